# Optimizing a Trainium2 kernel written in Bass

```python
import math
import jax, jax.numpy as jnp
from jax import lax
import numpy as np

D_MODEL = 1024
BATCH = 8
SEQ = 4096
DEPTH = 1

CHUNK = 64
EPS = 1e-6

RWKV_HEADS = 8
RWKV_HEAD_DIM = 64
RWKV_WIDTH = RWKV_HEADS * RWKV_HEAD_DIM
LORA_W = 64
LORA_A = 64
LORA_G = 128
GN_EPS = 64e-5
RWKV_SPLITS = [RWKV_WIDTH, 2 * RWKV_WIDTH, 3 * RWKV_WIDTH, 3 * RWKV_WIDTH + LORA_W, 3 * RWKV_WIDTH + LORA_W + LORA_A]
N_RWKV_COLS = 3 * RWKV_WIDTH + LORA_W + LORA_A + LORA_G

S5_GROUP = 16
S5_GROUPS = 32
S5_WIDTH = S5_GROUP * S5_GROUPS
S5_STATE = 64
DT_MIN = 1e-3
DT_MAX = 1e-1

N_IN_COLS = N_RWKV_COLS + S5_WIDTH + 2 * D_MODEL

PEER_HEADS = 8
PEER_KEYS = 128
PEER_EXPERTS = PEER_KEYS * PEER_KEYS
PEER_TOPK = 16
PEER_KEY_DIM = 128
PEER_HALF = PEER_KEY_DIM // 2
PEER_BLOCK = 128

kernel_name = 'hybrid_rwkv7_s5_peer_block'


def _rms_norm(x, gain):
    xf = x.astype(jnp.float32)
    y = xf * lax.rsqrt(jnp.mean(xf * xf, axis=-1, keepdims=True) + EPS)
    return (y * gain.astype(jnp.float32)).astype(x.dtype)


def _token_shift(p):
    return jnp.pad(p[:, :-1], ((0, 0), (1, 0), (0, 0)))


def _rwkv7_time_mix(p, mu, w_lora_up, w0, a_lora_up, a0, g_lora_up, k_k, k_a, r_k, ln_w, ln_b, w_o):
    f32 = jnp.float32
    B, S, _ = p.shape
    p = p.astype(f32)
    p = p + (_token_shift(p) - p) * mu.astype(f32)
    r, k, v, xw, xa, xg = jnp.split(p, RWKV_SPLITS, axis=-1)
    w_log = -jax.nn.softplus(-(w0.astype(f32) + jnp.tanh(xw) @ w_lora_up.astype(f32))) - 0.5
    decay = jnp.exp(-jnp.exp(w_log))
    a = jax.nn.sigmoid(a0.astype(f32) + xa @ a_lora_up.astype(f32))
    g = jax.nn.sigmoid(xg) @ g_lora_up.astype(f32)
    kk = k * k_k.astype(f32)
    k = k * (1.0 + (a - 1.0) * k_a.astype(f32))

    def heads(t):
        return t.reshape(B, S, RWKV_HEADS, RWKV_HEAD_DIM)

    r, k, v, decay, a, kk = heads(r), heads(k), heads(v), heads(decay), heads(a), heads(kk)
    kk = kk / jnp.maximum(jnp.sqrt(jnp.sum(kk * kk, axis=-1, keepdims=True)), 1e-12)

    def step(state, inp):
        r_t, k_t, v_t, w_t, kk_t, a_t = inp
        sa = jnp.einsum('bhvk,bhk->bhv', state, -kk_t)
        state = (state * w_t[:, :, None, :]
                 + sa[..., None] * (kk_t * a_t)[:, :, None, :]
                 + v_t[..., None] * k_t[:, :, None, :])
        y_t = jnp.einsum('bhvk,bhk->bhv', state, r_t)
        return state, y_t

    def seq_first(t):
        return jnp.swapaxes(t, 0, 1)

    state0 = jnp.zeros((B, RWKV_HEADS, RWKV_HEAD_DIM, RWKV_HEAD_DIM), f32)
    _, ys = lax.scan(step, state0, (seq_first(r), seq_first(k), seq_first(v),
                                    seq_first(decay), seq_first(kk), seq_first(a)))
    y = jnp.swapaxes(ys, 0, 1)
    mean = jnp.mean(y, axis=-1, keepdims=True)
    var = jnp.mean(jnp.square(y - mean), axis=-1, keepdims=True)
    y = ((y - mean) * lax.rsqrt(var + GN_EPS)).reshape(B, S, RWKV_WIDTH)
    y = y * ln_w.astype(f32) + ln_b.astype(f32)
    bonus = jnp.sum(r * k * r_k.astype(f32), axis=-1, keepdims=True) * v
    y = (y + bonus.reshape(B, S, RWKV_WIDTH)) * g
    return y @ w_o.astype(f32)


def _complex_linear_combine(e1, e2):
    a1r, a1i, b1r, b1i = e1
    a2r, a2i, b2r, b2i = e2
    ar = a2r * a1r - a2i * a1i
    ai = a2r * a1i + a2i * a1r
    br = a2r * b1r - a2i * b1i + b2r
    bi = a2r * b1i + a2i * b1r + b2i
    return ar, ai, br, bi


def _s5_ssm_glu(u, log_dt, a_re, a_im, b_re, b_im, c_re, c_im, d, w_glu):
    f32 = jnp.float32
    B, S, _ = u.shape
    uf = u.astype(f32).reshape(B, S, S5_GROUPS, S5_GROUP)
    a_re = a_re.astype(f32)
    a_im = a_im.astype(f32)
    dt = jnp.exp(log_dt.astype(f32))[:, None]
    mag = jnp.exp(a_re * dt)
    lam_re = mag * jnp.cos(a_im * dt)
    lam_im = mag * jnp.sin(a_im * dt)
    den = a_re * a_re + a_im * a_im
    num_re = lam_re - 1.0
    f_re = (num_re * a_re + lam_im * a_im) / den
    f_im = (lam_im * a_re - num_re * a_im) / den
    b_re = b_re.astype(f32)
    b_im = b_im.astype(f32)
    bb_re = f_re[..., None] * b_re - f_im[..., None] * b_im
    bb_im = f_re[..., None] * b_im + f_im[..., None] * b_re
    bu_re = jnp.einsum('bsgh,gph->bsgp', uf, bb_re)
    bu_im = jnp.einsum('bsgh,gph->bsgp', uf, bb_im)
    lam_re_t = jnp.broadcast_to(lam_re, (1, S, S5_GROUPS, S5_STATE))
    lam_im_t = jnp.broadcast_to(lam_im, (1, S, S5_GROUPS, S5_STATE))
    _, _, xs_re, xs_im = lax.associative_scan(
        _complex_linear_combine, (lam_re_t, lam_im_t, bu_re, bu_im), axis=1)
    y = (jnp.einsum('bsgp,ghp->bsgh', xs_re, c_re.astype(f32))
         - jnp.einsum('bsgp,ghp->bsgh', xs_im, c_im.astype(f32))
         + d.astype(f32) * uf)
    y = jax.nn.gelu(y.reshape(B, S, S5_WIDTH))
    z = y @ w_glu.astype(f32)
    z_val, z_gate = jnp.split(z, 2, axis=-1)
    return z_val * jax.nn.sigmoid(z_gate)


def _peer_ffn(xn, wq, subkeys, table_u, table_v):
    B, S, D = xn.shape
    xb = xn.reshape((B * S) // PEER_BLOCK, PEER_BLOCK, D)

    def block(xt):
        q = (xt @ wq).reshape(PEER_BLOCK, PEER_HEADS, 2, PEER_HALF).astype(jnp.float32)
        s = jnp.einsum('thcd,hcnd->thcn', q, subkeys.astype(jnp.float32))
        sv, si = lax.top_k(s, PEER_TOPK)
        cand = sv[:, :, 0, :, None] + sv[:, :, 1, None, :]
        cv, ci = lax.top_k(cand.reshape(PEER_BLOCK, PEER_HEADS, PEER_TOPK * PEER_TOPK), PEER_TOPK)
        i1 = jnp.take_along_axis(si[:, :, 0], ci // PEER_TOPK, axis=-1)
        i2 = jnp.take_along_axis(si[:, :, 1], ci % PEER_TOPK, axis=-1)
        e = i1 * PEER_KEYS + i2
        gate = jax.nn.softmax(cv, axis=-1)
        u_sel = table_u[e]
        hid = jax.nn.gelu(jnp.einsum('td,thkd->thk', xt, u_sel).astype(jnp.float32))
        return jnp.einsum('thk,thkd->td', (gate * hid).astype(xt.dtype), table_v[e])

    return lax.map(block, xb).reshape(B, S, D)


def setup_inputs(seed: int = 0) -> dict:
    key = jax.random.key(seed)
    ks = jax.random.split(key, 32)
    f32 = jnp.float32
    L = DEPTH

    def nrm(k, shape, scale):
        return jax.random.normal(k, shape, f32) * scale

    def gain(k, shape):
        return 1.0 + 0.02 * jax.random.normal(k, shape, f32)

    a_im_init = math.pi * jnp.arange(S5_STATE, dtype=f32)
    return {
        'x': jax.random.normal(ks[0], (BATCH, SEQ, D_MODEL), f32),
        'norm_mix': gain(ks[1], (L, D_MODEL)),
        'w_in': nrm(ks[2], (L, D_MODEL, N_IN_COLS), D_MODEL ** -0.5),
        'b_gate': nrm(ks[3], (L, 2 * D_MODEL), 0.1),
        'mu_rwkv': jax.random.uniform(ks[4], (L, N_RWKV_COLS), f32),
        'w_lora_up': nrm(ks[5], (L, LORA_W, RWKV_WIDTH), 0.1 * LORA_W ** -0.5),
        'w0': jax.random.uniform(ks[6], (L, RWKV_WIDTH), f32, -6.0, 1.0),
        'a_lora_up': nrm(ks[7], (L, LORA_A, RWKV_WIDTH), 0.1 * LORA_A ** -0.5),
        'a0': nrm(ks[8], (L, RWKV_WIDTH), 0.1),
        'g_lora_up': nrm(ks[9], (L, LORA_G, RWKV_WIDTH), LORA_G ** -0.5),
        'k_k': 0.85 + 0.02 * jax.random.normal(ks[10], (L, RWKV_WIDTH), f32),
        'k_a': gain(ks[11], (L, RWKV_WIDTH)),
        'r_k': nrm(ks[12], (L, RWKV_HEADS, RWKV_HEAD_DIM), 0.1),
        'ln_x_w': gain(ks[13], (L, RWKV_WIDTH)),
        'ln_x_b': nrm(ks[14], (L, RWKV_WIDTH), 0.02),
        'w_o_rwkv': nrm(ks[15], (L, RWKV_WIDTH, D_MODEL), RWKV_WIDTH ** -0.5),
        's5_log_dt': jax.random.uniform(ks[16], (L, S5_GROUPS), f32, math.log(DT_MIN), math.log(DT_MAX)),
        's5_a_re': -0.5 * jnp.exp(0.01 * jax.random.normal(ks[17], (L, S5_GROUPS, S5_STATE), f32)),
        's5_a_im': a_im_init + 0.01 * jax.random.normal(ks[18], (L, S5_GROUPS, S5_STATE), f32),
        's5_b_re': nrm(ks[19], (L, S5_GROUPS, S5_STATE, S5_GROUP), (2 * S5_GROUP) ** -0.5),
        's5_b_im': nrm(ks[20], (L, S5_GROUPS, S5_STATE, S5_GROUP), (2 * S5_GROUP) ** -0.5),
        's5_c_re': nrm(ks[21], (L, S5_GROUPS, S5_GROUP, S5_STATE), (2 * S5_STATE) ** -0.5),
        's5_c_im': nrm(ks[22], (L, S5_GROUPS, S5_GROUP, S5_STATE), (2 * S5_STATE) ** -0.5),
        's5_d': nrm(ks[23], (L, S5_GROUPS, S5_GROUP), 1.0),
        'w_glu_s5': nrm(ks[24], (L, S5_WIDTH, 2 * D_MODEL), S5_WIDTH ** -0.5),
        'w_out': nrm(ks[25], (L, D_MODEL, D_MODEL), D_MODEL ** -0.5),
        'norm_ffn': gain(ks[26], (L, D_MODEL)),
        'peer_wq': nrm(ks[27], (L, D_MODEL, PEER_HEADS * PEER_KEY_DIM), D_MODEL ** -0.5),
        'peer_subkeys': nrm(ks[28], (L, PEER_HEADS, 2, PEER_KEYS, PEER_HALF), PEER_HALF ** -0.5),
        'peer_u': nrm(ks[29], (L, PEER_EXPERTS, D_MODEL), D_MODEL ** -0.5),
        'peer_v': nrm(ks[30], (L, PEER_EXPERTS, D_MODEL), PEER_HEADS ** -0.5),
        'norm_final': gain(ks[31], (D_MODEL,)),
    }


def reference(x, norm_mix, w_in, b_gate, mu_rwkv, w_lora_up, w0, a_lora_up, a0, g_lora_up,
              k_k, k_a, r_k, ln_x_w, ln_x_b, w_o_rwkv, s5_log_dt, s5_a_re, s5_a_im,
              s5_b_re, s5_b_im, s5_c_re, s5_c_im, s5_d, w_glu_s5, w_out, norm_ffn,
              peer_wq, peer_subkeys, peer_u, peer_v, norm_final):
    h = x
    for l in range(DEPTH):
        xn = _rms_norm(h, norm_mix[l])
        p = xn @ w_in[l]
        p_rwkv = p[..., :N_RWKV_COLS]
        p_s5 = p[..., N_RWKV_COLS:N_RWKV_COLS + S5_WIDTH]
        p_gate = p[..., N_RWKV_COLS + S5_WIDTH:].astype(jnp.float32) + b_gate[l].astype(jnp.float32)
        y_a = _rwkv7_time_mix(p_rwkv, mu_rwkv[l], w_lora_up[l], w0[l], a_lora_up[l], a0[l],
                              g_lora_up[l], k_k[l], k_a[l], r_k[l], ln_x_w[l], ln_x_b[l], w_o_rwkv[l])
        y_b = _s5_ssm_glu(p_s5, s5_log_dt[l], s5_a_re[l], s5_a_im[l], s5_b_re[l], s5_b_im[l],
                          s5_c_re[l], s5_c_im[l], s5_d[l], w_glu_s5[l])
        g_a, g_b = jnp.split(jax.nn.sigmoid(p_gate), 2, axis=-1)
        mixed = (g_a * y_a + g_b * y_b).astype(h.dtype)
        h = h + mixed @ w_out[l]
        h = h + _peer_ffn(_rms_norm(h, norm_ffn[l]), peer_wq[l], peer_subkeys[l], peer_u[l], peer_v[l])
    return _rms_norm(h, norm_final)
```

```python
import numpy as np
from contextlib import ExitStack
import concourse.bass as bass
import concourse.mybir as mybir
from concourse.bass_utils import run_bass_kernel_spmd

F32 = mybir.dt.float32
BF16 = mybir.dt.bfloat16
I32 = mybir.dt.int32
U32 = mybir.dt.uint32
ALU = mybir.AluOpType
AF = mybir.ActivationFunctionType
AX = mybir.AxisListType

D = 1024
NRW = 1792
NCOL = 4352
SEQ = 4096
NCT = 34
C0 = float(np.exp(-0.5))
PI = float(np.pi)
GN_EPS = 64e-5
NB = 6
NEG = -1.0e30


class Sched:
    def __init__(self, nc, es):
        self.nc = nc
        self.es = es
        self.engs = {'v': nc.vector, 'a': nc.scalar, 'p': nc.tensor, 'g': nc.gpsimd, 's': nc.sync}
        self.sems = {}
        self.val = {}
        self.waited = {e: {} for e in self.engs}
        self.lastw = {}
        self.readers = {}
        self.nins = 0
        for e in 'vapg':
            self._mk(e)

    def _mk(self, key):
        self.sems[key] = self.es.enter_context(self.nc.semaphore('sem_' + key))
        self.val[key] = 0

    def _wait(self, e, k, v):
        if self.waited[e].get(k, 0) >= v:
            return
        self.engs[e].wait_ge(self.sems[k], v)
        self.waited[e][k] = v

    def _deps(self, e, reads, writes):
        for b in reads:
            if b in self.lastw:
                self._wait(e, *self.lastw[b])
        for b in writes:
            if b in self.lastw:
                self._wait(e, *self.lastw[b])
            for k, v in self.readers.get(b, {}).items():
                self._wait(e, k, v)

    def _commit(self, tok, reads, writes):
        k, v = tok
        for b in reads:
            self.readers.setdefault(b, {})[k] = v
        for b in writes:
            self.lastw[b] = tok
            self.readers[b] = {}

    def op(self, e, reads, writes, fn):
        self._deps(e, reads, writes)
        ins = fn(self.engs[e])
        self.val[e] += 1
        ins.then_inc(self.sems[e], 1)
        self._commit((e, self.val[e]), reads, writes)
        self.nins += 1

    def dma(self, q, semkey, out, in_, reads, writes, in_offset=None):
        if semkey not in self.sems:
            self._mk(semkey)
        self._deps(q, reads, writes)
        if self.val[semkey] > 0:
            self._wait(q, semkey, self.val[semkey])
        eng = self.engs[q]
        if in_offset is not None:
            ins = eng.indirect_dma_start(out=out, out_offset=None, in_=in_, in_offset=in_offset)
        else:
            ins = eng.dma_start(out=out, in_=in_)
        self.val[semkey] += 16
        ins.then_inc(self.sems[semkey], 16)
        self._commit((semkey, self.val[semkey]), reads, writes)
        self.nins += 1

    def barrier(self):
        for e in self.engs:
            for k, v in self.val.items():
                if v > 0:
                    self._wait(e, k, v)
        self.lastw = {}
        self.readers = {}


def bc(ap, shape, axis):
    return ap.unsqueeze(axis).to_broadcast(shape)


def gelu_tanh(op, src, srck, tmp, tmpk, dst, dstk):
    op('g', [srck], [tmpk], lambda e: e.tensor_tensor(out=tmp, in0=src, in1=src, op=ALU.mult))
    op('v', [tmpk], [tmpk], lambda e: e.tensor_scalar(out=tmp, in0=tmp, scalar1=0.044715, scalar2=1.0, op0=ALU.mult, op1=ALU.add))
    op('v', [tmpk, srck], [tmpk], lambda e: e.tensor_tensor(out=tmp, in0=tmp, in1=src, op=ALU.mult))
    op('a', [tmpk], [tmpk], lambda e: e.activation(out=tmp, in_=tmp, func=AF.Sigmoid, scale=1.5957691216057308))
    op('v', [tmpk, srck], [dstk], lambda e: e.tensor_tensor(out=dst, in0=tmp, in1=src, op=ALU.mult))


class _Stop(Exception):
    pass


def build_nc(NT=32, debug=False, phases=(1, 2), stop=None):
    nc = bass.Bass("TRN2", target_bir_lowering=False)

    def mark(name):
        if stop == name:
            raise _Stop()

    def din(name, shape, dt=F32):
        return nc.dram_tensor(name, shape, dt, kind="ExternalInput").ap()

    x_d = din("x", [SEQ, D])
    w_in_d = din("w_in", [D, NCOL])
    gainP_d = din("gainP", [128, 8])
    bgate_d = din("bgate", [128, 16])
    mu_d = din("mu", [128, 14])
    pv_d = din("pvec", [128, 6, 4])
    lora_d = din("lora", [128, 512])
    glora_d = din("glora", [128, 512])
    lnw_d = din("lnw", [128, 512])
    lnb_d = din("lnb", [128, 512])
    w_o_d = din("w_o", [512, D])
    w_glu_d = din("w_glu", [512, 2 * D])
    w_out_d = din("w_out", [D, D])
    s5p_d = din("s5p", [128, 3, 16])
    bbre_d = din("bbre", [128, 2048])
    bbim_d = din("bbim", [128, 2048])
    ccre_d = din("ccre", [128, 2048])
    ccim_d = din("ccim", [128, 2048])
    gffn_d = din("gffn", [128, D])
    gfin_d = din("gfin", [128, D])
    wq_d = din("wq", [D, D])
    subk_d = din("subk", [128, 1024])
    pu_d = din("peer_u", [16384, D])
    pvv_d = din("peer_v", [16384, D])
    ident_d = din("ident", [128, 128])
    masks_d = din("masks", [128, 5, 128])
    sel2_d = din("sel2", [128, 2])
    out_d = nc.dram_tensor("out", [SEQ, D], F32, kind="ExternalOutput").ap()
    h1_d = nc.dram_tensor("h1s", [SEQ, D], F32, kind="ExternalOutput" if debug else "Internal").ap()
    wsc_d = nc.dram_tensor("wsc", [NCT, 128, 1024], BF16, kind="Internal").ap()

    with ExitStack() as es0:
        sch = Sched(nc, es0)
        op = sch.op
        dma = sch.dma
        PS = [es0.enter_context(nc.psum_tensor("ps%d" % i, [128, 512], F32)) for i in range(8)]

        def psk(i):
            return "ps%d" % i

        def P3(i):
            return PS[i][:].rearrange("p (a b) -> p a b", a=4)

        if 1 in phases:
         try:
          with ExitStack() as es:
            def sb(name, shape, dt=F32):
                return es.enter_context(nc.sbuf_tensor("s_" + name, shape, dt))

            ident = sb("ident", [128, 128])
            masks = sb("masks", [128, 5, 128])
            sel2 = sb("sel2", [128, 2])
            gainP = sb("gainP", [128, 8])
            bgate = sb("bgate", [128, 16])
            mu = sb("mu", [128, 14])
            pvec = sb("pvec", [128, 6, 4])
            lnw = sb("lnw", [128, 512])
            lnb = sb("lnb", [128, 512])
            s5p = sb("s5p", [128, 3, 16])
            sp = sb("sp", [128, 24, 16])
            lora = sb("lora", [128, 512], BF16)
            glora = sb("glora", [128, 512], BF16)
            w_o = sb("w_o", [128, 4, 1024], BF16)
            w_glu = sb("w_glu", [128, 4, 2048], BF16)
            w_out = sb("w_out", [128, 8, 1024], BF16)
            BBre = sb("BBre", [128, 16, 128], BF16)
            BBim = sb("BBim", [128, 16, 128], BF16)
            CCre = sb("CCre", [128, 16, 128], BF16)
            CCimN = sb("CCimN", [128, 16, 128], BF16)
            Ere = sb("Ere", [128, 16, 128])
            Eim = sb("Eim", [128, 16, 128])
            SCR = sb("SCR", [128, 8192])
            esp = ExitStack()
            stgb = [esp.enter_context(nc.sbuf_tensor("s_stgb%d" % i, [128, 1024], BF16)) for i in range(2)]

            for t_sb, t_d in [(ident, ident_d), (masks, masks_d), (sel2, sel2_d), (gainP, gainP_d),
                              (bgate, bgate_d), (mu, mu_d), (pvec, pv_d), (lnw, lnw_d), (lnb, lnb_d),
                              (s5p, s5p_d)]:
                dma('s', 'dc', t_sb[:], t_d, [], ["c"])
            maskSL = masks[:, 0, :]
            maskSU = masks[:, 1, :]
            maskUI = masks[:, 2, :]
            maskBD = masks[:, 3, :]
            scanmask = masks[:, 4, :]

            stg = [SCR[:, 0:2048], SCR[:, 2048:4096]]
            tmpA = SCR[:, 4096:6144]
            tmpB = SCR[:, 6144:8192]

            w_in_v = w_in_d.rearrange("(kt p) c -> p kt c", p=128)
            for c in range(NCT):
                b = c % 2
                sg = "stg%d" % b
                sgb = "stgb%d" % b
                dma('s', 'dst%d' % b, stg[b][:, 0:1024].rearrange("p (k c) -> p k c", k=8),
                    w_in_v[:, :, c * 128:(c + 1) * 128], [], [sg])
                op('v', [sg, "c"], [sgb], lambda e, b=b: e.tensor_tensor(
                    out=stgb[b][:].rearrange("p (k c) -> p k c", k=8),
                    in0=stg[b][:, 0:1024].rearrange("p (k c) -> p k c", k=8),
                    in1=bc(gainP[:], [128, 8, 128], 2), op=ALU.mult))
                dma('s', 'dsb%d' % b, wsc_d[c], stgb[b][:], [sgb], ["wsc%d" % c])

            ldn = [0]

            def load_cast(dst_ap, src_ap, width, dstkey):
                b = ldn[0] % 2
                ldn[0] += 1
                sg = "stg%d" % b
                dma('s', 'dst%d' % b, stg[b][:, 0:width], src_ap, [], [sg])
                if b == 0:
                    op('v', [sg], [dstkey], lambda e: e.tensor_copy(out=dst_ap, in_=stg[b][:, 0:width]))
                else:
                    op('a', [sg], [dstkey], lambda e: e.activation(out=dst_ap, in_=stg[b][:, 0:width], func=AF.Copy))

            load_cast(lora[:], lora_d, 512, "lora")
            load_cast(glora[:], glora_d, 512, "glora")
            for k in range(4):
                load_cast(w_o[:, k, :], w_o_d[k * 128:(k + 1) * 128, :], 1024, "w_o")
            for k in range(4):
                load_cast(w_glu[:, k, :], w_glu_d[k * 128:(k + 1) * 128, :], 2048, "w_glu")
            for k in range(8):
                load_cast(w_out[:, k, :], w_out_d[k * 128:(k + 1) * 128, :], 1024, "w_out")
            load_cast(BBre[:].rearrange("p a b -> p (a b)"), bbre_d, 2048, "BB")
            load_cast(BBim[:].rearrange("p a b -> p (a b)"), bbim_d, 2048, "BB")

            def V2(i):
                return sp[:, i, :]
            a_re = s5p[:, 0, :]
            a_im = s5p[:, 1, :]
            ldt = s5p[:, 2, :]
            DT, TH, LAB, CS, SN, R, M_, LRE, LIM, NRE, DEN, FRE, FIM, T1, T2, CS2, SN2 = range(17)

            def vv(out_i, a, b_, o):
                op('v', ["c", "sp"], ["sp"], lambda e: e.tensor_tensor(out=V2(out_i), in0=a, in1=b_, op=o))
            op('a', ["c"], ["sp"], lambda e: e.activation(out=V2(DT), in_=ldt, func=AF.Exp))
            vv(TH, a_im, V2(DT), ALU.mult)
            vv(T1, a_re, V2(DT), ALU.mult)
            op('a', ["sp"], ["sp"], lambda e: e.activation(out=V2(LAB), in_=V2(T1), func=AF.Exp))

            def sin_of(dst, shift):
                op('v', ["sp"], ["sp"], lambda e: e.tensor_scalar(out=V2(R), in0=V2(TH), scalar1=float(shift), scalar2=None, op0=ALU.add))
                for _ in range(4):
                    op('v', ["sp"], ["sp"], lambda e: e.tensor_scalar(out=V2(M_), in0=V2(R), scalar1=PI, scalar2=-2.0 * PI, op0=ALU.is_ge, op1=ALU.mult))
                    vv(R, V2(R), V2(M_), ALU.add)
                op('a', ["sp"], ["sp"], lambda e: e.activation(out=V2(dst), in_=V2(R), func=AF.Sin))
            sin_of(SN, 0.0)
            sin_of(CS, PI / 2)
            vv(LRE, V2(LAB), V2(CS), ALU.mult)
            vv(LIM, V2(LAB), V2(SN), ALU.mult)
            op('v', ["sp"], ["sp"], lambda e: e.tensor_scalar(out=V2(NRE), in0=V2(LRE), scalar1=-1.0, scalar2=None, op0=ALU.add))
            vv(T1, a_re, a_re, ALU.mult)
            vv(T2, a_im, a_im, ALU.mult)
            vv(DEN, V2(T1), V2(T2), ALU.add)
            op('v', ["sp"], ["sp"], lambda e: e.reciprocal(out=V2(DEN), in_=V2(DEN)))
            vv(T1, V2(NRE), a_re, ALU.mult)
            vv(T2, V2(LIM), a_im, ALU.mult)
            vv(T1, V2(T1), V2(T2), ALU.add)
            vv(FRE, V2(T1), V2(DEN), ALU.mult)
            vv(T1, V2(LIM), a_re, ALU.mult)
            vv(T2, V2(NRE), a_im, ALU.mult)
            vv(T1, V2(T1), V2(T2), ALU.subtract)
            vv(FIM, V2(T1), V2(DEN), ALU.mult)

            dma('s', 'dst0', stg[0], ccre_d, [], ["stg0"])
            dma('s', 'dst1', stg[1], ccim_d, [], ["stg1"])

            def c3(a):
                return a.rearrange("p (j m) -> p j m", j=16)
            fre_b = bc(V2(FRE), [128, 16, 128], 2)
            fim_b = bc(V2(FIM), [128, 16, 128], 2)
            op('v', ["stg0", "sp"], ["tmpA"], lambda e: e.tensor_tensor(out=c3(tmpA), in0=c3(stg[0]), in1=fre_b, op=ALU.mult))
            op('v', ["stg1", "sp"], ["tmpB"], lambda e: e.tensor_tensor(out=c3(tmpB), in0=c3(stg[1]), in1=fim_b, op=ALU.mult))
            op('v', ["tmpA", "tmpB"], ["CC"], lambda e: e.tensor_tensor(out=CCre[:].rearrange("p a b -> p (a b)"), in0=tmpA, in1=tmpB, op=ALU.subtract))
            op('v', ["stg0", "sp", "CC"], ["tmpA"], lambda e: e.tensor_tensor(out=c3(tmpA), in0=c3(stg[0]), in1=fim_b, op=ALU.mult))
            op('v', ["stg1", "sp", "CC"], ["tmpB"], lambda e: e.tensor_tensor(out=c3(tmpB), in0=c3(stg[1]), in1=fre_b, op=ALU.mult))
            op('v', ["tmpA", "tmpB"], ["tmpA"], lambda e: e.tensor_tensor(out=tmpA, in0=tmpA, in1=tmpB, op=ALU.add))
            op('v', ["tmpA"], ["CC"], lambda e: e.tensor_scalar(out=CCimN[:].rearrange("p a b -> p (a b)"), in0=tmpA, scalar1=-1.0, scalar2=None, op0=ALU.mult))

            op('v', [], ["E"], lambda e: e.memset(Ere[:, :, 0:1], 1.0))
            op('v', [], ["E"], lambda e: e.memset(Eim[:, :, 0:1], 0.0))
            op('v', ["sp"], ["sp"], lambda e: e.tensor_copy(out=V2(CS2), in_=V2(CS)))
            op('v', ["sp"], ["sp"], lambda e: e.tensor_copy(out=V2(SN2), in_=V2(SN)))
            et0 = tmpA[:, 0:1024].rearrange("p (a b) -> p a b", a=16)
            et1 = tmpB[:, 0:1024].rearrange("p (a b) -> p a b", a=16)
            for lv in range(7):
                m = 1 << lv
                shp = [128, 16, m]
                cb = bc(V2(CS2), shp, 2)
                sbb = bc(V2(SN2), shp, 2)
                op('v', ["E", "sp", "tmpA"], ["tmpA"], lambda e, m=m, cb=cb: e.tensor_tensor(out=et0[:, :, 0:m], in0=Ere[:, :, 0:m], in1=cb, op=ALU.mult))
                op('v', ["E", "sp", "tmpB"], ["tmpB"], lambda e, m=m, sbb=sbb: e.tensor_tensor(out=et1[:, :, 0:m], in0=Eim[:, :, 0:m], in1=sbb, op=ALU.mult))
                op('v', ["tmpA", "tmpB", "E"], ["E"], lambda e, m=m: e.tensor_tensor(out=Ere[:, :, m:2 * m], in0=et0[:, :, 0:m], in1=et1[:, :, 0:m], op=ALU.subtract))
                op('v', ["E", "sp", "tmpA"], ["tmpA"], lambda e, m=m, sbb=sbb: e.tensor_tensor(out=et0[:, :, 0:m], in0=Ere[:, :, 0:m], in1=sbb, op=ALU.mult))
                op('v', ["E", "sp", "tmpB"], ["tmpB"], lambda e, m=m, cb=cb: e.tensor_tensor(out=et1[:, :, 0:m], in0=Eim[:, :, 0:m], in1=cb, op=ALU.mult))
                op('v', ["tmpA", "tmpB", "E"], ["E"], lambda e, m=m: e.tensor_tensor(out=Eim[:, :, m:2 * m], in0=et0[:, :, 0:m], in1=et1[:, :, 0:m], op=ALU.add))
                vv(T1, V2(CS2), V2(CS2), ALU.mult)
                vv(T2, V2(SN2), V2(SN2), ALU.mult)
                op('v', ["sp"], ["sp"], lambda e: e.scalar_tensor_tensor(out=V2(SN2), in0=V2(CS2), scalar=2.0, in1=V2(SN2), op0=ALU.mult, op1=ALU.mult))
                vv(CS2, V2(T1), V2(T2), ALU.subtract)

            sch.barrier()
            esp.close()
            mark("prep")

            PF = sb("PF", [128, 14, 129])
            Stp = sb("Stp", [128, 4, 128])
            cw = sb("cw", [128, 2, 16])
            op('v', [], ["PF"], lambda e: e.memset(PF[:], 0.0))
            op('v', [], ["Stp"], lambda e: e.memset(Stp[:], 0.0))
            op('v', [], ["cw"], lambda e: e.memset(cw[:], 0.0))

            xt = [sb("xt%d" % i, [128, D]) for i in range(2)]
            stat = sb("stat", [128, 8])
            xnF = sb("xnF", [128, 8, 128], BF16)
            wch = [sb("wch%d" % i, [128, 8, 128], BF16) for i in range(4)]
            L = sb("L", [128, 14, 128])
            uF = sb("uF", [128, 4, 128])
            uB = sb("uB", [128, 4, 128], BF16)
            gates = sb("gates", [128, 16, 128], BF16)
            lorain = sb("lorain", [128, 128], BF16)
            sgx = sb("sgx", [128, 128], BF16)
            TT = [SCR[:, i * 512:(i + 1) * 512].rearrange("p (a b) -> p a b", a=4) for i in range(16)]
            TK = ["T%d" % i for i in range(16)]
            (iSIG, iCS, iPT, iPTM, iPINV, iAV, iKK, iTMP, iKMOD, iRTL, iATL, iBTL, iKTL, iRKR, iY2, iX) = range(16)
            VT = sb("VT", [128, 512])
            BT = sb("BT", [128, 512])
            KT = sb("KT", [128, 512])
            gT = sb("gT", [128, 512])
            mats = {nm: sb("m_" + nm, [128, 4, 128]) for nm in ["X", "XT", "X2", "X2T", "QT", "akT", "rbT", "rkT"]}
            RHS = sb("RHS", [128, 512])
            SA = sb("SA", [128, 512])
            PTbd = sb("PTbd", [128, 4, 128])
            Y = sb("Y", [128, 512])
            gst = sb("gst", [128, 6, 8])
            bon = sb("bon", [128, 8])
            roF = sb("roF", [128, 4, 128], BF16)
            mixed = sb("mixed", [128, 8, 128])
            mixedB = sb("mixedB", [128, 8, 128], BF16)
            xre = sb("xre", [128, 4, 128], BF16)
            xim = sb("xim", [128, 4, 128], BF16)
            ygB = sb("ygB", [128, 4, 128], BF16)
            zg = sb("zg", [128, 8, 128])
            h1 = sb("h1", [128, D])
            cwn = sb("cwn", [128, 6, 4])

            w0 = pvec[:, 0, :]
            a0 = pvec[:, 1, :]
            k_k = pvec[:, 2, :]
            k_a = pvec[:, 3, :]
            r_k = pvec[:, 4, :]
            s5d = pvec[:, 5, :]
            S4 = [128, 4, 128]

            def tt(eng, o, ok, a, ak, b_, bk, alu):
                op(eng, [ak, bk], [ok], lambda e: e.tensor_tensor(out=o, in0=a, in1=b_, op=alu))

            dma('s', 'dx0', xt[0][:], x_d[0:128, :], [], ["xt0"])

            for it in range(NT):
                xb = it % 2
                xk = "xt%d" % xb
                xcur = xt[xb]
                if it + 1 < NT:
                    dma('s', 'dx%d' % (1 - xb), xt[1 - xb][:], x_d[(it + 1) * 128:(it + 2) * 128, :], [], ["xt%d" % (1 - xb)])
                op('a', [xk], [TK[iX], "stat"], lambda e: e.activation(out=SCR[:, iX * 512:iX * 512 + 512], in_=xcur[:, 0:512], func=AF.Square, accum_out=stat[:, 0:1]))
                op('a', [xk], [TK[iX], "stat"], lambda e: e.activation(out=SCR[:, iX * 512:iX * 512 + 512], in_=xcur[:, 512:1024], func=AF.Square, accum_out=stat[:, 3:4]))
                op('v', ["stat"], ["stat"], lambda e: e.tensor_tensor(out=stat[:, 0:1], in0=stat[:, 0:1], in1=stat[:, 3:4], op=ALU.add))
                op('a', ["stat"], ["stat"], lambda e: e.activation(out=stat[:, 1:2], in_=stat[:, 0:1], func=AF.Sqrt, bias=1e-6, scale=1.0 / D))
                op('v', ["stat"], ["stat"], lambda e: e.reciprocal(out=stat[:, 2:3], in_=stat[:, 1:2]))
                op('v', [xk, "stat"], [xk], lambda e: e.tensor_scalar(out=xcur[:], in0=xcur[:], scalar1=stat[:, 2:3], scalar2=None, op0=ALU.mult))
                mark("norm")
                for half in range(2):
                    pk = psk(half)

                    def tr(e, half=half):
                        ins = None
                        for q in range(4):
                            kt = half * 4 + q
                            ins = e.transpose(out=PS[half][:, q * 128:(q + 1) * 128], in_=xcur[:, kt * 128:(kt + 1) * 128], identity=ident[:])
                        return ins
                    op('p', [xk, "c"], [pk], tr)
                    if half == 0:
                        op('v', [pk], ["xnF"], lambda e: e.tensor_copy(out=xnF[:, 0:4, :], in_=P3(0)))
                    else:
                        op('a', [pk], ["xnF"], lambda e: e.activation(out=xnF[:, 4:8, :], in_=P3(1), func=AF.Copy))
                mark("xnF")
                for c in range(NCT):
                    mark("proj%d" % c)
                    wb = c % 4
                    wk = "wch%d" % wb
                    dma('s', 'dw%d' % wb, wch[wb][:].rearrange("p a b -> p (a b)"), wsc_d[c], ["wsc%d" % c], [wk])
                    pb = 2 + (c % 4)
                    pk = psk(pb)

                    def mm(e, wb=wb, pb=pb):
                        ins = None
                        for kt in range(8):
                            ins = e.matmul(PS[pb][:, 0:128], lhsT=wch[wb][:, kt, :], rhs=xnF[:, kt, :], start=(kt == 0), stop=(kt == 7))
                        return ins
                    op('p', [wk, "xnF"], [pk], mm)
                    if c < 14:
                        if c % 2 == 0:
                            op('v', [pk], ["PF"], lambda e, c=c, pb=pb: e.tensor_copy(out=PF[:, c, 1:129], in_=PS[pb][:, 0:128]))
                        else:
                            op('a', [pk], ["PF"], lambda e, c=c, pb=pb: e.activation(out=PF[:, c, 1:129], in_=PS[pb][:, 0:128], func=AF.Copy))
                    elif c < 18:
                        op('v', [pk], ["uF"], lambda e, c=c, pb=pb: e.tensor_copy(out=uF[:, c - 14, :], in_=PS[pb][:, 0:128]))
                        op('a', ["uF"], ["uB"], lambda e, c=c, pb=pb: e.activation(out=uB[:, c - 14, :], in_=uF[:, c - 14, :], func=AF.Copy))
                    else:
                        op('a', [pk, "c"], ["gates"], lambda e, c=c, pb=pb: e.activation(out=gates[:, c - 18, :], in_=PS[pb][:, 0:128], func=AF.Sigmoid, bias=bgate[:, c - 18:c - 17]))
                mark("proj")
                op('v', ["PF"], ["L"], lambda e: e.tensor_tensor(out=L[:], in0=PF[:, :, 0:128], in1=PF[:, :, 1:129], op=ALU.subtract))
                op('g', ["L", "c"], ["L"], lambda e: e.tensor_tensor(out=L[:], in0=L[:], in1=bc(mu[:], [128, 14, 128], 2), op=ALU.mult))
                op('v', ["L", "PF"], ["L"], lambda e: e.tensor_tensor(out=L[:], in0=L[:], in1=PF[:, :, 1:129], op=ALU.add))
                op('v', ["PF"], ["PF"], lambda e: e.tensor_copy(out=PF[:, :, 0:1], in_=PF[:, :, 128:129]))
                rF = L[:, 0:4, :]
                kF = L[:, 4:8, :]
                vF = L[:, 8:12, :]
                sig, cs, Pt, Ptm1, Pinv, av, kk, tmp, kmod, rtl, atl, btl, ktl, rkr = [TT[i] for i in range(14)]
                op('a', ["L"], ["lorain"], lambda e: e.activation(out=lorain[0:64, :], in_=L[0:64, 12, :], func=AF.Tanh))
                op('v', ["L"], ["lorain"], lambda e: e.tensor_copy(out=lorain[64:128, :], in_=L[64:128, 12, :]))
                op('a', ["L"], ["sgx"], lambda e: e.activation(out=sgx[:], in_=L[:, 13, :], func=AF.Sigmoid))

                def mm_lw(e):
                    ins = None
                    for j in range(4):
                        ins = e.matmul(PS[0][:, j * 128:(j + 1) * 128], lhsT=lora[0:64, j * 128:(j + 1) * 128], rhs=lorain[0:64, :], start=True, stop=True)
                    return ins
                op('p', ["lora", "lorain"], [psk(0)], mm_lw)

                def mm_la(e):
                    ins = None
                    for j in range(4):
                        ins = e.matmul(PS[1][:, j * 128:(j + 1) * 128], lhsT=lora[64:128, j * 128:(j + 1) * 128], rhs=lorain[64:128, :], start=True, stop=True)
                    return ins
                op('p', ["lora", "lorain"], [psk(1)], mm_la)
                op('p', ["glora", "sgx"], [psk(6)], lambda e: e.matmul(PS[6][:], lhsT=sgx[:], rhs=glora[:], start=True, stop=True))
                for j in range(4):
                    op('a', [psk(0), "c"], [TK[iSIG]], lambda e, j=j: e.activation(out=sig[:, j, :], in_=PS[0][:, j * 128:(j + 1) * 128], func=AF.Sigmoid, bias=w0[:, j:j + 1]))
                    op('a', [psk(1), "c"], [TK[iAV]], lambda e, j=j: e.activation(out=av[:, j, :], in_=PS[1][:, j * 128:(j + 1) * 128], func=AF.Sigmoid, bias=a0[:, j:j + 1]))
                op('v', [psk(6)], ["gT"], lambda e: e.tensor_copy(out=gT[:], in_=PS[6][:]))
                for j in range(4):
                    op('v', [TK[iSIG], "c"], [TK[iCS]], lambda e, j=j: e.tensor_tensor_scan(out=cs[:, j, :], data0=scanmask, data1=sig[:, j, :], initial=0.0, op0=ALU.mult, op1=ALU.add))
                op('a', [TK[iCS]], [TK[iPT]], lambda e: e.activation(out=Pt, in_=cs, func=AF.Exp, scale=-C0))
                op('a', [TK[iCS]], [TK[iPINV]], lambda e: e.activation(out=Pinv, in_=cs, func=AF.Exp, scale=C0))
                tt('v', Ptm1, TK[iPTM], cs, TK[iCS], sig, TK[iSIG], ALU.subtract)
                op('a', [TK[iPTM]], [TK[iPTM]], lambda e: e.activation(out=Ptm1, in_=Ptm1, func=AF.Exp, scale=-C0))
                tt('g', kk, TK[iKK], kF, "L", bc(k_k, S4, 2), "c", ALU.mult)
                tt('g', tmp, TK[iTMP], kk, TK[iKK], kk, TK[iKK], ALU.mult)

                def mm_n(e):
                    ins = None
                    for j in range(4):
                        ins = e.matmul(PS[7][:, j * 128:(j + 1) * 128], lhsT=maskBD, rhs=tmp[:, j, :], start=True, stop=True)
                    return ins
                op('p', [TK[iTMP], "c"], [psk(7)], mm_n)
                op('a', [psk(7)], [TK[iTMP]], lambda e: e.activation(out=tmp, in_=P3(7), func=AF.Sqrt))
                op('v', [TK[iTMP]], [TK[iTMP]], lambda e: e.tensor_scalar(out=tmp, in0=tmp, scalar1=1e-12, scalar2=None, op0=ALU.max))
                op('v', [TK[iTMP]], [TK[iTMP]], lambda e: e.reciprocal(out=tmp, in_=tmp))
                tt('v', kk, TK[iKK], kk, TK[iKK], tmp, TK[iTMP], ALU.mult)
                tt('g', kmod, TK[iKMOD], av, TK[iAV], bc(k_a, S4, 2), "c", ALU.mult)
                tt('g', kmod, TK[iKMOD], kmod, TK[iKMOD], bc(k_a, S4, 2), "c", ALU.subtract)
                op('v', [TK[iKMOD], "L"], [TK[iKMOD]], lambda e: e.scalar_tensor_tensor(out=kmod, in0=kmod, scalar=1.0, in1=kF, op0=ALU.add, op1=ALU.mult))
                tt('v', rtl, TK[iRTL], rF, "L", Pt, TK[iPT], ALU.mult)
                op('v', [TK[iKK], TK[iPTM]], [TK[iATL]], lambda e: e.scalar_tensor_tensor(out=atl, in0=kk, scalar=-1.0, in1=Ptm1, op0=ALU.mult, op1=ALU.mult))
                tt('g', btl, TK[iBTL], kk, TK[iKK], av, TK[iAV], ALU.mult)
                tt('v', btl, TK[iBTL], btl, TK[iBTL], Pinv, TK[iPINV], ALU.mult)
                tt('v', ktl, TK[iKTL], kmod, TK[iKMOD], Pinv, TK[iPINV], ALU.mult)
                tt('g', rkr, TK[iRKR], rF, "L", kmod, TK[iKMOD], ALU.mult)
                tt('g', rkr, TK[iRKR], rkr, TK[iRKR], bc(r_k, S4, 2), "c", ALU.mult)
                op('v', [TK[iPT], "c"], ["PTbd"], lambda e: e.tensor_tensor(out=PTbd[:], in0=bc(maskBD, S4, 1), in1=Pt[:, :, 127:128].to_broadcast(S4), op=ALU.mult))

                def mm_b(e):
                    ins = None
                    for j in range(4):
                        ins = e.matmul(PS[7][:, 2 * j:2 * j + 2], lhsT=rkr[:, j, :], rhs=sel2[:], start=True, stop=True)
                    return ins
                op('p', [TK[iRKR], "c"], [psk(7)], mm_b)
                op('v', [psk(7)], ["bon"], lambda e: e.tensor_copy(out=bon[:], in_=PS[7][:, 0:8]))
                atl_m = [TT[0], TT[1]]
                rtl_m = [TT[2], TT[3]]
                for hh in range(2):
                    op('v' if hh == 0 else 'g', [TK[iATL], "c"], [TK[hh]], lambda e, hh=hh: e.tensor_scalar(out=atl_m[hh], in0=atl, scalar1=sel2[:, hh:hh + 1], scalar2=None, op0=ALU.mult))
                    op('v' if hh == 0 else 'g', [TK[iRTL], "c"], [TK[2 + hh]], lambda e, hh=hh: e.tensor_scalar(out=rtl_m[hh], in0=rtl, scalar1=sel2[:, hh:hh + 1], scalar2=None, op0=ALU.mult))
                mark("elem")
                for src, srckey, dst, dkey, pb in [(vF, "L", VT, "VT", 0), (btl, TK[iBTL], BT, "BT", 1), (ktl, TK[iKTL], KT, "KT", 6)]:
                    def trf(e, src=src, pb=pb):
                        ins = None
                        for j in range(4):
                            ins = e.transpose(out=PS[pb][:, j * 128:(j + 1) * 128], in_=src[:, j, :], identity=ident[:])
                        return ins
                    op('p', [srckey, "c"], [psk(pb)], trf)
                    if pb == 1:
                        op('a', [psk(pb)], [dkey], lambda e, dst=dst, pb=pb: e.activation(out=dst[:], in_=PS[pb][:], func=AF.Copy))
                    else:
                        op('v', [psk(pb)], [dkey], lambda e, dst=dst, pb=pb: e.tensor_copy(out=dst[:], in_=PS[pb][:]))
                mark("trans")
                for hg in range(2):
                    def hsl(t, hh, hg=hg):
                        h = hg * 4 + hh
                        return t[(h % 2) * 64:(h % 2) * 64 + 64, h // 2, :]
                    specs = [("X", "A", btl, maskSL, 2), ("XT", btl, "A", maskSU, 3),
                             ("akT", ktl, "A", maskSU, 4), ("rbT", btl, "R", maskUI, 5), ("rkT", ktl, "R", maskUI, 7)]

                    def pick(t, hl, hg=hg):
                        h = hg * 4 + hl
                        j, hh = h // 2, h % 2
                        if isinstance(t, str):
                            return (atl_m if t == "A" else rtl_m)[hh][:, j, :]
                        return t[:, j, :]
                    for nm, lt, rt, mk, pb in specs:
                        def mmT(e, lt=lt, rt=rt, pb=pb):
                            ins = None
                            for hl in range(4):
                                ins = e.matmul(PS[pb][:, hl * 128:(hl + 1) * 128], lhsT=pick(lt, hl), rhs=pick(rt, hl), start=True, stop=True)
                            return ins
                        op('p', [TK[0], TK[1], TK[2], TK[3], TK[iBTL], TK[iKTL]], [psk(pb)], mmT)
                        op('v', [psk(pb), "c"], ["m_" + nm], lambda e, nm=nm, mk=mk, pb=pb: e.tensor_tensor(
                            out=mats[nm][:], in0=P3(pb), in1=bc(mk, S4, 1), op=ALU.mult))
                        mark("mats_" + nm)
                    op('g', ["m_XT", "c"], ["m_QT"], lambda e: e.tensor_tensor(out=mats["QT"][:], in0=mats["XT"][:], in1=bc(ident[:], S4, 1), op=ALU.add))
                    mark("mats_qt")
                    cur, curT, nx, nxT = "X", "XT", "X2", "X2T"
                    for step in range(6):
                        def sq(e, a=curT, b_=cur):
                            ins = None
                            for hh in range(4):
                                ins = e.matmul(PS[2][:, hh * 128:(hh + 1) * 128], lhsT=mats[a][:, hh, :], rhs=mats[b_][:, hh, :], start=True, stop=True)
                            return ins
                        op('p', ["m_" + cur, "m_" + curT], [psk(2)], sq)
                        op('a', [psk(2)], ["m_" + nx], lambda e, nx=nx: e.activation(out=mats[nx][:], in_=P3(2), func=AF.Copy))
                        if step < 5:
                            def sqT(e, a=cur, b_=curT):
                                ins = None
                                for hh in range(4):
                                    ins = e.matmul(PS[3][:, hh * 128:(hh + 1) * 128], lhsT=mats[a][:, hh, :], rhs=mats[b_][:, hh, :], start=True, stop=True)
                                return ins
                            op('p', ["m_" + cur, "m_" + curT], [psk(3)], sqT)
                            op('v', [psk(3)], ["m_" + nxT], lambda e, nxT=nxT: e.tensor_copy(out=mats[nxT][:], in_=P3(3)))

                        def qu(e, a=nx):
                            ins = None
                            for hh in range(4):
                                ins = e.matmul(PS[4][:, hh * 128:(hh + 1) * 128], lhsT=mats[a][:, hh, :], rhs=mats["QT"][:, hh, :], start=True, stop=True)
                            return ins
                        op('p', ["m_" + nx, "m_QT"], [psk(4)], qu)
                        op('v', [psk(4), "m_QT"], ["m_QT"], lambda e: e.tensor_tensor(out=mats["QT"][:], in0=mats["QT"][:], in1=P3(4), op=ALU.add))
                        cur, curT, nx, nxT = nx, nxT, cur, curT
                        mark("mats_s%d" % step)
                    mark("mats")
                    c0 = hg * 256

                    def mm_rhs(e, hg=hg):
                        ins = None
                        for hl in range(4):
                            h = hg * 4 + hl
                            j = h // 2
                            hh = h % 2
                            reg = PS[0][:, hl * 64:(hl + 1) * 64]
                            e.matmul(reg, lhsT=atl[:, j, :], rhs=Stp[:, j, hh * 64:(hh + 1) * 64], start=True, stop=False)
                            ins = e.matmul(reg, lhsT=mats["akT"][:, hl, :], rhs=VT[:, h * 64:(h + 1) * 64], start=False, stop=True)
                        return ins
                    op('p', [TK[iATL], "Stp", "m_akT", "VT"], [psk(0)], mm_rhs)
                    op('v', [psk(0)], ["RHS"], lambda e, c0=c0: e.tensor_copy(out=RHS[:, c0:c0 + 256], in_=PS[0][:, 0:256]))

                    def mm_sa(e, hg=hg):
                        ins = None
                        for hl in range(4):
                            h = hg * 4 + hl
                            ins = e.matmul(PS[1][:, hl * 64:(hl + 1) * 64], lhsT=mats["QT"][:, hl, :], rhs=RHS[:, h * 64:(h + 1) * 64], start=True, stop=True)
                        return ins
                    op('p', ["m_QT", "RHS"], [psk(1)], mm_sa)
                    op('v', [psk(1)], ["SA"], lambda e, c0=c0: e.tensor_copy(out=SA[:, c0:c0 + 256], in_=PS[1][:, 0:256]))

                    def mm_y(e, hg=hg):
                        ins = None
                        for hl in range(4):
                            h = hg * 4 + hl
                            j = h // 2
                            hh = h % 2
                            reg = PS[6][:, hl * 64:(hl + 1) * 64]
                            e.matmul(reg, lhsT=rtl[:, j, :], rhs=Stp[:, j, hh * 64:(hh + 1) * 64], start=True, stop=False)
                            e.matmul(reg, lhsT=mats["rbT"][:, hl, :], rhs=SA[:, h * 64:(h + 1) * 64], start=False, stop=False)
                            ins = e.matmul(reg, lhsT=mats["rkT"][:, hl, :], rhs=VT[:, h * 64:(h + 1) * 64], start=False, stop=True)
                        return ins
                    op('p', [TK[iRTL], "Stp", "m_rbT", "m_rkT", "SA", "VT"], [psk(6)], mm_y)
                    op('a', [psk(6)], ["Y"], lambda e, c0=c0: e.activation(out=Y[:, c0:c0 + 256], in_=PS[6][:, 0:256], func=AF.Copy))

                    def mm_st(e, hg=hg):
                        ins = None
                        for jj in range(2):
                            j = hg * 2 + jj
                            reg = PS[5][:, jj * 128:(jj + 1) * 128]
                            e.matmul(reg, lhsT=ident[:], rhs=Stp[:, j, :], start=True, stop=False)
                            e.matmul(reg, lhsT=BT[:, j * 128:(j + 1) * 128], rhs=SA[:, j * 128:(j + 1) * 128], start=False, stop=False)
                            ins = e.matmul(reg, lhsT=KT[:, j * 128:(j + 1) * 128], rhs=VT[:, j * 128:(j + 1) * 128], start=False, stop=True)
                        return ins
                    op('p', ["Stp", "BT", "KT", "SA", "VT", "c"], [psk(5)], mm_st)
                    op('v', [psk(5), "PTbd"], ["Stp"], lambda e, hg=hg: e.tensor_tensor(
                        out=Stp[:, 2 * hg:2 * hg + 2, :], in0=PS[5][:, 0:256].rearrange("p (a b) -> p a b", a=2),
                        in1=PTbd[:, 2 * hg:2 * hg + 2, :], op=ALU.mult))
                mark("chain")
                Y3 = Y[:].rearrange("p (h v) -> p h v", h=8)
                Ysq = SCR[:, iY2 * 512:(iY2 + 1) * 512]
                G8 = [128, 8, 64]
                op('v', ["Y"], ["gst"], lambda e: e.tensor_reduce(out=gst[:, 0, :], in_=Y3, axis=AX.X, op=ALU.add))
                op('a', ["Y"], [TK[iY2]], lambda e: e.activation(out=Ysq, in_=Y[:], func=AF.Square))
                op('v', [TK[iY2]], ["gst"], lambda e: e.tensor_reduce(out=gst[:, 1, :], in_=Ysq.rearrange("p (h v) -> p h v", h=8), axis=AX.X, op=ALU.add))
                op('v', ["gst"], ["gst"], lambda e: e.tensor_scalar(out=gst[:, 2, :], in0=gst[:, 0, :], scalar1=1.0 / 64, scalar2=None, op0=ALU.mult))
                op('v', ["gst"], ["gst"], lambda e: e.tensor_tensor(out=gst[:, 3, :], in0=gst[:, 2, :], in1=gst[:, 2, :], op=ALU.mult))
                op('v', ["gst"], ["gst"], lambda e: e.scalar_tensor_tensor(out=gst[:, 4, :], in0=gst[:, 1, :], scalar=1.0 / 64, in1=gst[:, 3, :], op0=ALU.mult, op1=ALU.subtract))
                op('a', ["gst"], ["gst"], lambda e: e.activation(out=gst[:, 5, :], in_=gst[:, 4, :], func=AF.Sqrt, bias=GN_EPS, scale=1.0))
                op('v', ["gst"], ["gst"], lambda e: e.reciprocal(out=gst[:, 5, :], in_=gst[:, 5, :]))
                op('v', ["Y", "gst"], ["Y"], lambda e: e.tensor_tensor(out=Y3, in0=Y3, in1=bc(gst[:, 2, :], G8, 2), op=ALU.subtract))
                op('v', ["Y", "gst"], ["Y"], lambda e: e.tensor_tensor(out=Y3, in0=Y3, in1=bc(gst[:, 5, :], G8, 2), op=ALU.mult))
                op('g', ["Y", "c"], ["Y"], lambda e: e.tensor_tensor(out=Y[:], in0=Y[:], in1=lnw[:], op=ALU.mult))
                op('g', ["Y", "c"], ["Y"], lambda e: e.tensor_tensor(out=Y[:], in0=Y[:], in1=lnb[:], op=ALU.add))
                op('v', ["VT", "bon"], [TK[iY2]], lambda e: e.tensor_tensor(out=Ysq.rearrange("p (h v) -> p h v", h=8), in0=VT[:].rearrange("p (h v) -> p h v", h=8), in1=bc(bon[:], G8, 2), op=ALU.mult))
                op('v', ["Y", TK[iY2]], ["Y"], lambda e: e.tensor_tensor(out=Y[:], in0=Y[:], in1=Ysq, op=ALU.add))
                op('v', ["Y", "gT"], ["Y"], lambda e: e.tensor_tensor(out=Y[:], in0=Y[:], in1=gT[:], op=ALU.mult))

                def tr_ro(e):
                    ins = None
                    for j in range(4):
                        ins = e.transpose(out=PS[0][:, j * 128:(j + 1) * 128], in_=Y[:, j * 128:(j + 1) * 128], identity=ident[:])
                    return ins
                op('p', ["Y", "c"], [psk(0)], tr_ro)
                op('a', [psk(0)], ["roF"], lambda e: e.activation(out=roF[:], in_=P3(0), func=AF.Copy))
                for half in range(2):
                    pb = 1 + half

                    def mm_o(e, half=half, pb=pb):
                        ins = None
                        for q in range(4):
                            dt_ = half * 4 + q
                            for kt in range(4):
                                ins = e.matmul(PS[pb][:, q * 128:(q + 1) * 128], lhsT=w_o[:, kt, dt_ * 128:(dt_ + 1) * 128], rhs=roF[:, kt, :], start=(kt == 0), stop=(kt == 3))
                        return ins
                    op('p', ["w_o", "roF"], [psk(pb)], mm_o)
                    op('v', [psk(pb), "gates"], ["mixed"], lambda e, half=half, pb=pb: e.tensor_tensor(out=mixed[:, half * 4:(half + 1) * 4, :], in0=P3(pb), in1=gates[:, half * 4:(half + 1) * 4, :], op=ALU.mult))
                mark("epi")
                s5a, s5b, s5c, s5d_, btre, btim, wre, wim = [TT[i] for i in range(8)]
                ka, kb, kc, kd, kbr, kbi, kwr, kwi = [TK[i] for i in range(8)]
                for kt in range(4):
                    Er = Ere[:, 4 * kt:4 * kt + 4, :]
                    Ei = Eim[:, 4 * kt:4 * kt + 4, :]

                    def mm_bu(e, kt=kt):
                        ins = None
                        for q in range(4):
                            j = 4 * kt + q
                            e.matmul(PS[3][:, q * 128:(q + 1) * 128], lhsT=BBre[:, j, :], rhs=uB[:, kt, :], start=True, stop=True)
                            ins = e.matmul(PS[4][:, q * 128:(q + 1) * 128], lhsT=BBim[:, j, :], rhs=uB[:, kt, :], start=True, stop=True)
                        return ins
                    op('p', ["BB", "uB"], [psk(3), psk(4)], mm_bu)
                    tt('v', s5a, ka, Er, "E", P3(3), psk(3), ALU.mult)
                    tt('v', s5b, kb, Ei, "E", P3(4), psk(4), ALU.mult)
                    tt('g', btre, kbr, s5a, ka, s5b, kb, ALU.add)
                    tt('v', s5c, kc, Er, "E", P3(4), psk(4), ALU.mult)
                    tt('v', s5d_, kd, Ei, "E", P3(3), psk(3), ALU.mult)
                    tt('g', btim, kbi, s5c, kc, s5d_, kd, ALU.subtract)
                    for q in range(4):
                        j = 4 * kt + q
                        op('v', [kbr, "cw", "sp"], [kwr], lambda e, q=q, j=j: e.tensor_tensor_scan(out=wre[:, q, :], data0=sp[:, LAB, j:j + 1].to_broadcast([128, 128]), data1=btre[:, q, :], initial=cw[:, 0, j:j + 1], op0=ALU.mult, op1=ALU.add))
                        op('v', [kbi, "cw", "sp"], [kwi], lambda e, q=q, j=j: e.tensor_tensor_scan(out=wim[:, q, :], data0=sp[:, LAB, j:j + 1].to_broadcast([128, 128]), data1=btim[:, q, :], initial=cw[:, 1, j:j + 1], op0=ALU.mult, op1=ALU.add))
                    c128 = sp[:, CS2, 4 * kt:4 * kt + 4]
                    s128 = sp[:, SN2, 4 * kt:4 * kt + 4]
                    wr127 = wre[:, :, 127]
                    wi127 = wim[:, :, 127]
                    tt('g', cwn[:, 0, :], "cwn", c128, "sp", wr127, kwr, ALU.mult)
                    tt('g', cwn[:, 1, :], "cwn", s128, "sp", wi127, kwi, ALU.mult)
                    tt('g', cwn[:, 2, :], "cwn", s128, "sp", wr127, kwr, ALU.mult)
                    tt('g', cwn[:, 3, :], "cwn", c128, "sp", wi127, kwi, ALU.mult)
                    tt('v', cw[:, 0, 4 * kt:4 * kt + 4], "cw", cwn[:, 0, :], "cwn", cwn[:, 1, :], "cwn", ALU.subtract)
                    tt('v', cw[:, 1, 4 * kt:4 * kt + 4], "cw", cwn[:, 2, :], "cwn", cwn[:, 3, :], "cwn", ALU.add)
                    tt('v', s5a, ka, Er, "E", wre, kwr, ALU.mult)
                    tt('v', s5b, kb, Ei, "E", wim, kwi, ALU.mult)
                    tt('g', xre[:], "xre", s5a, ka, s5b, kb, ALU.subtract)
                    tt('v', s5c, kc, Ei, "E", wre, kwr, ALU.mult)
                    tt('v', s5d_, kd, Er, "E", wim, kwi, ALU.mult)
                    tt('g', xim[:], "xim", s5c, kc, s5d_, kd, ALU.add)

                    def mm_c(e, kt=kt):
                        ins = None
                        for q in range(4):
                            j = 4 * kt + q
                            e.matmul(PS[5][:, kt * 128:(kt + 1) * 128], lhsT=CCre[:, j, :], rhs=xre[:, q, :], start=(q == 0), stop=False)
                            ins = e.matmul(PS[5][:, kt * 128:(kt + 1) * 128], lhsT=CCimN[:, j, :], rhs=xim[:, q, :], start=False, stop=(q == 3))
                        return ins
                    op('p', ["CC", "xre", "xim"], [psk(5)], mm_c)
                tt('g', s5a, ka, uF[:], "uF", bc(s5d, S4, 2), "c", ALU.mult)
                tt('v', s5a, ka, s5a, ka, P3(5), psk(5), ALU.add)
                gelu_tanh(op, s5a, ka, s5b, kb, ygB[:], "ygB")
                for half in range(2):
                    for vg in range(2):
                        pb = 6 + vg

                        def mm_g(e, half=half, vg=vg, pb=pb):
                            ins = None
                            for q in range(4):
                                col = vg * 8 + half * 4 + q
                                for kt in range(4):
                                    ins = e.matmul(PS[pb][:, q * 128:(q + 1) * 128], lhsT=w_glu[:, kt, col * 128:(col + 1) * 128], rhs=ygB[:, kt, :], start=(kt == 0), stop=(kt == 3))
                            return ins
                        op('p', ["w_glu", "ygB"], [psk(pb)], mm_g)
                    zh = zg[:, half * 4:(half + 1) * 4, :]
                    op('a', [psk(7)], ["zg"], lambda e, zh=zh: e.activation(out=zh, in_=P3(7), func=AF.Sigmoid))
                    tt('v', zh, "zg", zh, "zg", P3(6), psk(6), ALU.mult)
                    tt('g', zh, "zg", zh, "zg", gates[:, 8 + half * 4:8 + (half + 1) * 4, :], "gates", ALU.mult)
                    tt('v', mixedB[:, half * 4:(half + 1) * 4, :], "mixedB", zh, "zg", mixed[:, half * 4:(half + 1) * 4, :], "mixed", ALU.add)
                mark("s5")
                for half in range(2):
                    pb = 1 + half

                    def mm_h(e, half=half, pb=pb):
                        ins = None
                        for kt in range(8):
                            ins = e.matmul(PS[pb][:], lhsT=mixedB[:, kt, :], rhs=w_out[:, kt, half * 512:(half + 1) * 512], start=(kt == 0), stop=(kt == 7))
                        return ins
                    op('p', ["w_out", "mixedB"], [psk(pb)], mm_h)
                    op('v', [psk(pb), xk, "stat"], ["h1"], lambda e, half=half, pb=pb: e.scalar_tensor_tensor(
                        out=h1[:, half * 512:(half + 1) * 512], in0=xcur[:, half * 512:(half + 1) * 512], scalar=stat[:, 1:2], in1=PS[pb][:], op0=ALU.mult, op1=ALU.add))
                dma('s', 'dh1', h1_d[it * 128:(it + 1) * 128, :], h1[:], ["h1"], ["h1d%d" % it])
            sch.barrier()
         except _Stop:
            sch.barrier()

        if 2 in phases:
          with ExitStack() as es:
            def sb(name, shape, dt=F32):
                return es.enter_context(nc.sbuf_tensor("s_" + name, shape, dt))
            ident = sb("ident2", [128, 128])
            gffn = sb("gffn", [128, D])
            gfin = sb("gfin", [128, D])
            wq = sb("wq", [128, 8, D])
            subk = sb("subk", [128, 8, 128])
            h1t = [sb("h1t%d" % i, [128, D]) for i in range(2)]
            xn2 = sb("xn2", [128, D])
            junk = sb("junk", [128, D])
            xn2F = sb("xn2F", [128, 8, 128])
            qF = sb("qF", [128, 8, 128])
            sc = sb("sc", [128, 16, 128])
            scr = sb("scr", [128, 256])
            sv = sb("sv", [128, 16, 16])
            siu = sb("siu", [128, 16, 16], U32)
            sif = sb("sif", [128, 16, 16])
            dsi = sb("dsi", [128, 16, 16])
            cand = sb("cand", [128, 8, 256])
            cv = sb("cv", [128, 8, 16])
            ciu = sb("ciu", [128, 8, 16], U32)
            iiu = sb("iiu", [128, 8, 16], U32)
            jju = sb("jju", [128, 8, 16], U32)
            iif = sb("iif", [128, 8, 16])
            jjf = sb("jjf", [128, 8, 16])
            i1 = sb("i1", [128, 8, 16])
            i2 = sb("i2", [128, 8, 16])
            lt = sb("lt", [128, 8, 16])
            ei = sb("ei", [128, 128], I32)
            gate = sb("gate", [128, 8, 16])
            sm = sb("sm", [128, 4, 8])
            hid = sb("hid", [128, 128])
            wgt = sb("wgt", [128, 128])
            stat2 = sb("stat2", [128, 8])
            U = [sb("U%d" % i, [128, D]) for i in range(NB)]
            Vb = [sb("Vb%d" % i, [128, D]) for i in range(NB)]
            acc = [sb("acc%d" % i, [128, D]) for i in range(2)]
            outt = sb("outt", [128, D])

            dma('s', 'dc2', ident[:], ident_d, [], ["c"])
            dma('s', 'dc2', gffn[:], gffn_d, [], ["c"])
            dma('s', 'dc2', gfin[:], gfin_d, [], ["c"])
            dma('s', 'dc2', subk[:].rearrange("p a b -> p (a b)"), subk_d, [], ["c"])
            dma('s', 'dc2', wq[:], wq_d.rearrange("(kt p) c -> p kt c", p=128), [], ["c"])

            def rms(src, srck, dstat):
                op('a', [srck], ["junk", "stat2"], lambda e: e.activation(out=junk[:, 0:512], in_=src[:, 0:512], func=AF.Square, accum_out=dstat[:, 0:1]))
                op('a', [srck], ["junk", "stat2"], lambda e: e.activation(out=junk[:, 512:1024], in_=src[:, 512:1024], func=AF.Square, accum_out=dstat[:, 3:4]))
                op('v', ["stat2"], ["stat2"], lambda e: e.tensor_tensor(out=dstat[:, 0:1], in0=dstat[:, 0:1], in1=dstat[:, 3:4], op=ALU.add))
                op('a', ["stat2"], ["stat2"], lambda e: e.activation(out=dstat[:, 1:2], in_=dstat[:, 0:1], func=AF.Sqrt, bias=1e-6, scale=1.0 / D))
                op('v', ["stat2"], ["stat2"], lambda e: e.reciprocal(out=dstat[:, 2:3], in_=dstat[:, 1:2]))

            def top16(seg, segk, width, vout, iout, outk):
                op('v', [segk], [outk], lambda e: e.max(out=vout[:, 0:8], in_=seg))
                op('v', [segk, outk], [outk], lambda e: e.max_index(out=iout[:, 0:8], in_max=vout[:, 0:8], in_values=seg))
                op('v', [segk, outk], ["scr"], lambda e: e.match_replace(out=scr[:, 0:width], in_to_replace=vout[:, 0:8], in_values=seg, imm_value=NEG))
                op('v', ["scr"], [outk], lambda e: e.max(out=vout[:, 8:16], in_=scr[:, 0:width]))
                op('v', ["scr", outk], [outk], lambda e: e.max_index(out=iout[:, 8:16], in_max=vout[:, 8:16], in_values=scr[:, 0:width]))

            dma('s', 'dh0', h1t[0][:], h1_d[0:128, :], ["h1d0"], ["h1t0"])
            gcount = [0, 0]
            for it in range(NT):
                hb = it % 2
                hk = "h1t%d" % hb
                hcur = h1t[hb]
                if it + 1 < NT:
                    dma('s', 'dh%d' % (1 - hb), h1t[1 - hb][:], h1_d[(it + 1) * 128:(it + 2) * 128, :], ["h1d%d" % (it + 1)], ["h1t%d" % (1 - hb)])
                rms(hcur, hk, stat2)
                op('v', [hk, "stat2", "c"], ["xn2"], lambda e: e.scalar_tensor_tensor(out=xn2[:], in0=hcur[:], scalar=stat2[:, 2:3], in1=gffn[:], op0=ALU.mult, op1=ALU.mult))
                for half in range(2):
                    def tr2(e, half=half):
                        ins = None
                        for q in range(4):
                            kt = half * 4 + q
                            ins = e.transpose(out=PS[half][:, q * 128:(q + 1) * 128], in_=xn2[:, kt * 128:(kt + 1) * 128], identity=ident[:])
                        return ins
                    op('p', ["xn2", "c"], [psk(half)], tr2)
                    if half == 0:
                        op('v', [psk(0)], ["xn2F"], lambda e: e.tensor_copy(out=xn2F[:, 0:4, :], in_=P3(0)))
                    else:
                        op('a', [psk(1)], ["xn2F"], lambda e: e.activation(out=xn2F[:, 4:8, :], in_=P3(1), func=AF.Copy))
                for half in range(2):
                    pb = 2 + half

                    def mm_q(e, half=half, pb=pb):
                        ins = None
                        for q in range(4):
                            ct = half * 4 + q
                            for kt in range(8):
                                ins = e.matmul(PS[pb][:, q * 128:(q + 1) * 128], lhsT=wq[:, kt, ct * 128:(ct + 1) * 128], rhs=xn2F[:, kt, :], start=(kt == 0), stop=(kt == 7))
                        return ins
                    op('p', ["c", "xn2F"], [psk(pb)], mm_q)
                    if half == 0:
                        op('v', [psk(pb)], ["qF"], lambda e, pb=pb: e.tensor_copy(out=qF[:, 0:4, :], in_=P3(pb)))
                    else:
                        op('a', [psk(pb)], ["qF"], lambda e, pb=pb: e.activation(out=qF[:, 4:8, :], in_=P3(pb), func=AF.Copy))
                sc4 = sc[:].rearrange("p (h c) n -> p h c n", c=2)
                for hg in range(2):
                    for c in range(2):
                        pb = 4 + hg * 2 + c

                        def mm_s(e, hg=hg, c=c, pb=pb):
                            ins = None
                            for q in range(4):
                                h = hg * 4 + q
                                ins = e.matmul(PS[pb][:, q * 128:(q + 1) * 128], lhsT=qF[c * 64:(c + 1) * 64, h, :], rhs=subk[c * 64:(c + 1) * 64, h, :], start=True, stop=True)
                            return ins
                        op('p', ["qF", "c"], [psk(pb)], mm_s)
                        if c == 0:
                            op('v', [psk(pb)], ["sc"], lambda e, hg=hg, c=c, pb=pb: e.tensor_copy(out=sc4[:, hg * 4:(hg + 1) * 4, c, :], in_=P3(pb)))
                        else:
                            op('a', [psk(pb)], ["sc"], lambda e, hg=hg, c=c, pb=pb: e.activation(out=sc4[:, hg * 4:(hg + 1) * 4, c, :], in_=P3(pb), func=AF.Copy))
                for s_ in range(16):
                    top16(sc[:, s_, :], "sc", 128, sv[:, s_, :], siu[:, s_, :], "svi")
                for h in range(8):
                    op('g', ["svi"], ["cand"], lambda e, h=h: e.tensor_tensor(
                        out=cand[:, h, :].rearrange("p (i j) -> p i j", i=16),
                        in0=bc(sv[:, 2 * h, :], [128, 16, 16], 2), in1=bc(sv[:, 2 * h + 1, :], [128, 16, 16], 1), op=ALU.add))
                for h in range(8):
                    top16(cand[:, h, :], "cand", 256, cv[:, h, :], ciu[:, h, :], "cvi")
                op('v', ["cvi"], ["iiu"], lambda e: e.tensor_single_scalar(out=iiu[:], in_=ciu[:], scalar=4, op=ALU.logical_shift_right))
                op('v', ["cvi"], ["jju"], lambda e: e.tensor_single_scalar(out=jju[:], in_=ciu[:], scalar=15, op=ALU.bitwise_and))
                op('v', ["iiu"], ["iif"], lambda e: e.tensor_copy(out=iif[:], in_=iiu[:]))
                op('v', ["jju"], ["jjf"], lambda e: e.tensor_copy(out=jjf[:], in_=jju[:]))
                op('v', ["svi"], ["sif"], lambda e: e.tensor_copy(out=sif[:], in_=siu[:]))
                op('v', [], ["i1"], lambda e: e.memset(i1[:], 0.0))
                op('v', [], ["i2"], lambda e: e.memset(i2[:], 0.0))
                sif4 = sif[:].rearrange("p (h c) k -> p h c k", c=2)
                for m in range(16):
                    op('v', ["iif", "sif"], ["lt"], lambda e, m=m: e.scalar_tensor_tensor(out=lt[:], in0=iif[:], scalar=float(m), in1=sif4[:, :, 0, m:m + 1].to_broadcast([128, 8, 16]), op0=ALU.is_equal, op1=ALU.mult))
                    op('v', ["lt", "i1"], ["i1"], lambda e: e.tensor_tensor(out=i1[:], in0=i1[:], in1=lt[:], op=ALU.add))
                    op('v', ["jjf", "sif"], ["dsi"], lambda e, m=m: e.scalar_tensor_tensor(out=dsi[:, 0:8, :], in0=jjf[:], scalar=float(m), in1=sif4[:, :, 1, m:m + 1].to_broadcast([128, 8, 16]), op0=ALU.is_equal, op1=ALU.mult))
                    op('g', ["dsi", "i2"], ["i2"], lambda e: e.tensor_tensor(out=i2[:], in0=i2[:], in1=dsi[:, 0:8, :], op=ALU.add))
                op('v', ["i1", "i2"], ["i1"], lambda e: e.scalar_tensor_tensor(out=i1[:], in0=i1[:], scalar=128.0, in1=i2[:], op0=ALU.mult, op1=ALU.add))
                op('v', ["i1"], ["ei"], lambda e: e.tensor_copy(out=ei[:], in_=i1[:].rearrange("p h k -> p (h k)")))
                op('v', ["cvi"], ["sm"], lambda e: e.tensor_reduce(out=sm[:, 0, :], in_=cv[:], axis=AX.X, op=ALU.max))
                op('v', ["cvi", "sm"], ["gate"], lambda e: e.tensor_tensor(out=gate[:], in0=cv[:], in1=bc(sm[:, 0, :], [128, 8, 16], 2), op=ALU.subtract))
                op('a', ["gate"], ["gate"], lambda e: e.activation(out=gate[:], in_=gate[:], func=AF.Exp))
                op('v', ["gate"], ["sm"], lambda e: e.tensor_reduce(out=sm[:, 1, :], in_=gate[:], axis=AX.X, op=ALU.add))
                op('v', ["sm"], ["sm"], lambda e: e.reciprocal(out=sm[:, 2, :], in_=sm[:, 1, :]))
                op('v', ["gate", "sm"], ["gate"], lambda e: e.tensor_tensor(out=gate[:], in0=gate[:], in1=bc(sm[:, 2, :], [128, 8, 16], 2), op=ALU.mult))
                op('v', [], ["hid"], lambda e: e.memset(hid[:], 0.0))
                for s_ in range(128):
                    b = gcount[0] % NB
                    gcount[0] += 1
                    dma('g', 'du%d' % b, U[b][:], pu_d, ["ei"], ["U%d" % b], in_offset=bass.IndirectOffsetOnAxis(ap=ei[:, s_:s_ + 1], axis=0))
                    op('v', ["U%d" % b, "xn2"], ["junk", "hid"], lambda e, b=b, s_=s_: e.scalar_tensor_tensor(
                        out=junk[:], in0=U[b][:], scalar=1.0, in1=xn2[:], op0=ALU.mult, op1=ALU.mult, accum_out=hid[:, s_:s_ + 1]))
                gelu_tanh(op, hid[:], "hid", lt[:].rearrange("p h k -> p (h k)"), "lt", wgt[:], "wgt")
                op('v', ["wgt", "gate"], ["wgt"], lambda e: e.tensor_tensor(out=wgt[:], in0=wgt[:], in1=gate[:].rearrange("p h k -> p (h k)"), op=ALU.mult))
                op('v', [], ["acc0"], lambda e: e.memset(acc[0][:], 0.0))
                op('g', [], ["acc1"], lambda e: e.memset(acc[1][:], 0.0))
                for s_ in range(128):
                    b = gcount[1] % NB
                    gcount[1] += 1
                    a_ = s_ % 2
                    dma('g', 'dv%d' % b, Vb[b][:], pvv_d, ["ei"], ["Vb%d" % b], in_offset=bass.IndirectOffsetOnAxis(ap=ei[:, s_:s_ + 1], axis=0))
                    op('v', ["Vb%d" % b, "wgt", "acc%d" % a_], ["acc%d" % a_], lambda e, b=b, s_=s_, a_=a_: e.scalar_tensor_tensor(
                        out=acc[a_][:], in0=Vb[b][:], scalar=wgt[:, s_:s_ + 1], in1=acc[a_][:], op0=ALU.mult, op1=ALU.add))
                op('v', ["acc0", "acc1"], ["acc0"], lambda e: e.tensor_tensor(out=acc[0][:], in0=acc[0][:], in1=acc[1][:], op=ALU.add))
                op('v', ["acc0", hk], ["acc0"], lambda e: e.tensor_tensor(out=acc[0][:], in0=acc[0][:], in1=hcur[:], op=ALU.add))
                rms(acc[0], "acc0", stat2)
                op('v', ["acc0", "stat2", "c"], ["outt"], lambda e: e.scalar_tensor_tensor(out=outt[:], in0=acc[0][:], scalar=stat2[:, 2:3], in1=gfin[:], op0=ALU.mult, op1=ALU.mult))
                dma('s', 'dout', out_d[it * 128:(it + 1) * 128, :], outt[:], ["outt"], ["od%d" % it])
            sch.barrier()
    return nc


def make_inputs(inp, b):
    f = lambda a: np.ascontiguousarray(np.asarray(a), dtype=np.float32)
    colT = lambda v, n: f(np.asarray(v).reshape(n, 128).T)
    m = {}
    m["x"] = f(inp["x"][b])
    m["w_in"] = f(inp["w_in"][0])
    m["gainP"] = colT(inp["norm_mix"][0], 8)
    m["bgate"] = colT(inp["b_gate"][0], 16)
    m["mu"] = colT(inp["mu_rwkv"][0], 14)
    pv = np.stack([colT(inp["w0"][0], 4), colT(inp["a0"][0], 4), colT(inp["k_k"][0], 4), colT(inp["k_a"][0], 4),
                   colT(np.asarray(inp["r_k"][0]).reshape(512), 4), colT(np.asarray(inp["s5_d"][0]).reshape(512), 4)], axis=1)
    m["pvec"] = f(pv)
    m["lora"] = f(np.concatenate([np.asarray(inp["w_lora_up"][0]), np.asarray(inp["a_lora_up"][0])], axis=0))
    m["glora"] = f(inp["g_lora_up"][0])
    m["lnw"] = f(np.broadcast_to(np.asarray(inp["ln_x_w"][0])[None, :], (128, 512)))
    m["lnb"] = f(np.broadcast_to(np.asarray(inp["ln_x_b"][0])[None, :], (128, 512)))
    m["w_o"] = f(inp["w_o_rwkv"][0])
    m["w_glu"] = f(inp["w_glu_s5"][0])
    m["w_out"] = f(inp["w_out"][0])
    a_re = np.asarray(inp["s5_a_re"][0]).reshape(16, 128).T
    a_im = np.asarray(inp["s5_a_im"][0]).reshape(16, 128).T
    ldt = np.repeat(np.asarray(inp["s5_log_dt"][0]), 64).reshape(16, 128).T
    m["s5p"] = f(np.stack([a_re, a_im, ldt], axis=1))
    bbre = np.zeros((128, 16, 128), np.float32)
    bbim = np.zeros((128, 16, 128), np.float32)
    ccre = np.zeros((128, 16, 128), np.float32)
    ccim = np.zeros((128, 16, 128), np.float32)
    b_re = np.asarray(inp["s5_b_re"][0]); b_im = np.asarray(inp["s5_b_im"][0])
    c_re = np.asarray(inp["s5_c_re"][0]); c_im = np.asarray(inp["s5_c_im"][0])
    for g in range(32):
        j, gl, g8 = g // 2, g % 2, g % 8
        bbre[g8 * 16:(g8 + 1) * 16, j, gl * 64:(gl + 1) * 64] = b_re[g].T
        bbim[g8 * 16:(g8 + 1) * 16, j, gl * 64:(gl + 1) * 64] = b_im[g].T
        ccre[gl * 64:(gl + 1) * 64, j, g8 * 16:(g8 + 1) * 16] = c_re[g].T
        ccim[gl * 64:(gl + 1) * 64, j, g8 * 16:(g8 + 1) * 16] = c_im[g].T
    m["bbre"] = bbre.reshape(128, 2048)
    m["bbim"] = bbim.reshape(128, 2048)
    m["ccre"] = ccre.reshape(128, 2048)
    m["ccim"] = ccim.reshape(128, 2048)
    m["gffn"] = f(np.broadcast_to(np.asarray(inp["norm_ffn"][0])[None, :], (128, D)))
    m["gfin"] = f(np.broadcast_to(np.asarray(inp["norm_final"])[None, :], (128, D)))
    m["wq"] = f(inp["peer_wq"][0])
    m["subk"] = f(np.asarray(inp["peer_subkeys"][0]).transpose(1, 3, 0, 2).reshape(128, 1024))
    m["peer_u"] = f(inp["peer_u"][0])
    m["peer_v"] = f(inp["peer_v"][0])
    m["ident"] = np.eye(128, dtype=np.float32)
    p = np.arange(128)[:, None]
    jx = np.arange(128)[None, :]
    masks = np.zeros((128, 5, 128), np.float32)
    masks[:, 0] = (jx < p)
    masks[:, 1] = (jx > p)
    masks[:, 2] = (jx >= p)
    masks[:, 3] = ((jx // 64) == (p // 64))
    masks[:, 4] = 1.0
    masks[:, 4, 0] = 0.0
    m["masks"] = masks
    sel2 = np.zeros((128, 2), np.float32)
    sel2[:64, 0] = 1.0
    sel2[64:, 1] = 1.0
    m["sel2"] = sel2
    return m


_NC_CACHE = {}


def kernel(**inputs):
    n = 8
    if "nc" not in _NC_CACHE:
        _NC_CACHE["nc"] = build_nc()
    nc = _NC_CACHE["nc"]
    in_maps = [make_inputs(inputs, b) for b in range(n)]
    res = run_bass_kernel_spmd(nc, in_maps, core_ids=list(range(n)))
    out = np.stack([np.asarray(r["out"], dtype=np.float32) for r in res.results], axis=0)
    return out
```

```python
import numpy as np
from contextlib import ExitStack
import concourse.bass as bass
import concourse.mybir as mybir
from concourse.bass_utils import run_bass_kernel_spmd

F32 = mybir.dt.float32
BF16 = mybir.dt.bfloat16
I32 = mybir.dt.int32
U32 = mybir.dt.uint32
ALU = mybir.AluOpType
AF = mybir.ActivationFunctionType
AX = mybir.AxisListType

D = 1024
NRW = 1792
NCOL = 4352
SEQ = 4096
NCT = 34
C0 = float(np.exp(-0.5))
PI = float(np.pi)
GN_EPS = 64e-5
NB = 16
NEG = -1.0e30


class Sched:
    def __init__(self, nc, es):
        self.nc = nc
        self.es = es
        self.engs = {'v': nc.vector, 'a': nc.scalar, 'p': nc.tensor, 'g': nc.gpsimd, 's': nc.sync}
        self.sems = {}
        self.val = {}
        self.waited = {e: {} for e in self.engs}
        self.lastw = {}
        self.readers = {}
        self.nins = 0
        for e in 'vapg':
            self._mk(e)

    def _mk(self, key):
        self.sems[key] = self.es.enter_context(self.nc.semaphore('sem_' + key))
        self.val[key] = 0

    def _wait(self, e, k, v):
        if self.waited[e].get(k, 0) >= v:
            return
        self.engs[e].wait_ge(self.sems[k], v)
        self.waited[e][k] = v

    def _deps(self, e, reads, writes):
        for b in reads:
            if b in self.lastw:
                self._wait(e, *self.lastw[b])
        for b in writes:
            if b in self.lastw:
                self._wait(e, *self.lastw[b])
            for k, v in self.readers.get(b, {}).items():
                self._wait(e, k, v)

    def _commit(self, tok, reads, writes):
        k, v = tok
        for b in reads:
            self.readers.setdefault(b, {})[k] = v
        for b in writes:
            self.lastw[b] = tok
            self.readers[b] = {}

    def op(self, e, reads, writes, fn):
        self._deps(e, reads, writes)
        ins = fn(self.engs[e])
        self.val[e] += 1
        ins.then_inc(self.sems[e], 1)
        self._commit((e, self.val[e]), reads, writes)
        self.nins += 1

    def dma(self, q, semkey, out, in_, reads, writes, in_offset=None):
        if semkey not in self.sems:
            self._mk(semkey)
        self._deps(q, reads, writes)
        if self.val[semkey] > 0:
            self._wait(q, semkey, self.val[semkey])
        eng = self.engs[q]
        if in_offset is not None:
            ins = eng.indirect_dma_start(out=out, out_offset=None, in_=in_, in_offset=in_offset)
        else:
            ins = eng.dma_start(out=out, in_=in_)
        self.val[semkey] += 16
        ins.then_inc(self.sems[semkey], 16)
        self._commit((semkey, self.val[semkey]), reads, writes)
        self.nins += 1

    def barrier(self):
        for e in self.engs:
            for k, v in self.val.items():
                if v > 0:
                    self._wait(e, k, v)
        self.lastw = {}
        self.readers = {}


def bc(ap, shape, axis):
    return ap.unsqueeze(axis).to_broadcast(shape)


def gelu_tanh(op, src, srck, tmp, tmpk, dst, dstk, sq_eng='g'):
    op(sq_eng, [srck], [tmpk], lambda e: e.tensor_tensor(out=tmp, in0=src, in1=src, op=ALU.mult))
    op('v', [tmpk], [tmpk], lambda e: e.tensor_scalar(out=tmp, in0=tmp, scalar1=0.044715, scalar2=1.0, op0=ALU.mult, op1=ALU.add))
    op('v', [tmpk, srck], [tmpk], lambda e: e.tensor_tensor(out=tmp, in0=tmp, in1=src, op=ALU.mult))
    op('a', [tmpk], [tmpk], lambda e: e.activation(out=tmp, in_=tmp, func=AF.Sigmoid, scale=1.5957691216057308))
    op('v', [tmpk, srck], [dstk], lambda e: e.tensor_tensor(out=dst, in0=tmp, in1=src, op=ALU.mult))


class _Stop(Exception):
    pass


def build_nc(NT=32, debug=False, phases=(1, 2), stop=None):
    nc = bass.Bass("TRN2", target_bir_lowering=False)

    def mark(name):
        if stop == name:
            raise _Stop()

    def din(name, shape, dt=F32):
        return nc.dram_tensor(name, shape, dt, kind="ExternalInput").ap()

    x_d = din("x", [SEQ, D])
    w_in_d = din("w_in", [D, NCOL])
    gainP_d = din("gainP", [128, 8])
    bgate_d = din("bgate", [128, 16])
    mu_d = din("mu", [128, 14])
    pv_d = din("pvec", [128, 6, 4])
    lora_d = din("lora", [128, 512])
    glora_d = din("glora", [128, 512])
    lnw_d = din("lnw", [128, 512])
    lnb_d = din("lnb", [128, 512])
    w_o_d = din("w_o", [512, D])
    w_glu_d = din("w_glu", [512, 2 * D])
    w_out_d = din("w_out", [D, D])
    s5p_d = din("s5p", [128, 3, 16])
    bbre_d = din("bbre", [128, 2048])
    bbim_d = din("bbim", [128, 2048])
    ccre_d = din("ccre", [128, 2048])
    ccim_d = din("ccim", [128, 2048])
    gffn_d = din("gffn", [128, D])
    gfin_d = din("gfin", [128, D])
    wq_d = din("wq", [D, D])
    subk_d = din("subk", [128, 1024])
    pu_d = din("peer_u", [16384, D])
    pvv_d = din("peer_v", [16384, D])
    ident_d = din("ident", [128, 128])
    masks_d = din("masks", [128, 5, 128])
    sel2_d = din("sel2", [128, 2])
    iota_d = din("iota16", [128, 16])
    out_d = nc.dram_tensor("out", [SEQ, D], F32, kind="ExternalOutput").ap()
    h1_d = nc.dram_tensor("h1s", [SEQ, D], F32, kind="ExternalOutput" if debug else "Internal").ap()
    wsc_d = nc.dram_tensor("wsc", [NCT, 128, 1024], BF16, kind="Internal").ap()
    tub_d = nc.dram_tensor("tub", [16384, D], BF16, kind="Internal").ap()
    tvb_d = nc.dram_tensor("tvb", [16384, D], BF16, kind="Internal").ap()

    with ExitStack() as es0:
        sch = Sched(nc, es0)
        op = sch.op
        dma = sch.dma
        PS = [es0.enter_context(nc.psum_tensor("ps%d" % i, [128, 512], F32)) for i in range(8)]

        def psk(i):
            return "ps%d" % i

        def P3(i):
            return PS[i][:].rearrange("p (a b) -> p a b", a=4)

        if 1 in phases:
         try:
          with ExitStack() as es:
            def sb(name, shape, dt=F32):
                return es.enter_context(nc.sbuf_tensor("s_" + name, shape, dt))

            ident = sb("ident", [128, 128])
            masks = sb("masks", [128, 5, 128])
            sel2 = sb("sel2", [128, 2])
            gainP = sb("gainP", [128, 8])
            bgate = sb("bgate", [128, 16])
            mu = sb("mu", [128, 14])
            pvec = sb("pvec", [128, 6, 4])
            lnw = sb("lnw", [128, 512])
            lnb = sb("lnb", [128, 512])
            s5p = sb("s5p", [128, 3, 16])
            sp = sb("sp", [128, 24, 16])
            lora = sb("lora", [128, 512], BF16)
            glora = sb("glora", [128, 512], BF16)
            w_o = sb("w_o", [128, 4, 1024], BF16)
            w_glu = sb("w_glu", [128, 4, 2048], BF16)
            w_out = sb("w_out", [128, 8, 1024], BF16)
            BBre = sb("BBre", [128, 16, 128], BF16)
            BBim = sb("BBim", [128, 16, 128], BF16)
            CCre = sb("CCre", [128, 16, 128], BF16)
            CCimN = sb("CCimN", [128, 16, 128], BF16)
            Ere = sb("Ere", [128, 16, 128])
            Eim = sb("Eim", [128, 16, 128])
            SCR = sb("SCR", [128, 8192])
            esp = ExitStack()
            stgb = [esp.enter_context(nc.sbuf_tensor("s_stgb%d" % i, [128, 1024], BF16)) for i in range(2)]

            for t_sb, t_d in [(ident, ident_d), (masks, masks_d), (sel2, sel2_d), (gainP, gainP_d),
                              (bgate, bgate_d), (mu, mu_d), (pvec, pv_d), (lnw, lnw_d), (lnb, lnb_d),
                              (s5p, s5p_d)]:
                dma('s', 'dc', t_sb[:], t_d, [], ["c"])
            maskSL = masks[:, 0, :]
            maskSU = masks[:, 1, :]
            maskUI = masks[:, 2, :]
            maskBD = masks[:, 3, :]
            scanmask = masks[:, 4, :]

            stg = [SCR[:, 0:2048], SCR[:, 2048:4096]]
            tmpA = SCR[:, 4096:6144]
            tmpB = SCR[:, 6144:8192]

            w_in_v = w_in_d.rearrange("(kt p) c -> p kt c", p=128)
            for c in range(NCT):
                b = c % 2
                sg = "stg%d" % b
                sgb = "stgb%d" % b
                dma('s', 'dst%d' % b, stg[b][:, 0:1024].rearrange("p (k c) -> p k c", k=8),
                    w_in_v[:, :, c * 128:(c + 1) * 128], [], [sg])
                op('v', [sg, "c"], [sgb], lambda e, b=b: e.tensor_tensor(
                    out=stgb[b][:].rearrange("p (k c) -> p k c", k=8),
                    in0=stg[b][:, 0:1024].rearrange("p (k c) -> p k c", k=8),
                    in1=bc(gainP[:], [128, 8, 128], 2), op=ALU.mult))
                dma('s', 'dsb%d' % b, wsc_d[c], stgb[b][:], [sgb], ["wsc%d" % c])

            ldn = [0]

            def load_cast(dst_ap, src_ap, width, dstkey):
                b = ldn[0] % 2
                ldn[0] += 1
                sg = "stg%d" % b
                dma('s', 'dst%d' % b, stg[b][:, 0:width], src_ap, [], [sg])
                if b == 0:
                    op('v', [sg], [dstkey], lambda e: e.tensor_copy(out=dst_ap, in_=stg[b][:, 0:width]))
                else:
                    op('a', [sg], [dstkey], lambda e: e.activation(out=dst_ap, in_=stg[b][:, 0:width], func=AF.Copy))

            load_cast(lora[:], lora_d, 512, "lora")
            load_cast(glora[:], glora_d, 512, "glora")
            for k in range(4):
                load_cast(w_o[:, k, :], w_o_d[k * 128:(k + 1) * 128, :], 1024, "w_o")
            for k in range(4):
                load_cast(w_glu[:, k, :], w_glu_d[k * 128:(k + 1) * 128, :], 2048, "w_glu")
            for k in range(8):
                load_cast(w_out[:, k, :], w_out_d[k * 128:(k + 1) * 128, :], 1024, "w_out")
            load_cast(BBre[:].rearrange("p a b -> p (a b)"), bbre_d, 2048, "BB")
            load_cast(BBim[:].rearrange("p a b -> p (a b)"), bbim_d, 2048, "BB")

            def V2(i):
                return sp[:, i, :]
            a_re = s5p[:, 0, :]
            a_im = s5p[:, 1, :]
            ldt = s5p[:, 2, :]
            DT, TH, LAB, CS, SN, R, M_, LRE, LIM, NRE, DEN, FRE, FIM, T1, T2, CS2, SN2 = range(17)

            def vv(out_i, a, b_, o):
                op('v', ["c", "sp"], ["sp"], lambda e: e.tensor_tensor(out=V2(out_i), in0=a, in1=b_, op=o))
            op('a', ["c"], ["sp"], lambda e: e.activation(out=V2(DT), in_=ldt, func=AF.Exp))
            vv(TH, a_im, V2(DT), ALU.mult)
            vv(T1, a_re, V2(DT), ALU.mult)
            op('a', ["sp"], ["sp"], lambda e: e.activation(out=V2(LAB), in_=V2(T1), func=AF.Exp))

            def sin_of(dst, shift):
                op('v', ["sp"], ["sp"], lambda e: e.tensor_scalar(out=V2(R), in0=V2(TH), scalar1=float(shift), scalar2=None, op0=ALU.add))
                for _ in range(4):
                    op('v', ["sp"], ["sp"], lambda e: e.tensor_scalar(out=V2(M_), in0=V2(R), scalar1=PI, scalar2=-2.0 * PI, op0=ALU.is_ge, op1=ALU.mult))
                    vv(R, V2(R), V2(M_), ALU.add)
                op('a', ["sp"], ["sp"], lambda e: e.activation(out=V2(dst), in_=V2(R), func=AF.Sin))
            sin_of(SN, 0.0)
            sin_of(CS, PI / 2)
            vv(LRE, V2(LAB), V2(CS), ALU.mult)
            vv(LIM, V2(LAB), V2(SN), ALU.mult)
            op('v', ["sp"], ["sp"], lambda e: e.tensor_scalar(out=V2(NRE), in0=V2(LRE), scalar1=-1.0, scalar2=None, op0=ALU.add))
            vv(T1, a_re, a_re, ALU.mult)
            vv(T2, a_im, a_im, ALU.mult)
            vv(DEN, V2(T1), V2(T2), ALU.add)
            op('v', ["sp"], ["sp"], lambda e: e.reciprocal(out=V2(DEN), in_=V2(DEN)))
            vv(T1, V2(NRE), a_re, ALU.mult)
            vv(T2, V2(LIM), a_im, ALU.mult)
            vv(T1, V2(T1), V2(T2), ALU.add)
            vv(FRE, V2(T1), V2(DEN), ALU.mult)
            vv(T1, V2(LIM), a_re, ALU.mult)
            vv(T2, V2(NRE), a_im, ALU.mult)
            vv(T1, V2(T1), V2(T2), ALU.subtract)
            vv(FIM, V2(T1), V2(DEN), ALU.mult)

            dma('s', 'dst0', stg[0], ccre_d, [], ["stg0"])
            dma('s', 'dst1', stg[1], ccim_d, [], ["stg1"])

            def c3(a):
                return a.rearrange("p (j m) -> p j m", j=16)
            fre_b = bc(V2(FRE), [128, 16, 128], 2)
            fim_b = bc(V2(FIM), [128, 16, 128], 2)
            op('v', ["stg0", "sp"], ["tmpA"], lambda e: e.tensor_tensor(out=c3(tmpA), in0=c3(stg[0]), in1=fre_b, op=ALU.mult))
            op('v', ["stg1", "sp"], ["tmpB"], lambda e: e.tensor_tensor(out=c3(tmpB), in0=c3(stg[1]), in1=fim_b, op=ALU.mult))
            op('v', ["tmpA", "tmpB"], ["CC"], lambda e: e.tensor_tensor(out=CCre[:].rearrange("p a b -> p (a b)"), in0=tmpA, in1=tmpB, op=ALU.subtract))
            op('v', ["stg0", "sp", "CC"], ["tmpA"], lambda e: e.tensor_tensor(out=c3(tmpA), in0=c3(stg[0]), in1=fim_b, op=ALU.mult))
            op('v', ["stg1", "sp", "CC"], ["tmpB"], lambda e: e.tensor_tensor(out=c3(tmpB), in0=c3(stg[1]), in1=fre_b, op=ALU.mult))
            op('v', ["tmpA", "tmpB"], ["tmpA"], lambda e: e.tensor_tensor(out=tmpA, in0=tmpA, in1=tmpB, op=ALU.add))
            op('v', ["tmpA"], ["CC"], lambda e: e.tensor_scalar(out=CCimN[:].rearrange("p a b -> p (a b)"), in0=tmpA, scalar1=-1.0, scalar2=None, op0=ALU.mult))

            op('v', [], ["E"], lambda e: e.memset(Ere[:, :, 0:1], 1.0))
            op('v', [], ["E"], lambda e: e.memset(Eim[:, :, 0:1], 0.0))
            op('v', ["sp"], ["sp"], lambda e: e.tensor_copy(out=V2(CS2), in_=V2(CS)))
            op('v', ["sp"], ["sp"], lambda e: e.tensor_copy(out=V2(SN2), in_=V2(SN)))
            et0 = tmpA[:, 0:1024].rearrange("p (a b) -> p a b", a=16)
            et1 = tmpB[:, 0:1024].rearrange("p (a b) -> p a b", a=16)
            for lv in range(7):
                m = 1 << lv
                shp = [128, 16, m]
                cb = bc(V2(CS2), shp, 2)
                sbb = bc(V2(SN2), shp, 2)
                op('v', ["E", "sp", "tmpA"], ["tmpA"], lambda e, m=m, cb=cb: e.tensor_tensor(out=et0[:, :, 0:m], in0=Ere[:, :, 0:m], in1=cb, op=ALU.mult))
                op('v', ["E", "sp", "tmpB"], ["tmpB"], lambda e, m=m, sbb=sbb: e.tensor_tensor(out=et1[:, :, 0:m], in0=Eim[:, :, 0:m], in1=sbb, op=ALU.mult))
                op('v', ["tmpA", "tmpB", "E"], ["E"], lambda e, m=m: e.tensor_tensor(out=Ere[:, :, m:2 * m], in0=et0[:, :, 0:m], in1=et1[:, :, 0:m], op=ALU.subtract))
                op('v', ["E", "sp", "tmpA"], ["tmpA"], lambda e, m=m, sbb=sbb: e.tensor_tensor(out=et0[:, :, 0:m], in0=Ere[:, :, 0:m], in1=sbb, op=ALU.mult))
                op('v', ["E", "sp", "tmpB"], ["tmpB"], lambda e, m=m, cb=cb: e.tensor_tensor(out=et1[:, :, 0:m], in0=Eim[:, :, 0:m], in1=cb, op=ALU.mult))
                op('v', ["tmpA", "tmpB", "E"], ["E"], lambda e, m=m: e.tensor_tensor(out=Eim[:, :, m:2 * m], in0=et0[:, :, 0:m], in1=et1[:, :, 0:m], op=ALU.add))
                vv(T1, V2(CS2), V2(CS2), ALU.mult)
                vv(T2, V2(SN2), V2(SN2), ALU.mult)
                op('v', ["sp"], ["sp"], lambda e: e.scalar_tensor_tensor(out=V2(SN2), in0=V2(CS2), scalar=2.0, in1=V2(SN2), op0=ALU.mult, op1=ALU.mult))
                vv(CS2, V2(T1), V2(T2), ALU.subtract)

            sch.barrier()
            esp.close()
            mark("prep")

            PF = sb("PF", [128, 14, 129])
            Stp = sb("Stp", [128, 4, 128])
            cw = sb("cw", [128, 2, 16])
            op('v', [], ["PF"], lambda e: e.memset(PF[:], 0.0))
            op('v', [], ["Stp"], lambda e: e.memset(Stp[:], 0.0))
            op('v', [], ["cw"], lambda e: e.memset(cw[:], 0.0))

            xt = [sb("xt%d" % i, [128, D]) for i in range(2)]
            stat = sb("stat", [128, 8])
            xnF = sb("xnF", [128, 8, 128], BF16)
            wch = [sb("wch%d" % i, [128, 8, 128], BF16) for i in range(4)]
            L = sb("L", [128, 14, 128])
            uF = sb("uF", [128, 4, 128])
            uB = sb("uB", [128, 4, 128], BF16)
            gates = sb("gates", [128, 16, 128], BF16)
            lorain = sb("lorain", [128, 128], BF16)
            sgx = sb("sgx", [128, 128], BF16)
            TT = [SCR[:, i * 512:(i + 1) * 512].rearrange("p (a b) -> p a b", a=4) for i in range(16)]
            TK = ["T%d" % i for i in range(16)]
            (iSIG, iCS, iPT, iPTM, iPINV, iAV, iKK, iTMP, iKMOD, iRTL, iATL, iBTL, iKTL, iRKR, iY2, iX) = range(16)
            VT = sb("VT", [128, 512])
            BT = sb("BT", [128, 512])
            KT = sb("KT", [128, 512])
            gT = sb("gT", [128, 512])
            mats = {nm: sb("m_" + nm, [128, 4, 128]) for nm in ["X", "XT", "X2", "X2T", "QT", "akT", "rbT", "rkT"]}
            RHS = sb("RHS", [128, 512])
            SA = sb("SA", [128, 512])
            PTbd = sb("PTbd", [128, 4, 128])
            Y = sb("Y", [128, 512])
            gst = sb("gst", [128, 6, 8])
            bon = sb("bon", [128, 8])
            roF = sb("roF", [128, 4, 128], BF16)
            mixed = sb("mixed", [128, 8, 128])
            mixedB = sb("mixedB", [128, 8, 128], BF16)
            xre = sb("xre", [128, 4, 128], BF16)
            xim = sb("xim", [128, 4, 128], BF16)
            ygB = sb("ygB", [128, 4, 128], BF16)
            zg = sb("zg", [128, 8, 128])
            h1 = sb("h1", [128, D])
            cwn = sb("cwn", [128, 6, 4])

            w0 = pvec[:, 0, :]
            a0 = pvec[:, 1, :]
            k_k = pvec[:, 2, :]
            k_a = pvec[:, 3, :]
            r_k = pvec[:, 4, :]
            s5d = pvec[:, 5, :]
            S4 = [128, 4, 128]

            def tt(eng, o, ok, a, ak, b_, bk, alu):
                op(eng, [ak, bk], [ok], lambda e: e.tensor_tensor(out=o, in0=a, in1=b_, op=alu))

            dma('s', 'dx0', xt[0][:], x_d[0:128, :], [], ["xt0"])

            for it in range(NT):
                xb = it % 2
                xk = "xt%d" % xb
                xcur = xt[xb]
                if it + 1 < NT:
                    dma('s', 'dx%d' % (1 - xb), xt[1 - xb][:], x_d[(it + 1) * 128:(it + 2) * 128, :], [], ["xt%d" % (1 - xb)])
                op('a', [xk], [TK[iX], "stat"], lambda e: e.activation(out=SCR[:, iX * 512:iX * 512 + 512], in_=xcur[:, 0:512], func=AF.Square, accum_out=stat[:, 0:1]))
                op('a', [xk], [TK[iX], "stat"], lambda e: e.activation(out=SCR[:, iX * 512:iX * 512 + 512], in_=xcur[:, 512:1024], func=AF.Square, accum_out=stat[:, 3:4]))
                op('v', ["stat"], ["stat"], lambda e: e.tensor_tensor(out=stat[:, 0:1], in0=stat[:, 0:1], in1=stat[:, 3:4], op=ALU.add))
                op('a', ["stat"], ["stat"], lambda e: e.activation(out=stat[:, 1:2], in_=stat[:, 0:1], func=AF.Sqrt, bias=1e-6, scale=1.0 / D))
                op('v', ["stat"], ["stat"], lambda e: e.reciprocal(out=stat[:, 2:3], in_=stat[:, 1:2]))
                op('v', [xk, "stat"], [xk], lambda e: e.tensor_scalar(out=xcur[:], in0=xcur[:], scalar1=stat[:, 2:3], scalar2=None, op0=ALU.mult))
                mark("norm")
                for half in range(2):
                    pk = psk(half)

                    def tr(e, half=half):
                        ins = None
                        for q in range(4):
                            kt = half * 4 + q
                            ins = e.transpose(out=PS[half][:, q * 128:(q + 1) * 128], in_=xcur[:, kt * 128:(kt + 1) * 128], identity=ident[:])
                        return ins
                    op('p', [xk, "c"], [pk], tr)
                    if half == 0:
                        op('v', [pk], ["xnF"], lambda e: e.tensor_copy(out=xnF[:, 0:4, :], in_=P3(0)))
                    else:
                        op('a', [pk], ["xnF"], lambda e: e.activation(out=xnF[:, 4:8, :], in_=P3(1), func=AF.Copy))
                mark("xnF")
                for c in range(NCT):
                    mark("proj%d" % c)
                    wb = c % 4
                    wk = "wch%d" % wb
                    dma('s', 'dw%d' % wb, wch[wb][:].rearrange("p a b -> p (a b)"), wsc_d[c], ["wsc%d" % c], [wk])
                    pb = 2 + (c % 4)
                    pk = psk(pb)

                    def mm(e, wb=wb, pb=pb):
                        ins = None
                        for kt in range(8):
                            ins = e.matmul(PS[pb][:, 0:128], lhsT=wch[wb][:, kt, :], rhs=xnF[:, kt, :], start=(kt == 0), stop=(kt == 7))
                        return ins
                    op('p', [wk, "xnF"], [pk], mm)
                    if c < 14:
                        if c % 2 == 0:
                            op('v', [pk], ["PF"], lambda e, c=c, pb=pb: e.tensor_copy(out=PF[:, c, 1:129], in_=PS[pb][:, 0:128]))
                        else:
                            op('a', [pk], ["PF"], lambda e, c=c, pb=pb: e.activation(out=PF[:, c, 1:129], in_=PS[pb][:, 0:128], func=AF.Copy))
                    elif c < 18:
                        op('v', [pk], ["uF"], lambda e, c=c, pb=pb: e.tensor_copy(out=uF[:, c - 14, :], in_=PS[pb][:, 0:128]))
                        op('a', ["uF"], ["uB"], lambda e, c=c, pb=pb: e.activation(out=uB[:, c - 14, :], in_=uF[:, c - 14, :], func=AF.Copy))
                    else:
                        op('a', [pk, "c"], ["gates"], lambda e, c=c, pb=pb: e.activation(out=gates[:, c - 18, :], in_=PS[pb][:, 0:128], func=AF.Sigmoid, bias=bgate[:, c - 18:c - 17]))
                mark("proj")
                op('v', ["PF"], ["L"], lambda e: e.tensor_tensor(out=L[:], in0=PF[:, :, 0:128], in1=PF[:, :, 1:129], op=ALU.subtract))
                op('g', ["L", "c"], ["L"], lambda e: e.tensor_tensor(out=L[:], in0=L[:], in1=bc(mu[:], [128, 14, 128], 2), op=ALU.mult))
                op('v', ["L", "PF"], ["L"], lambda e: e.tensor_tensor(out=L[:], in0=L[:], in1=PF[:, :, 1:129], op=ALU.add))
                op('v', ["PF"], ["PF"], lambda e: e.tensor_copy(out=PF[:, :, 0:1], in_=PF[:, :, 128:129]))
                rF = L[:, 0:4, :]
                kF = L[:, 4:8, :]
                vF = L[:, 8:12, :]
                sig, cs, Pt, Ptm1, Pinv, av, kk, tmp, kmod, rtl, atl, btl, ktl, rkr = [TT[i] for i in range(14)]
                op('a', ["L"], ["lorain"], lambda e: e.activation(out=lorain[0:64, :], in_=L[0:64, 12, :], func=AF.Tanh))
                op('v', ["L"], ["lorain"], lambda e: e.tensor_copy(out=lorain[64:128, :], in_=L[64:128, 12, :]))
                op('a', ["L"], ["sgx"], lambda e: e.activation(out=sgx[:], in_=L[:, 13, :], func=AF.Sigmoid))

                def mm_lw(e):
                    ins = None
                    for j in range(4):
                        ins = e.matmul(PS[0][:, j * 128:(j + 1) * 128], lhsT=lora[0:64, j * 128:(j + 1) * 128], rhs=lorain[0:64, :], start=True, stop=True)
                    return ins
                op('p', ["lora", "lorain"], [psk(0)], mm_lw)

                def mm_la(e):
                    ins = None
                    for j in range(4):
                        ins = e.matmul(PS[1][:, j * 128:(j + 1) * 128], lhsT=lora[64:128, j * 128:(j + 1) * 128], rhs=lorain[64:128, :], start=True, stop=True)
                    return ins
                op('p', ["lora", "lorain"], [psk(1)], mm_la)
                op('p', ["glora", "sgx"], [psk(6)], lambda e: e.matmul(PS[6][:], lhsT=sgx[:], rhs=glora[:], start=True, stop=True))
                for j in range(4):
                    op('a', [psk(0), "c"], [TK[iSIG]], lambda e, j=j: e.activation(out=sig[:, j, :], in_=PS[0][:, j * 128:(j + 1) * 128], func=AF.Sigmoid, bias=w0[:, j:j + 1]))
                    op('a', [psk(1), "c"], [TK[iAV]], lambda e, j=j: e.activation(out=av[:, j, :], in_=PS[1][:, j * 128:(j + 1) * 128], func=AF.Sigmoid, bias=a0[:, j:j + 1]))
                op('v', [psk(6)], ["gT"], lambda e: e.tensor_copy(out=gT[:], in_=PS[6][:]))
                for j in range(4):
                    op('v', [TK[iSIG], "c"], [TK[iCS]], lambda e, j=j: e.tensor_tensor_scan(out=cs[:, j, :], data0=scanmask, data1=sig[:, j, :], initial=0.0, op0=ALU.mult, op1=ALU.add))
                op('a', [TK[iCS]], [TK[iPT]], lambda e: e.activation(out=Pt, in_=cs, func=AF.Exp, scale=-C0))
                op('a', [TK[iCS]], [TK[iPINV]], lambda e: e.activation(out=Pinv, in_=cs, func=AF.Exp, scale=C0))
                tt('v', Ptm1, TK[iPTM], cs, TK[iCS], sig, TK[iSIG], ALU.subtract)
                op('a', [TK[iPTM]], [TK[iPTM]], lambda e: e.activation(out=Ptm1, in_=Ptm1, func=AF.Exp, scale=-C0))
                tt('g', kk, TK[iKK], kF, "L", bc(k_k, S4, 2), "c", ALU.mult)
                tt('g', tmp, TK[iTMP], kk, TK[iKK], kk, TK[iKK], ALU.mult)

                def mm_n(e):
                    ins = None
                    for j in range(4):
                        ins = e.matmul(PS[7][:, j * 128:(j + 1) * 128], lhsT=maskBD, rhs=tmp[:, j, :], start=True, stop=True)
                    return ins
                op('p', [TK[iTMP], "c"], [psk(7)], mm_n)
                op('a', [psk(7)], [TK[iTMP]], lambda e: e.activation(out=tmp, in_=P3(7), func=AF.Sqrt))
                op('v', [TK[iTMP]], [TK[iTMP]], lambda e: e.tensor_scalar(out=tmp, in0=tmp, scalar1=1e-12, scalar2=None, op0=ALU.max))
                op('v', [TK[iTMP]], [TK[iTMP]], lambda e: e.reciprocal(out=tmp, in_=tmp))
                tt('v', kk, TK[iKK], kk, TK[iKK], tmp, TK[iTMP], ALU.mult)
                tt('g', kmod, TK[iKMOD], av, TK[iAV], bc(k_a, S4, 2), "c", ALU.mult)
                tt('g', kmod, TK[iKMOD], kmod, TK[iKMOD], bc(k_a, S4, 2), "c", ALU.subtract)
                op('v', [TK[iKMOD], "L"], [TK[iKMOD]], lambda e: e.scalar_tensor_tensor(out=kmod, in0=kmod, scalar=1.0, in1=kF, op0=ALU.add, op1=ALU.mult))
                tt('v', rtl, TK[iRTL], rF, "L", Pt, TK[iPT], ALU.mult)
                op('v', [TK[iKK], TK[iPTM]], [TK[iATL]], lambda e: e.scalar_tensor_tensor(out=atl, in0=kk, scalar=-1.0, in1=Ptm1, op0=ALU.mult, op1=ALU.mult))
                tt('g', btl, TK[iBTL], kk, TK[iKK], av, TK[iAV], ALU.mult)
                tt('v', btl, TK[iBTL], btl, TK[iBTL], Pinv, TK[iPINV], ALU.mult)
                tt('v', ktl, TK[iKTL], kmod, TK[iKMOD], Pinv, TK[iPINV], ALU.mult)
                tt('g', rkr, TK[iRKR], rF, "L", kmod, TK[iKMOD], ALU.mult)
                tt('g', rkr, TK[iRKR], rkr, TK[iRKR], bc(r_k, S4, 2), "c", ALU.mult)
                op('v', [TK[iPT], "c"], ["PTbd"], lambda e: e.tensor_tensor(out=PTbd[:], in0=bc(maskBD, S4, 1), in1=Pt[:, :, 127:128].to_broadcast(S4), op=ALU.mult))

                def mm_b(e):
                    ins = None
                    for j in range(4):
                        ins = e.matmul(PS[7][:, 2 * j:2 * j + 2], lhsT=rkr[:, j, :], rhs=sel2[:], start=True, stop=True)
                    return ins
                op('p', [TK[iRKR], "c"], [psk(7)], mm_b)
                op('v', [psk(7)], ["bon"], lambda e: e.tensor_copy(out=bon[:], in_=PS[7][:, 0:8]))
                atl_m = [TT[0], TT[1]]
                rtl_m = [TT[2], TT[3]]
                for hh in range(2):
                    op('v' if hh == 0 else 'g', [TK[iATL], "c"], [TK[hh]], lambda e, hh=hh: e.tensor_scalar(out=atl_m[hh], in0=atl, scalar1=sel2[:, hh:hh + 1], scalar2=None, op0=ALU.mult))
                    op('v' if hh == 0 else 'g', [TK[iRTL], "c"], [TK[2 + hh]], lambda e, hh=hh: e.tensor_scalar(out=rtl_m[hh], in0=rtl, scalar1=sel2[:, hh:hh + 1], scalar2=None, op0=ALU.mult))
                mark("elem")
                for src, srckey, dst, dkey, pb in [(vF, "L", VT, "VT", 0), (btl, TK[iBTL], BT, "BT", 1), (ktl, TK[iKTL], KT, "KT", 6)]:
                    def trf(e, src=src, pb=pb):
                        ins = None
                        for j in range(4):
                            ins = e.transpose(out=PS[pb][:, j * 128:(j + 1) * 128], in_=src[:, j, :], identity=ident[:])
                        return ins
                    op('p', [srckey, "c"], [psk(pb)], trf)
                    if pb == 1:
                        op('a', [psk(pb)], [dkey], lambda e, dst=dst, pb=pb: e.activation(out=dst[:], in_=PS[pb][:], func=AF.Copy))
                    else:
                        op('v', [psk(pb)], [dkey], lambda e, dst=dst, pb=pb: e.tensor_copy(out=dst[:], in_=PS[pb][:]))
                mark("trans")
                for hg in range(2):
                    def hsl(t, hh, hg=hg):
                        h = hg * 4 + hh
                        return t[(h % 2) * 64:(h % 2) * 64 + 64, h // 2, :]
                    specs = [("X", "A", btl, maskSL, 2), ("XT", btl, "A", maskSU, 3),
                             ("akT", ktl, "A", maskSU, 4), ("rbT", btl, "R", maskUI, 5), ("rkT", ktl, "R", maskUI, 7)]

                    def pick(t, hl, hg=hg):
                        h = hg * 4 + hl
                        j, hh = h // 2, h % 2
                        if isinstance(t, str):
                            return (atl_m if t == "A" else rtl_m)[hh][:, j, :]
                        return t[:, j, :]
                    for nm, lt, rt, mk, pb in specs:
                        def mmT(e, lt=lt, rt=rt, pb=pb):
                            ins = None
                            for hl in range(4):
                                ins = e.matmul(PS[pb][:, hl * 128:(hl + 1) * 128], lhsT=pick(lt, hl), rhs=pick(rt, hl), start=True, stop=True)
                            return ins
                        op('p', [TK[0], TK[1], TK[2], TK[3], TK[iBTL], TK[iKTL]], [psk(pb)], mmT)
                        op('v', [psk(pb), "c"], ["m_" + nm], lambda e, nm=nm, mk=mk, pb=pb: e.tensor_tensor(
                            out=mats[nm][:], in0=P3(pb), in1=bc(mk, S4, 1), op=ALU.mult))
                        mark("mats_" + nm)
                    op('g', ["m_XT", "c"], ["m_QT"], lambda e: e.tensor_tensor(out=mats["QT"][:], in0=mats["XT"][:], in1=bc(ident[:], S4, 1), op=ALU.add))
                    mark("mats_qt")
                    cur, curT, nx, nxT = "X", "XT", "X2", "X2T"
                    for step in range(6):
                        def sq(e, a=curT, b_=cur):
                            ins = None
                            for hh in range(4):
                                ins = e.matmul(PS[2][:, hh * 128:(hh + 1) * 128], lhsT=mats[a][:, hh, :], rhs=mats[b_][:, hh, :], start=True, stop=True)
                            return ins
                        op('p', ["m_" + cur, "m_" + curT], [psk(2)], sq)
                        op('a', [psk(2)], ["m_" + nx], lambda e, nx=nx: e.activation(out=mats[nx][:], in_=P3(2), func=AF.Copy))
                        if step < 5:
                            def sqT(e, a=cur, b_=curT):
                                ins = None
                                for hh in range(4):
                                    ins = e.matmul(PS[3][:, hh * 128:(hh + 1) * 128], lhsT=mats[a][:, hh, :], rhs=mats[b_][:, hh, :], start=True, stop=True)
                                return ins
                            op('p', ["m_" + cur, "m_" + curT], [psk(3)], sqT)
                            op('v', [psk(3)], ["m_" + nxT], lambda e, nxT=nxT: e.tensor_copy(out=mats[nxT][:], in_=P3(3)))

                        def qu(e, a=nx):
                            ins = None
                            for hh in range(4):
                                ins = e.matmul(PS[4][:, hh * 128:(hh + 1) * 128], lhsT=mats[a][:, hh, :], rhs=mats["QT"][:, hh, :], start=True, stop=True)
                            return ins
                        op('p', ["m_" + nx, "m_QT"], [psk(4)], qu)
                        op('v', [psk(4), "m_QT"], ["m_QT"], lambda e: e.tensor_tensor(out=mats["QT"][:], in0=mats["QT"][:], in1=P3(4), op=ALU.add))
                        cur, curT, nx, nxT = nx, nxT, cur, curT
                        mark("mats_s%d" % step)
                    mark("mats")
                    c0 = hg * 256

                    def mm_rhs(e, hg=hg):
                        ins = None
                        for hl in range(4):
                            h = hg * 4 + hl
                            j = h // 2
                            hh = h % 2
                            reg = PS[0][:, hl * 64:(hl + 1) * 64]
                            e.matmul(reg, lhsT=atl[:, j, :], rhs=Stp[:, j, hh * 64:(hh + 1) * 64], start=True, stop=False)
                            ins = e.matmul(reg, lhsT=mats["akT"][:, hl, :], rhs=VT[:, h * 64:(h + 1) * 64], start=False, stop=True)
                        return ins
                    op('p', [TK[iATL], "Stp", "m_akT", "VT"], [psk(0)], mm_rhs)
                    op('v', [psk(0)], ["RHS"], lambda e, c0=c0: e.tensor_copy(out=RHS[:, c0:c0 + 256], in_=PS[0][:, 0:256]))

                    def mm_sa(e, hg=hg):
                        ins = None
                        for hl in range(4):
                            h = hg * 4 + hl
                            ins = e.matmul(PS[1][:, hl * 64:(hl + 1) * 64], lhsT=mats["QT"][:, hl, :], rhs=RHS[:, h * 64:(h + 1) * 64], start=True, stop=True)
                        return ins
                    op('p', ["m_QT", "RHS"], [psk(1)], mm_sa)
                    op('v', [psk(1)], ["SA"], lambda e, c0=c0: e.tensor_copy(out=SA[:, c0:c0 + 256], in_=PS[1][:, 0:256]))

                    def mm_y(e, hg=hg):
                        ins = None
                        for hl in range(4):
                            h = hg * 4 + hl
                            j = h // 2
                            hh = h % 2
                            reg = PS[6][:, hl * 64:(hl + 1) * 64]
                            e.matmul(reg, lhsT=rtl[:, j, :], rhs=Stp[:, j, hh * 64:(hh + 1) * 64], start=True, stop=False)
                            e.matmul(reg, lhsT=mats["rbT"][:, hl, :], rhs=SA[:, h * 64:(h + 1) * 64], start=False, stop=False)
                            ins = e.matmul(reg, lhsT=mats["rkT"][:, hl, :], rhs=VT[:, h * 64:(h + 1) * 64], start=False, stop=True)
                        return ins
                    op('p', [TK[iRTL], "Stp", "m_rbT", "m_rkT", "SA", "VT"], [psk(6)], mm_y)
                    op('a', [psk(6)], ["Y"], lambda e, c0=c0: e.activation(out=Y[:, c0:c0 + 256], in_=PS[6][:, 0:256], func=AF.Copy))

                    def mm_st(e, hg=hg):
                        ins = None
                        for jj in range(2):
                            j = hg * 2 + jj
                            reg = PS[5][:, jj * 128:(jj + 1) * 128]
                            e.matmul(reg, lhsT=ident[:], rhs=Stp[:, j, :], start=True, stop=False)
                            e.matmul(reg, lhsT=BT[:, j * 128:(j + 1) * 128], rhs=SA[:, j * 128:(j + 1) * 128], start=False, stop=False)
                            ins = e.matmul(reg, lhsT=KT[:, j * 128:(j + 1) * 128], rhs=VT[:, j * 128:(j + 1) * 128], start=False, stop=True)
                        return ins
                    op('p', ["Stp", "BT", "KT", "SA", "VT", "c"], [psk(5)], mm_st)
                    op('v', [psk(5), "PTbd"], ["Stp"], lambda e, hg=hg: e.tensor_tensor(
                        out=Stp[:, 2 * hg:2 * hg + 2, :], in0=PS[5][:, 0:256].rearrange("p (a b) -> p a b", a=2),
                        in1=PTbd[:, 2 * hg:2 * hg + 2, :], op=ALU.mult))
                mark("chain")
                Y3 = Y[:].rearrange("p (h v) -> p h v", h=8)
                Ysq = SCR[:, iY2 * 512:(iY2 + 1) * 512]
                G8 = [128, 8, 64]
                op('v', ["Y"], ["gst"], lambda e: e.tensor_reduce(out=gst[:, 0, :], in_=Y3, axis=AX.X, op=ALU.add))
                op('a', ["Y"], [TK[iY2]], lambda e: e.activation(out=Ysq, in_=Y[:], func=AF.Square))
                op('v', [TK[iY2]], ["gst"], lambda e: e.tensor_reduce(out=gst[:, 1, :], in_=Ysq.rearrange("p (h v) -> p h v", h=8), axis=AX.X, op=ALU.add))
                op('v', ["gst"], ["gst"], lambda e: e.tensor_scalar(out=gst[:, 2, :], in0=gst[:, 0, :], scalar1=1.0 / 64, scalar2=None, op0=ALU.mult))
                op('v', ["gst"], ["gst"], lambda e: e.tensor_tensor(out=gst[:, 3, :], in0=gst[:, 2, :], in1=gst[:, 2, :], op=ALU.mult))
                op('v', ["gst"], ["gst"], lambda e: e.scalar_tensor_tensor(out=gst[:, 4, :], in0=gst[:, 1, :], scalar=1.0 / 64, in1=gst[:, 3, :], op0=ALU.mult, op1=ALU.subtract))
                op('a', ["gst"], ["gst"], lambda e: e.activation(out=gst[:, 5, :], in_=gst[:, 4, :], func=AF.Sqrt, bias=GN_EPS, scale=1.0))
                op('v', ["gst"], ["gst"], lambda e: e.reciprocal(out=gst[:, 5, :], in_=gst[:, 5, :]))
                op('v', ["Y", "gst"], ["Y"], lambda e: e.tensor_tensor(out=Y3, in0=Y3, in1=bc(gst[:, 2, :], G8, 2), op=ALU.subtract))
                op('v', ["Y", "gst"], ["Y"], lambda e: e.tensor_tensor(out=Y3, in0=Y3, in1=bc(gst[:, 5, :], G8, 2), op=ALU.mult))
                op('g', ["Y", "c"], ["Y"], lambda e: e.tensor_tensor(out=Y[:], in0=Y[:], in1=lnw[:], op=ALU.mult))
                op('g', ["Y", "c"], ["Y"], lambda e: e.tensor_tensor(out=Y[:], in0=Y[:], in1=lnb[:], op=ALU.add))
                op('v', ["VT", "bon"], [TK[iY2]], lambda e: e.tensor_tensor(out=Ysq.rearrange("p (h v) -> p h v", h=8), in0=VT[:].rearrange("p (h v) -> p h v", h=8), in1=bc(bon[:], G8, 2), op=ALU.mult))
                op('v', ["Y", TK[iY2]], ["Y"], lambda e: e.tensor_tensor(out=Y[:], in0=Y[:], in1=Ysq, op=ALU.add))
                op('v', ["Y", "gT"], ["Y"], lambda e: e.tensor_tensor(out=Y[:], in0=Y[:], in1=gT[:], op=ALU.mult))

                def tr_ro(e):
                    ins = None
                    for j in range(4):
                        ins = e.transpose(out=PS[0][:, j * 128:(j + 1) * 128], in_=Y[:, j * 128:(j + 1) * 128], identity=ident[:])
                    return ins
                op('p', ["Y", "c"], [psk(0)], tr_ro)
                op('a', [psk(0)], ["roF"], lambda e: e.activation(out=roF[:], in_=P3(0), func=AF.Copy))
                for half in range(2):
                    pb = 1 + half

                    def mm_o(e, half=half, pb=pb):
                        ins = None
                        for q in range(4):
                            dt_ = half * 4 + q
                            for kt in range(4):
                                ins = e.matmul(PS[pb][:, q * 128:(q + 1) * 128], lhsT=w_o[:, kt, dt_ * 128:(dt_ + 1) * 128], rhs=roF[:, kt, :], start=(kt == 0), stop=(kt == 3))
                        return ins
                    op('p', ["w_o", "roF"], [psk(pb)], mm_o)
                    op('v', [psk(pb), "gates"], ["mixed"], lambda e, half=half, pb=pb: e.tensor_tensor(out=mixed[:, half * 4:(half + 1) * 4, :], in0=P3(pb), in1=gates[:, half * 4:(half + 1) * 4, :], op=ALU.mult))
                mark("epi")
                s5a, s5b, s5c, s5d_, btre, btim, wre, wim = [TT[i] for i in range(8)]
                ka, kb, kc, kd, kbr, kbi, kwr, kwi = [TK[i] for i in range(8)]
                for kt in range(4):
                    Er = Ere[:, 4 * kt:4 * kt + 4, :]
                    Ei = Eim[:, 4 * kt:4 * kt + 4, :]

                    def mm_bu(e, kt=kt):
                        ins = None
                        for q in range(4):
                            j = 4 * kt + q
                            e.matmul(PS[3][:, q * 128:(q + 1) * 128], lhsT=BBre[:, j, :], rhs=uB[:, kt, :], start=True, stop=True)
                            ins = e.matmul(PS[4][:, q * 128:(q + 1) * 128], lhsT=BBim[:, j, :], rhs=uB[:, kt, :], start=True, stop=True)
                        return ins
                    op('p', ["BB", "uB"], [psk(3), psk(4)], mm_bu)
                    tt('v', s5a, ka, Er, "E", P3(3), psk(3), ALU.mult)
                    tt('v', s5b, kb, Ei, "E", P3(4), psk(4), ALU.mult)
                    tt('g', btre, kbr, s5a, ka, s5b, kb, ALU.add)
                    tt('v', s5c, kc, Er, "E", P3(4), psk(4), ALU.mult)
                    tt('v', s5d_, kd, Ei, "E", P3(3), psk(3), ALU.mult)
                    tt('g', btim, kbi, s5c, kc, s5d_, kd, ALU.subtract)
                    for q in range(4):
                        j = 4 * kt + q
                        op('v', [kbr, "cw", "sp"], [kwr], lambda e, q=q, j=j: e.tensor_tensor_scan(out=wre[:, q, :], data0=sp[:, LAB, j:j + 1].to_broadcast([128, 128]), data1=btre[:, q, :], initial=cw[:, 0, j:j + 1], op0=ALU.mult, op1=ALU.add))
                        op('v', [kbi, "cw", "sp"], [kwi], lambda e, q=q, j=j: e.tensor_tensor_scan(out=wim[:, q, :], data0=sp[:, LAB, j:j + 1].to_broadcast([128, 128]), data1=btim[:, q, :], initial=cw[:, 1, j:j + 1], op0=ALU.mult, op1=ALU.add))
                    c128 = sp[:, CS2, 4 * kt:4 * kt + 4]
                    s128 = sp[:, SN2, 4 * kt:4 * kt + 4]
                    wr127 = wre[:, :, 127]
                    wi127 = wim[:, :, 127]
                    tt('g', cwn[:, 0, :], "cwn", c128, "sp", wr127, kwr, ALU.mult)
                    tt('g', cwn[:, 1, :], "cwn", s128, "sp", wi127, kwi, ALU.mult)
                    tt('g', cwn[:, 2, :], "cwn", s128, "sp", wr127, kwr, ALU.mult)
                    tt('g', cwn[:, 3, :], "cwn", c128, "sp", wi127, kwi, ALU.mult)
                    tt('v', cw[:, 0, 4 * kt:4 * kt + 4], "cw", cwn[:, 0, :], "cwn", cwn[:, 1, :], "cwn", ALU.subtract)
                    tt('v', cw[:, 1, 4 * kt:4 * kt + 4], "cw", cwn[:, 2, :], "cwn", cwn[:, 3, :], "cwn", ALU.add)
                    tt('v', s5a, ka, Er, "E", wre, kwr, ALU.mult)
                    tt('v', s5b, kb, Ei, "E", wim, kwi, ALU.mult)
                    tt('g', xre[:], "xre", s5a, ka, s5b, kb, ALU.subtract)
                    tt('v', s5c, kc, Ei, "E", wre, kwr, ALU.mult)
                    tt('v', s5d_, kd, Er, "E", wim, kwi, ALU.mult)
                    tt('g', xim[:], "xim", s5c, kc, s5d_, kd, ALU.add)

                    def mm_c(e, kt=kt):
                        ins = None
                        for q in range(4):
                            j = 4 * kt + q
                            e.matmul(PS[5][:, kt * 128:(kt + 1) * 128], lhsT=CCre[:, j, :], rhs=xre[:, q, :], start=(q == 0), stop=False)
                            ins = e.matmul(PS[5][:, kt * 128:(kt + 1) * 128], lhsT=CCimN[:, j, :], rhs=xim[:, q, :], start=False, stop=(q == 3))
                        return ins
                    op('p', ["CC", "xre", "xim"], [psk(5)], mm_c)
                tt('g', s5a, ka, uF[:], "uF", bc(s5d, S4, 2), "c", ALU.mult)
                tt('v', s5a, ka, s5a, ka, P3(5), psk(5), ALU.add)
                gelu_tanh(op, s5a, ka, s5b, kb, ygB[:], "ygB")
                for half in range(2):
                    for vg in range(2):
                        pb = 6 + vg

                        def mm_g(e, half=half, vg=vg, pb=pb):
                            ins = None
                            for q in range(4):
                                col = vg * 8 + half * 4 + q
                                for kt in range(4):
                                    ins = e.matmul(PS[pb][:, q * 128:(q + 1) * 128], lhsT=w_glu[:, kt, col * 128:(col + 1) * 128], rhs=ygB[:, kt, :], start=(kt == 0), stop=(kt == 3))
                            return ins
                        op('p', ["w_glu", "ygB"], [psk(pb)], mm_g)
                    zh = zg[:, half * 4:(half + 1) * 4, :]
                    op('a', [psk(7)], ["zg"], lambda e, zh=zh: e.activation(out=zh, in_=P3(7), func=AF.Sigmoid))
                    tt('v', zh, "zg", zh, "zg", P3(6), psk(6), ALU.mult)
                    tt('g', zh, "zg", zh, "zg", gates[:, 8 + half * 4:8 + (half + 1) * 4, :], "gates", ALU.mult)
                    tt('v', mixedB[:, half * 4:(half + 1) * 4, :], "mixedB", zh, "zg", mixed[:, half * 4:(half + 1) * 4, :], "mixed", ALU.add)
                mark("s5")
                for half in range(2):
                    pb = 1 + half

                    def mm_h(e, half=half, pb=pb):
                        ins = None
                        for kt in range(8):
                            ins = e.matmul(PS[pb][:], lhsT=mixedB[:, kt, :], rhs=w_out[:, kt, half * 512:(half + 1) * 512], start=(kt == 0), stop=(kt == 7))
                        return ins
                    op('p', ["w_out", "mixedB"], [psk(pb)], mm_h)
                    op('v', [psk(pb), xk, "stat"], ["h1"], lambda e, half=half, pb=pb: e.scalar_tensor_tensor(
                        out=h1[:, half * 512:(half + 1) * 512], in0=xcur[:, half * 512:(half + 1) * 512], scalar=stat[:, 1:2], in1=PS[pb][:], op0=ALU.mult, op1=ALU.add))
                dma('s', 'dh1', h1_d[it * 128:(it + 1) * 128, :], h1[:], ["h1"], ["h1d%d" % it])
            sch.barrier()
         except _Stop:
            sch.barrier()

        if 2 in phases:
          with ExitStack() as es:
            def sb(name, shape, dt=F32):
                return es.enter_context(nc.sbuf_tensor("s_" + name, shape, dt))
            NCV = 6
            RB = 4
            cin = [sb("cin%d" % i, [128, RB, D]) for i in range(NCV)]
            cout = [sb("cout%d" % i, [128, RB, D], BF16) for i in range(NCV)]
            n = 0
            for src_d, dst_d in [(pu_d, tub_d), (pvv_d, tvb_d)]:
                sv_ = src_d.rearrange("(c r p) d -> c p r d", p=128, r=RB)
                dv_ = dst_d.rearrange("(c r p) d -> c p r d", p=128, r=RB)
                for c in range(16384 // (128 * RB)):
                    b = n % NCV
                    n += 1
                    dma('s', 'dci%d' % b, cin[b][:], sv_[c], [], ["cin%d" % b])
                    if n % 3 == 0:
                        op('v', ["cin%d" % b], ["cout%d" % b], lambda e, b=b: e.tensor_copy(out=cout[b][:], in_=cin[b][:]))
                    elif n % 3 == 1:
                        op('a', ["cin%d" % b], ["cout%d" % b], lambda e, b=b: e.activation(out=cout[b][:], in_=cin[b][:], func=AF.Copy))
                    else:
                        op('g', ["cin%d" % b], ["cout%d" % b], lambda e, b=b: e.tensor_copy(out=cout[b][:], in_=cin[b][:]))
                    dma('a', 'dco%d' % b, dv_[c], cout[b][:], ["cout%d" % b], ["tb"])
            sch.barrier()

        if 2 in phases:
          with ExitStack() as es:
            def sb(name, shape, dt=F32):
                return es.enter_context(nc.sbuf_tensor("s_" + name, shape, dt))
            ident = sb("ident2", [128, 128])
            gffn = sb("gffn", [128, D])
            gfin = sb("gfin", [128, D])
            wq = sb("wq", [128, 8, D])
            subk = sb("subk", [128, 8, 128])
            h1t = [sb("h1t%d" % i, [128, D]) for i in range(2)]
            xn2 = [sb("xn2_%d" % i, [128, D]) for i in range(2)]
            xn2b = [sb("xn2b_%d" % i, [128, D], BF16) for i in range(2)]
            junk = sb("junk", [128, D])
            junkb = sb("junkb", [128, D], BF16)
            xn2F = sb("xn2F", [128, 8, 128])
            qF = sb("qF", [128, 8, 128])
            sc = sb("sc", [128, 16, 128])
            scr = sb("scr", [128, 16, 128])
            iota16 = sb("iota16", [128, 16])
            eq = sb("eq", [128, 8, 16, 16])
            sv = sb("sv", [128, 16, 16])
            siu = sb("siu", [128, 16, 16], U32)
            sif = sb("sif", [128, 16, 16])
            dsi = sb("dsi", [128, 8, 16])
            cand = sb("cand", [128, 8, 256])
            cv = sb("cv", [128, 8, 16])
            ciu = sb("ciu", [128, 8, 16], U32)
            iiu = sb("iiu", [128, 8, 16], U32)
            jju = sb("jju", [128, 8, 16], U32)
            iif = sb("iif", [128, 8, 16])
            jjf = sb("jjf", [128, 8, 16])
            i1 = sb("i1", [128, 8, 16])
            i2 = sb("i2", [128, 8, 16])
            lt = sb("lt", [128, 8, 16])
            gtmp = sb("gtmp", [128, 128])
            ei = [sb("ei%d" % i, [128, 128], I32) for i in range(2)]
            gate = [sb("gate%d" % i, [128, 8, 16]) for i in range(2)]
            sm = sb("sm", [128, 4, 8])
            hid = sb("hid", [128, 128])
            wgt = [sb("wgt%d" % i, [128, 128]) for i in range(2)]
            statA = sb("statA", [128, 8])
            statV = sb("statV", [128, 8])
            NDG = 4
            dg = [sb("dg%d" % i, [128, 128], BF16) for i in range(NDG)]
            U = [sb("U%d" % i, [128, D], BF16) for i in range(NB)]
            Vb = [sb("Vb%d" % i, [128, D], BF16) for i in range(NB)]
            h2 = sb("h2", [128, D])
            outt = sb("outt", [128, D])

            dma('s', 'dc2', ident[:], ident_d, [], ["c"])
            dma('s', 'dc2', gffn[:], gffn_d, [], ["c"])
            dma('s', 'dc2', iota16[:], iota_d, [], ["c"])
            dma('s', 'dc2', gfin[:], gfin_d, [], ["c"])
            dma('s', 'dc2', subk[:].rearrange("p a b -> p (a b)"), subk_d, [], ["c"])
            dma('s', 'dc2', wq[:], wq_d.rearrange("(kt p) c -> p kt c", p=128), [], ["c"])

            def rms(src, srck, dstat, dk):
                op('a', [srck], ["junk", dk], lambda e: e.activation(out=junk[:, 0:512], in_=src[:, 0:512], func=AF.Square, accum_out=dstat[:, 0:1]))
                op('a', [srck], ["junk", dk], lambda e: e.activation(out=junk[:, 512:1024], in_=src[:, 512:1024], func=AF.Square, accum_out=dstat[:, 3:4]))
                op('v', [dk], [dk], lambda e: e.tensor_tensor(out=dstat[:, 0:1], in0=dstat[:, 0:1], in1=dstat[:, 3:4], op=ALU.add))
                op('a', [dk], [dk], lambda e: e.activation(out=dstat[:, 1:2], in_=dstat[:, 0:1], func=AF.Sqrt, bias=1e-6, scale=1.0 / D))
                op('v', [dk], [dk], lambda e: e.reciprocal(out=dstat[:, 2:3], in_=dstat[:, 1:2]))

            def top16_multi(segs, segk, width, vouts, iouts, okeys):
                n = len(segs)
                scrv = [scr[:].rearrange("p a b -> p (a b)")[:, i * width:(i + 1) * width] for i in range(n)]
                sk = ["scr%d" % i for i in range(n)]
                for i in range(n):
                    op('v', [segk], [okeys[i]], lambda e, i=i: e.max(out=vouts[i][:, 0:8], in_=segs[i]))
                for i in range(n):
                    op('v', [segk, okeys[i]], [okeys[i]], lambda e, i=i: e.max_index(out=iouts[i][:, 0:8], in_max=vouts[i][:, 0:8], in_values=segs[i]))
                for i in range(n):
                    op('v', [segk, okeys[i]], [sk[i]], lambda e, i=i: e.match_replace(out=scrv[i], in_to_replace=vouts[i][:, 0:8], in_values=segs[i], imm_value=NEG))
                for i in range(n):
                    op('v', [sk[i]], [okeys[i]], lambda e, i=i: e.max(out=vouts[i][:, 8:16], in_=scrv[i]))
                for i in range(n):
                    op('v', [sk[i], okeys[i]], [okeys[i]], lambda e, i=i: e.max_index(out=iouts[i][:, 8:16], in_max=vouts[i][:, 8:16], in_values=scrv[i]))

            gcount = [0, 0, 0]

            def stage_A(it):
                p = it % 2
                hk = "h1t%d" % p
                hcur = h1t[p]
                xk = "xn2_%d" % p
                xc = xn2[p]
                dma('s', 'dh%d' % p, hcur[:], h1_d[it * 128:(it + 1) * 128, :], ["h1d%d" % it], [hk])
                rms(hcur, hk, statA, "statA")
                op('v', [hk, "statA", "c"], [xk], lambda e: e.scalar_tensor_tensor(out=xc[:], in0=hcur[:], scalar=statA[:, 2:3], in1=gffn[:], op0=ALU.mult, op1=ALU.mult))
                op('a', [xk], ["xn2b_%d" % p], lambda e: e.activation(out=xn2b[p][:], in_=xc[:], func=AF.Copy))
                for half in range(2):
                    def tr2(e, half=half):
                        ins = None
                        for q in range(4):
                            kt = half * 4 + q
                            ins = e.transpose(out=PS[half][:, q * 128:(q + 1) * 128], in_=xc[:, kt * 128:(kt + 1) * 128], identity=ident[:])
                        return ins
                    op('p', [xk, "c"], [psk(half)], tr2)
                    if half == 0:
                        op('v', [psk(0)], ["xn2F"], lambda e: e.tensor_copy(out=xn2F[:, 0:4, :], in_=P3(0)))
                    else:
                        op('a', [psk(1)], ["xn2F"], lambda e: e.activation(out=xn2F[:, 4:8, :], in_=P3(1), func=AF.Copy))
                for half in range(2):
                    pb = 2 + half

                    def mm_q(e, half=half, pb=pb):
                        ins = None
                        for q in range(4):
                            ct = half * 4 + q
                            for kt in range(8):
                                ins = e.matmul(PS[pb][:, q * 128:(q + 1) * 128], lhsT=wq[:, kt, ct * 128:(ct + 1) * 128], rhs=xn2F[:, kt, :], start=(kt == 0), stop=(kt == 7))
                        return ins
                    op('p', ["c", "xn2F"], [psk(pb)], mm_q)
                    if half == 0:
                        op('v', [psk(pb)], ["qF"], lambda e, pb=pb: e.tensor_copy(out=qF[:, 0:4, :], in_=P3(pb)))
                    else:
                        op('a', [psk(pb)], ["qF"], lambda e, pb=pb: e.activation(out=qF[:, 4:8, :], in_=P3(pb), func=AF.Copy))
                sc4 = sc[:].rearrange("p (h c) n -> p h c n", c=2)
                for hg in range(2):
                    for c in range(2):
                        pb = 4 + c

                        def mm_s(e, hg=hg, c=c, pb=pb):
                            ins = None
                            for q in range(4):
                                h = hg * 4 + q
                                ins = e.matmul(PS[pb][:, q * 128:(q + 1) * 128], lhsT=qF[c * 64:(c + 1) * 64, h, :], rhs=subk[c * 64:(c + 1) * 64, h, :], start=True, stop=True)
                            return ins
                        op('p', ["qF", "c"], [psk(pb)], mm_s)
                        if c == 0:
                            op('v', [psk(pb)], ["sc"], lambda e, hg=hg, c=c, pb=pb: e.tensor_copy(out=sc4[:, hg * 4:(hg + 1) * 4, c, :], in_=P3(pb)))
                        else:
                            op('a', [psk(pb)], ["sc"], lambda e, hg=hg, c=c, pb=pb: e.activation(out=sc4[:, hg * 4:(hg + 1) * 4, c, :], in_=P3(pb), func=AF.Copy))

            svk = ["svi%d" % i for i in range(16)]
            cvk = ["cvi%d" % i for i in range(8)]

            def stage_A2(it):
                p = it % 2
                top16_multi([sc[:, i, :] for i in range(16)], "sc", 128, [sv[:, i, :] for i in range(16)], [siu[:, i, :] for i in range(16)], svk)
                for h in range(8):
                    op('v', [svk[2 * h], svk[2 * h + 1]], ["cand"], lambda e, h=h: e.tensor_tensor(
                        out=cand[:, h, :].rearrange("p (i j) -> p i j", i=16),
                        in0=bc(sv[:, 2 * h, :], [128, 16, 16], 2), in1=bc(sv[:, 2 * h + 1, :], [128, 16, 16], 1), op=ALU.add))
                top16_multi([cand[:, h, :] for h in range(8)], "cand", 256, [cv[:, h, :] for h in range(8)], [ciu[:, h, :] for h in range(8)], cvk)
                op('v', cvk, ["iiu"], lambda e: e.tensor_single_scalar(out=iiu[:], in_=ciu[:], scalar=4, op=ALU.logical_shift_right))
                op('v', cvk, ["jju"], lambda e: e.tensor_single_scalar(out=jju[:], in_=ciu[:], scalar=15, op=ALU.bitwise_and))
                op('v', ["iiu"], ["iif"], lambda e: e.tensor_copy(out=iif[:], in_=iiu[:]))
                op('v', ["jju"], ["jjf"], lambda e: e.tensor_copy(out=jjf[:], in_=jju[:]))
                op('v', svk, ["sif"], lambda e: e.tensor_copy(out=sif[:], in_=siu[:]))
                sif4 = sif[:].rearrange("p (h c) k -> p h c k", c=2)
                E4 = [128, 8, 16, 16]
                iota4 = iota16[:].unsqueeze(1).unsqueeze(1).to_broadcast(E4)
                for (idxf, idk, c_, dst, dk) in [(iif, "iif", 0, i1, "i1"), (jjf, "jjf", 1, i2, "i2")]:
                    op('v', [idk, "c", "eq"], ["eq"], lambda e, idxf=idxf: e.tensor_tensor(out=eq[:], in0=idxf[:].unsqueeze(3).to_broadcast(E4), in1=iota4, op=ALU.is_equal))
                    op('v', ["eq", "sif"], ["eq"], lambda e, c_=c_: e.tensor_tensor(out=eq[:], in0=eq[:], in1=sif4[:, :, c_, :].unsqueeze(2).to_broadcast(E4), op=ALU.mult))
                    op('v', ["eq"], [dk], lambda e, dst=dst: e.tensor_reduce(out=dst[:], in_=eq[:], axis=AX.X, op=ALU.add))
                op('v', ["i1", "i2"], ["i1"], lambda e: e.scalar_tensor_tensor(out=i1[:], in0=i1[:], scalar=128.0, in1=i2[:], op0=ALU.mult, op1=ALU.add))
                op('v', ["i1"], ["ei%d" % p], lambda e: e.tensor_copy(out=ei[p][:], in_=i1[:].rearrange("p h k -> p (h k)")))

            def stage_A3(it):
                p = it % 2
                gk = "gate%d" % p
                g_ = gate[p]
                op('v', cvk, ["sm"], lambda e: e.tensor_reduce(out=sm[:, 0, :], in_=cv[:], axis=AX.X, op=ALU.max))
                op('v', cvk + ["sm"], [gk], lambda e: e.tensor_tensor(out=g_[:], in0=cv[:], in1=bc(sm[:, 0, :], [128, 8, 16], 2), op=ALU.subtract))
                op('a', [gk], [gk], lambda e: e.activation(out=g_[:], in_=g_[:], func=AF.Exp))
                op('v', [gk], ["sm"], lambda e: e.tensor_reduce(out=sm[:, 1, :], in_=g_[:], axis=AX.X, op=ALU.add))
                op('v', ["sm"], ["sm"], lambda e: e.reciprocal(out=sm[:, 2, :], in_=sm[:, 1, :]))
                op('v', [gk, "sm"], [gk], lambda e: e.tensor_tensor(out=g_[:], in0=g_[:], in1=bc(sm[:, 2, :], [128, 8, 16], 2), op=ALU.mult))

            def stage_U(it):
                p = it % 2
                xk = "xn2_%d" % p
                xc = xn2[p]
                eik = "ei%d" % p
                op('v', [], ["hid"], lambda e: e.memset(hid[:], 0.0))
                for s_ in range(128):
                    b = gcount[0] % NB
                    gcount[0] += 1
                    dma('g', 'du%d' % b, U[b][:], tub_d, [eik], ["U%d" % b], in_offset=bass.IndirectOffsetOnAxis(ap=ei[p][:, s_:s_ + 1], axis=0))
                    op('v', ["U%d" % b, "xn2b_%d" % p], ["junkb", "hid"], lambda e, b=b, s_=s_: e.scalar_tensor_tensor(
                        out=junkb[:], in0=U[b][:], scalar=1.0, in1=xn2b[p][:], op0=ALU.mult, op1=ALU.mult, accum_out=hid[:, s_:s_ + 1]))
                wk = "wgt%d" % p
                gelu_tanh(op, hid[:], "hid", gtmp[:], "gtmp", wgt[p][:], wk, sq_eng='v')
                op('v', [wk, "gate%d" % p], [wk], lambda e: e.tensor_tensor(out=wgt[p][:], in0=wgt[p][:], in1=gate[p][:].rearrange("p h k -> p (h k)"), op=ALU.mult))

            def stage_V(it):
                p = it % 2
                hk = "h1t%d" % p
                hcur = h1t[p]
                eik = "ei%d" % p
                wk = "wgt%d" % p
                for s_ in range(128):
                    b = gcount[1] % NB
                    gcount[1] += 1
                    r = gcount[2] % NDG
                    gcount[2] += 1
                    dma('g', 'dv%d' % b, Vb[b][:], tvb_d, [eik], ["Vb%d" % b], in_offset=bass.IndirectOffsetOnAxis(ap=ei[p][:, s_:s_ + 1], axis=0))
                    op('a', [wk, "c"], ["dg%d" % r], lambda e, r=r, s_=s_: e.activation(out=dg[r][:], in_=ident[:], func=AF.Copy, scale=wgt[p][:, s_:s_ + 1]))

                    def mm_v(e, b=b, r=r, s_=s_):
                        e.matmul(PS[6][:], lhsT=dg[r][:], rhs=Vb[b][:, 0:512], start=(s_ == 0), stop=(s_ == 127))
                        return e.matmul(PS[7][:], lhsT=dg[r][:], rhs=Vb[b][:, 512:1024], start=(s_ == 0), stop=(s_ == 127))
                    op('p', ["dg%d" % r, "Vb%d" % b], [psk(6), psk(7)], mm_v)

            def stage_Vf(it):
                p = it % 2
                hk = "h1t%d" % p
                hcur = h1t[p]
                op('v', [psk(6), hk], ["h2"], lambda e: e.tensor_tensor(out=h2[:, 0:512], in0=PS[6][:], in1=hcur[:, 0:512], op=ALU.add))
                op('v', [psk(7), hk], ["h2"], lambda e: e.tensor_tensor(out=h2[:, 512:1024], in0=PS[7][:], in1=hcur[:, 512:1024], op=ALU.add))
                rms(h2, "h2", statV, "statV")
                op('v', ["h2", "statV", "c"], ["outt"], lambda e: e.scalar_tensor_tensor(out=outt[:], in0=h2[:], scalar=statV[:, 2:3], in1=gfin[:], op0=ALU.mult, op1=ALU.mult))
                dma('s', 'dout', out_d[it * 128:(it + 1) * 128, :], outt[:], ["outt"], ["od%d" % it])

            stage_A(0)
            stage_A2(0)
            stage_A3(0)
            for it in range(NT):
                if it > 0:
                    stage_Vf(it - 1)
                if it + 1 < NT:
                    stage_A(it + 1)
                stage_U(it)
                if it + 1 < NT:
                    stage_A2(it + 1)
                stage_V(it)
                if it + 1 < NT:
                    stage_A3(it + 1)
            stage_Vf(NT - 1)
            sch.barrier()
    return nc


def make_inputs(inp, b):
    f = lambda a: np.ascontiguousarray(np.asarray(a), dtype=np.float32)
    colT = lambda v, n: f(np.asarray(v).reshape(n, 128).T)
    m = {}
    m["x"] = f(inp["x"][b])
    m["w_in"] = f(inp["w_in"][0])
    m["gainP"] = colT(inp["norm_mix"][0], 8)
    m["bgate"] = colT(inp["b_gate"][0], 16)
    m["mu"] = colT(inp["mu_rwkv"][0], 14)
    pv = np.stack([colT(inp["w0"][0], 4), colT(inp["a0"][0], 4), colT(inp["k_k"][0], 4), colT(inp["k_a"][0], 4),
                   colT(np.asarray(inp["r_k"][0]).reshape(512), 4), colT(np.asarray(inp["s5_d"][0]).reshape(512), 4)], axis=1)
    m["pvec"] = f(pv)
    m["lora"] = f(np.concatenate([np.asarray(inp["w_lora_up"][0]), np.asarray(inp["a_lora_up"][0])], axis=0))
    m["glora"] = f(inp["g_lora_up"][0])
    m["lnw"] = f(np.broadcast_to(np.asarray(inp["ln_x_w"][0])[None, :], (128, 512)))
    m["lnb"] = f(np.broadcast_to(np.asarray(inp["ln_x_b"][0])[None, :], (128, 512)))
    m["w_o"] = f(inp["w_o_rwkv"][0])
    m["w_glu"] = f(inp["w_glu_s5"][0])
    m["w_out"] = f(inp["w_out"][0])
    a_re = np.asarray(inp["s5_a_re"][0]).reshape(16, 128).T
    a_im = np.asarray(inp["s5_a_im"][0]).reshape(16, 128).T
    ldt = np.repeat(np.asarray(inp["s5_log_dt"][0]), 64).reshape(16, 128).T
    m["s5p"] = f(np.stack([a_re, a_im, ldt], axis=1))
    bbre = np.zeros((128, 16, 128), np.float32)
    bbim = np.zeros((128, 16, 128), np.float32)
    ccre = np.zeros((128, 16, 128), np.float32)
    ccim = np.zeros((128, 16, 128), np.float32)
    b_re = np.asarray(inp["s5_b_re"][0]); b_im = np.asarray(inp["s5_b_im"][0])
    c_re = np.asarray(inp["s5_c_re"][0]); c_im = np.asarray(inp["s5_c_im"][0])
    for g in range(32):
        j, gl, g8 = g // 2, g % 2, g % 8
        bbre[g8 * 16:(g8 + 1) * 16, j, gl * 64:(gl + 1) * 64] = b_re[g].T
        bbim[g8 * 16:(g8 + 1) * 16, j, gl * 64:(gl + 1) * 64] = b_im[g].T
        ccre[gl * 64:(gl + 1) * 64, j, g8 * 16:(g8 + 1) * 16] = c_re[g].T
        ccim[gl * 64:(gl + 1) * 64, j, g8 * 16:(g8 + 1) * 16] = c_im[g].T
    m["bbre"] = bbre.reshape(128, 2048)
    m["bbim"] = bbim.reshape(128, 2048)
    m["ccre"] = ccre.reshape(128, 2048)
    m["ccim"] = ccim.reshape(128, 2048)
    m["gffn"] = f(np.broadcast_to(np.asarray(inp["norm_ffn"][0])[None, :], (128, D)))
    m["gfin"] = f(np.broadcast_to(np.asarray(inp["norm_final"])[None, :], (128, D)))
    m["wq"] = f(inp["peer_wq"][0])
    m["subk"] = f(np.asarray(inp["peer_subkeys"][0]).transpose(1, 3, 0, 2).reshape(128, 1024))
    m["peer_u"] = f(inp["peer_u"][0])
    m["peer_v"] = f(inp["peer_v"][0])
    m["ident"] = np.eye(128, dtype=np.float32)
    p = np.arange(128)[:, None]
    jx = np.arange(128)[None, :]
    masks = np.zeros((128, 5, 128), np.float32)
    masks[:, 0] = (jx < p)
    masks[:, 1] = (jx > p)
    masks[:, 2] = (jx >= p)
    masks[:, 3] = ((jx // 64) == (p // 64))
    masks[:, 4] = 1.0
    masks[:, 4, 0] = 0.0
    m["masks"] = masks
    sel2 = np.zeros((128, 2), np.float32)
    sel2[:64, 0] = 1.0
    sel2[64:, 1] = 1.0
    m["sel2"] = sel2
    m["iota16"] = np.ascontiguousarray(np.broadcast_to(np.arange(16, dtype=np.float32)[None, :], (128, 16)))
    return m


_NC_CACHE = {}


def kernel(**inputs):
    n = 8
    if "nc" not in _NC_CACHE:
        _NC_CACHE["nc"] = build_nc()
    nc = _NC_CACHE["nc"]
    in_maps = [make_inputs(inputs, b) for b in range(n)]
    res = run_bass_kernel_spmd(nc, in_maps, core_ids=list(range(n)))
    out = np.stack([np.asarray(r["out"], dtype=np.float32) for r in res.results], axis=0)
    return out
```

```python
import numpy as np
from contextlib import ExitStack
import concourse.bass as bass
import concourse.mybir as mybir
from concourse.bass_utils import run_bass_kernel_spmd

F32 = mybir.dt.float32
BF16 = mybir.dt.bfloat16
I32 = mybir.dt.int32
U32 = mybir.dt.uint32
ALU = mybir.AluOpType
AF = mybir.ActivationFunctionType
AX = mybir.AxisListType

D = 1024
NRW = 1792
NCOL = 4352
SEQ = 4096
NCT = 34
C0 = float(np.exp(-0.5))
PI = float(np.pi)
GN_EPS = 64e-5
NB = 16
NEG = -1.0e30


class Sched:
    def __init__(self, nc, es):
        self.nc = nc
        self.es = es
        self.engs = {'v': nc.vector, 'a': nc.scalar, 'p': nc.tensor, 'g': nc.gpsimd, 's': nc.sync}
        self.sems = {}
        self.val = {}
        self.waited = {e: {} for e in self.engs}
        self.lastw = {}
        self.readers = {}
        self.nins = 0
        for e in 'vapg':
            self._mk(e)

    def _mk(self, key):
        self.sems[key] = self.es.enter_context(self.nc.semaphore('sem_' + key))
        self.val[key] = 0

    def _wait(self, e, k, v):
        if self.waited[e].get(k, 0) >= v:
            return
        self.engs[e].wait_ge(self.sems[k], v)
        self.waited[e][k] = v

    def _deps(self, e, reads, writes):
        for b in reads:
            if b in self.lastw:
                self._wait(e, *self.lastw[b])
        for b in writes:
            if b in self.lastw:
                self._wait(e, *self.lastw[b])
            for k, v in self.readers.get(b, {}).items():
                self._wait(e, k, v)

    def _commit(self, tok, reads, writes):
        k, v = tok
        for b in reads:
            self.readers.setdefault(b, {})[k] = v
        for b in writes:
            self.lastw[b] = tok
            self.readers[b] = {}

    def op(self, e, reads, writes, fn):
        self._deps(e, reads, writes)
        ins = fn(self.engs[e])
        self.val[e] += 1
        ins.then_inc(self.sems[e], 1)
        self._commit((e, self.val[e]), reads, writes)
        self.nins += 1

    def dma(self, q, semkey, out, in_, reads, writes, in_offset=None):
        if semkey not in self.sems:
            self._mk(semkey)
        self._deps(q, reads, writes)
        if self.val[semkey] > 0:
            self._wait(q, semkey, self.val[semkey])
        eng = self.engs[q]
        if in_offset is not None:
            ins = eng.indirect_dma_start(out=out, out_offset=None, in_=in_, in_offset=in_offset)
        else:
            ins = eng.dma_start(out=out, in_=in_)
        self.val[semkey] += 16
        ins.then_inc(self.sems[semkey], 16)
        self._commit((semkey, self.val[semkey]), reads, writes)
        self.nins += 1

    def barrier(self):
        for e in self.engs:
            for k, v in self.val.items():
                if v > 0:
                    self._wait(e, k, v)
        self.lastw = {}
        self.readers = {}


def bc(ap, shape, axis):
    return ap.unsqueeze(axis).to_broadcast(shape)


def gelu_tanh(op, src, srck, tmp, tmpk, dst, dstk, sq_eng='g'):
    op(sq_eng, [srck], [tmpk], lambda e: e.tensor_tensor(out=tmp, in0=src, in1=src, op=ALU.mult))
    op('v', [tmpk], [tmpk], lambda e: e.tensor_scalar(out=tmp, in0=tmp, scalar1=0.044715, scalar2=1.0, op0=ALU.mult, op1=ALU.add))
    op('v', [tmpk, srck], [tmpk], lambda e: e.tensor_tensor(out=tmp, in0=tmp, in1=src, op=ALU.mult))
    op('a', [tmpk], [tmpk], lambda e: e.activation(out=tmp, in_=tmp, func=AF.Sigmoid, scale=1.5957691216057308))
    op('v', [tmpk, srck], [dstk], lambda e: e.tensor_tensor(out=dst, in0=tmp, in1=src, op=ALU.mult))


class _Stop(Exception):
    pass


def build_nc(NT=32, debug=False, phases=(1, 2), stop=None):
    nc = bass.Bass("TRN2", target_bir_lowering=False)

    def mark(name):
        if stop == name:
            raise _Stop()

    def din(name, shape, dt=F32):
        return nc.dram_tensor(name, shape, dt, kind="ExternalInput").ap()

    x_d = din("x", [SEQ, D])
    w_in_d = din("w_in", [D, NCOL])
    gainP_d = din("gainP", [128, 8])
    bgate_d = din("bgate", [128, 16])
    mu_d = din("mu", [128, 14])
    pv_d = din("pvec", [128, 6, 4])
    lora_d = din("lora", [128, 512])
    glora_d = din("glora", [128, 512])
    lnw_d = din("lnw", [128, 512])
    lnb_d = din("lnb", [128, 512])
    w_o_d = din("w_o", [512, D])
    w_glu_d = din("w_glu", [512, 2 * D])
    w_out_d = din("w_out", [D, D])
    s5p_d = din("s5p", [128, 3, 16])
    bbre_d = din("bbre", [128, 2048])
    bbim_d = din("bbim", [128, 2048])
    ccre_d = din("ccre", [128, 2048])
    ccim_d = din("ccim", [128, 2048])
    gffn_d = din("gffn", [128, D])
    gfin_d = din("gfin", [128, D])
    wq_d = din("wq", [D, D])
    subk_d = din("subk", [128, 1024])
    pu_d = din("peer_u", [16384, D])
    pvv_d = din("peer_v", [16384, D])
    ident_d = din("ident", [128, 128])
    masks_d = din("masks", [128, 5, 128])
    sel2_d = din("sel2", [128, 2])
    iota_d = din("iota16", [128, 16])
    out_d = nc.dram_tensor("out", [SEQ, D], F32, kind="ExternalOutput").ap()
    h1_d = nc.dram_tensor("h1s", [SEQ, D], F32, kind="ExternalOutput" if debug else "Internal").ap()
    wsc_d = nc.dram_tensor("wsc", [NCT, 128, 1024], BF16, kind="Internal").ap()
    tuv_d = nc.dram_tensor("tuv", [16384, 2, D], BF16, kind="Internal").ap()

    with ExitStack() as es0:
        sch = Sched(nc, es0)
        op = sch.op
        dma = sch.dma
        PS = [es0.enter_context(nc.psum_tensor("ps%d" % i, [128, 512], F32)) for i in range(8)]

        def psk(i):
            return "ps%d" % i

        def P3(i):
            return PS[i][:].rearrange("p (a b) -> p a b", a=4)

        if 1 in phases:
         try:
          with ExitStack() as es:
            def sb(name, shape, dt=F32):
                return es.enter_context(nc.sbuf_tensor("s_" + name, shape, dt))

            ident = sb("ident", [128, 128])
            masks = sb("masks", [128, 5, 128])
            sel2 = sb("sel2", [128, 2])
            gainP = sb("gainP", [128, 8])
            bgate = sb("bgate", [128, 16])
            mu = sb("mu", [128, 14])
            pvec = sb("pvec", [128, 6, 4])
            lnw = sb("lnw", [128, 512])
            lnb = sb("lnb", [128, 512])
            s5p = sb("s5p", [128, 3, 16])
            sp = sb("sp", [128, 24, 16])
            lora = sb("lora", [128, 512], BF16)
            glora = sb("glora", [128, 512], BF16)
            w_o = sb("w_o", [128, 4, 1024], BF16)
            w_glu = sb("w_glu", [128, 4, 2048], BF16)
            w_out = sb("w_out", [128, 8, 1024], BF16)
            BBre = sb("BBre", [128, 16, 128], BF16)
            BBim = sb("BBim", [128, 16, 128], BF16)
            CCre = sb("CCre", [128, 16, 128], BF16)
            CCimN = sb("CCimN", [128, 16, 128], BF16)
            Ere = sb("Ere", [128, 16, 128])
            Eim = sb("Eim", [128, 16, 128])
            SCR = sb("SCR", [128, 8192])
            esp = ExitStack()
            stgb = [esp.enter_context(nc.sbuf_tensor("s_stgb%d" % i, [128, 1024], BF16)) for i in range(2)]

            for t_sb, t_d in [(ident, ident_d), (masks, masks_d), (sel2, sel2_d), (gainP, gainP_d),
                              (bgate, bgate_d), (mu, mu_d), (pvec, pv_d), (lnw, lnw_d), (lnb, lnb_d),
                              (s5p, s5p_d)]:
                dma('s', 'dc', t_sb[:], t_d, [], ["c"])
            maskSL = masks[:, 0, :]
            maskSU = masks[:, 1, :]
            maskUI = masks[:, 2, :]
            maskBD = masks[:, 3, :]
            scanmask = masks[:, 4, :]

            stg = [SCR[:, 0:2048], SCR[:, 2048:4096]]
            tmpA = SCR[:, 4096:6144]
            tmpB = SCR[:, 6144:8192]

            w_in_v = w_in_d.rearrange("(kt p) c -> p kt c", p=128)
            for c in range(NCT):
                b = c % 2
                sg = "stg%d" % b
                sgb = "stgb%d" % b
                dma('s', 'dst%d' % b, stg[b][:, 0:1024].rearrange("p (k c) -> p k c", k=8),
                    w_in_v[:, :, c * 128:(c + 1) * 128], [], [sg])
                op('v', [sg, "c"], [sgb], lambda e, b=b: e.tensor_tensor(
                    out=stgb[b][:].rearrange("p (k c) -> p k c", k=8),
                    in0=stg[b][:, 0:1024].rearrange("p (k c) -> p k c", k=8),
                    in1=bc(gainP[:], [128, 8, 128], 2), op=ALU.mult))
                dma('s', 'dsb%d' % b, wsc_d[c], stgb[b][:], [sgb], ["wsc%d" % c])

            ldn = [0]

            def load_cast(dst_ap, src_ap, width, dstkey):
                b = ldn[0] % 2
                ldn[0] += 1
                sg = "stg%d" % b
                dma('s', 'dst%d' % b, stg[b][:, 0:width], src_ap, [], [sg])
                if b == 0:
                    op('v', [sg], [dstkey], lambda e: e.tensor_copy(out=dst_ap, in_=stg[b][:, 0:width]))
                else:
                    op('a', [sg], [dstkey], lambda e: e.activation(out=dst_ap, in_=stg[b][:, 0:width], func=AF.Copy))

            load_cast(lora[:], lora_d, 512, "lora")
            load_cast(glora[:], glora_d, 512, "glora")
            for k in range(4):
                load_cast(w_o[:, k, :], w_o_d[k * 128:(k + 1) * 128, :], 1024, "w_o")
            for k in range(4):
                load_cast(w_glu[:, k, :], w_glu_d[k * 128:(k + 1) * 128, :], 2048, "w_glu")
            for k in range(8):
                load_cast(w_out[:, k, :], w_out_d[k * 128:(k + 1) * 128, :], 1024, "w_out")
            load_cast(BBre[:].rearrange("p a b -> p (a b)"), bbre_d, 2048, "BB")
            load_cast(BBim[:].rearrange("p a b -> p (a b)"), bbim_d, 2048, "BB")

            def V2(i):
                return sp[:, i, :]
            a_re = s5p[:, 0, :]
            a_im = s5p[:, 1, :]
            ldt = s5p[:, 2, :]
            DT, TH, LAB, CS, SN, R, M_, LRE, LIM, NRE, DEN, FRE, FIM, T1, T2, CS2, SN2 = range(17)

            def vv(out_i, a, b_, o):
                op('v', ["c", "sp"], ["sp"], lambda e: e.tensor_tensor(out=V2(out_i), in0=a, in1=b_, op=o))
            op('a', ["c"], ["sp"], lambda e: e.activation(out=V2(DT), in_=ldt, func=AF.Exp))
            vv(TH, a_im, V2(DT), ALU.mult)
            vv(T1, a_re, V2(DT), ALU.mult)
            op('a', ["sp"], ["sp"], lambda e: e.activation(out=V2(LAB), in_=V2(T1), func=AF.Exp))

            def sin_of(dst, shift):
                op('v', ["sp"], ["sp"], lambda e: e.tensor_scalar(out=V2(R), in0=V2(TH), scalar1=float(shift), scalar2=None, op0=ALU.add))
                for _ in range(4):
                    op('v', ["sp"], ["sp"], lambda e: e.tensor_scalar(out=V2(M_), in0=V2(R), scalar1=PI, scalar2=-2.0 * PI, op0=ALU.is_ge, op1=ALU.mult))
                    vv(R, V2(R), V2(M_), ALU.add)
                op('a', ["sp"], ["sp"], lambda e: e.activation(out=V2(dst), in_=V2(R), func=AF.Sin))
            sin_of(SN, 0.0)
            sin_of(CS, PI / 2)
            vv(LRE, V2(LAB), V2(CS), ALU.mult)
            vv(LIM, V2(LAB), V2(SN), ALU.mult)
            op('v', ["sp"], ["sp"], lambda e: e.tensor_scalar(out=V2(NRE), in0=V2(LRE), scalar1=-1.0, scalar2=None, op0=ALU.add))
            vv(T1, a_re, a_re, ALU.mult)
            vv(T2, a_im, a_im, ALU.mult)
            vv(DEN, V2(T1), V2(T2), ALU.add)
            op('v', ["sp"], ["sp"], lambda e: e.reciprocal(out=V2(DEN), in_=V2(DEN)))
            vv(T1, V2(NRE), a_re, ALU.mult)
            vv(T2, V2(LIM), a_im, ALU.mult)
            vv(T1, V2(T1), V2(T2), ALU.add)
            vv(FRE, V2(T1), V2(DEN), ALU.mult)
            vv(T1, V2(LIM), a_re, ALU.mult)
            vv(T2, V2(NRE), a_im, ALU.mult)
            vv(T1, V2(T1), V2(T2), ALU.subtract)
            vv(FIM, V2(T1), V2(DEN), ALU.mult)

            dma('s', 'dst0', stg[0], ccre_d, [], ["stg0"])
            dma('s', 'dst1', stg[1], ccim_d, [], ["stg1"])

            def c3(a):
                return a.rearrange("p (j m) -> p j m", j=16)
            fre_b = bc(V2(FRE), [128, 16, 128], 2)
            fim_b = bc(V2(FIM), [128, 16, 128], 2)
            op('v', ["stg0", "sp"], ["tmpA"], lambda e: e.tensor_tensor(out=c3(tmpA), in0=c3(stg[0]), in1=fre_b, op=ALU.mult))
            op('v', ["stg1", "sp"], ["tmpB"], lambda e: e.tensor_tensor(out=c3(tmpB), in0=c3(stg[1]), in1=fim_b, op=ALU.mult))
            op('v', ["tmpA", "tmpB"], ["CC"], lambda e: e.tensor_tensor(out=CCre[:].rearrange("p a b -> p (a b)"), in0=tmpA, in1=tmpB, op=ALU.subtract))
            op('v', ["stg0", "sp", "CC"], ["tmpA"], lambda e: e.tensor_tensor(out=c3(tmpA), in0=c3(stg[0]), in1=fim_b, op=ALU.mult))
            op('v', ["stg1", "sp", "CC"], ["tmpB"], lambda e: e.tensor_tensor(out=c3(tmpB), in0=c3(stg[1]), in1=fre_b, op=ALU.mult))
            op('v', ["tmpA", "tmpB"], ["tmpA"], lambda e: e.tensor_tensor(out=tmpA, in0=tmpA, in1=tmpB, op=ALU.add))
            op('v', ["tmpA"], ["CC"], lambda e: e.tensor_scalar(out=CCimN[:].rearrange("p a b -> p (a b)"), in0=tmpA, scalar1=-1.0, scalar2=None, op0=ALU.mult))

            op('v', [], ["E"], lambda e: e.memset(Ere[:, :, 0:1], 1.0))
            op('v', [], ["E"], lambda e: e.memset(Eim[:, :, 0:1], 0.0))
            op('v', ["sp"], ["sp"], lambda e: e.tensor_copy(out=V2(CS2), in_=V2(CS)))
            op('v', ["sp"], ["sp"], lambda e: e.tensor_copy(out=V2(SN2), in_=V2(SN)))
            et0 = tmpA[:, 0:1024].rearrange("p (a b) -> p a b", a=16)
            et1 = tmpB[:, 0:1024].rearrange("p (a b) -> p a b", a=16)
            for lv in range(7):
                m = 1 << lv
                shp = [128, 16, m]
                cb = bc(V2(CS2), shp, 2)
                sbb = bc(V2(SN2), shp, 2)
                op('v', ["E", "sp", "tmpA"], ["tmpA"], lambda e, m=m, cb=cb: e.tensor_tensor(out=et0[:, :, 0:m], in0=Ere[:, :, 0:m], in1=cb, op=ALU.mult))
                op('v', ["E", "sp", "tmpB"], ["tmpB"], lambda e, m=m, sbb=sbb: e.tensor_tensor(out=et1[:, :, 0:m], in0=Eim[:, :, 0:m], in1=sbb, op=ALU.mult))
                op('v', ["tmpA", "tmpB", "E"], ["E"], lambda e, m=m: e.tensor_tensor(out=Ere[:, :, m:2 * m], in0=et0[:, :, 0:m], in1=et1[:, :, 0:m], op=ALU.subtract))
                op('v', ["E", "sp", "tmpA"], ["tmpA"], lambda e, m=m, sbb=sbb: e.tensor_tensor(out=et0[:, :, 0:m], in0=Ere[:, :, 0:m], in1=sbb, op=ALU.mult))
                op('v', ["E", "sp", "tmpB"], ["tmpB"], lambda e, m=m, cb=cb: e.tensor_tensor(out=et1[:, :, 0:m], in0=Eim[:, :, 0:m], in1=cb, op=ALU.mult))
                op('v', ["tmpA", "tmpB", "E"], ["E"], lambda e, m=m: e.tensor_tensor(out=Eim[:, :, m:2 * m], in0=et0[:, :, 0:m], in1=et1[:, :, 0:m], op=ALU.add))
                vv(T1, V2(CS2), V2(CS2), ALU.mult)
                vv(T2, V2(SN2), V2(SN2), ALU.mult)
                op('v', ["sp"], ["sp"], lambda e: e.scalar_tensor_tensor(out=V2(SN2), in0=V2(CS2), scalar=2.0, in1=V2(SN2), op0=ALU.mult, op1=ALU.mult))
                vv(CS2, V2(T1), V2(T2), ALU.subtract)

            sch.barrier()
            esp.close()
            mark("prep")

            PF = sb("PF", [128, 14, 129])
            Stp = sb("Stp", [128, 4, 128])
            cw = sb("cw", [128, 2, 16])
            op('v', [], ["PF"], lambda e: e.memset(PF[:], 0.0))
            op('v', [], ["Stp"], lambda e: e.memset(Stp[:], 0.0))
            op('v', [], ["cw0", "cw1", "cw2", "cw3"], lambda e: e.memset(cw[:], 0.0))

            xt = [sb("xt%d" % i, [128, D]) for i in range(2)]
            stat = sb("stat", [128, 8])
            xnF = sb("xnF", [128, 8, 128], BF16)
            wch = [sb("wch%d" % i, [128, 8, 128], BF16) for i in range(3)]
            L = sb("L", [128, 14, 128])
            uF = sb("uF", [128, 4, 128])
            uB = sb("uB", [128, 4, 128], BF16)
            gates = sb("gates", [128, 16, 128], BF16)
            lorain = sb("lorain", [128, 128], BF16)
            sgx = sb("sgx", [128, 128], BF16)
            TT = [SCR[:, i * 512:(i + 1) * 512].rearrange("p (a b) -> p a b", a=4) for i in range(16)]
            TK = ["T%d" % i for i in range(16)]
            (iSIG, iCS, iPT, iPTM, iPINV, iAV, iKK, iTMP, iKMOD, iRTL, iATL, iBTL, iKTL, iRKR, iY2, iX) = range(16)
            VT = sb("VT", [128, 512])
            BT = sb("BT", [128, 512])
            KT = sb("KT", [128, 512])
            gT = sb("gT", [128, 512])
            mats = {nm: sb("m_" + nm, [128, 4, 128]) for nm in ["X", "XT", "X2", "X2T", "QT", "akT", "rbT", "rkT"]}
            RHS = sb("RHS", [128, 512])
            SA = sb("SA", [128, 512])
            PTbd = sb("PTbd", [128, 4, 128])
            Y = sb("Y", [128, 512])
            gst = sb("gst", [128, 6, 8])
            bon = sb("bon", [128, 8])
            roF = sb("roF", [128, 4, 128], BF16)
            mixed = sb("mixed", [128, 8, 128])
            mixedB = sb("mixedB", [128, 8, 128], BF16)
            xre2 = [sb("xre%d" % i, [128, 4, 128], BF16) for i in range(2)]
            xim2 = [sb("xim%d" % i, [128, 4, 128], BF16) for i in range(2)]
            ygB = sb("ygB", [128, 4, 128], BF16)
            zg = sb("zg", [128, 8, 128])
            h1 = sb("h1", [128, D])
            cwn2 = [sb("cwn%d" % i, [128, 4, 4]) for i in range(2)]

            w0 = pvec[:, 0, :]
            a0 = pvec[:, 1, :]
            k_k = pvec[:, 2, :]
            k_a = pvec[:, 3, :]
            r_k = pvec[:, 4, :]
            s5d = pvec[:, 5, :]
            S4 = [128, 4, 128]

            def tt(eng, o, ok, a, ak, b_, bk, alu):
                op(eng, [ak, bk], [ok], lambda e: e.tensor_tensor(out=o, in0=a, in1=b_, op=alu))

            dma('s', 'dx0', xt[0][:], x_d[0:128, :], [], ["xt0"])

            for it in range(NT):
                xb = it % 2
                xk = "xt%d" % xb
                xcur = xt[xb]
                if it + 1 < NT:
                    dma('s', 'dx%d' % (1 - xb), xt[1 - xb][:], x_d[(it + 1) * 128:(it + 2) * 128, :], [], ["xt%d" % (1 - xb)])
                op('a', [xk], [TK[iX], "stat"], lambda e: e.activation(out=SCR[:, iX * 512:iX * 512 + 512], in_=xcur[:, 0:512], func=AF.Square, accum_out=stat[:, 0:1]))
                op('a', [xk], [TK[iX], "stat"], lambda e: e.activation(out=SCR[:, iX * 512:iX * 512 + 512], in_=xcur[:, 512:1024], func=AF.Square, accum_out=stat[:, 3:4]))
                op('v', ["stat"], ["stat"], lambda e: e.tensor_tensor(out=stat[:, 0:1], in0=stat[:, 0:1], in1=stat[:, 3:4], op=ALU.add))
                op('a', ["stat"], ["stat"], lambda e: e.activation(out=stat[:, 1:2], in_=stat[:, 0:1], func=AF.Sqrt, bias=1e-6, scale=1.0 / D))
                op('v', ["stat"], ["stat"], lambda e: e.reciprocal(out=stat[:, 2:3], in_=stat[:, 1:2]))
                op('v', [xk, "stat"], [xk], lambda e: e.tensor_scalar(out=xcur[:], in0=xcur[:], scalar1=stat[:, 2:3], scalar2=None, op0=ALU.mult))
                mark("norm")
                for half in range(2):
                    pk = psk(half)

                    def tr(e, half=half):
                        ins = None
                        for q in range(4):
                            kt = half * 4 + q
                            ins = e.transpose(out=PS[half][:, q * 128:(q + 1) * 128], in_=xcur[:, kt * 128:(kt + 1) * 128], identity=ident[:])
                        return ins
                    op('p', [xk, "c"], [pk], tr)
                    if half == 0:
                        op('v', [pk], ["xnF"], lambda e: e.tensor_copy(out=xnF[:, 0:4, :], in_=P3(0)))
                    else:
                        op('a', [pk], ["xnF"], lambda e: e.activation(out=xnF[:, 4:8, :], in_=P3(1), func=AF.Copy))
                mark("xnF")
                for c in range(NCT):
                    mark("proj%d" % c)
                    wb = c % 3
                    wk = "wch%d" % wb
                    dma('s', 'dw%d' % wb, wch[wb][:].rearrange("p a b -> p (a b)"), wsc_d[c], ["wsc%d" % c], [wk])
                    pb = 2 + (c % 4)
                    pk = psk(pb)

                    def mm(e, wb=wb, pb=pb):
                        ins = None
                        for kt in range(8):
                            ins = e.matmul(PS[pb][:, 0:128], lhsT=wch[wb][:, kt, :], rhs=xnF[:, kt, :], start=(kt == 0), stop=(kt == 7))
                        return ins
                    op('p', [wk, "xnF"], [pk], mm)
                    if c < 14:
                        if c % 2 == 0:
                            op('v', [pk], ["PF"], lambda e, c=c, pb=pb: e.tensor_copy(out=PF[:, c, 1:129], in_=PS[pb][:, 0:128]))
                        else:
                            op('a', [pk], ["PF"], lambda e, c=c, pb=pb: e.activation(out=PF[:, c, 1:129], in_=PS[pb][:, 0:128], func=AF.Copy))
                    elif c < 18:
                        op('v', [pk], ["uF"], lambda e, c=c, pb=pb: e.tensor_copy(out=uF[:, c - 14, :], in_=PS[pb][:, 0:128]))
                        op('a', ["uF"], ["uB"], lambda e, c=c, pb=pb: e.activation(out=uB[:, c - 14, :], in_=uF[:, c - 14, :], func=AF.Copy))
                    else:
                        op('a', [pk, "c"], ["gates"], lambda e, c=c, pb=pb: e.activation(out=gates[:, c - 18, :], in_=PS[pb][:, 0:128], func=AF.Sigmoid, bias=bgate[:, c - 18:c - 17]))
                mark("proj")
                op('v', ["PF"], ["L"], lambda e: e.tensor_tensor(out=L[:], in0=PF[:, :, 0:128], in1=PF[:, :, 1:129], op=ALU.subtract))
                op('g', ["L", "c"], ["L"], lambda e: e.tensor_tensor(out=L[:], in0=L[:], in1=bc(mu[:], [128, 14, 128], 2), op=ALU.mult))
                op('v', ["L", "PF"], ["L"], lambda e: e.tensor_tensor(out=L[:], in0=L[:], in1=PF[:, :, 1:129], op=ALU.add))
                op('v', ["PF"], ["PF"], lambda e: e.tensor_copy(out=PF[:, :, 0:1], in_=PF[:, :, 128:129]))
                rF = L[:, 0:4, :]
                kF = L[:, 4:8, :]
                vF = L[:, 8:12, :]
                sig, cs, Pt, Ptm1, Pinv, av, kk, tmp, kmod, rtl, atl, btl, ktl, rkr = [TT[i] for i in range(14)]
                op('a', ["L"], ["lorain"], lambda e: e.activation(out=lorain[0:64, :], in_=L[0:64, 12, :], func=AF.Tanh))
                op('v', ["L"], ["lorain"], lambda e: e.tensor_copy(out=lorain[64:128, :], in_=L[64:128, 12, :]))
                op('a', ["L"], ["sgx"], lambda e: e.activation(out=sgx[:], in_=L[:, 13, :], func=AF.Sigmoid))

                def mm_lw(e):
                    ins = None
                    for j in range(4):
                        ins = e.matmul(PS[0][:, j * 128:(j + 1) * 128], lhsT=lora[0:64, j * 128:(j + 1) * 128], rhs=lorain[0:64, :], start=True, stop=True)
                    return ins
                op('p', ["lora", "lorain"], [psk(0)], mm_lw)

                def mm_la(e):
                    ins = None
                    for j in range(4):
                        ins = e.matmul(PS[1][:, j * 128:(j + 1) * 128], lhsT=lora[64:128, j * 128:(j + 1) * 128], rhs=lorain[64:128, :], start=True, stop=True)
                    return ins
                op('p', ["lora", "lorain"], [psk(1)], mm_la)
                op('p', ["glora", "sgx"], [psk(6)], lambda e: e.matmul(PS[6][:], lhsT=sgx[:], rhs=glora[:], start=True, stop=True))
                for j in range(4):
                    op('a', [psk(0), "c"], [TK[iSIG]], lambda e, j=j: e.activation(out=sig[:, j, :], in_=PS[0][:, j * 128:(j + 1) * 128], func=AF.Sigmoid, bias=w0[:, j:j + 1]))
                    op('a', [psk(1), "c"], [TK[iAV]], lambda e, j=j: e.activation(out=av[:, j, :], in_=PS[1][:, j * 128:(j + 1) * 128], func=AF.Sigmoid, bias=a0[:, j:j + 1]))
                op('v', [psk(6)], ["gT"], lambda e: e.tensor_copy(out=gT[:], in_=PS[6][:]))
                for j in range(4):
                    op('v', [TK[iSIG], "c"], [TK[iCS]], lambda e, j=j: e.tensor_tensor_scan(out=cs[:, j, :], data0=scanmask, data1=sig[:, j, :], initial=0.0, op0=ALU.mult, op1=ALU.add))
                op('a', [TK[iCS]], [TK[iPT]], lambda e: e.activation(out=Pt, in_=cs, func=AF.Exp, scale=-C0))
                op('a', [TK[iCS]], [TK[iPINV]], lambda e: e.activation(out=Pinv, in_=cs, func=AF.Exp, scale=C0))
                tt('v', Ptm1, TK[iPTM], cs, TK[iCS], sig, TK[iSIG], ALU.subtract)
                op('a', [TK[iPTM]], [TK[iPTM]], lambda e: e.activation(out=Ptm1, in_=Ptm1, func=AF.Exp, scale=-C0))
                tt('g', kk, TK[iKK], kF, "L", bc(k_k, S4, 2), "c", ALU.mult)
                tt('g', tmp, TK[iTMP], kk, TK[iKK], kk, TK[iKK], ALU.mult)

                def mm_n(e):
                    ins = None
                    for j in range(4):
                        ins = e.matmul(PS[7][:, j * 128:(j + 1) * 128], lhsT=maskBD, rhs=tmp[:, j, :], start=True, stop=True)
                    return ins
                op('p', [TK[iTMP], "c"], [psk(7)], mm_n)
                op('a', [psk(7)], [TK[iTMP]], lambda e: e.activation(out=tmp, in_=P3(7), func=AF.Sqrt))
                op('v', [TK[iTMP]], [TK[iTMP]], lambda e: e.tensor_scalar(out=tmp, in0=tmp, scalar1=1e-12, scalar2=None, op0=ALU.max))
                op('v', [TK[iTMP]], [TK[iTMP]], lambda e: e.reciprocal(out=tmp, in_=tmp))
                tt('v', kk, TK[iKK], kk, TK[iKK], tmp, TK[iTMP], ALU.mult)
                tt('g', kmod, TK[iKMOD], av, TK[iAV], bc(k_a, S4, 2), "c", ALU.mult)
                tt('g', kmod, TK[iKMOD], kmod, TK[iKMOD], bc(k_a, S4, 2), "c", ALU.subtract)
                op('v', [TK[iKMOD], "L"], [TK[iKMOD]], lambda e: e.scalar_tensor_tensor(out=kmod, in0=kmod, scalar=1.0, in1=kF, op0=ALU.add, op1=ALU.mult))
                tt('v', rtl, TK[iRTL], rF, "L", Pt, TK[iPT], ALU.mult)
                op('v', [TK[iKK], TK[iPTM]], [TK[iATL]], lambda e: e.scalar_tensor_tensor(out=atl, in0=kk, scalar=-1.0, in1=Ptm1, op0=ALU.mult, op1=ALU.mult))
                tt('g', btl, TK[iBTL], kk, TK[iKK], av, TK[iAV], ALU.mult)
                tt('v', btl, TK[iBTL], btl, TK[iBTL], Pinv, TK[iPINV], ALU.mult)
                tt('v', ktl, TK[iKTL], kmod, TK[iKMOD], Pinv, TK[iPINV], ALU.mult)
                tt('g', rkr, TK[iRKR], rF, "L", kmod, TK[iKMOD], ALU.mult)
                tt('g', rkr, TK[iRKR], rkr, TK[iRKR], bc(r_k, S4, 2), "c", ALU.mult)
                op('v', [TK[iPT], "c"], ["PTbd"], lambda e: e.tensor_tensor(out=PTbd[:], in0=bc(maskBD, S4, 1), in1=Pt[:, :, 127:128].to_broadcast(S4), op=ALU.mult))

                def mm_b(e):
                    ins = None
                    for j in range(4):
                        ins = e.matmul(PS[7][:, 2 * j:2 * j + 2], lhsT=rkr[:, j, :], rhs=sel2[:], start=True, stop=True)
                    return ins
                op('p', [TK[iRKR], "c"], [psk(7)], mm_b)
                op('v', [psk(7)], ["bon"], lambda e: e.tensor_copy(out=bon[:], in_=PS[7][:, 0:8]))
                atl_m = [TT[0], TT[1]]
                rtl_m = [TT[2], TT[3]]
                for hh in range(2):
                    op('v' if hh == 0 else 'g', [TK[iATL], "c"], [TK[hh]], lambda e, hh=hh: e.tensor_scalar(out=atl_m[hh], in0=atl, scalar1=sel2[:, hh:hh + 1], scalar2=None, op0=ALU.mult))
                    op('v' if hh == 0 else 'g', [TK[iRTL], "c"], [TK[2 + hh]], lambda e, hh=hh: e.tensor_scalar(out=rtl_m[hh], in0=rtl, scalar1=sel2[:, hh:hh + 1], scalar2=None, op0=ALU.mult))
                mark("elem")
                for src, srckey, dst, dkey, pb in [(vF, "L", VT, "VT", 0), (btl, TK[iBTL], BT, "BT", 1), (ktl, TK[iKTL], KT, "KT", 6)]:
                    def trf(e, src=src, pb=pb):
                        ins = None
                        for j in range(4):
                            ins = e.transpose(out=PS[pb][:, j * 128:(j + 1) * 128], in_=src[:, j, :], identity=ident[:])
                        return ins
                    op('p', [srckey, "c"], [psk(pb)], trf)
                    if pb == 1:
                        op('a', [psk(pb)], [dkey], lambda e, dst=dst, pb=pb: e.activation(out=dst[:], in_=PS[pb][:], func=AF.Copy))
                    else:
                        op('v', [psk(pb)], [dkey], lambda e, dst=dst, pb=pb: e.tensor_copy(out=dst[:], in_=PS[pb][:]))
                mark("trans")
                for hg in range(2):
                    def hsl(t, hh, hg=hg):
                        h = hg * 4 + hh
                        return t[(h % 2) * 64:(h % 2) * 64 + 64, h // 2, :]
                    specs = [("X", "A", btl, maskSL, 2), ("XT", btl, "A", maskSU, 3),
                             ("akT", ktl, "A", maskSU, 4), ("rbT", btl, "R", maskUI, 5), ("rkT", ktl, "R", maskUI, 7)]

                    def pick(t, hl, hg=hg):
                        h = hg * 4 + hl
                        j, hh = h // 2, h % 2
                        if isinstance(t, str):
                            return (atl_m if t == "A" else rtl_m)[hh][:, j, :]
                        return t[:, j, :]
                    for nm, lt, rt, mk, pb in specs:
                        def mmT(e, lt=lt, rt=rt, pb=pb):
                            ins = None
                            for hl in range(4):
                                ins = e.matmul(PS[pb][:, hl * 128:(hl + 1) * 128], lhsT=pick(lt, hl), rhs=pick(rt, hl), start=True, stop=True)
                            return ins
                        op('p', [TK[0], TK[1], TK[2], TK[3], TK[iBTL], TK[iKTL]], [psk(pb)], mmT)
                        op('v', [psk(pb), "c"], ["m_" + nm], lambda e, nm=nm, mk=mk, pb=pb: e.tensor_tensor(
                            out=mats[nm][:], in0=P3(pb), in1=bc(mk, S4, 1), op=ALU.mult))
                        mark("mats_" + nm)
                    op('g', ["m_XT", "c"], ["m_QT"], lambda e: e.tensor_tensor(out=mats["QT"][:], in0=mats["XT"][:], in1=bc(ident[:], S4, 1), op=ALU.add))
                    mark("mats_qt")
                    cur, curT, nx, nxT = "X", "XT", "X2", "X2T"
                    for step in range(6):
                        def sq(e, a=curT, b_=cur):
                            ins = None
                            for hh in range(4):
                                ins = e.matmul(PS[2][:, hh * 128:(hh + 1) * 128], lhsT=mats[a][:, hh, :], rhs=mats[b_][:, hh, :], start=True, stop=True)
                            return ins
                        op('p', ["m_" + cur, "m_" + curT], [psk(2)], sq)
                        op('a', [psk(2)], ["m_" + nx], lambda e, nx=nx: e.activation(out=mats[nx][:], in_=P3(2), func=AF.Copy))
                        if step < 5:
                            def sqT(e, a=cur, b_=curT):
                                ins = None
                                for hh in range(4):
                                    ins = e.matmul(PS[3][:, hh * 128:(hh + 1) * 128], lhsT=mats[a][:, hh, :], rhs=mats[b_][:, hh, :], start=True, stop=True)
                                return ins
                            op('p', ["m_" + cur, "m_" + curT], [psk(3)], sqT)
                            op('v', [psk(3)], ["m_" + nxT], lambda e, nxT=nxT: e.tensor_copy(out=mats[nxT][:], in_=P3(3)))

                        def qu(e, a=nx):
                            ins = None
                            for hh in range(4):
                                ins = e.matmul(PS[4][:, hh * 128:(hh + 1) * 128], lhsT=mats[a][:, hh, :], rhs=mats["QT"][:, hh, :], start=True, stop=True)
                            return ins
                        op('p', ["m_" + nx, "m_QT"], [psk(4)], qu)
                        op('v', [psk(4), "m_QT"], ["m_QT"], lambda e: e.tensor_tensor(out=mats["QT"][:], in0=mats["QT"][:], in1=P3(4), op=ALU.add))
                        cur, curT, nx, nxT = nx, nxT, cur, curT
                        mark("mats_s%d" % step)
                    mark("mats")
                    c0 = hg * 256

                    def mm_rhs(e, hg=hg):
                        ins = None
                        for hl in range(4):
                            h = hg * 4 + hl
                            j = h // 2
                            hh = h % 2
                            reg = PS[0][:, hl * 64:(hl + 1) * 64]
                            e.matmul(reg, lhsT=atl[:, j, :], rhs=Stp[:, j, hh * 64:(hh + 1) * 64], start=True, stop=False)
                            ins = e.matmul(reg, lhsT=mats["akT"][:, hl, :], rhs=VT[:, h * 64:(h + 1) * 64], start=False, stop=True)
                        return ins
                    op('p', [TK[iATL], "Stp", "m_akT", "VT"], [psk(0)], mm_rhs)
                    op('v', [psk(0)], ["RHS"], lambda e, c0=c0: e.tensor_copy(out=RHS[:, c0:c0 + 256], in_=PS[0][:, 0:256]))

                    def mm_sa(e, hg=hg):
                        ins = None
                        for hl in range(4):
                            h = hg * 4 + hl
                            ins = e.matmul(PS[1][:, hl * 64:(hl + 1) * 64], lhsT=mats["QT"][:, hl, :], rhs=RHS[:, h * 64:(h + 1) * 64], start=True, stop=True)
                        return ins
                    op('p', ["m_QT", "RHS"], [psk(1)], mm_sa)
                    op('v', [psk(1)], ["SA"], lambda e, c0=c0: e.tensor_copy(out=SA[:, c0:c0 + 256], in_=PS[1][:, 0:256]))

                    def mm_y(e, hg=hg):
                        ins = None
                        for hl in range(4):
                            h = hg * 4 + hl
                            j = h // 2
                            hh = h % 2
                            reg = PS[6][:, hl * 64:(hl + 1) * 64]
                            e.matmul(reg, lhsT=rtl[:, j, :], rhs=Stp[:, j, hh * 64:(hh + 1) * 64], start=True, stop=False)
                            e.matmul(reg, lhsT=mats["rbT"][:, hl, :], rhs=SA[:, h * 64:(h + 1) * 64], start=False, stop=False)
                            ins = e.matmul(reg, lhsT=mats["rkT"][:, hl, :], rhs=VT[:, h * 64:(h + 1) * 64], start=False, stop=True)
                        return ins
                    op('p', [TK[iRTL], "Stp", "m_rbT", "m_rkT", "SA", "VT"], [psk(6)], mm_y)
                    op('a', [psk(6)], ["Y"], lambda e, c0=c0: e.activation(out=Y[:, c0:c0 + 256], in_=PS[6][:, 0:256], func=AF.Copy))

                    def mm_st(e, hg=hg):
                        ins = None
                        for jj in range(2):
                            j = hg * 2 + jj
                            reg = PS[5][:, jj * 128:(jj + 1) * 128]
                            e.matmul(reg, lhsT=ident[:], rhs=Stp[:, j, :], start=True, stop=False)
                            e.matmul(reg, lhsT=BT[:, j * 128:(j + 1) * 128], rhs=SA[:, j * 128:(j + 1) * 128], start=False, stop=False)
                            ins = e.matmul(reg, lhsT=KT[:, j * 128:(j + 1) * 128], rhs=VT[:, j * 128:(j + 1) * 128], start=False, stop=True)
                        return ins
                    op('p', ["Stp", "BT", "KT", "SA", "VT", "c"], [psk(5)], mm_st)
                    op('v', [psk(5), "PTbd"], ["Stp"], lambda e, hg=hg: e.tensor_tensor(
                        out=Stp[:, 2 * hg:2 * hg + 2, :], in0=PS[5][:, 0:256].rearrange("p (a b) -> p a b", a=2),
                        in1=PTbd[:, 2 * hg:2 * hg + 2, :], op=ALU.mult))
                mark("chain")
                Y3 = Y[:].rearrange("p (h v) -> p h v", h=8)
                Ysq = SCR[:, iY2 * 512:(iY2 + 1) * 512]
                G8 = [128, 8, 64]
                op('v', ["Y"], ["gst"], lambda e: e.tensor_reduce(out=gst[:, 0, :], in_=Y3, axis=AX.X, op=ALU.add))
                op('a', ["Y"], [TK[iY2]], lambda e: e.activation(out=Ysq, in_=Y[:], func=AF.Square))
                op('v', [TK[iY2]], ["gst"], lambda e: e.tensor_reduce(out=gst[:, 1, :], in_=Ysq.rearrange("p (h v) -> p h v", h=8), axis=AX.X, op=ALU.add))
                op('v', ["gst"], ["gst"], lambda e: e.tensor_scalar(out=gst[:, 2, :], in0=gst[:, 0, :], scalar1=1.0 / 64, scalar2=None, op0=ALU.mult))
                op('v', ["gst"], ["gst"], lambda e: e.tensor_tensor(out=gst[:, 3, :], in0=gst[:, 2, :], in1=gst[:, 2, :], op=ALU.mult))
                op('v', ["gst"], ["gst"], lambda e: e.scalar_tensor_tensor(out=gst[:, 4, :], in0=gst[:, 1, :], scalar=1.0 / 64, in1=gst[:, 3, :], op0=ALU.mult, op1=ALU.subtract))
                op('a', ["gst"], ["gst"], lambda e: e.activation(out=gst[:, 5, :], in_=gst[:, 4, :], func=AF.Sqrt, bias=GN_EPS, scale=1.0))
                op('v', ["gst"], ["gst"], lambda e: e.reciprocal(out=gst[:, 5, :], in_=gst[:, 5, :]))
                op('v', ["Y", "gst"], ["Y"], lambda e: e.tensor_tensor(out=Y3, in0=Y3, in1=bc(gst[:, 2, :], G8, 2), op=ALU.subtract))
                op('v', ["Y", "gst"], ["Y"], lambda e: e.tensor_tensor(out=Y3, in0=Y3, in1=bc(gst[:, 5, :], G8, 2), op=ALU.mult))
                op('g', ["Y", "c"], ["Y"], lambda e: e.tensor_tensor(out=Y[:], in0=Y[:], in1=lnw[:], op=ALU.mult))
                op('g', ["Y", "c"], ["Y"], lambda e: e.tensor_tensor(out=Y[:], in0=Y[:], in1=lnb[:], op=ALU.add))
                op('v', ["VT", "bon"], [TK[iY2]], lambda e: e.tensor_tensor(out=Ysq.rearrange("p (h v) -> p h v", h=8), in0=VT[:].rearrange("p (h v) -> p h v", h=8), in1=bc(bon[:], G8, 2), op=ALU.mult))
                op('v', ["Y", TK[iY2]], ["Y"], lambda e: e.tensor_tensor(out=Y[:], in0=Y[:], in1=Ysq, op=ALU.add))
                op('v', ["Y", "gT"], ["Y"], lambda e: e.tensor_tensor(out=Y[:], in0=Y[:], in1=gT[:], op=ALU.mult))

                def tr_ro(e):
                    ins = None
                    for j in range(4):
                        ins = e.transpose(out=PS[0][:, j * 128:(j + 1) * 128], in_=Y[:, j * 128:(j + 1) * 128], identity=ident[:])
                    return ins
                op('p', ["Y", "c"], [psk(0)], tr_ro)
                op('a', [psk(0)], ["roF"], lambda e: e.activation(out=roF[:], in_=P3(0), func=AF.Copy))
                for half in range(2):
                    pb = 1 + half

                    def mm_o(e, half=half, pb=pb):
                        ins = None
                        for q in range(4):
                            dt_ = half * 4 + q
                            for kt in range(4):
                                ins = e.matmul(PS[pb][:, q * 128:(q + 1) * 128], lhsT=w_o[:, kt, dt_ * 128:(dt_ + 1) * 128], rhs=roF[:, kt, :], start=(kt == 0), stop=(kt == 3))
                        return ins
                    op('p', ["w_o", "roF"], [psk(pb)], mm_o)
                    op('v', [psk(pb), "gates"], ["mixed"], lambda e, half=half, pb=pb: e.tensor_tensor(out=mixed[:, half * 4:(half + 1) * 4, :], in0=P3(pb), in1=gates[:, half * 4:(half + 1) * 4, :], op=ALU.mult))
                mark("epi")
                def s5_group(kt, ts):
                    s5a, s5b, s5c, s5d_, btre, btim, wre, wim = [TT[8 * ts + i] for i in range(8)]
                    ka, kb, kc, kd, kbr, kbi, kwr, kwi = [TK[8 * ts + i] for i in range(8)]
                    pa, pb_ = (3, 4) if ts == 0 else (1, 2)
                    xr, xi = xre2[ts], xim2[ts]
                    xrk, xik = "xre%d" % ts, "xim%d" % ts
                    cn = cwn2[ts]
                    cnk = "cwn%d" % ts
                    cwk = "cw%d" % kt
                    Er = Ere[:, 4 * kt:4 * kt + 4, :]
                    Ei = Eim[:, 4 * kt:4 * kt + 4, :]

                    def mm_bu(e):
                        ins = None
                        for q in range(4):
                            j = 4 * kt + q
                            e.matmul(PS[pa][:, q * 128:(q + 1) * 128], lhsT=BBre[:, j, :], rhs=uB[:, kt, :], start=True, stop=True)
                            ins = e.matmul(PS[pb_][:, q * 128:(q + 1) * 128], lhsT=BBim[:, j, :], rhs=uB[:, kt, :], start=True, stop=True)
                        return ins
                    op('p', ["BB", "uB"], [psk(pa), psk(pb_)], mm_bu)
                    yield
                    tt('v', s5a, ka, Er, "E", P3(pa), psk(pa), ALU.mult)
                    yield
                    tt('v', s5b, kb, Ei, "E", P3(pb_), psk(pb_), ALU.mult)
                    yield
                    tt('g', btre, kbr, s5a, ka, s5b, kb, ALU.add)
                    yield
                    tt('v', s5c, kc, Er, "E", P3(pb_), psk(pb_), ALU.mult)
                    yield
                    tt('v', s5d_, kd, Ei, "E", P3(pa), psk(pa), ALU.mult)
                    yield
                    tt('g', btim, kbi, s5c, kc, s5d_, kd, ALU.subtract)
                    yield
                    for q in range(4):
                        j = 4 * kt + q
                        op('v', [kbr, cwk, "sp"], [kwr], lambda e, q=q, j=j: e.tensor_tensor_scan(out=wre[:, q, :], data0=sp[:, LAB, j:j + 1].to_broadcast([128, 128]), data1=btre[:, q, :], initial=cw[:, 0, j:j + 1], op0=ALU.mult, op1=ALU.add))
                        yield
                        op('v', [kbi, cwk, "sp"], [kwi], lambda e, q=q, j=j: e.tensor_tensor_scan(out=wim[:, q, :], data0=sp[:, LAB, j:j + 1].to_broadcast([128, 128]), data1=btim[:, q, :], initial=cw[:, 1, j:j + 1], op0=ALU.mult, op1=ALU.add))
                        yield
                    c128 = sp[:, CS2, 4 * kt:4 * kt + 4]
                    s128 = sp[:, SN2, 4 * kt:4 * kt + 4]
                    wr127 = wre[:, :, 127]
                    wi127 = wim[:, :, 127]
                    tt('g', cn[:, 0, :], cnk, c128, "sp", wr127, kwr, ALU.mult)
                    yield
                    tt('g', cn[:, 1, :], cnk, s128, "sp", wi127, kwi, ALU.mult)
                    yield
                    tt('g', cn[:, 2, :], cnk, s128, "sp", wr127, kwr, ALU.mult)
                    yield
                    tt('g', cn[:, 3, :], cnk, c128, "sp", wi127, kwi, ALU.mult)
                    yield
                    tt('g', cw[:, 0, 4 * kt:4 * kt + 4], cwk, cn[:, 0, :], cnk, cn[:, 1, :], cnk, ALU.subtract)
                    yield
                    tt('g', cw[:, 1, 4 * kt:4 * kt + 4], cwk, cn[:, 2, :], cnk, cn[:, 3, :], cnk, ALU.add)
                    yield
                    tt('v', s5a, ka, Er, "E", wre, kwr, ALU.mult)
                    yield
                    tt('v', s5b, kb, Ei, "E", wim, kwi, ALU.mult)
                    yield
                    tt('g', xr[:], xrk, s5a, ka, s5b, kb, ALU.subtract)
                    yield
                    tt('v', s5c, kc, Ei, "E", wre, kwr, ALU.mult)
                    yield
                    tt('v', s5d_, kd, Er, "E", wim, kwi, ALU.mult)
                    yield
                    tt('g', xi[:], xik, s5c, kc, s5d_, kd, ALU.add)
                    yield

                    def mm_c(e):
                        ins = None
                        for q in range(4):
                            j = 4 * kt + q
                            e.matmul(PS[5][:, kt * 128:(kt + 1) * 128], lhsT=CCre[:, j, :], rhs=xr[:, q, :], start=(q == 0), stop=False)
                            ins = e.matmul(PS[5][:, kt * 128:(kt + 1) * 128], lhsT=CCimN[:, j, :], rhs=xi[:, q, :], start=False, stop=(q == 3))
                        return ins
                    op('p', ["CC", xrk, xik], [psk(5)], mm_c)
                    yield

                for pair in range(2):
                    gens = [s5_group(2 * pair, 0), s5_group(2 * pair + 1, 1)]
                    live = [True, True]
                    while any(live):
                        for gi in range(2):
                            if live[gi]:
                                try:
                                    next(gens[gi])
                                except StopIteration:
                                    live[gi] = False
                s5a, s5b = TT[0], TT[1]
                ka, kb = TK[0], TK[1]
                tt('g', s5a, ka, uF[:], "uF", bc(s5d, S4, 2), "c", ALU.mult)
                tt('v', s5a, ka, s5a, ka, P3(5), psk(5), ALU.add)
                gelu_tanh(op, s5a, ka, s5b, kb, ygB[:], "ygB")
                for half in range(2):
                    for vg in range(2):
                        pb = 6 + vg

                        def mm_g(e, half=half, vg=vg, pb=pb):
                            ins = None
                            for q in range(4):
                                col = vg * 8 + half * 4 + q
                                for kt in range(4):
                                    ins = e.matmul(PS[pb][:, q * 128:(q + 1) * 128], lhsT=w_glu[:, kt, col * 128:(col + 1) * 128], rhs=ygB[:, kt, :], start=(kt == 0), stop=(kt == 3))
                            return ins
                        op('p', ["w_glu", "ygB"], [psk(pb)], mm_g)
                    zh = zg[:, half * 4:(half + 1) * 4, :]
                    op('a', [psk(7)], ["zg"], lambda e, zh=zh: e.activation(out=zh, in_=P3(7), func=AF.Sigmoid))
                    tt('v', zh, "zg", zh, "zg", P3(6), psk(6), ALU.mult)
                    tt('g', zh, "zg", zh, "zg", gates[:, 8 + half * 4:8 + (half + 1) * 4, :], "gates", ALU.mult)
                    tt('v', mixedB[:, half * 4:(half + 1) * 4, :], "mixedB", zh, "zg", mixed[:, half * 4:(half + 1) * 4, :], "mixed", ALU.add)
                mark("s5")
                for half in range(2):
                    pb = 1 + half

                    def mm_h(e, half=half, pb=pb):
                        ins = None
                        for kt in range(8):
                            ins = e.matmul(PS[pb][:], lhsT=mixedB[:, kt, :], rhs=w_out[:, kt, half * 512:(half + 1) * 512], start=(kt == 0), stop=(kt == 7))
                        return ins
                    op('p', ["w_out", "mixedB"], [psk(pb)], mm_h)
                    op('v', [psk(pb), xk, "stat"], ["h1"], lambda e, half=half, pb=pb: e.scalar_tensor_tensor(
                        out=h1[:, half * 512:(half + 1) * 512], in0=xcur[:, half * 512:(half + 1) * 512], scalar=stat[:, 1:2], in1=PS[pb][:], op0=ALU.mult, op1=ALU.add))
                dma('s', 'dh1', h1_d[it * 128:(it + 1) * 128, :], h1[:], ["h1"], ["h1d%d" % it])
            sch.barrier()
         except _Stop:
            sch.barrier()

        if 2 in phases:
          with ExitStack() as es:
            def sb(name, shape, dt=F32):
                return es.enter_context(nc.sbuf_tensor("s_" + name, shape, dt))
            NCV = 6
            RB = 4
            cin = [sb("cin%d" % i, [128, RB, D]) for i in range(NCV)]
            cout = [sb("cout%d" % i, [128, RB, D], BF16) for i in range(NCV)]
            n = 0
            for src_d, dst_d in [(pu_d, tuv_d[:, 0, :]), (pvv_d, tuv_d[:, 1, :])]:
                sv_ = src_d.rearrange("(c r p) d -> c p r d", p=128, r=RB)
                dv_ = dst_d.rearrange("(c r p) d -> c p r d", p=128, r=RB)
                for c in range(16384 // (128 * RB)):
                    b = n % NCV
                    n += 1
                    dma('s', 'dci%d' % b, cin[b][:], sv_[c], [], ["cin%d" % b])
                    if n % 3 == 0:
                        op('v', ["cin%d" % b], ["cout%d" % b], lambda e, b=b: e.tensor_copy(out=cout[b][:], in_=cin[b][:]))
                    elif n % 3 == 1:
                        op('a', ["cin%d" % b], ["cout%d" % b], lambda e, b=b: e.activation(out=cout[b][:], in_=cin[b][:], func=AF.Copy))
                    else:
                        op('g', ["cin%d" % b], ["cout%d" % b], lambda e, b=b: e.tensor_copy(out=cout[b][:], in_=cin[b][:]))
                    dma('a', 'dco%d' % b, dv_[c], cout[b][:], ["cout%d" % b], ["tb"])
            sch.barrier()

        if 2 in phases:
          with ExitStack() as es:
            def sb(name, shape, dt=F32):
                return es.enter_context(nc.sbuf_tensor("s_" + name, shape, dt))
            ident = sb("ident2", [128, 128])
            gffn = sb("gffn", [128, D])
            gfin = sb("gfin", [128, D])
            wq = sb("wq", [128, 8, D])
            subk = sb("subk", [128, 8, 128])
            h1t = [sb("h1t%d" % i, [128, D]) for i in range(2)]
            xn2 = [sb("xn2_0", [128, D])] * 2
            xn2b = [sb("xn2b_%d" % i, [128, D], BF16) for i in range(2)]
            junkb = sb("junkb", [128, D], BF16)
            xn2F = sb("xn2F", [128, 8, 128])
            qF = sb("qF", [128, 8, 128])
            sc = sb("sc", [128, 16, 128])
            scr = sb("scr", [128, 16, 128])
            iota16 = sb("iota16", [128, 16])
            eq = sb("eq", [128, 8, 16, 16])
            junk = eq[:].rearrange("p a b c -> p (a b c)")
            sv = sb("sv", [128, 16, 16])
            siu = sb("siu", [128, 16, 16], U32)
            sif = sb("sif", [128, 16, 16])
            dsi = sb("dsi", [128, 8, 16])
            cand = sb("cand", [128, 8, 256])
            cv = sb("cv", [128, 8, 16])
            ciu = sb("ciu", [128, 8, 16], U32)
            iiu = sb("iiu", [128, 8, 16], U32)
            jju = sb("jju", [128, 8, 16], U32)
            iif = sb("iif", [128, 8, 16])
            jjf = sb("jjf", [128, 8, 16])
            i1 = sb("i1", [128, 8, 16])
            i2 = sb("i2", [128, 8, 16])
            lt = sb("lt", [128, 8, 16])
            gtmp = sb("gtmp", [128, 128])
            ei = [sb("ei%d" % i, [128, 128], I32) for i in range(2)]
            gate = [sb("gate%d" % i, [128, 8, 16]) for i in range(2)]
            sm = sb("sm", [128, 4, 8])
            hid = sb("hid", [128, 128])
            wgt = [sb("wgt%d" % i, [128, 128]) for i in range(2)]
            statA = sb("statA", [128, 8])
            statV = sb("statV", [128, 8])
            NDG = 4
            dg = [sb("dg%d" % i, [128, 128], BF16) for i in range(NDG)]
            NR = 20
            UV = [sb("UV%d" % i, [128, 2, D], BF16) for i in range(NR)]
            hid2 = [sb("hid%d" % i, [128, 128]) for i in range(2)]
            gt2 = [sb("gtg%d" % i, [128, 8]) for i in range(2)]
            h2 = sb("h2", [128, D])
            outt = sb("outt", [128, D])

            dma('s', 'dc2', ident[:], ident_d, [], ["c"])
            dma('s', 'dc2', gffn[:], gffn_d, [], ["c"])
            dma('s', 'dc2', iota16[:], iota_d, [], ["c"])
            dma('s', 'dc2', gfin[:], gfin_d, [], ["c"])
            dma('s', 'dc2', subk[:].rearrange("p a b -> p (a b)"), subk_d, [], ["c"])
            dma('s', 'dc2', wq[:], wq_d.rearrange("(kt p) c -> p kt c", p=128), [], ["c"])

            def rms(src, srck, dstat, dk, op=op):
                op('a', [srck], ["eq", dk], lambda e: e.activation(out=junk[:, 0:512], in_=src[:, 0:512], func=AF.Square, accum_out=dstat[:, 0:1]))
                op('a', [srck], ["eq", dk], lambda e: e.activation(out=junk[:, 512:1024], in_=src[:, 512:1024], func=AF.Square, accum_out=dstat[:, 3:4]))
                op('v', [dk], [dk], lambda e: e.tensor_tensor(out=dstat[:, 0:1], in0=dstat[:, 0:1], in1=dstat[:, 3:4], op=ALU.add))
                op('a', [dk], [dk], lambda e: e.activation(out=dstat[:, 1:2], in_=dstat[:, 0:1], func=AF.Sqrt, bias=1e-6, scale=1.0 / D))
                op('v', [dk], [dk], lambda e: e.reciprocal(out=dstat[:, 2:3], in_=dstat[:, 1:2]))

            def top16_multi(segs, segk, width, vouts, iouts, okeys):
                n = len(segs)
                scrv = [scr[:].rearrange("p a b -> p (a b)")[:, i * width:(i + 1) * width] for i in range(n)]
                sk = ["scr%d" % i for i in range(n)]
                for i in range(n):
                    qop('v', [segk], [okeys[i]], lambda e, i=i: e.max(out=vouts[i][:, 0:8], in_=segs[i]))
                for i in range(n):
                    qop('v', [segk, okeys[i]], [okeys[i]], lambda e, i=i: e.max_index(out=iouts[i][:, 0:8], in_max=vouts[i][:, 0:8], in_values=segs[i]))
                for i in range(n):
                    qop('v', [segk, okeys[i]], [sk[i]], lambda e, i=i: e.match_replace(out=scrv[i], in_to_replace=vouts[i][:, 0:8], in_values=segs[i], imm_value=NEG))
                for i in range(n):
                    qop('v', [sk[i]], [okeys[i]], lambda e, i=i: e.max(out=vouts[i][:, 8:16], in_=scrv[i]))
                for i in range(n):
                    qop('v', [sk[i], okeys[i]], [okeys[i]], lambda e, i=i: e.max_index(out=iouts[i][:, 8:16], in_max=vouts[i][:, 8:16], in_values=scrv[i]))

            gcount = [0, 0, 0]

            Aq = []

            def qop(*a):
                Aq.append(('op', a))

            def qdma(*a, **k):
                Aq.append(('dma', a, k))

            def drain(n=None):
                k = 0
                while Aq and (n is None or k < n):
                    item = Aq.pop(0)
                    if item[0] == 'op':
                        op(*item[1])
                    else:
                        dma(*item[1], **item[2])
                    k += 1

            def stage_A(it):
                p = it % 2
                hk = "h1t%d" % p
                hcur = h1t[p]
                xk = "xn2_0"
                xc = xn2[0]
                qdma('s', 'dh%d' % p, hcur[:], h1_d[it * 128:(it + 1) * 128, :], ["h1d%d" % it], [hk])
                rms(hcur, hk, statA, "statA", op=qop)
                qop('v', [hk, "statA", "c"], [xk], lambda e: e.scalar_tensor_tensor(out=xc[:], in0=hcur[:], scalar=statA[:, 2:3], in1=gffn[:], op0=ALU.mult, op1=ALU.mult))
                qop('a', [xk], ["xn2b_%d" % p], lambda e: e.activation(out=xn2b[p][:], in_=xc[:], func=AF.Copy))
                for half in range(2):
                    def tr2(e, half=half):
                        ins = None
                        for q in range(4):
                            kt = half * 4 + q
                            ins = e.transpose(out=PS[half][:, q * 128:(q + 1) * 128], in_=xc[:, kt * 128:(kt + 1) * 128], identity=ident[:])
                        return ins
                    qop('p', [xk, "c"], [psk(half)], tr2)
                    if half == 0:
                        qop('v', [psk(0)], ["xn2F"], lambda e: e.tensor_copy(out=xn2F[:, 0:4, :], in_=P3(0)))
                    else:
                        qop('a', [psk(1)], ["xn2F"], lambda e: e.activation(out=xn2F[:, 4:8, :], in_=P3(1), func=AF.Copy))
                for half in range(2):
                    pb = 2 + half

                    def mm_q(e, half=half, pb=pb):
                        ins = None
                        for q in range(4):
                            ct = half * 4 + q
                            for kt in range(8):
                                ins = e.matmul(PS[pb][:, q * 128:(q + 1) * 128], lhsT=wq[:, kt, ct * 128:(ct + 1) * 128], rhs=xn2F[:, kt, :], start=(kt == 0), stop=(kt == 7))
                        return ins
                    qop('p', ["c", "xn2F"], [psk(pb)], mm_q)
                    if half == 0:
                        qop('v', [psk(pb)], ["qF"], lambda e, pb=pb: e.tensor_copy(out=qF[:, 0:4, :], in_=P3(pb)))
                    else:
                        qop('a', [psk(pb)], ["qF"], lambda e, pb=pb: e.activation(out=qF[:, 4:8, :], in_=P3(pb), func=AF.Copy))
                sc4 = sc[:].rearrange("p (h c) n -> p h c n", c=2)
                for hg in range(2):
                    for c in range(2):
                        pb = 4 + c

                        def mm_s(e, hg=hg, c=c, pb=pb):
                            ins = None
                            for q in range(4):
                                h = hg * 4 + q
                                ins = e.matmul(PS[pb][:, q * 128:(q + 1) * 128], lhsT=qF[c * 64:(c + 1) * 64, h, :], rhs=subk[c * 64:(c + 1) * 64, h, :], start=True, stop=True)
                            return ins
                        qop('p', ["qF", "c"], [psk(pb)], mm_s)
                        if c == 0:
                            qop('v', [psk(pb)], ["sc"], lambda e, hg=hg, c=c, pb=pb: e.tensor_copy(out=sc4[:, hg * 4:(hg + 1) * 4, c, :], in_=P3(pb)))
                        else:
                            qop('a', [psk(pb)], ["sc"], lambda e, hg=hg, c=c, pb=pb: e.activation(out=sc4[:, hg * 4:(hg + 1) * 4, c, :], in_=P3(pb), func=AF.Copy))

            svk = ["svi%d" % i for i in range(16)]
            cvk = ["cvi%d" % i for i in range(8)]

            def stage_A2(it):
                p = it % 2
                top16_multi([sc[:, i, :] for i in range(16)], "sc", 128, [sv[:, i, :] for i in range(16)], [siu[:, i, :] for i in range(16)], svk)
                for h in range(8):
                    qop('v', [svk[2 * h], svk[2 * h + 1]], ["cand"], lambda e, h=h: e.tensor_tensor(
                        out=cand[:, h, :].rearrange("p (i j) -> p i j", i=16),
                        in0=bc(sv[:, 2 * h, :], [128, 16, 16], 2), in1=bc(sv[:, 2 * h + 1, :], [128, 16, 16], 1), op=ALU.add))
                top16_multi([cand[:, h, :] for h in range(8)], "cand", 256, [cv[:, h, :] for h in range(8)], [ciu[:, h, :] for h in range(8)], cvk)
                qop('v', cvk, ["iiu"], lambda e: e.tensor_single_scalar(out=iiu[:], in_=ciu[:], scalar=4, op=ALU.logical_shift_right))
                qop('v', cvk, ["jju"], lambda e: e.tensor_single_scalar(out=jju[:], in_=ciu[:], scalar=15, op=ALU.bitwise_and))
                qop('v', ["iiu"], ["iif"], lambda e: e.tensor_copy(out=iif[:], in_=iiu[:]))
                qop('v', ["jju"], ["jjf"], lambda e: e.tensor_copy(out=jjf[:], in_=jju[:]))
                qop('v', svk, ["sif"], lambda e: e.tensor_copy(out=sif[:], in_=siu[:]))
                sif4 = sif[:].rearrange("p (h c) k -> p h c k", c=2)
                E4 = [128, 8, 16, 16]
                iota4 = iota16[:].unsqueeze(1).unsqueeze(1).to_broadcast(E4)
                for (idxf, idk, c_, dst, dk) in [(iif, "iif", 0, i1, "i1"), (jjf, "jjf", 1, i2, "i2")]:
                    qop('v', [idk, "c", "eq"], ["eq"], lambda e, idxf=idxf: e.tensor_tensor(out=eq[:], in0=idxf[:].unsqueeze(3).to_broadcast(E4), in1=iota4, op=ALU.is_equal))
                    qop('v', ["eq", "sif"], ["eq"], lambda e, c_=c_: e.tensor_tensor(out=eq[:], in0=eq[:], in1=sif4[:, :, c_, :].unsqueeze(2).to_broadcast(E4), op=ALU.mult))
                    qop('v', ["eq"], [dk], lambda e, dst=dst: e.tensor_reduce(out=dst[:], in_=eq[:], axis=AX.X, op=ALU.add))
                qop('v', ["i1", "i2"], ["i1"], lambda e: e.scalar_tensor_tensor(out=i1[:], in0=i1[:], scalar=128.0, in1=i2[:], op0=ALU.mult, op1=ALU.add))
                qop('v', ["i1"], ["ei%d" % p], lambda e: e.tensor_copy(out=ei[p][:], in_=i1[:].rearrange("p h k -> p (h k)")))

            def stage_A3(it):
                p = it % 2
                gk = "gate%d" % p
                g_ = gate[p]
                qop('v', cvk, ["sm"], lambda e: e.tensor_reduce(out=sm[:, 0, :], in_=cv[:], axis=AX.X, op=ALU.max))
                qop('v', cvk + ["sm"], [gk], lambda e: e.tensor_tensor(out=g_[:], in0=cv[:], in1=bc(sm[:, 0, :], [128, 8, 16], 2), op=ALU.subtract))
                qop('a', [gk], [gk], lambda e: e.activation(out=g_[:], in_=g_[:], func=AF.Exp))
                qop('v', [gk], ["sm"], lambda e: e.tensor_reduce(out=sm[:, 1, :], in_=g_[:], axis=AX.X, op=ALU.add))
                qop('v', ["sm"], ["sm"], lambda e: e.reciprocal(out=sm[:, 2, :], in_=sm[:, 1, :]))
                qop('v', [gk, "sm"], [gk], lambda e: e.tensor_tensor(out=g_[:], in0=g_[:], in1=bc(sm[:, 2, :], [128, 8, 16], 2), op=ALU.mult))

            GS = 4
            NGR = 128 // GS
            slot_buf = {}

            def dots(it, g):
                p = it % 2
                hd = hid2[p]
                hk_ = "hid%d" % p
                if g == 0:
                    op('v', [], [hk_], lambda e: e.memset(hd[:], 0.0))
                for s_ in range(g * GS, (g + 1) * GS):
                    b = gcount[0] % NR
                    gcount[0] += 1
                    slot_buf[(it, s_)] = b
                    dma('g', 'duv%d' % b, UV[b][:].rearrange("p a d -> p (a d)"), tuv_d.rearrange("e a d -> e (a d)"), ["ei%d" % p], ["UV%d" % b],
                        in_offset=bass.IndirectOffsetOnAxis(ap=ei[p][:, s_:s_ + 1], axis=0))
                    op('v', ["UV%d" % b, "xn2b_%d" % p], ["junkb", hk_], lambda e, b=b, s_=s_: e.scalar_tensor_tensor(
                        out=junkb[:], in0=UV[b][:, 0, :], scalar=1.0, in1=xn2b[p][:], op0=ALU.mult, op1=ALU.mult, accum_out=hd[:, s_:s_ + 1]))

            def weights(it, g):
                p = it % 2
                hd = hid2[p]
                hk_ = "hid%d" % p
                wk = "wgt%d" % p
                q = g % 2
                sl = slice(g * GS, (g + 1) * GS)
                gelu_tanh(op, hd[:, sl], hk_, gt2[q][:, 0:GS], "gtg%d" % q, wgt[p][:, sl], wk, sq_eng='v')
                op('v', [wk, "gate%d" % p], [wk], lambda e: e.tensor_tensor(out=wgt[p][:, sl], in0=wgt[p][:, sl], in1=gate[p][:].rearrange("p h k -> p (h k)")[:, sl], op=ALU.mult))

            def accum(it, g):
                p = it % 2
                wk = "wgt%d" % p
                for s_ in range(g * GS, (g + 1) * GS):
                    b = slot_buf.pop((it, s_))
                    r = gcount[2] % NDG
                    gcount[2] += 1
                    op('a', [wk, "c"], ["dg%d" % r], lambda e, r=r, s_=s_: e.activation(out=dg[r][:], in_=ident[:], func=AF.Copy, scale=wgt[p][:, s_:s_ + 1]))

                    def mm_v(e, b=b, r=r, s_=s_):
                        e.matmul(PS[6][:], lhsT=dg[r][:], rhs=UV[b][:, 1, 0:512], start=(s_ == 0), stop=(s_ == 127))
                        return e.matmul(PS[7][:], lhsT=dg[r][:], rhs=UV[b][:, 1, 512:1024], start=(s_ == 0), stop=(s_ == 127))
                    op('p', ["dg%d" % r, "UV%d" % b], [psk(6), psk(7)], mm_v)

            def stage_Vf(it):
                p = it % 2
                hk = "h1t%d" % p
                hcur = h1t[p]
                op('v', [psk(6), hk], ["h2"], lambda e: e.tensor_tensor(out=h2[:, 0:512], in0=PS[6][:], in1=hcur[:, 0:512], op=ALU.add))
                op('v', [psk(7), hk], ["h2"], lambda e: e.tensor_tensor(out=h2[:, 512:1024], in0=PS[7][:], in1=hcur[:, 512:1024], op=ALU.add))
                rms(h2, "h2", statV, "statV")
                op('v', ["h2", "statV", "c"], ["outt"], lambda e: e.scalar_tensor_tensor(out=outt[:], in0=h2[:], scalar=statV[:, 2:3], in1=gfin[:], op0=ALU.mult, op1=ALU.mult))
                dma('s', 'dout', out_d[it * 128:(it + 1) * 128, :], outt[:], ["outt"], ["od%d" % it])

            stage_A(0)
            stage_A2(0)
            stage_A3(0)
            drain()
            prev = None
            for it in range(NT):
                per = 0
                if it + 1 < NT:
                    stage_A(it + 1)
                    stage_A2(it + 1)
                    stage_A3(it + 1)
                    per = (len(Aq) + NGR - 5) // (NGR - 4)
                for g in range(NGR):
                    dots(it, g)
                    if prev is not None:
                        weights(*prev)
                        accum(*prev)
                        if prev[1] == NGR - 1:
                            stage_Vf(prev[0])
                    prev = (it, g)
                    if per:
                        drain(per)
                drain()
            weights(*prev)
            accum(*prev)
            stage_Vf(prev[0])
            sch.barrier()
    return nc


def make_inputs(inp, b):
    f = lambda a: np.ascontiguousarray(np.asarray(a), dtype=np.float32)
    colT = lambda v, n: f(np.asarray(v).reshape(n, 128).T)
    m = {}
    m["x"] = f(inp["x"][b])
    m["w_in"] = f(inp["w_in"][0])
    m["gainP"] = colT(inp["norm_mix"][0], 8)
    m["bgate"] = colT(inp["b_gate"][0], 16)
    m["mu"] = colT(inp["mu_rwkv"][0], 14)
    pv = np.stack([colT(inp["w0"][0], 4), colT(inp["a0"][0], 4), colT(inp["k_k"][0], 4), colT(inp["k_a"][0], 4),
                   colT(np.asarray(inp["r_k"][0]).reshape(512), 4), colT(np.asarray(inp["s5_d"][0]).reshape(512), 4)], axis=1)
    m["pvec"] = f(pv)
    m["lora"] = f(np.concatenate([np.asarray(inp["w_lora_up"][0]), np.asarray(inp["a_lora_up"][0])], axis=0))
    m["glora"] = f(inp["g_lora_up"][0])
    m["lnw"] = f(np.broadcast_to(np.asarray(inp["ln_x_w"][0])[None, :], (128, 512)))
    m["lnb"] = f(np.broadcast_to(np.asarray(inp["ln_x_b"][0])[None, :], (128, 512)))
    m["w_o"] = f(inp["w_o_rwkv"][0])
    m["w_glu"] = f(inp["w_glu_s5"][0])
    m["w_out"] = f(inp["w_out"][0])
    a_re = np.asarray(inp["s5_a_re"][0]).reshape(16, 128).T
    a_im = np.asarray(inp["s5_a_im"][0]).reshape(16, 128).T
    ldt = np.repeat(np.asarray(inp["s5_log_dt"][0]), 64).reshape(16, 128).T
    m["s5p"] = f(np.stack([a_re, a_im, ldt], axis=1))
    bbre = np.zeros((128, 16, 128), np.float32)
    bbim = np.zeros((128, 16, 128), np.float32)
    ccre = np.zeros((128, 16, 128), np.float32)
    ccim = np.zeros((128, 16, 128), np.float32)
    b_re = np.asarray(inp["s5_b_re"][0]); b_im = np.asarray(inp["s5_b_im"][0])
    c_re = np.asarray(inp["s5_c_re"][0]); c_im = np.asarray(inp["s5_c_im"][0])
    for g in range(32):
        j, gl, g8 = g // 2, g % 2, g % 8
        bbre[g8 * 16:(g8 + 1) * 16, j, gl * 64:(gl + 1) * 64] = b_re[g].T
        bbim[g8 * 16:(g8 + 1) * 16, j, gl * 64:(gl + 1) * 64] = b_im[g].T
        ccre[gl * 64:(gl + 1) * 64, j, g8 * 16:(g8 + 1) * 16] = c_re[g].T
        ccim[gl * 64:(gl + 1) * 64, j, g8 * 16:(g8 + 1) * 16] = c_im[g].T
    m["bbre"] = bbre.reshape(128, 2048)
    m["bbim"] = bbim.reshape(128, 2048)
    m["ccre"] = ccre.reshape(128, 2048)
    m["ccim"] = ccim.reshape(128, 2048)
    m["gffn"] = f(np.broadcast_to(np.asarray(inp["norm_ffn"][0])[None, :], (128, D)))
    m["gfin"] = f(np.broadcast_to(np.asarray(inp["norm_final"])[None, :], (128, D)))
    m["wq"] = f(inp["peer_wq"][0])
    m["subk"] = f(np.asarray(inp["peer_subkeys"][0]).transpose(1, 3, 0, 2).reshape(128, 1024))
    m["peer_u"] = f(inp["peer_u"][0])
    m["peer_v"] = f(inp["peer_v"][0])
    m["ident"] = np.eye(128, dtype=np.float32)
    p = np.arange(128)[:, None]
    jx = np.arange(128)[None, :]
    masks = np.zeros((128, 5, 128), np.float32)
    masks[:, 0] = (jx < p)
    masks[:, 1] = (jx > p)
    masks[:, 2] = (jx >= p)
    masks[:, 3] = ((jx // 64) == (p // 64))
    masks[:, 4] = 1.0
    masks[:, 4, 0] = 0.0
    m["masks"] = masks
    sel2 = np.zeros((128, 2), np.float32)
    sel2[:64, 0] = 1.0
    sel2[64:, 1] = 1.0
    m["sel2"] = sel2
    m["iota16"] = np.ascontiguousarray(np.broadcast_to(np.arange(16, dtype=np.float32)[None, :], (128, 16)))
    return m


_NC_CACHE = {}


def kernel(**inputs):
    n = 8
    if "nc" not in _NC_CACHE:
        _NC_CACHE["nc"] = build_nc()
    nc = _NC_CACHE["nc"]
    in_maps = [make_inputs(inputs, b) for b in range(n)]
    res = run_bass_kernel_spmd(nc, in_maps, core_ids=list(range(n)))
    out = np.stack([np.asarray(r["out"], dtype=np.float32) for r in res.results], axis=0)
    return out
```

```python
import numpy as np
from contextlib import ExitStack
import concourse.bass as bass
import concourse.mybir as mybir
from concourse.bass_utils import run_bass_kernel_spmd

F32 = mybir.dt.float32
BF16 = mybir.dt.bfloat16
I32 = mybir.dt.int32
U32 = mybir.dt.uint32
ALU = mybir.AluOpType
AF = mybir.ActivationFunctionType
AX = mybir.AxisListType

D = 1024
NRW = 1792
NCOL = 4352
SEQ = 4096
NCT = 34
C0 = float(np.exp(-0.5))
PI = float(np.pi)
GN_EPS = 64e-5
NB = 16
NEG = -1.0e30
SEQ_STREAMS = False


class Sched:
    def __init__(self, nc, es):
        self.nc = nc
        self.es = es
        self.engs = {'v': nc.vector, 'a': nc.scalar, 'p': nc.tensor, 'g': nc.gpsimd, 's': nc.sync}
        self.sems = {}
        self.val = {}
        self.waited = {e: {} for e in self.engs}
        self.lastw = {}
        self.readers = {}
        self.nins = 0
        for e in 'vapg':
            self._mk(e)

    def _mk(self, key):
        self.sems[key] = self.es.enter_context(self.nc.semaphore('sem_' + key))
        self.val[key] = 0

    def _wait(self, e, k, v):
        if self.waited[e].get(k, 0) >= v:
            return
        self.engs[e].wait_ge(self.sems[k], v)
        self.waited[e][k] = v

    def _deps(self, e, reads, writes):
        for b in reads:
            if b in self.lastw:
                self._wait(e, *self.lastw[b])
        for b in writes:
            if b in self.lastw:
                self._wait(e, *self.lastw[b])
            for k, v in self.readers.get(b, {}).items():
                self._wait(e, k, v)

    def _commit(self, tok, reads, writes):
        k, v = tok
        for b in reads:
            self.readers.setdefault(b, {})[k] = v
        for b in writes:
            self.lastw[b] = tok
            self.readers[b] = {}

    def op(self, e, reads, writes, fn):
        self._deps(e, reads, writes)
        ins = fn(self.engs[e])
        self.val[e] += 1
        ins.then_inc(self.sems[e], 1)
        self._commit((e, self.val[e]), reads, writes)
        self.nins += 1

    def dma(self, q, semkey, out, in_, reads, writes, in_offset=None):
        if semkey not in self.sems:
            self._mk(semkey)
        self._deps(q, reads, writes)
        if self.val[semkey] > 0:
            self._wait(q, semkey, self.val[semkey])
        eng = self.engs[q]
        if in_offset is not None:
            ins = eng.indirect_dma_start(out=out, out_offset=None, in_=in_, in_offset=in_offset)
        else:
            ins = eng.dma_start(out=out, in_=in_)
        self.val[semkey] += 16
        ins.then_inc(self.sems[semkey], 16)
        self._commit((semkey, self.val[semkey]), reads, writes)
        self.nins += 1

    def barrier(self):
        for e in self.engs:
            for k, v in self.val.items():
                if v > 0:
                    self._wait(e, k, v)
        self.lastw = {}
        self.readers = {}


def bc(ap, shape, axis):
    return ap.unsqueeze(axis).to_broadcast(shape)


def gelu_tanh(op, src, srck, tmp, tmpk, dst, dstk, sq_eng='g'):
    op(sq_eng, [srck], [tmpk], lambda e: e.tensor_tensor(out=tmp, in0=src, in1=src, op=ALU.mult))
    op('v', [tmpk], [tmpk], lambda e: e.tensor_scalar(out=tmp, in0=tmp, scalar1=0.044715, scalar2=1.0, op0=ALU.mult, op1=ALU.add))
    op('v', [tmpk, srck], [tmpk], lambda e: e.tensor_tensor(out=tmp, in0=tmp, in1=src, op=ALU.mult))
    op('a', [tmpk], [tmpk], lambda e: e.activation(out=tmp, in_=tmp, func=AF.Sigmoid, scale=1.5957691216057308))
    op('v', [tmpk, srck], [dstk], lambda e: e.tensor_tensor(out=dst, in0=tmp, in1=src, op=ALU.mult))


class _Stop(Exception):
    pass


def build_nc(NT=32, debug=False, phases=(1, 2), stop=None):
    nc = bass.Bass("TRN2", target_bir_lowering=False)

    def mark(name):
        if stop == name:
            raise _Stop()

    def din(name, shape, dt=F32):
        return nc.dram_tensor(name, shape, dt, kind="ExternalInput").ap()

    x_d = din("x", [SEQ, D])
    w_in_d = din("w_in", [D, NCOL])
    gainP_d = din("gainP", [128, 8])
    bgate_d = din("bgate", [128, 16])
    mu_d = din("mu", [128, 14])
    pv_d = din("pvec", [128, 6, 4])
    lora_d = din("lora", [128, 512])
    glora_d = din("glora", [128, 512])
    lnw_d = din("lnw", [128, 512])
    lnb_d = din("lnb", [128, 512])
    w_o_d = din("w_o", [512, D])
    w_glu_d = din("w_glu", [512, 2 * D])
    w_out_d = din("w_out", [D, D])
    s5p_d = din("s5p", [128, 3, 16])
    bbre_d = din("bbre", [128, 2048])
    bbim_d = din("bbim", [128, 2048])
    ccre_d = din("ccre", [128, 2048])
    ccim_d = din("ccim", [128, 2048])
    gffn_d = din("gffn", [128, D])
    gfin_d = din("gfin", [128, D])
    wq_d = din("wq", [D, D])
    subk_d = din("subk", [128, 1024])
    pu_d = din("peer_u", [16384, D])
    pvv_d = din("peer_v", [16384, D])
    ident_d = din("ident", [128, 128])
    masks_d = din("masks", [128, 5, 128])
    sel2_d = din("sel2", [128, 2])
    iota_d = din("iota16", [128, 16])
    out_d = nc.dram_tensor("out", [SEQ, D], F32, kind="ExternalOutput").ap()
    h1_d = nc.dram_tensor("h1s", [SEQ, D], F32, kind="ExternalOutput" if debug else "Internal").ap()
    wsc_d = nc.dram_tensor("wsc", [NCT, 128, 1024], BF16, kind="Internal").ap()
    wo_d = nc.dram_tensor("wosc", [8, 128, 1024], BF16, kind="Internal").ap()
    tuv_d = nc.dram_tensor("tuv", [16384, 2, D], BF16, kind="Internal").ap()

    with ExitStack() as es0:
        sch = Sched(nc, es0)
        op = sch.op
        dma = sch.dma
        PS = [es0.enter_context(nc.psum_tensor("ps%d" % i, [128, 512], F32)) for i in range(8)]

        def psk(i):
            return "ps%d" % i

        def P3(i):
            return PS[i][:].rearrange("p (a b) -> p a b", a=4)

        if 1 in phases:
         try:
          with ExitStack() as es:
            def sb(name, shape, dt=F32):
                return es.enter_context(nc.sbuf_tensor("s_" + name, shape, dt))

            opq = [None]

            def op(*a):
                if opq[0] is None:
                    sch.op(*a)
                else:
                    opq[0].append(a)

            ident = sb("ident", [128, 128])
            masks = sb("masks", [128, 5, 128])
            sel2 = sb("sel2", [128, 2])
            gainP = sb("gainP", [128, 8])
            bgate = sb("bgate", [128, 16])
            mu = sb("mu", [128, 14])
            pvec = sb("pvec", [128, 6, 4])
            lnw = sb("lnw", [128, 512])
            lnb = sb("lnb", [128, 512])
            s5p = sb("s5p", [128, 3, 16])
            sp = sb("sp", [128, 24, 16])
            lora = sb("lora", [128, 512], BF16)
            glora = sb("glora", [128, 512], BF16)
            w_o = sb("w_o", [128, 4, 1024], BF16)
            w_glu = sb("w_glu", [128, 4, 2048], BF16)
            BBre = sb("BBre", [128, 16, 128], BF16)
            BBim = sb("BBim", [128, 16, 128], BF16)
            CCre = sb("CCre", [128, 16, 128], BF16)
            CCimN = sb("CCimN", [128, 16, 128], BF16)
            Ere = sb("Ere", [128, 16, 128])
            Eim = sb("Eim", [128, 16, 128])
            SCR = sb("SCR", [128, 8192])
            esp = ExitStack()
            stgb = [esp.enter_context(nc.sbuf_tensor("s_stgb%d" % i, [128, 1024], BF16)) for i in range(2)]

            for t_sb, t_d in [(ident, ident_d), (masks, masks_d), (sel2, sel2_d), (gainP, gainP_d),
                              (bgate, bgate_d), (mu, mu_d), (pvec, pv_d), (lnw, lnw_d), (lnb, lnb_d),
                              (s5p, s5p_d)]:
                dma('s', 'dc', t_sb[:], t_d, [], ["c"])
            maskSL = masks[:, 0, :]
            maskSU = masks[:, 1, :]
            maskUI = masks[:, 2, :]
            maskBD = masks[:, 3, :]
            scanmask = masks[:, 4, :]

            stg = [SCR[:, 0:2048], SCR[:, 2048:4096]]
            tmpA = SCR[:, 4096:6144]
            tmpB = SCR[:, 6144:8192]

            w_in_v = w_in_d.rearrange("(kt p) c -> p kt c", p=128)
            for c in range(NCT):
                b = c % 2
                sg = "stg%d" % b
                sgb = "stgb%d" % b
                dma('s', 'dst%d' % b, stg[b][:, 0:1024].rearrange("p (k c) -> p k c", k=8),
                    w_in_v[:, :, c * 128:(c + 1) * 128], [], [sg])
                op('v', [sg, "c"], [sgb], lambda e, b=b: e.tensor_tensor(
                    out=stgb[b][:].rearrange("p (k c) -> p k c", k=8),
                    in0=stg[b][:, 0:1024].rearrange("p (k c) -> p k c", k=8),
                    in1=bc(gainP[:], [128, 8, 128], 2), op=ALU.mult))
                dma('s', 'dsb%d' % b, wsc_d[c], stgb[b][:], [sgb], ["wsc%d" % c])

            ldn = [0]

            def load_cast(dst_ap, src_ap, width, dstkey):
                b = ldn[0] % 2
                ldn[0] += 1
                sg = "stg%d" % b
                dma('s', 'dst%d' % b, stg[b][:, 0:width], src_ap, [], [sg])
                if b == 0:
                    op('v', [sg], [dstkey], lambda e: e.tensor_copy(out=dst_ap, in_=stg[b][:, 0:width]))
                else:
                    op('a', [sg], [dstkey], lambda e: e.activation(out=dst_ap, in_=stg[b][:, 0:width], func=AF.Copy))

            load_cast(lora[:], lora_d, 512, "lora")
            load_cast(glora[:], glora_d, 512, "glora")
            for k in range(4):
                load_cast(w_o[:, k, :], w_o_d[k * 128:(k + 1) * 128, :], 1024, "w_o")
            for k in range(4):
                load_cast(w_glu[:, k, :], w_glu_d[k * 128:(k + 1) * 128, :], 2048, "w_glu")
            for k in range(8):
                b = k % 2
                dma('s', 'dst%d' % b, stg[b][:, 0:1024], w_out_d[k * 128:(k + 1) * 128, :], [], ["stg%d" % b])
                op('v', ["stg%d" % b], ["stgb%d" % b], lambda e, b=b: e.tensor_copy(out=stgb[b][:], in_=stg[b][:, 0:1024]))
                dma('s', 'dsb%d' % b, wo_d[k], stgb[b][:], ["stgb%d" % b], ["wo%d" % k])
            load_cast(BBre[:].rearrange("p a b -> p (a b)"), bbre_d, 2048, "BB")
            load_cast(BBim[:].rearrange("p a b -> p (a b)"), bbim_d, 2048, "BB")

            def V2(i):
                return sp[:, i, :]
            a_re = s5p[:, 0, :]
            a_im = s5p[:, 1, :]
            ldt = s5p[:, 2, :]
            DT, TH, LAB, CS, SN, R, M_, LRE, LIM, NRE, DEN, FRE, FIM, T1, T2, CS2, SN2 = range(17)

            def vv(out_i, a, b_, o):
                op('v', ["c", "sp"], ["sp"], lambda e: e.tensor_tensor(out=V2(out_i), in0=a, in1=b_, op=o))
            op('a', ["c"], ["sp"], lambda e: e.activation(out=V2(DT), in_=ldt, func=AF.Exp))
            vv(TH, a_im, V2(DT), ALU.mult)
            vv(T1, a_re, V2(DT), ALU.mult)
            op('a', ["sp"], ["sp"], lambda e: e.activation(out=V2(LAB), in_=V2(T1), func=AF.Exp))

            def sin_of(dst, shift):
                op('v', ["sp"], ["sp"], lambda e: e.tensor_scalar(out=V2(R), in0=V2(TH), scalar1=float(shift), scalar2=None, op0=ALU.add))
                for _ in range(4):
                    op('v', ["sp"], ["sp"], lambda e: e.tensor_scalar(out=V2(M_), in0=V2(R), scalar1=PI, scalar2=-2.0 * PI, op0=ALU.is_ge, op1=ALU.mult))
                    vv(R, V2(R), V2(M_), ALU.add)
                op('a', ["sp"], ["sp"], lambda e: e.activation(out=V2(dst), in_=V2(R), func=AF.Sin))
            sin_of(SN, 0.0)
            sin_of(CS, PI / 2)
            vv(LRE, V2(LAB), V2(CS), ALU.mult)
            vv(LIM, V2(LAB), V2(SN), ALU.mult)
            op('v', ["sp"], ["sp"], lambda e: e.tensor_scalar(out=V2(NRE), in0=V2(LRE), scalar1=-1.0, scalar2=None, op0=ALU.add))
            vv(T1, a_re, a_re, ALU.mult)
            vv(T2, a_im, a_im, ALU.mult)
            vv(DEN, V2(T1), V2(T2), ALU.add)
            op('v', ["sp"], ["sp"], lambda e: e.reciprocal(out=V2(DEN), in_=V2(DEN)))
            vv(T1, V2(NRE), a_re, ALU.mult)
            vv(T2, V2(LIM), a_im, ALU.mult)
            vv(T1, V2(T1), V2(T2), ALU.add)
            vv(FRE, V2(T1), V2(DEN), ALU.mult)
            vv(T1, V2(LIM), a_re, ALU.mult)
            vv(T2, V2(NRE), a_im, ALU.mult)
            vv(T1, V2(T1), V2(T2), ALU.subtract)
            vv(FIM, V2(T1), V2(DEN), ALU.mult)

            dma('s', 'dst0', stg[0], ccre_d, [], ["stg0"])
            dma('s', 'dst1', stg[1], ccim_d, [], ["stg1"])

            def c3(a):
                return a.rearrange("p (j m) -> p j m", j=16)
            fre_b = bc(V2(FRE), [128, 16, 128], 2)
            fim_b = bc(V2(FIM), [128, 16, 128], 2)
            op('v', ["stg0", "sp"], ["tmpA"], lambda e: e.tensor_tensor(out=c3(tmpA), in0=c3(stg[0]), in1=fre_b, op=ALU.mult))
            op('v', ["stg1", "sp"], ["tmpB"], lambda e: e.tensor_tensor(out=c3(tmpB), in0=c3(stg[1]), in1=fim_b, op=ALU.mult))
            op('v', ["tmpA", "tmpB"], ["CC"], lambda e: e.tensor_tensor(out=CCre[:].rearrange("p a b -> p (a b)"), in0=tmpA, in1=tmpB, op=ALU.subtract))
            op('v', ["stg0", "sp", "CC"], ["tmpA"], lambda e: e.tensor_tensor(out=c3(tmpA), in0=c3(stg[0]), in1=fim_b, op=ALU.mult))
            op('v', ["stg1", "sp", "CC"], ["tmpB"], lambda e: e.tensor_tensor(out=c3(tmpB), in0=c3(stg[1]), in1=fre_b, op=ALU.mult))
            op('v', ["tmpA", "tmpB"], ["tmpA"], lambda e: e.tensor_tensor(out=tmpA, in0=tmpA, in1=tmpB, op=ALU.add))
            op('v', ["tmpA"], ["CC"], lambda e: e.tensor_scalar(out=CCimN[:].rearrange("p a b -> p (a b)"), in0=tmpA, scalar1=-1.0, scalar2=None, op0=ALU.mult))

            op('v', [], ["E"], lambda e: e.memset(Ere[:, :, 0:1], 1.0))
            op('v', [], ["E"], lambda e: e.memset(Eim[:, :, 0:1], 0.0))
            op('v', ["sp"], ["sp"], lambda e: e.tensor_copy(out=V2(CS2), in_=V2(CS)))
            op('v', ["sp"], ["sp"], lambda e: e.tensor_copy(out=V2(SN2), in_=V2(SN)))
            et0 = tmpA[:, 0:1024].rearrange("p (a b) -> p a b", a=16)
            et1 = tmpB[:, 0:1024].rearrange("p (a b) -> p a b", a=16)
            for lv in range(7):
                m = 1 << lv
                shp = [128, 16, m]
                cb = bc(V2(CS2), shp, 2)
                sbb = bc(V2(SN2), shp, 2)
                op('v', ["E", "sp", "tmpA"], ["tmpA"], lambda e, m=m, cb=cb: e.tensor_tensor(out=et0[:, :, 0:m], in0=Ere[:, :, 0:m], in1=cb, op=ALU.mult))
                op('v', ["E", "sp", "tmpB"], ["tmpB"], lambda e, m=m, sbb=sbb: e.tensor_tensor(out=et1[:, :, 0:m], in0=Eim[:, :, 0:m], in1=sbb, op=ALU.mult))
                op('v', ["tmpA", "tmpB", "E"], ["E"], lambda e, m=m: e.tensor_tensor(out=Ere[:, :, m:2 * m], in0=et0[:, :, 0:m], in1=et1[:, :, 0:m], op=ALU.subtract))
                op('v', ["E", "sp", "tmpA"], ["tmpA"], lambda e, m=m, sbb=sbb: e.tensor_tensor(out=et0[:, :, 0:m], in0=Ere[:, :, 0:m], in1=sbb, op=ALU.mult))
                op('v', ["E", "sp", "tmpB"], ["tmpB"], lambda e, m=m, cb=cb: e.tensor_tensor(out=et1[:, :, 0:m], in0=Eim[:, :, 0:m], in1=cb, op=ALU.mult))
                op('v', ["tmpA", "tmpB", "E"], ["E"], lambda e, m=m: e.tensor_tensor(out=Eim[:, :, m:2 * m], in0=et0[:, :, 0:m], in1=et1[:, :, 0:m], op=ALU.add))
                vv(T1, V2(CS2), V2(CS2), ALU.mult)
                vv(T2, V2(SN2), V2(SN2), ALU.mult)
                op('v', ["sp"], ["sp"], lambda e: e.scalar_tensor_tensor(out=V2(SN2), in0=V2(CS2), scalar=2.0, in1=V2(SN2), op0=ALU.mult, op1=ALU.mult))
                vv(CS2, V2(T1), V2(T2), ALU.subtract)

            sch.barrier()
            esp.close()
            mark("prep")

            PF = sb("PF", [128, 14, 129])
            Stp = sb("Stp", [128, 4, 128])
            cw = sb("cw", [128, 2, 16])
            op('v', [], ["PF"], lambda e: e.memset(PF[:], 0.0))
            op('v', [], ["Stp"], lambda e: e.memset(Stp[:], 0.0))
            op('v', [], ["cw0", "cw1", "cw2", "cw3"], lambda e: e.memset(cw[:], 0.0))

            xt = [sb("xt%d" % i, [128, D]) for i in range(2)]
            stat = sb("stat", [128, 8])
            xnF = sb("xnF", [128, 8, 128], BF16)
            wch = [sb("wch%d" % i, [128, 8, 128], BF16) for i in range(3)]
            L = sb("L", [128, 14, 128])
            uF = sb("uF", [128, 4, 128])
            uB = sb("uB", [128, 4, 128], BF16)
            gates = sb("gates", [128, 16, 128], BF16)
            lorain = sb("lorain", [128, 128], BF16)
            sgx = sb("sgx", [128, 128], BF16)
            TT = [SCR[:, i * 512:(i + 1) * 512].rearrange("p (a b) -> p a b", a=4) for i in range(16)]
            TK = ["T%d" % i for i in range(16)]
            (iSIG, iCS, iPT, iPTM, iPINV, iAV, iKK, iTMP, iKMOD, iRTL, iATL, iBTL, iKTL, iRKR, iY2, iX) = range(16)
            VT = sb("VT", [128, 512])
            BT = sb("BT", [128, 512])
            KT = sb("KT", [128, 512])
            gT = sb("gT", [128, 512])
            mats = {nm: sb("m_" + nm, [128, 4, 128]) for nm in ["X", "XT", "X2", "X2T", "QT", "akT", "rbT", "rkT"]}
            RHS = sb("RHS", [128, 512])
            SA = sb("SA", [128, 512])
            PTbd = sb("PTbd", [128, 4, 128])
            Y = sb("Y", [128, 512])
            gst = sb("gst", [128, 6, 8])
            bon = sb("bon", [128, 8])
            roF = sb("roF", [128, 4, 128], BF16)
            mixed = sb("mixed", [128, 8, 128])
            mixedB = sb("mixedB", [128, 8, 128], BF16)
            xre2 = [sb("xre%d" % i, [128, 4, 128], BF16) for i in range(2)]
            xim2 = [sb("xim%d" % i, [128, 4, 128], BF16) for i in range(2)]
            ygB = sb("ygB", [128, 4, 128], BF16)
            zg = sb("zg", [128, 8, 128])
            ST = sb("ST", [128, 8, 512])
            cwn2 = [sb("cwn%d" % i, [128, 4, 4]) for i in range(2)]

            w0 = pvec[:, 0, :]
            a0 = pvec[:, 1, :]
            k_k = pvec[:, 2, :]
            k_a = pvec[:, 3, :]
            r_k = pvec[:, 4, :]
            s5d = pvec[:, 5, :]
            S4 = [128, 4, 128]

            def tt(eng, o, ok, a, ak, b_, bk, alu):
                op(eng, [ak, bk], [ok], lambda e: e.tensor_tensor(out=o, in0=a, in1=b_, op=alu))

            dma('s', 'dx0', xt[0][:], x_d[0:128, :], [], ["xt0"])

            for it in range(NT):
                xb = it % 2
                xk = "xt%d" % xb
                xcur = xt[xb]
                if it + 1 < NT:
                    dma('s', 'dx%d' % (1 - xb), xt[1 - xb][:], x_d[(it + 1) * 128:(it + 2) * 128, :], [], ["xt%d" % (1 - xb)])
                op('a', [xk], [TK[iX], "stat"], lambda e: e.activation(out=SCR[:, iX * 512:iX * 512 + 512], in_=xcur[:, 0:512], func=AF.Square, accum_out=stat[:, 0:1]))
                op('a', [xk], [TK[iX], "stat"], lambda e: e.activation(out=SCR[:, iX * 512:iX * 512 + 512], in_=xcur[:, 512:1024], func=AF.Square, accum_out=stat[:, 3:4]))
                op('v', ["stat"], ["stat"], lambda e: e.tensor_tensor(out=stat[:, 0:1], in0=stat[:, 0:1], in1=stat[:, 3:4], op=ALU.add))
                op('a', ["stat"], ["stat"], lambda e: e.activation(out=stat[:, 1:2], in_=stat[:, 0:1], func=AF.Sqrt, bias=1e-6, scale=1.0 / D))
                op('v', ["stat"], ["stat"], lambda e: e.reciprocal(out=stat[:, 2:3], in_=stat[:, 1:2]))
                op('v', [xk, "stat"], [xk], lambda e: e.tensor_scalar(out=xcur[:], in0=xcur[:], scalar1=stat[:, 2:3], scalar2=None, op0=ALU.mult))
                mark("norm")
                for half in range(2):
                    pk = psk(half)

                    def tr(e, half=half):
                        ins = None
                        for q in range(4):
                            kt = half * 4 + q
                            ins = e.transpose(out=PS[half][:, q * 128:(q + 1) * 128], in_=xcur[:, kt * 128:(kt + 1) * 128], identity=ident[:])
                        return ins
                    op('p', [xk, "c"], [pk], tr)
                    if half == 0:
                        op('v', [pk], ["xnF"], lambda e: e.tensor_copy(out=xnF[:, 0:4, :], in_=P3(0)))
                    else:
                        op('a', [pk], ["xnF"], lambda e: e.activation(out=xnF[:, 4:8, :], in_=P3(1), func=AF.Copy))
                mark("xnF")
                for c in range(NCT):
                    mark("proj%d" % c)
                    wb = c % 3
                    wk = "wch%d" % wb
                    dma('s', 'dw%d' % wb, wch[wb][:].rearrange("p a b -> p (a b)"), wsc_d[c], ["wsc%d" % c], [wk])
                    pb = 2 + (c % 4)
                    pk = psk(pb)

                    def mm(e, wb=wb, pb=pb):
                        ins = None
                        for kt in range(8):
                            ins = e.matmul(PS[pb][:, 0:128], lhsT=wch[wb][:, kt, :], rhs=xnF[:, kt, :], start=(kt == 0), stop=(kt == 7))
                        return ins
                    op('p', [wk, "xnF"], [pk], mm)
                    if c < 14:
                        if c % 2 == 0:
                            op('v', [pk], ["PF"], lambda e, c=c, pb=pb: e.tensor_copy(out=PF[:, c, 1:129], in_=PS[pb][:, 0:128]))
                        else:
                            op('a', [pk], ["PF"], lambda e, c=c, pb=pb: e.activation(out=PF[:, c, 1:129], in_=PS[pb][:, 0:128], func=AF.Copy))
                    elif c < 18:
                        op('v', [pk], ["uF"], lambda e, c=c, pb=pb: e.tensor_copy(out=uF[:, c - 14, :], in_=PS[pb][:, 0:128]))
                        op('a', ["uF"], ["uB"], lambda e, c=c, pb=pb: e.activation(out=uB[:, c - 14, :], in_=uF[:, c - 14, :], func=AF.Copy))
                    else:
                        op('a', [pk, "c"], ["gates"], lambda e, c=c, pb=pb: e.activation(out=gates[:, c - 18, :], in_=PS[pb][:, 0:128], func=AF.Sigmoid, bias=bgate[:, c - 18:c - 17]))
                mark("proj")
                op('v', ["PF"], ["L"], lambda e: e.tensor_tensor(out=L[:], in0=PF[:, :, 0:128], in1=PF[:, :, 1:129], op=ALU.subtract))
                op('g', ["L", "c"], ["L"], lambda e: e.tensor_tensor(out=L[:], in0=L[:], in1=bc(mu[:], [128, 14, 128], 2), op=ALU.mult))
                op('v', ["L", "PF"], ["L"], lambda e: e.tensor_tensor(out=L[:], in0=L[:], in1=PF[:, :, 1:129], op=ALU.add))
                op('v', ["PF"], ["PF"], lambda e: e.tensor_copy(out=PF[:, :, 0:1], in_=PF[:, :, 128:129]))
                qR, qS = [], []
                opq[0] = qR
                rF = L[:, 0:4, :]
                kF = L[:, 4:8, :]
                vF = L[:, 8:12, :]
                sig, cs, Pt, Ptm1, Pinv, av, kk, tmp, kmod, rtl, atl, btl, ktl, rkr = [TT[i] for i in range(14)]
                op('a', ["L"], ["lorain"], lambda e: e.activation(out=lorain[0:64, :], in_=L[0:64, 12, :], func=AF.Tanh))
                op('v', ["L"], ["lorain"], lambda e: e.tensor_copy(out=lorain[64:128, :], in_=L[64:128, 12, :]))
                op('a', ["L"], ["sgx"], lambda e: e.activation(out=sgx[:], in_=L[:, 13, :], func=AF.Sigmoid))

                def mm_lw(e):
                    ins = None
                    for j in range(4):
                        ins = e.matmul(PS[0][:, j * 128:(j + 1) * 128], lhsT=lora[0:64, j * 128:(j + 1) * 128], rhs=lorain[0:64, :], start=True, stop=True)
                    return ins
                op('p', ["lora", "lorain"], [psk(0)], mm_lw)

                def mm_la(e):
                    ins = None
                    for j in range(4):
                        ins = e.matmul(PS[1][:, j * 128:(j + 1) * 128], lhsT=lora[64:128, j * 128:(j + 1) * 128], rhs=lorain[64:128, :], start=True, stop=True)
                    return ins
                op('p', ["lora", "lorain"], [psk(1)], mm_la)
                op('p', ["glora", "sgx"], [psk(6)], lambda e: e.matmul(PS[6][:], lhsT=sgx[:], rhs=glora[:], start=True, stop=True))
                for j in range(4):
                    op('a', [psk(0), "c"], [TK[iSIG]], lambda e, j=j: e.activation(out=sig[:, j, :], in_=PS[0][:, j * 128:(j + 1) * 128], func=AF.Sigmoid, bias=w0[:, j:j + 1]))
                    op('a', [psk(1), "c"], [TK[iAV]], lambda e, j=j: e.activation(out=av[:, j, :], in_=PS[1][:, j * 128:(j + 1) * 128], func=AF.Sigmoid, bias=a0[:, j:j + 1]))
                op('v', [psk(6)], ["gT"], lambda e: e.tensor_copy(out=gT[:], in_=PS[6][:]))
                for j in range(4):
                    op('v', [TK[iSIG], "c"], [TK[iCS]], lambda e, j=j: e.tensor_tensor_scan(out=cs[:, j, :], data0=scanmask, data1=sig[:, j, :], initial=0.0, op0=ALU.mult, op1=ALU.add))
                op('a', [TK[iCS]], [TK[iPT]], lambda e: e.activation(out=Pt, in_=cs, func=AF.Exp, scale=-C0))
                op('a', [TK[iCS]], [TK[iPINV]], lambda e: e.activation(out=Pinv, in_=cs, func=AF.Exp, scale=C0))
                tt('v', Ptm1, TK[iPTM], cs, TK[iCS], sig, TK[iSIG], ALU.subtract)
                op('a', [TK[iPTM]], [TK[iPTM]], lambda e: e.activation(out=Ptm1, in_=Ptm1, func=AF.Exp, scale=-C0))
                tt('g', kk, TK[iKK], kF, "L", bc(k_k, S4, 2), "c", ALU.mult)
                tt('g', tmp, TK[iTMP], kk, TK[iKK], kk, TK[iKK], ALU.mult)

                def mm_n(e):
                    ins = None
                    for j in range(4):
                        ins = e.matmul(PS[7][:, j * 128:(j + 1) * 128], lhsT=maskBD, rhs=tmp[:, j, :], start=True, stop=True)
                    return ins
                op('p', [TK[iTMP], "c"], [psk(7)], mm_n)
                op('a', [psk(7)], [TK[iTMP]], lambda e: e.activation(out=tmp, in_=P3(7), func=AF.Sqrt))
                op('v', [TK[iTMP]], [TK[iTMP]], lambda e: e.tensor_scalar(out=tmp, in0=tmp, scalar1=1e-12, scalar2=None, op0=ALU.max))
                op('v', [TK[iTMP]], [TK[iTMP]], lambda e: e.reciprocal(out=tmp, in_=tmp))
                tt('v', kk, TK[iKK], kk, TK[iKK], tmp, TK[iTMP], ALU.mult)
                tt('g', kmod, TK[iKMOD], av, TK[iAV], bc(k_a, S4, 2), "c", ALU.mult)
                tt('g', kmod, TK[iKMOD], kmod, TK[iKMOD], bc(k_a, S4, 2), "c", ALU.subtract)
                op('v', [TK[iKMOD], "L"], [TK[iKMOD]], lambda e: e.scalar_tensor_tensor(out=kmod, in0=kmod, scalar=1.0, in1=kF, op0=ALU.add, op1=ALU.mult))
                tt('v', rtl, TK[iRTL], rF, "L", Pt, TK[iPT], ALU.mult)
                op('v', [TK[iKK], TK[iPTM]], [TK[iATL]], lambda e: e.scalar_tensor_tensor(out=atl, in0=kk, scalar=-1.0, in1=Ptm1, op0=ALU.mult, op1=ALU.mult))
                tt('g', btl, TK[iBTL], kk, TK[iKK], av, TK[iAV], ALU.mult)
                tt('v', btl, TK[iBTL], btl, TK[iBTL], Pinv, TK[iPINV], ALU.mult)
                tt('v', ktl, TK[iKTL], kmod, TK[iKMOD], Pinv, TK[iPINV], ALU.mult)
                tt('g', rkr, TK[iRKR], rF, "L", kmod, TK[iKMOD], ALU.mult)
                tt('g', rkr, TK[iRKR], rkr, TK[iRKR], bc(r_k, S4, 2), "c", ALU.mult)
                op('v', [TK[iPT], "c"], ["PTbd"], lambda e: e.tensor_tensor(out=PTbd[:], in0=bc(maskBD, S4, 1), in1=Pt[:, :, 127:128].to_broadcast(S4), op=ALU.mult))

                def mm_b(e):
                    ins = None
                    for j in range(4):
                        ins = e.matmul(PS[7][:, 2 * j:2 * j + 2], lhsT=rkr[:, j, :], rhs=sel2[:], start=True, stop=True)
                    return ins
                op('p', [TK[iRKR], "c"], [psk(7)], mm_b)
                op('v', [psk(7)], ["bon"], lambda e: e.tensor_copy(out=bon[:], in_=PS[7][:, 0:8]))
                atl_m = [TT[0], TT[1]]
                rtl_m = [TT[2], TT[3]]
                for hh in range(2):
                    op('v' if hh == 0 else 'g', [TK[iATL], "c"], [TK[hh]], lambda e, hh=hh: e.tensor_scalar(out=atl_m[hh], in0=atl, scalar1=sel2[:, hh:hh + 1], scalar2=None, op0=ALU.mult))
                    op('v' if hh == 0 else 'g', [TK[iRTL], "c"], [TK[2 + hh]], lambda e, hh=hh: e.tensor_scalar(out=rtl_m[hh], in0=rtl, scalar1=sel2[:, hh:hh + 1], scalar2=None, op0=ALU.mult))
                mark("elem")
                for src, srckey, dst, dkey, pb in [(vF, "L", VT, "VT", 0), (btl, TK[iBTL], BT, "BT", 1), (ktl, TK[iKTL], KT, "KT", 6)]:
                    def trf(e, src=src, pb=pb):
                        ins = None
                        for j in range(4):
                            ins = e.transpose(out=PS[pb][:, j * 128:(j + 1) * 128], in_=src[:, j, :], identity=ident[:])
                        return ins
                    op('p', [srckey, "c"], [psk(pb)], trf)
                    if pb == 1:
                        op('a', [psk(pb)], [dkey], lambda e, dst=dst, pb=pb: e.activation(out=dst[:], in_=PS[pb][:], func=AF.Copy))
                    else:
                        op('v', [psk(pb)], [dkey], lambda e, dst=dst, pb=pb: e.tensor_copy(out=dst[:], in_=PS[pb][:]))
                mark("trans")
                for hg in range(2):
                    def hsl(t, hh, hg=hg):
                        h = hg * 4 + hh
                        return t[(h % 2) * 64:(h % 2) * 64 + 64, h // 2, :]
                    specs = [("X", "A", btl, maskSL, 2), ("XT", btl, "A", maskSU, 0),
                             ("akT", ktl, "A", maskSU, 1), ("rbT", btl, "R", maskUI, 6), ("rkT", ktl, "R", maskUI, 7)]

                    def pick(t, hl, hg=hg):
                        h = hg * 4 + hl
                        j, hh = h // 2, h % 2
                        if isinstance(t, str):
                            return (atl_m if t == "A" else rtl_m)[hh][:, j, :]
                        return t[:, j, :]
                    for nm, lt, rt, mk, pb in specs:
                        def mmT(e, lt=lt, rt=rt, pb=pb, pick=pick):
                            ins = None
                            for hl in range(4):
                                ins = e.matmul(PS[pb][:, hl * 128:(hl + 1) * 128], lhsT=pick(lt, hl), rhs=pick(rt, hl), start=True, stop=True)
                            return ins
                        op('p', [TK[0], TK[1], TK[2], TK[3], TK[iBTL], TK[iKTL]], [psk(pb)], mmT)
                        op('v', [psk(pb), "c"], ["m_" + nm], lambda e, nm=nm, mk=mk, pb=pb: e.tensor_tensor(
                            out=mats[nm][:], in0=P3(pb), in1=bc(mk, S4, 1), op=ALU.mult))
                        mark("mats_" + nm)
                    op('g', ["m_XT", "c"], ["m_QT"], lambda e: e.tensor_tensor(out=mats["QT"][:], in0=mats["XT"][:], in1=bc(ident[:], S4, 1), op=ALU.add))
                    mark("mats_qt")
                    cur, curT, nx, nxT = "X", "XT", "X2", "X2T"
                    for step in range(6):
                        def sq(e, a=curT, b_=cur):
                            ins = None
                            for hh in range(4):
                                ins = e.matmul(PS[2][:, hh * 128:(hh + 1) * 128], lhsT=mats[a][:, hh, :], rhs=mats[b_][:, hh, :], start=True, stop=True)
                            return ins
                        op('p', ["m_" + cur, "m_" + curT], [psk(2)], sq)
                        op('a', [psk(2)], ["m_" + nx], lambda e, nx=nx: e.activation(out=mats[nx][:], in_=P3(2), func=AF.Copy))
                        if step < 5:
                            def sqT(e, a=cur, b_=curT):
                                ins = None
                                for hh in range(4):
                                    ins = e.matmul(PS[0][:, hh * 128:(hh + 1) * 128], lhsT=mats[a][:, hh, :], rhs=mats[b_][:, hh, :], start=True, stop=True)
                                return ins
                            op('p', ["m_" + cur, "m_" + curT], [psk(0)], sqT)
                            op('v', [psk(0)], ["m_" + nxT], lambda e, nxT=nxT: e.tensor_copy(out=mats[nxT][:], in_=P3(0)))

                        def qu(e, a=nx):
                            ins = None
                            for hh in range(4):
                                ins = e.matmul(PS[1][:, hh * 128:(hh + 1) * 128], lhsT=mats[a][:, hh, :], rhs=mats["QT"][:, hh, :], start=True, stop=True)
                            return ins
                        op('p', ["m_" + nx, "m_QT"], [psk(1)], qu)
                        op('v', [psk(1), "m_QT"], ["m_QT"], lambda e: e.tensor_tensor(out=mats["QT"][:], in0=mats["QT"][:], in1=P3(1), op=ALU.add))
                        cur, curT, nx, nxT = nx, nxT, cur, curT
                        mark("mats_s%d" % step)
                    mark("mats")
                    c0 = hg * 256

                    def mm_rhs(e, hg=hg):
                        ins = None
                        for hl in range(4):
                            h = hg * 4 + hl
                            j = h // 2
                            hh = h % 2
                            reg = PS[6][:, hl * 64:(hl + 1) * 64]
                            e.matmul(reg, lhsT=atl[:, j, :], rhs=Stp[:, j, hh * 64:(hh + 1) * 64], start=True, stop=False)
                            ins = e.matmul(reg, lhsT=mats["akT"][:, hl, :], rhs=VT[:, h * 64:(h + 1) * 64], start=False, stop=True)
                        return ins
                    op('p', [TK[iATL], "Stp", "m_akT", "VT"], [psk(6)], mm_rhs)
                    op('v', [psk(6)], ["RHS"], lambda e, c0=c0: e.tensor_copy(out=RHS[:, c0:c0 + 256], in_=PS[6][:, 0:256]))

                    def mm_sa(e, hg=hg):
                        ins = None
                        for hl in range(4):
                            h = hg * 4 + hl
                            ins = e.matmul(PS[7][:, hl * 64:(hl + 1) * 64], lhsT=mats["QT"][:, hl, :], rhs=RHS[:, h * 64:(h + 1) * 64], start=True, stop=True)
                        return ins
                    op('p', ["m_QT", "RHS"], [psk(7)], mm_sa)
                    op('v', [psk(7)], ["SA"], lambda e, c0=c0: e.tensor_copy(out=SA[:, c0:c0 + 256], in_=PS[7][:, 0:256]))

                    def mm_y(e, hg=hg):
                        ins = None
                        for hl in range(4):
                            h = hg * 4 + hl
                            j = h // 2
                            hh = h % 2
                            reg = PS[2][:, hl * 64:(hl + 1) * 64]
                            e.matmul(reg, lhsT=rtl[:, j, :], rhs=Stp[:, j, hh * 64:(hh + 1) * 64], start=True, stop=False)
                            e.matmul(reg, lhsT=mats["rbT"][:, hl, :], rhs=SA[:, h * 64:(h + 1) * 64], start=False, stop=False)
                            ins = e.matmul(reg, lhsT=mats["rkT"][:, hl, :], rhs=VT[:, h * 64:(h + 1) * 64], start=False, stop=True)
                        return ins
                    op('p', [TK[iRTL], "Stp", "m_rbT", "m_rkT", "SA", "VT"], [psk(2)], mm_y)
                    op('a', [psk(2)], ["Y"], lambda e, c0=c0: e.activation(out=Y[:, c0:c0 + 256], in_=PS[2][:, 0:256], func=AF.Copy))

                    def mm_st(e, hg=hg):
                        ins = None
                        for jj in range(2):
                            j = hg * 2 + jj
                            reg = PS[0][:, jj * 128:(jj + 1) * 128]
                            e.matmul(reg, lhsT=ident[:], rhs=Stp[:, j, :], start=True, stop=False)
                            e.matmul(reg, lhsT=BT[:, j * 128:(j + 1) * 128], rhs=SA[:, j * 128:(j + 1) * 128], start=False, stop=False)
                            ins = e.matmul(reg, lhsT=KT[:, j * 128:(j + 1) * 128], rhs=VT[:, j * 128:(j + 1) * 128], start=False, stop=True)
                        return ins
                    op('p', ["Stp", "BT", "KT", "SA", "VT", "c"], [psk(0)], mm_st)
                    op('v', [psk(0), "PTbd"], ["Stp"], lambda e, hg=hg: e.tensor_tensor(
                        out=Stp[:, 2 * hg:2 * hg + 2, :], in0=PS[0][:, 0:256].rearrange("p (a b) -> p a b", a=2),
                        in1=PTbd[:, 2 * hg:2 * hg + 2, :], op=ALU.mult))
                mark("chain")
                Y3 = Y[:].rearrange("p (h v) -> p h v", h=8)
                Ysq = SCR[:, iY2 * 512:(iY2 + 1) * 512]
                G8 = [128, 8, 64]
                op('v', ["Y"], ["gst"], lambda e: e.tensor_reduce(out=gst[:, 0, :], in_=Y3, axis=AX.X, op=ALU.add))
                op('a', ["Y"], [TK[iY2]], lambda e: e.activation(out=Ysq, in_=Y[:], func=AF.Square))
                op('v', [TK[iY2]], ["gst"], lambda e: e.tensor_reduce(out=gst[:, 1, :], in_=Ysq.rearrange("p (h v) -> p h v", h=8), axis=AX.X, op=ALU.add))
                op('v', ["gst"], ["gst"], lambda e: e.tensor_scalar(out=gst[:, 2, :], in0=gst[:, 0, :], scalar1=1.0 / 64, scalar2=None, op0=ALU.mult))
                op('v', ["gst"], ["gst"], lambda e: e.tensor_tensor(out=gst[:, 3, :], in0=gst[:, 2, :], in1=gst[:, 2, :], op=ALU.mult))
                op('v', ["gst"], ["gst"], lambda e: e.scalar_tensor_tensor(out=gst[:, 4, :], in0=gst[:, 1, :], scalar=1.0 / 64, in1=gst[:, 3, :], op0=ALU.mult, op1=ALU.subtract))
                op('a', ["gst"], ["gst"], lambda e: e.activation(out=gst[:, 5, :], in_=gst[:, 4, :], func=AF.Sqrt, bias=GN_EPS, scale=1.0))
                op('v', ["gst"], ["gst"], lambda e: e.reciprocal(out=gst[:, 5, :], in_=gst[:, 5, :]))
                op('v', ["Y", "gst"], ["Y"], lambda e: e.tensor_tensor(out=Y3, in0=Y3, in1=bc(gst[:, 2, :], G8, 2), op=ALU.subtract))
                op('v', ["Y", "gst"], ["Y"], lambda e: e.tensor_tensor(out=Y3, in0=Y3, in1=bc(gst[:, 5, :], G8, 2), op=ALU.mult))
                op('g', ["Y", "c"], ["Y"], lambda e: e.tensor_tensor(out=Y[:], in0=Y[:], in1=lnw[:], op=ALU.mult))
                op('g', ["Y", "c"], ["Y"], lambda e: e.tensor_tensor(out=Y[:], in0=Y[:], in1=lnb[:], op=ALU.add))
                op('v', ["VT", "bon"], [TK[iY2]], lambda e: e.tensor_tensor(out=Ysq.rearrange("p (h v) -> p h v", h=8), in0=VT[:].rearrange("p (h v) -> p h v", h=8), in1=bc(bon[:], G8, 2), op=ALU.mult))
                op('v', ["Y", TK[iY2]], ["Y"], lambda e: e.tensor_tensor(out=Y[:], in0=Y[:], in1=Ysq, op=ALU.add))
                op('v', ["Y", "gT"], ["Y"], lambda e: e.tensor_tensor(out=Y[:], in0=Y[:], in1=gT[:], op=ALU.mult))

                def tr_ro(e):
                    ins = None
                    for j in range(4):
                        ins = e.transpose(out=PS[0][:, j * 128:(j + 1) * 128], in_=Y[:, j * 128:(j + 1) * 128], identity=ident[:])
                    return ins
                op('p', ["Y", "c"], [psk(0)], tr_ro)
                op('a', [psk(0)], ["roF"], lambda e: e.activation(out=roF[:], in_=P3(0), func=AF.Copy))
                for half in range(2):
                    pb = 1 + half

                    def mm_o(e, half=half, pb=pb):
                        ins = None
                        for q in range(4):
                            dt_ = half * 4 + q
                            for kt in range(4):
                                ins = e.matmul(PS[pb][:, q * 128:(q + 1) * 128], lhsT=w_o[:, kt, dt_ * 128:(dt_ + 1) * 128], rhs=roF[:, kt, :], start=(kt == 0), stop=(kt == 3))
                        return ins
                    op('p', ["w_o", "roF"], [psk(pb)], mm_o)
                    op('v', [psk(pb), "gates"], ["mixed"], lambda e, half=half, pb=pb: e.tensor_tensor(out=mixed[:, half * 4:(half + 1) * 4, :], in0=P3(pb), in1=gates[:, half * 4:(half + 1) * 4, :], op=ALU.mult))
                mark("epi")
                opq[0] = qS
                def s5_group(kt, ts):
                    s5a, s5b, s5c, s5d_, btre, btim, wre, wim = [ST[:, i, :].rearrange("p (a b) -> p a b", a=4) for i in range(8)]
                    ka, kb, kc, kd, kbr, kbi, kwr, kwi = ["ST%d" % i for i in range(8)]
                    pa, pb_ = 3, 4
                    xr, xi = xre2[ts], xim2[ts]
                    xrk, xik = "xre%d" % ts, "xim%d" % ts
                    cn = cwn2[ts]
                    cnk = "cwn%d" % ts
                    cwk = "cw%d" % kt
                    Er = Ere[:, 4 * kt:4 * kt + 4, :]
                    Ei = Eim[:, 4 * kt:4 * kt + 4, :]

                    def mm_bu(e):
                        ins = None
                        for q in range(4):
                            j = 4 * kt + q
                            e.matmul(PS[pa][:, q * 128:(q + 1) * 128], lhsT=BBre[:, j, :], rhs=uB[:, kt, :], start=True, stop=True)
                            ins = e.matmul(PS[pb_][:, q * 128:(q + 1) * 128], lhsT=BBim[:, j, :], rhs=uB[:, kt, :], start=True, stop=True)
                        return ins
                    op('p', ["BB", "uB"], [psk(pa), psk(pb_)], mm_bu)
                    yield
                    tt('v', s5a, ka, Er, "E", P3(pa), psk(pa), ALU.mult)
                    yield
                    tt('v', s5b, kb, Ei, "E", P3(pb_), psk(pb_), ALU.mult)
                    yield
                    tt('g', btre, kbr, s5a, ka, s5b, kb, ALU.add)
                    yield
                    tt('v', s5c, kc, Er, "E", P3(pb_), psk(pb_), ALU.mult)
                    yield
                    tt('v', s5d_, kd, Ei, "E", P3(pa), psk(pa), ALU.mult)
                    yield
                    tt('g', btim, kbi, s5c, kc, s5d_, kd, ALU.subtract)
                    yield
                    for q in range(4):
                        j = 4 * kt + q
                        op('v', [kbr, cwk, "sp"], [kwr], lambda e, q=q, j=j: e.tensor_tensor_scan(out=wre[:, q, :], data0=sp[:, LAB, j:j + 1].to_broadcast([128, 128]), data1=btre[:, q, :], initial=cw[:, 0, j:j + 1], op0=ALU.mult, op1=ALU.add))
                        yield
                        op('v', [kbi, cwk, "sp"], [kwi], lambda e, q=q, j=j: e.tensor_tensor_scan(out=wim[:, q, :], data0=sp[:, LAB, j:j + 1].to_broadcast([128, 128]), data1=btim[:, q, :], initial=cw[:, 1, j:j + 1], op0=ALU.mult, op1=ALU.add))
                        yield
                    c128 = sp[:, CS2, 4 * kt:4 * kt + 4]
                    s128 = sp[:, SN2, 4 * kt:4 * kt + 4]
                    wr127 = wre[:, :, 127]
                    wi127 = wim[:, :, 127]
                    tt('g', cn[:, 0, :], cnk, c128, "sp", wr127, kwr, ALU.mult)
                    yield
                    tt('g', cn[:, 1, :], cnk, s128, "sp", wi127, kwi, ALU.mult)
                    yield
                    tt('g', cn[:, 2, :], cnk, s128, "sp", wr127, kwr, ALU.mult)
                    yield
                    tt('g', cn[:, 3, :], cnk, c128, "sp", wi127, kwi, ALU.mult)
                    yield
                    tt('g', cw[:, 0, 4 * kt:4 * kt + 4], cwk, cn[:, 0, :], cnk, cn[:, 1, :], cnk, ALU.subtract)
                    yield
                    tt('g', cw[:, 1, 4 * kt:4 * kt + 4], cwk, cn[:, 2, :], cnk, cn[:, 3, :], cnk, ALU.add)
                    yield
                    tt('v', s5a, ka, Er, "E", wre, kwr, ALU.mult)
                    yield
                    tt('v', s5b, kb, Ei, "E", wim, kwi, ALU.mult)
                    yield
                    tt('g', xr[:], xrk, s5a, ka, s5b, kb, ALU.subtract)
                    yield
                    tt('v', s5c, kc, Ei, "E", wre, kwr, ALU.mult)
                    yield
                    tt('v', s5d_, kd, Er, "E", wim, kwi, ALU.mult)
                    yield
                    tt('g', xi[:], xik, s5c, kc, s5d_, kd, ALU.add)
                    yield

                    def mm_c(e):
                        ins = None
                        for q in range(4):
                            j = 4 * kt + q
                            e.matmul(PS[5][:, kt * 128:(kt + 1) * 128], lhsT=CCre[:, j, :], rhs=xr[:, q, :], start=(q == 0), stop=False)
                            ins = e.matmul(PS[5][:, kt * 128:(kt + 1) * 128], lhsT=CCimN[:, j, :], rhs=xi[:, q, :], start=False, stop=(q == 3))
                        return ins
                    op('p', ["CC", xrk, xik], [psk(5)], mm_c)
                    yield

                for kt_ in range(4):
                    for _ in s5_group(kt_, 0):
                        pass
                s5a, s5b = ST[:, 0, :].rearrange("p (a b) -> p a b", a=4), ST[:, 1, :].rearrange("p (a b) -> p a b", a=4)
                ka, kb = "ST0", "ST1"
                opq[0] = None
                nR, nS = len(qR), len(qS)
                iR = iS = 0
                while iR < nR or iS < nS:
                    if iS >= nS or (iR < nR and (SEQ_STREAMS or iR * nS <= iS * nR)):
                        sch.op(*qR[iR])
                        iR += 1
                    else:
                        sch.op(*qS[iS])
                        iS += 1
                tt('g', s5a, ka, uF[:], "uF", bc(s5d, S4, 2), "c", ALU.mult)
                tt('v', s5a, ka, s5a, ka, P3(5), psk(5), ALU.add)
                gelu_tanh(op, s5a, ka, s5b, kb, ygB[:], "ygB")
                for half in range(2):
                    for vg in range(2):
                        pb = 6 + vg

                        def mm_g(e, half=half, vg=vg, pb=pb):
                            ins = None
                            for q in range(4):
                                col = vg * 8 + half * 4 + q
                                for kt in range(4):
                                    ins = e.matmul(PS[pb][:, q * 128:(q + 1) * 128], lhsT=w_glu[:, kt, col * 128:(col + 1) * 128], rhs=ygB[:, kt, :], start=(kt == 0), stop=(kt == 3))
                            return ins
                        op('p', ["w_glu", "ygB"], [psk(pb)], mm_g)
                    zh = zg[:, half * 4:(half + 1) * 4, :]
                    op('a', [psk(7)], ["zg"], lambda e, zh=zh: e.activation(out=zh, in_=P3(7), func=AF.Sigmoid))
                    tt('v', zh, "zg", zh, "zg", P3(6), psk(6), ALU.mult)
                    tt('g', zh, "zg", zh, "zg", gates[:, 8 + half * 4:8 + (half + 1) * 4, :], "gates", ALU.mult)
                    tt('v', mixedB[:, half * 4:(half + 1) * 4, :], "mixedB", zh, "zg", mixed[:, half * 4:(half + 1) * 4, :], "mixed", ALU.add)
                mark("s5")
                wviews = [w_[:].rearrange("p a b -> p (a b)") for w_ in wch]
                for kt in range(8):
                    wb = (NCT + kt) % 3
                    wk = "wch%d" % wb
                    dma('s', 'dw%d' % wb, wviews[wb], wo_d[kt], [], [wk])

                    def mm_h(e, kt=kt, wb=wb):
                        e.matmul(PS[1][:], lhsT=mixedB[:, kt, :], rhs=wviews[wb][:, 0:512], start=(kt == 0), stop=(kt == 7))
                        return e.matmul(PS[2][:], lhsT=mixedB[:, kt, :], rhs=wviews[wb][:, 512:1024], start=(kt == 0), stop=(kt == 7))
                    op('p', [wk, "mixedB"], [psk(1), psk(2)], mm_h)
                for half in range(2):
                    pb = 1 + half
                    op('v', [psk(pb), xk, "stat"], [xk], lambda e, half=half, pb=pb: e.scalar_tensor_tensor(
                        out=xcur[:, half * 512:(half + 1) * 512], in0=xcur[:, half * 512:(half + 1) * 512], scalar=stat[:, 1:2], in1=PS[pb][:], op0=ALU.mult, op1=ALU.add))
                dma('s', 'dh1', h1_d[it * 128:(it + 1) * 128, :], xcur[:], [xk], ["h1d%d" % it])
            sch.barrier()
         except _Stop:
            sch.barrier()

        if 2 in phases:
          with ExitStack() as es:
            def sb(name, shape, dt=F32):
                return es.enter_context(nc.sbuf_tensor("s_" + name, shape, dt))
            NCV = 6
            RB = 4
            cin = [sb("cin%d" % i, [128, RB, D]) for i in range(NCV)]
            cout = [sb("cout%d" % i, [128, RB, D], BF16) for i in range(NCV)]
            n = 0
            for src_d, dst_d in [(pu_d, tuv_d[:, 0, :]), (pvv_d, tuv_d[:, 1, :])]:
                sv_ = src_d.rearrange("(c r p) d -> c p r d", p=128, r=RB)
                dv_ = dst_d.rearrange("(c r p) d -> c p r d", p=128, r=RB)
                for c in range(16384 // (128 * RB)):
                    b = n % NCV
                    n += 1
                    dma('s', 'dci%d' % b, cin[b][:], sv_[c], [], ["cin%d" % b])
                    if n % 3 == 0:
                        op('v', ["cin%d" % b], ["cout%d" % b], lambda e, b=b: e.tensor_copy(out=cout[b][:], in_=cin[b][:]))
                    elif n % 3 == 1:
                        op('a', ["cin%d" % b], ["cout%d" % b], lambda e, b=b: e.activation(out=cout[b][:], in_=cin[b][:], func=AF.Copy))
                    else:
                        op('g', ["cin%d" % b], ["cout%d" % b], lambda e, b=b: e.tensor_copy(out=cout[b][:], in_=cin[b][:]))
                    dma('a', 'dco%d' % b, dv_[c], cout[b][:], ["cout%d" % b], ["tb"])
            sch.barrier()

        if 2 in phases:
          with ExitStack() as es:
            def sb(name, shape, dt=F32):
                return es.enter_context(nc.sbuf_tensor("s_" + name, shape, dt))
            ident = sb("ident2", [128, 128])
            gffn = sb("gffn", [128, D])
            gfin = sb("gfin", [128, D])
            wq = sb("wq", [128, 8, D])
            subk = sb("subk", [128, 8, 128])
            h1t = [sb("h1t%d" % i, [128, D]) for i in range(2)]
            xn2 = [sb("xn2_0", [128, D])] * 2
            xn2b = [sb("xn2b_%d" % i, [128, D], BF16) for i in range(2)]
            junkb = sb("junkb", [128, D], BF16)
            xn2F = sb("xn2F", [128, 8, 128])
            qF = sb("qF", [128, 8, 128])
            sc = sb("sc", [128, 16, 128])
            scr = sb("scr", [128, 16, 128])
            iota16 = sb("iota16", [128, 16])
            eq = sb("eq", [128, 8, 16, 16])
            junk = eq[:].rearrange("p a b c -> p (a b c)")
            sv = sb("sv", [128, 16, 16])
            siu = sb("siu", [128, 16, 16], U32)
            sif = sb("sif", [128, 16, 16])
            dsi = sb("dsi", [128, 8, 16])
            cand = sb("cand", [128, 8, 256])
            cv = sb("cv", [128, 8, 16])
            ciu = sb("ciu", [128, 8, 16], U32)
            iiu = sb("iiu", [128, 8, 16], U32)
            jju = sb("jju", [128, 8, 16], U32)
            iif = sb("iif", [128, 8, 16])
            jjf = sb("jjf", [128, 8, 16])
            i1 = sb("i1", [128, 8, 16])
            i2 = sb("i2", [128, 8, 16])
            lt = sb("lt", [128, 8, 16])
            gtmp = sb("gtmp", [128, 128])
            ei = [sb("ei%d" % i, [128, 128], I32) for i in range(2)]
            gate = [sb("gate%d" % i, [128, 8, 16]) for i in range(2)]
            sm = sb("sm", [128, 4, 8])
            hid = sb("hid", [128, 128])
            wgt = [sb("wgt%d" % i, [128, 128]) for i in range(2)]
            statA = sb("statA", [128, 8])
            statV = sb("statV", [128, 8])
            NDG = 4
            dg = [sb("dg%d" % i, [128, 128], BF16) for i in range(NDG)]
            NR = 20
            UV = [sb("UV%d" % i, [128, 2, D], BF16) for i in range(NR)]
            hid2 = [sb("hid%d" % i, [128, 128]) for i in range(2)]
            gt2 = [sb("gtg%d" % i, [128, 8]) for i in range(2)]
            h2 = sb("h2", [128, D])
            outt = sb("outt", [128, D])

            dma('s', 'dc2', ident[:], ident_d, [], ["c"])
            dma('s', 'dc2', gffn[:], gffn_d, [], ["c"])
            dma('s', 'dc2', iota16[:], iota_d, [], ["c"])
            dma('s', 'dc2', gfin[:], gfin_d, [], ["c"])
            dma('s', 'dc2', subk[:].rearrange("p a b -> p (a b)"), subk_d, [], ["c"])
            dma('s', 'dc2', wq[:], wq_d.rearrange("(kt p) c -> p kt c", p=128), [], ["c"])

            def rms(src, srck, dstat, dk, op=op):
                op('a', [srck], ["eq", dk], lambda e: e.activation(out=junk[:, 0:512], in_=src[:, 0:512], func=AF.Square, accum_out=dstat[:, 0:1]))
                op('a', [srck], ["eq", dk], lambda e: e.activation(out=junk[:, 512:1024], in_=src[:, 512:1024], func=AF.Square, accum_out=dstat[:, 3:4]))
                op('v', [dk], [dk], lambda e: e.tensor_tensor(out=dstat[:, 0:1], in0=dstat[:, 0:1], in1=dstat[:, 3:4], op=ALU.add))
                op('a', [dk], [dk], lambda e: e.activation(out=dstat[:, 1:2], in_=dstat[:, 0:1], func=AF.Sqrt, bias=1e-6, scale=1.0 / D))
                op('v', [dk], [dk], lambda e: e.reciprocal(out=dstat[:, 2:3], in_=dstat[:, 1:2]))

            def top16_multi(segs, segk, width, vouts, iouts, okeys):
                n = len(segs)
                scrv = [scr[:].rearrange("p a b -> p (a b)")[:, i * width:(i + 1) * width] for i in range(n)]
                sk = ["scr%d" % i for i in range(n)]
                for i in range(n):
                    qop('v', [segk], [okeys[i]], lambda e, i=i: e.max(out=vouts[i][:, 0:8], in_=segs[i]))
                for i in range(n):
                    qop('v', [segk, okeys[i]], [okeys[i]], lambda e, i=i: e.max_index(out=iouts[i][:, 0:8], in_max=vouts[i][:, 0:8], in_values=segs[i]))
                for i in range(n):
                    qop('v', [segk, okeys[i]], [sk[i]], lambda e, i=i: e.match_replace(out=scrv[i], in_to_replace=vouts[i][:, 0:8], in_values=segs[i], imm_value=NEG))
                for i in range(n):
                    qop('v', [sk[i]], [okeys[i]], lambda e, i=i: e.max(out=vouts[i][:, 8:16], in_=scrv[i]))
                for i in range(n):
                    qop('v', [sk[i], okeys[i]], [okeys[i]], lambda e, i=i: e.max_index(out=iouts[i][:, 8:16], in_max=vouts[i][:, 8:16], in_values=scrv[i]))

            gcount = [0, 0, 0]

            Aq = []

            def qop(*a):
                Aq.append(('op', a))

            def qdma(*a, **k):
                Aq.append(('dma', a, k))

            def drain(n=None):
                k = 0
                while Aq and (n is None or k < n):
                    item = Aq.pop(0)
                    if item[0] == 'op':
                        op(*item[1])
                    else:
                        dma(*item[1], **item[2])
                    k += 1

            def stage_A(it):
                p = it % 2
                hk = "h1t%d" % p
                hcur = h1t[p]
                xk = "xn2_0"
                xc = xn2[0]
                qdma('s', 'dh%d' % p, hcur[:], h1_d[it * 128:(it + 1) * 128, :], ["h1d%d" % it], [hk])
                rms(hcur, hk, statA, "statA", op=qop)
                qop('v', [hk, "statA", "c"], [xk], lambda e: e.scalar_tensor_tensor(out=xc[:], in0=hcur[:], scalar=statA[:, 2:3], in1=gffn[:], op0=ALU.mult, op1=ALU.mult))
                qop('a', [xk], ["xn2b_%d" % p], lambda e: e.activation(out=xn2b[p][:], in_=xc[:], func=AF.Copy))
                for half in range(2):
                    def tr2(e, half=half):
                        ins = None
                        for q in range(4):
                            kt = half * 4 + q
                            ins = e.transpose(out=PS[half][:, q * 128:(q + 1) * 128], in_=xc[:, kt * 128:(kt + 1) * 128], identity=ident[:])
                        return ins
                    qop('p', [xk, "c"], [psk(half)], tr2)
                    if half == 0:
                        qop('v', [psk(0)], ["xn2F"], lambda e: e.tensor_copy(out=xn2F[:, 0:4, :], in_=P3(0)))
                    else:
                        qop('a', [psk(1)], ["xn2F"], lambda e: e.activation(out=xn2F[:, 4:8, :], in_=P3(1), func=AF.Copy))
                for half in range(2):
                    pb = 2 + half

                    def mm_q(e, half=half, pb=pb):
                        ins = None
                        for q in range(4):
                            ct = half * 4 + q
                            for kt in range(8):
                                ins = e.matmul(PS[pb][:, q * 128:(q + 1) * 128], lhsT=wq[:, kt, ct * 128:(ct + 1) * 128], rhs=xn2F[:, kt, :], start=(kt == 0), stop=(kt == 7))
                        return ins
                    qop('p', ["c", "xn2F"], [psk(pb)], mm_q)
                    if half == 0:
                        qop('v', [psk(pb)], ["qF"], lambda e, pb=pb: e.tensor_copy(out=qF[:, 0:4, :], in_=P3(pb)))
                    else:
                        qop('a', [psk(pb)], ["qF"], lambda e, pb=pb: e.activation(out=qF[:, 4:8, :], in_=P3(pb), func=AF.Copy))
                sc4 = sc[:].rearrange("p (h c) n -> p h c n", c=2)
                for hg in range(2):
                    for c in range(2):
                        pb = 4 + c

                        def mm_s(e, hg=hg, c=c, pb=pb):
                            ins = None
                            for q in range(4):
                                h = hg * 4 + q
                                ins = e.matmul(PS[pb][:, q * 128:(q + 1) * 128], lhsT=qF[c * 64:(c + 1) * 64, h, :], rhs=subk[c * 64:(c + 1) * 64, h, :], start=True, stop=True)
                            return ins
                        qop('p', ["qF", "c"], [psk(pb)], mm_s)
                        if c == 0:
                            qop('v', [psk(pb)], ["sc"], lambda e, hg=hg, c=c, pb=pb: e.tensor_copy(out=sc4[:, hg * 4:(hg + 1) * 4, c, :], in_=P3(pb)))
                        else:
                            qop('a', [psk(pb)], ["sc"], lambda e, hg=hg, c=c, pb=pb: e.activation(out=sc4[:, hg * 4:(hg + 1) * 4, c, :], in_=P3(pb), func=AF.Copy))

            svk = ["svi%d" % i for i in range(16)]
            cvk = ["cvi%d" % i for i in range(8)]

            def stage_A2(it):
                p = it % 2
                top16_multi([sc[:, i, :] for i in range(16)], "sc", 128, [sv[:, i, :] for i in range(16)], [siu[:, i, :] for i in range(16)], svk)
                for h in range(8):
                    qop('v', [svk[2 * h], svk[2 * h + 1]], ["cand"], lambda e, h=h: e.tensor_tensor(
                        out=cand[:, h, :].rearrange("p (i j) -> p i j", i=16),
                        in0=bc(sv[:, 2 * h, :], [128, 16, 16], 2), in1=bc(sv[:, 2 * h + 1, :], [128, 16, 16], 1), op=ALU.add))
                top16_multi([cand[:, h, :] for h in range(8)], "cand", 256, [cv[:, h, :] for h in range(8)], [ciu[:, h, :] for h in range(8)], cvk)
                qop('v', cvk, ["iiu"], lambda e: e.tensor_single_scalar(out=iiu[:], in_=ciu[:], scalar=4, op=ALU.logical_shift_right))
                qop('v', cvk, ["jju"], lambda e: e.tensor_single_scalar(out=jju[:], in_=ciu[:], scalar=15, op=ALU.bitwise_and))
                qop('v', ["iiu"], ["iif"], lambda e: e.tensor_copy(out=iif[:], in_=iiu[:]))
                qop('v', ["jju"], ["jjf"], lambda e: e.tensor_copy(out=jjf[:], in_=jju[:]))
                qop('v', svk, ["sif"], lambda e: e.tensor_copy(out=sif[:], in_=siu[:]))
                sif4 = sif[:].rearrange("p (h c) k -> p h c k", c=2)
                E4 = [128, 8, 16, 16]
                iota4 = iota16[:].unsqueeze(1).unsqueeze(1).to_broadcast(E4)
                for (idxf, idk, c_, dst, dk) in [(iif, "iif", 0, i1, "i1"), (jjf, "jjf", 1, i2, "i2")]:
                    qop('v', [idk, "c", "eq"], ["eq"], lambda e, idxf=idxf: e.tensor_tensor(out=eq[:], in0=idxf[:].unsqueeze(3).to_broadcast(E4), in1=iota4, op=ALU.is_equal))
                    qop('v', ["eq", "sif"], ["eq"], lambda e, c_=c_: e.tensor_tensor(out=eq[:], in0=eq[:], in1=sif4[:, :, c_, :].unsqueeze(2).to_broadcast(E4), op=ALU.mult))
                    qop('v', ["eq"], [dk], lambda e, dst=dst: e.tensor_reduce(out=dst[:], in_=eq[:], axis=AX.X, op=ALU.add))
                qop('v', ["i1", "i2"], ["i1"], lambda e: e.scalar_tensor_tensor(out=i1[:], in0=i1[:], scalar=128.0, in1=i2[:], op0=ALU.mult, op1=ALU.add))
                qop('v', ["i1"], ["ei%d" % p], lambda e: e.tensor_copy(out=ei[p][:], in_=i1[:].rearrange("p h k -> p (h k)")))

            def stage_A3(it):
                p = it % 2
                gk = "gate%d" % p
                g_ = gate[p]
                qop('v', cvk, ["sm"], lambda e: e.tensor_reduce(out=sm[:, 0, :], in_=cv[:], axis=AX.X, op=ALU.max))
                qop('v', cvk + ["sm"], [gk], lambda e: e.tensor_tensor(out=g_[:], in0=cv[:], in1=bc(sm[:, 0, :], [128, 8, 16], 2), op=ALU.subtract))
                qop('a', [gk], [gk], lambda e: e.activation(out=g_[:], in_=g_[:], func=AF.Exp))
                qop('v', [gk], ["sm"], lambda e: e.tensor_reduce(out=sm[:, 1, :], in_=g_[:], axis=AX.X, op=ALU.add))
                qop('v', ["sm"], ["sm"], lambda e: e.reciprocal(out=sm[:, 2, :], in_=sm[:, 1, :]))
                qop('v', [gk, "sm"], [gk], lambda e: e.tensor_tensor(out=g_[:], in0=g_[:], in1=bc(sm[:, 2, :], [128, 8, 16], 2), op=ALU.mult))

            GS = 4
            NGR = 128 // GS
            slot_buf = {}

            def dots(it, g):
                p = it % 2
                hd = hid2[p]
                hk_ = "hid%d" % p
                if g == 0:
                    op('v', [], [hk_], lambda e: e.memset(hd[:], 0.0))
                for s_ in range(g * GS, (g + 1) * GS):
                    b = gcount[0] % NR
                    gcount[0] += 1
                    slot_buf[(it, s_)] = b
                    dma('g', 'duv%d' % b, UV[b][:].rearrange("p a d -> p (a d)"), tuv_d.rearrange("e a d -> e (a d)"), ["ei%d" % p], ["UV%d" % b],
                        in_offset=bass.IndirectOffsetOnAxis(ap=ei[p][:, s_:s_ + 1], axis=0))
                    op('v', ["UV%d" % b, "xn2b_%d" % p], ["junkb", hk_], lambda e, b=b, s_=s_: e.scalar_tensor_tensor(
                        out=junkb[:], in0=UV[b][:, 0, :], scalar=1.0, in1=xn2b[p][:], op0=ALU.mult, op1=ALU.mult, accum_out=hd[:, s_:s_ + 1]))

            def weights(it, g):
                p = it % 2
                hd = hid2[p]
                hk_ = "hid%d" % p
                wk = "wgt%d" % p
                q = g % 2
                sl = slice(g * GS, (g + 1) * GS)
                gelu_tanh(op, hd[:, sl], hk_, gt2[q][:, 0:GS], "gtg%d" % q, wgt[p][:, sl], wk, sq_eng='v')
                op('v', [wk, "gate%d" % p], [wk], lambda e: e.tensor_tensor(out=wgt[p][:, sl], in0=wgt[p][:, sl], in1=gate[p][:].rearrange("p h k -> p (h k)")[:, sl], op=ALU.mult))

            def accum(it, g):
                p = it % 2
                wk = "wgt%d" % p
                for s_ in range(g * GS, (g + 1) * GS):
                    b = slot_buf.pop((it, s_))
                    r = gcount[2] % NDG
                    gcount[2] += 1
                    op('a', [wk, "c"], ["dg%d" % r], lambda e, r=r, s_=s_: e.activation(out=dg[r][:], in_=ident[:], func=AF.Copy, scale=wgt[p][:, s_:s_ + 1]))

                    def mm_v(e, b=b, r=r, s_=s_):
                        e.matmul(PS[6][:], lhsT=dg[r][:], rhs=UV[b][:, 1, 0:512], start=(s_ == 0), stop=(s_ == 127))
                        return e.matmul(PS[7][:], lhsT=dg[r][:], rhs=UV[b][:, 1, 512:1024], start=(s_ == 0), stop=(s_ == 127))
                    op('p', ["dg%d" % r, "UV%d" % b], [psk(6), psk(7)], mm_v)

            def stage_Vf(it):
                p = it % 2
                hk = "h1t%d" % p
                hcur = h1t[p]
                op('v', [psk(6), hk], ["h2"], lambda e: e.tensor_tensor(out=h2[:, 0:512], in0=PS[6][:], in1=hcur[:, 0:512], op=ALU.add))
                op('v', [psk(7), hk], ["h2"], lambda e: e.tensor_tensor(out=h2[:, 512:1024], in0=PS[7][:], in1=hcur[:, 512:1024], op=ALU.add))
                rms(h2, "h2", statV, "statV")
                op('v', ["h2", "statV", "c"], ["outt"], lambda e: e.scalar_tensor_tensor(out=outt[:], in0=h2[:], scalar=statV[:, 2:3], in1=gfin[:], op0=ALU.mult, op1=ALU.mult))
                dma('s', 'dout', out_d[it * 128:(it + 1) * 128, :], outt[:], ["outt"], ["od%d" % it])

            stage_A(0)
            stage_A2(0)
            stage_A3(0)
            drain()
            prev = None
            for it in range(NT):
                per = 0
                if it + 1 < NT:
                    stage_A(it + 1)
                    stage_A2(it + 1)
                    stage_A3(it + 1)
                    per = (len(Aq) + NGR - 5) // (NGR - 4)
                for g in range(NGR):
                    dots(it, g)
                    if prev is not None:
                        weights(*prev)
                        accum(*prev)
                        if prev[1] == NGR - 1:
                            stage_Vf(prev[0])
                    prev = (it, g)
                    if per:
                        drain(per)
                drain()
            weights(*prev)
            accum(*prev)
            stage_Vf(prev[0])
            sch.barrier()
    return nc


def make_inputs(inp, b):
    f = lambda a: np.ascontiguousarray(np.asarray(a), dtype=np.float32)
    colT = lambda v, n: f(np.asarray(v).reshape(n, 128).T)
    m = {}
    m["x"] = f(inp["x"][b])
    m["w_in"] = f(inp["w_in"][0])
    m["gainP"] = colT(inp["norm_mix"][0], 8)
    m["bgate"] = colT(inp["b_gate"][0], 16)
    m["mu"] = colT(inp["mu_rwkv"][0], 14)
    pv = np.stack([colT(inp["w0"][0], 4), colT(inp["a0"][0], 4), colT(inp["k_k"][0], 4), colT(inp["k_a"][0], 4),
                   colT(np.asarray(inp["r_k"][0]).reshape(512), 4), colT(np.asarray(inp["s5_d"][0]).reshape(512), 4)], axis=1)
    m["pvec"] = f(pv)
    m["lora"] = f(np.concatenate([np.asarray(inp["w_lora_up"][0]), np.asarray(inp["a_lora_up"][0])], axis=0))
    m["glora"] = f(inp["g_lora_up"][0])
    m["lnw"] = f(np.broadcast_to(np.asarray(inp["ln_x_w"][0])[None, :], (128, 512)))
    m["lnb"] = f(np.broadcast_to(np.asarray(inp["ln_x_b"][0])[None, :], (128, 512)))
    m["w_o"] = f(inp["w_o_rwkv"][0])
    m["w_glu"] = f(inp["w_glu_s5"][0])
    m["w_out"] = f(inp["w_out"][0])
    a_re = np.asarray(inp["s5_a_re"][0]).reshape(16, 128).T
    a_im = np.asarray(inp["s5_a_im"][0]).reshape(16, 128).T
    ldt = np.repeat(np.asarray(inp["s5_log_dt"][0]), 64).reshape(16, 128).T
    m["s5p"] = f(np.stack([a_re, a_im, ldt], axis=1))
    bbre = np.zeros((128, 16, 128), np.float32)
    bbim = np.zeros((128, 16, 128), np.float32)
    ccre = np.zeros((128, 16, 128), np.float32)
    ccim = np.zeros((128, 16, 128), np.float32)
    b_re = np.asarray(inp["s5_b_re"][0]); b_im = np.asarray(inp["s5_b_im"][0])
    c_re = np.asarray(inp["s5_c_re"][0]); c_im = np.asarray(inp["s5_c_im"][0])
    for g in range(32):
        j, gl, g8 = g // 2, g % 2, g % 8
        bbre[g8 * 16:(g8 + 1) * 16, j, gl * 64:(gl + 1) * 64] = b_re[g].T
        bbim[g8 * 16:(g8 + 1) * 16, j, gl * 64:(gl + 1) * 64] = b_im[g].T
        ccre[gl * 64:(gl + 1) * 64, j, g8 * 16:(g8 + 1) * 16] = c_re[g].T
        ccim[gl * 64:(gl + 1) * 64, j, g8 * 16:(g8 + 1) * 16] = c_im[g].T
    m["bbre"] = bbre.reshape(128, 2048)
    m["bbim"] = bbim.reshape(128, 2048)
    m["ccre"] = ccre.reshape(128, 2048)
    m["ccim"] = ccim.reshape(128, 2048)
    m["gffn"] = f(np.broadcast_to(np.asarray(inp["norm_ffn"][0])[None, :], (128, D)))
    m["gfin"] = f(np.broadcast_to(np.asarray(inp["norm_final"])[None, :], (128, D)))
    m["wq"] = f(inp["peer_wq"][0])
    m["subk"] = f(np.asarray(inp["peer_subkeys"][0]).transpose(1, 3, 0, 2).reshape(128, 1024))
    m["peer_u"] = f(inp["peer_u"][0])
    m["peer_v"] = f(inp["peer_v"][0])
    m["ident"] = np.eye(128, dtype=np.float32)
    p = np.arange(128)[:, None]
    jx = np.arange(128)[None, :]
    masks = np.zeros((128, 5, 128), np.float32)
    masks[:, 0] = (jx < p)
    masks[:, 1] = (jx > p)
    masks[:, 2] = (jx >= p)
    masks[:, 3] = ((jx // 64) == (p // 64))
    masks[:, 4] = 1.0
    masks[:, 4, 0] = 0.0
    m["masks"] = masks
    sel2 = np.zeros((128, 2), np.float32)
    sel2[:64, 0] = 1.0
    sel2[64:, 1] = 1.0
    m["sel2"] = sel2
    m["iota16"] = np.ascontiguousarray(np.broadcast_to(np.arange(16, dtype=np.float32)[None, :], (128, 16)))
    return m


_NC_CACHE = {}


def kernel(**inputs):
    n = 8
    if "nc" not in _NC_CACHE:
        _NC_CACHE["nc"] = build_nc()
    nc = _NC_CACHE["nc"]
    in_maps = [make_inputs(inputs, b) for b in range(n)]
    res = run_bass_kernel_spmd(nc, in_maps, core_ids=list(range(n)))
    out = np.stack([np.asarray(r["out"], dtype=np.float32) for r in res.results], axis=0)
    return out
```

```python
import numpy as np
from contextlib import ExitStack
import concourse.bass as bass
import concourse.mybir as mybir
from concourse.bass_utils import run_bass_kernel_spmd

F32 = mybir.dt.float32
BF16 = mybir.dt.bfloat16
I32 = mybir.dt.int32
U32 = mybir.dt.uint32
ALU = mybir.AluOpType
AF = mybir.ActivationFunctionType
AX = mybir.AxisListType

D = 1024
NRW = 1792
NCOL = 4352
SEQ = 4096
NCT = 34
C0 = float(np.exp(-0.5))
PI = float(np.pi)
GN_EPS = 64e-5
NB = 16
NEG = -1.0e30
SEQ_STREAMS = False


class Sched:
    def __init__(self, nc, es):
        self.nc = nc
        self.es = es
        self.engs = {'v': nc.vector, 'a': nc.scalar, 'p': nc.tensor, 'g': nc.gpsimd, 's': nc.sync}
        self.sems = {}
        self.val = {}
        self.waited = {e: {} for e in self.engs}
        self.lastw = {}
        self.readers = {}
        self.nins = 0
        for e in 'vapg':
            self._mk(e)

    def _mk(self, key):
        self.sems[key] = self.es.enter_context(self.nc.semaphore('sem_' + key))
        self.val[key] = 0

    def _wait(self, e, k, v):
        if self.waited[e].get(k, 0) >= v:
            return
        self.engs[e].wait_ge(self.sems[k], v)
        self.waited[e][k] = v

    def _deps(self, e, reads, writes):
        for b in reads:
            if b in self.lastw:
                self._wait(e, *self.lastw[b])
        for b in writes:
            if b in self.lastw:
                self._wait(e, *self.lastw[b])
            for k, v in self.readers.get(b, {}).items():
                self._wait(e, k, v)

    def _commit(self, tok, reads, writes):
        k, v = tok
        for b in reads:
            self.readers.setdefault(b, {})[k] = v
        for b in writes:
            self.lastw[b] = tok
            self.readers[b] = {}

    def op(self, e, reads, writes, fn):
        self._deps(e, reads, writes)
        ins = fn(self.engs[e])
        self.val[e] += 1
        ins.then_inc(self.sems[e], 1)
        self._commit((e, self.val[e]), reads, writes)
        self.nins += 1

    def dma(self, q, semkey, out, in_, reads, writes, in_offset=None):
        if semkey not in self.sems:
            self._mk(semkey)
        self._deps(q, reads, writes)
        if self.val[semkey] > 0:
            self._wait(q, semkey, self.val[semkey])
        eng = self.engs[q]
        if in_offset is not None:
            ins = eng.indirect_dma_start(out=out, out_offset=None, in_=in_, in_offset=in_offset)
        else:
            ins = eng.dma_start(out=out, in_=in_)
        self.val[semkey] += 16
        ins.then_inc(self.sems[semkey], 16)
        self._commit((semkey, self.val[semkey]), reads, writes)
        self.nins += 1

    def barrier(self):
        for e in self.engs:
            for k, v in self.val.items():
                if v > 0:
                    self._wait(e, k, v)
        self.lastw = {}
        self.readers = {}


def bc(ap, shape, axis):
    return ap.unsqueeze(axis).to_broadcast(shape)


def gelu_tanh(op, src, srck, tmp, tmpk, dst, dstk, sq_eng='g'):
    op(sq_eng, [srck], [tmpk], lambda e: e.tensor_tensor(out=tmp, in0=src, in1=src, op=ALU.mult))
    op('v', [tmpk], [tmpk], lambda e: e.tensor_scalar(out=tmp, in0=tmp, scalar1=0.044715, scalar2=1.0, op0=ALU.mult, op1=ALU.add))
    op('v', [tmpk, srck], [tmpk], lambda e: e.tensor_tensor(out=tmp, in0=tmp, in1=src, op=ALU.mult))
    op('a', [tmpk], [tmpk], lambda e: e.activation(out=tmp, in_=tmp, func=AF.Sigmoid, scale=1.5957691216057308))
    op('v', [tmpk, srck], [dstk], lambda e: e.tensor_tensor(out=dst, in0=tmp, in1=src, op=ALU.mult))


class _Stop(Exception):
    pass


def build_nc(NT=32, debug=False, phases=(1, 2), stop=None):
    nc = bass.Bass("TRN2", target_bir_lowering=False)

    def mark(name):
        if stop == name:
            raise _Stop()

    def din(name, shape, dt=F32):
        return nc.dram_tensor(name, shape, dt, kind="ExternalInput").ap()

    x_d = din("x", [SEQ, D])
    w_in_d = din("w_in", [D, NCOL])
    gainP_d = din("gainP", [128, 8])
    bgate_d = din("bgate", [128, 16])
    mu_d = din("mu", [128, 14])
    pv_d = din("pvec", [128, 6, 4])
    lora_d = din("lora", [128, 512])
    glora_d = din("glora", [128, 512])
    lnw_d = din("lnw", [128, 512])
    lnb_d = din("lnb", [128, 512])
    w_o_d = din("w_o", [512, D])
    w_glu_d = din("w_glu", [512, 2 * D])
    w_out_d = din("w_out", [D, D])
    s5p_d = din("s5p", [128, 3, 16])
    bbre_d = din("bbre", [128, 2048])
    bbim_d = din("bbim", [128, 2048])
    ccre_d = din("ccre", [128, 2048])
    ccim_d = din("ccim", [128, 2048])
    gffn_d = din("gffn", [128, D])
    gfin_d = din("gfin", [128, D])
    wq_d = din("wq", [D, D])
    subk_d = din("subk", [128, 1024])
    pu_d = din("peer_u", [16384, D])
    pvv_d = din("peer_v", [16384, D])
    ident_d = din("ident", [128, 128])
    masks_d = din("masks", [128, 5, 128])
    sel2_d = din("sel2", [128, 2])
    iota_d = din("iota16", [128, 16])
    out_d = nc.dram_tensor("out", [SEQ, D], F32, kind="ExternalOutput").ap()
    h1_d = nc.dram_tensor("h1s", [SEQ, D], F32, kind="ExternalOutput" if debug else "Internal").ap()
    wsc_d = nc.dram_tensor("wsc", [NCT, 128, 1024], BF16, kind="Internal").ap()
    wo_d = nc.dram_tensor("wosc", [8, 128, 1024], BF16, kind="Internal").ap()
    tuv_d = nc.dram_tensor("tuv", [16384, 2, D], BF16, kind="Internal").ap()

    with ExitStack() as es0:
        sch = Sched(nc, es0)
        op = sch.op
        dma = sch.dma
        PS = [es0.enter_context(nc.psum_tensor("ps%d" % i, [128, 512], F32)) for i in range(8)]

        def psk(i):
            return "ps%d" % i

        def P3(i):
            return PS[i][:].rearrange("p (a b) -> p a b", a=4)

        if 1 in phases:
         try:
          with ExitStack() as es:
            def sb(name, shape, dt=F32):
                return es.enter_context(nc.sbuf_tensor("s_" + name, shape, dt))

            opq = [None]

            def op(*a):
                if opq[0] is None:
                    sch.op(*a)
                else:
                    opq[0].append(a)

            ident = sb("ident", [128, 128])
            masks = sb("masks", [128, 5, 128])
            sel2 = sb("sel2", [128, 2])
            gainP = sb("gainP", [128, 8])
            bgate = sb("bgate", [128, 16])
            mu = sb("mu", [128, 14])
            pvec = sb("pvec", [128, 6, 4])
            lnw = sb("lnw", [128, 512])
            lnb = sb("lnb", [128, 512])
            s5p = sb("s5p", [128, 3, 16])
            sp = sb("sp", [128, 24, 16])
            lora = sb("lora", [128, 512], BF16)
            glora = sb("glora", [128, 512], BF16)
            w_o = sb("w_o", [128, 4, 1024], BF16)
            w_glu = sb("w_glu", [128, 4, 2048], BF16)
            BBre = sb("BBre", [128, 16, 128], BF16)
            BBim = sb("BBim", [128, 16, 128], BF16)
            CCre = sb("CCre", [128, 16, 128], BF16)
            CCimN = sb("CCimN", [128, 16, 128], BF16)
            Ere = sb("Ere", [128, 16, 128])
            Eim = sb("Eim", [128, 16, 128])
            SCR = sb("SCR", [128, 8192])
            esp = ExitStack()
            stgb = [esp.enter_context(nc.sbuf_tensor("s_stgb%d" % i, [128, 1024], BF16)) for i in range(2)]

            for t_sb, t_d in [(ident, ident_d), (masks, masks_d), (sel2, sel2_d), (gainP, gainP_d),
                              (bgate, bgate_d), (mu, mu_d), (pvec, pv_d), (lnw, lnw_d), (lnb, lnb_d),
                              (s5p, s5p_d)]:
                dma('s', 'dc', t_sb[:], t_d, [], ["c"])
            maskSL = masks[:, 0, :]
            maskSU = masks[:, 1, :]
            maskUI = masks[:, 2, :]
            maskBD = masks[:, 3, :]
            scanmask = masks[:, 4, :]

            stg = [SCR[:, 0:2048], SCR[:, 2048:4096]]
            tmpA = SCR[:, 4096:6144]
            tmpB = SCR[:, 6144:8192]

            w_in_v = w_in_d.rearrange("(kt p) c -> p kt c", p=128)
            for c in range(NCT):
                b = c % 2
                sg = "stg%d" % b
                sgb = "stgb%d" % b
                dma('s', 'dst%d' % b, stg[b][:, 0:1024].rearrange("p (k c) -> p k c", k=8),
                    w_in_v[:, :, c * 128:(c + 1) * 128], [], [sg])
                op('v', [sg, "c"], [sgb], lambda e, b=b: e.tensor_tensor(
                    out=stgb[b][:].rearrange("p (k c) -> p k c", k=8),
                    in0=stg[b][:, 0:1024].rearrange("p (k c) -> p k c", k=8),
                    in1=bc(gainP[:], [128, 8, 128], 2), op=ALU.mult))
                dma('s', 'dsb%d' % b, wsc_d[c], stgb[b][:], [sgb], ["wsc%d" % c])

            ldn = [0]

            def load_cast(dst_ap, src_ap, width, dstkey):
                b = ldn[0] % 2
                ldn[0] += 1
                sg = "stg%d" % b
                dma('s', 'dst%d' % b, stg[b][:, 0:width], src_ap, [], [sg])
                if b == 0:
                    op('v', [sg], [dstkey], lambda e: e.tensor_copy(out=dst_ap, in_=stg[b][:, 0:width]))
                else:
                    op('a', [sg], [dstkey], lambda e: e.activation(out=dst_ap, in_=stg[b][:, 0:width], func=AF.Copy))

            load_cast(lora[:], lora_d, 512, "lora")
            load_cast(glora[:], glora_d, 512, "glora")
            for k in range(4):
                load_cast(w_o[:, k, :], w_o_d[k * 128:(k + 1) * 128, :], 1024, "w_o")
            for k in range(4):
                load_cast(w_glu[:, k, :], w_glu_d[k * 128:(k + 1) * 128, :], 2048, "w_glu")
            for k in range(8):
                b = k % 2
                dma('s', 'dst%d' % b, stg[b][:, 0:1024], w_out_d[k * 128:(k + 1) * 128, :], [], ["stg%d" % b])
                op('v', ["stg%d" % b], ["stgb%d" % b], lambda e, b=b: e.tensor_copy(out=stgb[b][:], in_=stg[b][:, 0:1024]))
                dma('s', 'dsb%d' % b, wo_d[k], stgb[b][:], ["stgb%d" % b], ["wo%d" % k])
            load_cast(BBre[:].rearrange("p a b -> p (a b)"), bbre_d, 2048, "BB")
            load_cast(BBim[:].rearrange("p a b -> p (a b)"), bbim_d, 2048, "BB")

            def V2(i):
                return sp[:, i, :]
            a_re = s5p[:, 0, :]
            a_im = s5p[:, 1, :]
            ldt = s5p[:, 2, :]
            DT, TH, LAB, CS, SN, R, M_, LRE, LIM, NRE, DEN, FRE, FIM, T1, T2, CS2, SN2 = range(17)

            def vv(out_i, a, b_, o):
                op('v', ["c", "sp"], ["sp"], lambda e: e.tensor_tensor(out=V2(out_i), in0=a, in1=b_, op=o))
            op('a', ["c"], ["sp"], lambda e: e.activation(out=V2(DT), in_=ldt, func=AF.Exp))
            vv(TH, a_im, V2(DT), ALU.mult)
            vv(T1, a_re, V2(DT), ALU.mult)
            op('a', ["sp"], ["sp"], lambda e: e.activation(out=V2(LAB), in_=V2(T1), func=AF.Exp))

            def sin_of(dst, shift):
                op('v', ["sp"], ["sp"], lambda e: e.tensor_scalar(out=V2(R), in0=V2(TH), scalar1=float(shift), scalar2=None, op0=ALU.add))
                for _ in range(4):
                    op('v', ["sp"], ["sp"], lambda e: e.tensor_scalar(out=V2(M_), in0=V2(R), scalar1=PI, scalar2=-2.0 * PI, op0=ALU.is_ge, op1=ALU.mult))
                    vv(R, V2(R), V2(M_), ALU.add)
                op('a', ["sp"], ["sp"], lambda e: e.activation(out=V2(dst), in_=V2(R), func=AF.Sin))
            sin_of(SN, 0.0)
            sin_of(CS, PI / 2)
            vv(LRE, V2(LAB), V2(CS), ALU.mult)
            vv(LIM, V2(LAB), V2(SN), ALU.mult)
            op('v', ["sp"], ["sp"], lambda e: e.tensor_scalar(out=V2(NRE), in0=V2(LRE), scalar1=-1.0, scalar2=None, op0=ALU.add))
            vv(T1, a_re, a_re, ALU.mult)
            vv(T2, a_im, a_im, ALU.mult)
            vv(DEN, V2(T1), V2(T2), ALU.add)
            op('v', ["sp"], ["sp"], lambda e: e.reciprocal(out=V2(DEN), in_=V2(DEN)))
            vv(T1, V2(NRE), a_re, ALU.mult)
            vv(T2, V2(LIM), a_im, ALU.mult)
            vv(T1, V2(T1), V2(T2), ALU.add)
            vv(FRE, V2(T1), V2(DEN), ALU.mult)
            vv(T1, V2(LIM), a_re, ALU.mult)
            vv(T2, V2(NRE), a_im, ALU.mult)
            vv(T1, V2(T1), V2(T2), ALU.subtract)
            vv(FIM, V2(T1), V2(DEN), ALU.mult)

            dma('s', 'dst0', stg[0], ccre_d, [], ["stg0"])
            dma('s', 'dst1', stg[1], ccim_d, [], ["stg1"])

            def c3(a):
                return a.rearrange("p (j m) -> p j m", j=16)
            fre_b = bc(V2(FRE), [128, 16, 128], 2)
            fim_b = bc(V2(FIM), [128, 16, 128], 2)
            op('v', ["stg0", "sp"], ["tmpA"], lambda e: e.tensor_tensor(out=c3(tmpA), in0=c3(stg[0]), in1=fre_b, op=ALU.mult))
            op('v', ["stg1", "sp"], ["tmpB"], lambda e: e.tensor_tensor(out=c3(tmpB), in0=c3(stg[1]), in1=fim_b, op=ALU.mult))
            op('v', ["tmpA", "tmpB"], ["CC"], lambda e: e.tensor_tensor(out=CCre[:].rearrange("p a b -> p (a b)"), in0=tmpA, in1=tmpB, op=ALU.subtract))
            op('v', ["stg0", "sp", "CC"], ["tmpA"], lambda e: e.tensor_tensor(out=c3(tmpA), in0=c3(stg[0]), in1=fim_b, op=ALU.mult))
            op('v', ["stg1", "sp", "CC"], ["tmpB"], lambda e: e.tensor_tensor(out=c3(tmpB), in0=c3(stg[1]), in1=fre_b, op=ALU.mult))
            op('v', ["tmpA", "tmpB"], ["tmpA"], lambda e: e.tensor_tensor(out=tmpA, in0=tmpA, in1=tmpB, op=ALU.add))
            op('v', ["tmpA"], ["CC"], lambda e: e.tensor_scalar(out=CCimN[:].rearrange("p a b -> p (a b)"), in0=tmpA, scalar1=-1.0, scalar2=None, op0=ALU.mult))

            op('v', [], ["E"], lambda e: e.memset(Ere[:, :, 0:1], 1.0))
            op('v', [], ["E"], lambda e: e.memset(Eim[:, :, 0:1], 0.0))
            op('v', ["sp"], ["sp"], lambda e: e.tensor_copy(out=V2(CS2), in_=V2(CS)))
            op('v', ["sp"], ["sp"], lambda e: e.tensor_copy(out=V2(SN2), in_=V2(SN)))
            et0 = tmpA[:, 0:1024].rearrange("p (a b) -> p a b", a=16)
            et1 = tmpB[:, 0:1024].rearrange("p (a b) -> p a b", a=16)
            for lv in range(7):
                m = 1 << lv
                shp = [128, 16, m]
                cb = bc(V2(CS2), shp, 2)
                sbb = bc(V2(SN2), shp, 2)
                op('v', ["E", "sp", "tmpA"], ["tmpA"], lambda e, m=m, cb=cb: e.tensor_tensor(out=et0[:, :, 0:m], in0=Ere[:, :, 0:m], in1=cb, op=ALU.mult))
                op('v', ["E", "sp", "tmpB"], ["tmpB"], lambda e, m=m, sbb=sbb: e.tensor_tensor(out=et1[:, :, 0:m], in0=Eim[:, :, 0:m], in1=sbb, op=ALU.mult))
                op('v', ["tmpA", "tmpB", "E"], ["E"], lambda e, m=m: e.tensor_tensor(out=Ere[:, :, m:2 * m], in0=et0[:, :, 0:m], in1=et1[:, :, 0:m], op=ALU.subtract))
                op('v', ["E", "sp", "tmpA"], ["tmpA"], lambda e, m=m, sbb=sbb: e.tensor_tensor(out=et0[:, :, 0:m], in0=Ere[:, :, 0:m], in1=sbb, op=ALU.mult))
                op('v', ["E", "sp", "tmpB"], ["tmpB"], lambda e, m=m, cb=cb: e.tensor_tensor(out=et1[:, :, 0:m], in0=Eim[:, :, 0:m], in1=cb, op=ALU.mult))
                op('v', ["tmpA", "tmpB", "E"], ["E"], lambda e, m=m: e.tensor_tensor(out=Eim[:, :, m:2 * m], in0=et0[:, :, 0:m], in1=et1[:, :, 0:m], op=ALU.add))
                vv(T1, V2(CS2), V2(CS2), ALU.mult)
                vv(T2, V2(SN2), V2(SN2), ALU.mult)
                op('v', ["sp"], ["sp"], lambda e: e.scalar_tensor_tensor(out=V2(SN2), in0=V2(CS2), scalar=2.0, in1=V2(SN2), op0=ALU.mult, op1=ALU.mult))
                vv(CS2, V2(T1), V2(T2), ALU.subtract)

            sch.barrier()
            esp.close()
            mark("prep")

            PF = sb("PF", [128, 14, 129])
            Stp = sb("Stp", [128, 4, 128])
            cw = sb("cw", [128, 2, 16])
            op('v', [], ["PF"], lambda e: e.memset(PF[:], 0.0))
            op('v', [], ["Stp"], lambda e: e.memset(Stp[:], 0.0))
            op('v', [], ["cw0", "cw1", "cw2", "cw3"], lambda e: e.memset(cw[:], 0.0))

            xt = [sb("xt%d" % i, [128, D]) for i in range(2)]
            stat = sb("stat", [128, 8])
            xnF = sb("xnF", [128, 8, 128], BF16)
            wch = [sb("wch%d" % i, [128, 8, 128], BF16) for i in range(3)]
            L = sb("L", [128, 14, 128])
            uF = sb("uF", [128, 4, 128])
            uB = sb("uB", [128, 4, 128], BF16)
            gates = sb("gates", [128, 16, 128], BF16)
            lorain = sb("lorain", [128, 128], BF16)
            sgx = sb("sgx", [128, 128], BF16)
            TT = [SCR[:, i * 512:(i + 1) * 512].rearrange("p (a b) -> p a b", a=4) for i in range(16)]
            TK = ["T%d" % i for i in range(16)]
            (iSIG, iCS, iPT, iPTM, iPINV, iAV, iKK, iTMP, iKMOD, iRTL, iATL, iBTL, iKTL, iRKR, iY2, iX) = range(16)
            VT = sb("VT", [128, 512])
            BT = sb("BT", [128, 512])
            KT = sb("KT", [128, 512])
            gT = sb("gT", [128, 512])
            mats = {nm: sb("m_" + nm, [128, 4, 128]) for nm in ["X", "XT", "X2", "X2T", "QT", "akT", "rbT", "rkT"]}
            RHS = sb("RHS", [128, 512])
            SA = sb("SA", [128, 512])
            PTbd = sb("PTbd", [128, 4, 128])
            Y = sb("Y", [128, 512])
            gst = sb("gst", [128, 6, 8])
            bon = sb("bon", [128, 8])
            roF = sb("roF", [128, 4, 128], BF16)
            mixed = sb("mixed", [128, 8, 128])
            mixedB = sb("mixedB", [128, 8, 128], BF16)
            xre2 = [sb("xre%d" % i, [128, 4, 128], BF16) for i in range(2)]
            xim2 = [sb("xim%d" % i, [128, 4, 128], BF16) for i in range(2)]
            ygB = sb("ygB", [128, 4, 128], BF16)
            zg = sb("zg", [128, 8, 128])
            ST = sb("ST", [128, 8, 512])
            cwn2 = [sb("cwn%d" % i, [128, 4, 4]) for i in range(2)]

            w0 = pvec[:, 0, :]
            a0 = pvec[:, 1, :]
            k_k = pvec[:, 2, :]
            k_a = pvec[:, 3, :]
            r_k = pvec[:, 4, :]
            s5d = pvec[:, 5, :]
            S4 = [128, 4, 128]

            def tt(eng, o, ok, a, ak, b_, bk, alu):
                op(eng, [ak, bk], [ok], lambda e: e.tensor_tensor(out=o, in0=a, in1=b_, op=alu))

            dma('s', 'dx0', xt[0][:], x_d[0:128, :], [], ["xt0"])

            for it in range(NT):
                xb = it % 2
                xk = "xt%d" % xb
                xcur = xt[xb]
                if it + 1 < NT:
                    dma('s', 'dx%d' % (1 - xb), xt[1 - xb][:], x_d[(it + 1) * 128:(it + 2) * 128, :], [], ["xt%d" % (1 - xb)])
                op('a', [xk], [TK[iX], "stat"], lambda e: e.activation(out=SCR[:, iX * 512:iX * 512 + 512], in_=xcur[:, 0:512], func=AF.Square, accum_out=stat[:, 0:1]))
                op('a', [xk], [TK[iX], "stat"], lambda e: e.activation(out=SCR[:, iX * 512:iX * 512 + 512], in_=xcur[:, 512:1024], func=AF.Square, accum_out=stat[:, 3:4]))
                op('v', ["stat"], ["stat"], lambda e: e.tensor_tensor(out=stat[:, 0:1], in0=stat[:, 0:1], in1=stat[:, 3:4], op=ALU.add))
                op('a', ["stat"], ["stat"], lambda e: e.activation(out=stat[:, 1:2], in_=stat[:, 0:1], func=AF.Sqrt, bias=1e-6, scale=1.0 / D))
                op('v', ["stat"], ["stat"], lambda e: e.reciprocal(out=stat[:, 2:3], in_=stat[:, 1:2]))
                op('v', [xk, "stat"], [xk], lambda e: e.tensor_scalar(out=xcur[:], in0=xcur[:], scalar1=stat[:, 2:3], scalar2=None, op0=ALU.mult))
                mark("norm")
                for half in range(2):
                    pk = psk(half)

                    def tr(e, half=half):
                        ins = None
                        for q in range(4):
                            kt = half * 4 + q
                            ins = e.transpose(out=PS[half][:, q * 128:(q + 1) * 128], in_=xcur[:, kt * 128:(kt + 1) * 128], identity=ident[:])
                        return ins
                    op('p', [xk, "c"], [pk], tr)
                    if half == 0:
                        op('v', [pk], ["xnF"], lambda e: e.tensor_copy(out=xnF[:, 0:4, :], in_=P3(0)))
                    else:
                        op('a', [pk], ["xnF"], lambda e: e.activation(out=xnF[:, 4:8, :], in_=P3(1), func=AF.Copy))
                mark("xnF")
                for c in range(NCT):
                    mark("proj%d" % c)
                    wb = c % 3
                    wk = "wch%d" % wb
                    dma('s', 'dw%d' % wb, wch[wb][:].rearrange("p a b -> p (a b)"), wsc_d[c], ["wsc%d" % c], [wk])
                    pb = 2 + (c % 4)
                    pk = psk(pb)

                    def mm(e, wb=wb, pb=pb):
                        ins = None
                        for kt in range(8):
                            ins = e.matmul(PS[pb][:, 0:128], lhsT=wch[wb][:, kt, :], rhs=xnF[:, kt, :], start=(kt == 0), stop=(kt == 7))
                        return ins
                    op('p', [wk, "xnF"], [pk], mm)
                    if c < 14:
                        if c % 2 == 0:
                            op('v', [pk], ["PF"], lambda e, c=c, pb=pb: e.tensor_copy(out=PF[:, c, 1:129], in_=PS[pb][:, 0:128]))
                        else:
                            op('a', [pk], ["PF"], lambda e, c=c, pb=pb: e.activation(out=PF[:, c, 1:129], in_=PS[pb][:, 0:128], func=AF.Copy))
                    elif c < 18:
                        op('v', [pk], ["uF"], lambda e, c=c, pb=pb: e.tensor_copy(out=uF[:, c - 14, :], in_=PS[pb][:, 0:128]))
                        op('a', ["uF"], ["uB"], lambda e, c=c, pb=pb: e.activation(out=uB[:, c - 14, :], in_=uF[:, c - 14, :], func=AF.Copy))
                    else:
                        op('a', [pk, "c"], ["gates"], lambda e, c=c, pb=pb: e.activation(out=gates[:, c - 18, :], in_=PS[pb][:, 0:128], func=AF.Sigmoid, bias=bgate[:, c - 18:c - 17]))
                mark("proj")
                op('v', ["PF"], ["L"], lambda e: e.tensor_tensor(out=L[:], in0=PF[:, :, 0:128], in1=PF[:, :, 1:129], op=ALU.subtract))
                op('g', ["L", "c"], ["L"], lambda e: e.tensor_tensor(out=L[:], in0=L[:], in1=bc(mu[:], [128, 14, 128], 2), op=ALU.mult))
                op('v', ["L", "PF"], ["L"], lambda e: e.tensor_tensor(out=L[:], in0=L[:], in1=PF[:, :, 1:129], op=ALU.add))
                op('v', ["PF"], ["PF"], lambda e: e.tensor_copy(out=PF[:, :, 0:1], in_=PF[:, :, 128:129]))
                qR, qS = [], []
                opq[0] = qR
                rF = L[:, 0:4, :]
                kF = L[:, 4:8, :]
                vF = L[:, 8:12, :]
                sig, cs, Pt, Ptm1, Pinv, av, kk, tmp, kmod, rtl, atl, btl, ktl, rkr = [TT[i] for i in range(14)]
                op('a', ["L"], ["lorain"], lambda e: e.activation(out=lorain[0:64, :], in_=L[0:64, 12, :], func=AF.Tanh))
                op('v', ["L"], ["lorain"], lambda e: e.tensor_copy(out=lorain[64:128, :], in_=L[64:128, 12, :]))
                op('a', ["L"], ["sgx"], lambda e: e.activation(out=sgx[:], in_=L[:, 13, :], func=AF.Sigmoid))

                def mm_lw(e):
                    ins = None
                    for j in range(4):
                        ins = e.matmul(PS[0][:, j * 128:(j + 1) * 128], lhsT=lora[0:64, j * 128:(j + 1) * 128], rhs=lorain[0:64, :], start=True, stop=True)
                    return ins
                op('p', ["lora", "lorain"], [psk(0)], mm_lw)

                def mm_la(e):
                    ins = None
                    for j in range(4):
                        ins = e.matmul(PS[1][:, j * 128:(j + 1) * 128], lhsT=lora[64:128, j * 128:(j + 1) * 128], rhs=lorain[64:128, :], start=True, stop=True)
                    return ins
                op('p', ["lora", "lorain"], [psk(1)], mm_la)
                op('p', ["glora", "sgx"], [psk(6)], lambda e: e.matmul(PS[6][:], lhsT=sgx[:], rhs=glora[:], start=True, stop=True))
                for j in range(4):
                    op('a', [psk(0), "c"], [TK[iSIG]], lambda e, j=j: e.activation(out=sig[:, j, :], in_=PS[0][:, j * 128:(j + 1) * 128], func=AF.Sigmoid, bias=w0[:, j:j + 1]))
                    op('a', [psk(1), "c"], [TK[iAV]], lambda e, j=j: e.activation(out=av[:, j, :], in_=PS[1][:, j * 128:(j + 1) * 128], func=AF.Sigmoid, bias=a0[:, j:j + 1]))
                op('v', [psk(6)], ["gT"], lambda e: e.tensor_copy(out=gT[:], in_=PS[6][:]))
                for j in range(4):
                    op('v', [TK[iSIG], "c"], [TK[iCS]], lambda e, j=j: e.tensor_tensor_scan(out=cs[:, j, :], data0=scanmask, data1=sig[:, j, :], initial=0.0, op0=ALU.mult, op1=ALU.add))
                op('a', [TK[iCS]], [TK[iPT]], lambda e: e.activation(out=Pt, in_=cs, func=AF.Exp, scale=-C0))
                op('a', [TK[iCS]], [TK[iPINV]], lambda e: e.activation(out=Pinv, in_=cs, func=AF.Exp, scale=C0))
                tt('v', Ptm1, TK[iPTM], cs, TK[iCS], sig, TK[iSIG], ALU.subtract)
                op('a', [TK[iPTM]], [TK[iPTM]], lambda e: e.activation(out=Ptm1, in_=Ptm1, func=AF.Exp, scale=-C0))
                tt('g', kk, TK[iKK], kF, "L", bc(k_k, S4, 2), "c", ALU.mult)
                tt('g', tmp, TK[iTMP], kk, TK[iKK], kk, TK[iKK], ALU.mult)

                def mm_n(e):
                    ins = None
                    for j in range(4):
                        ins = e.matmul(PS[7][:, j * 128:(j + 1) * 128], lhsT=maskBD, rhs=tmp[:, j, :], start=True, stop=True)
                    return ins
                op('p', [TK[iTMP], "c"], [psk(7)], mm_n)
                op('a', [psk(7)], [TK[iTMP]], lambda e: e.activation(out=tmp, in_=P3(7), func=AF.Sqrt))
                op('v', [TK[iTMP]], [TK[iTMP]], lambda e: e.tensor_scalar(out=tmp, in0=tmp, scalar1=1e-12, scalar2=None, op0=ALU.max))
                op('v', [TK[iTMP]], [TK[iTMP]], lambda e: e.reciprocal(out=tmp, in_=tmp))
                tt('v', kk, TK[iKK], kk, TK[iKK], tmp, TK[iTMP], ALU.mult)
                tt('g', kmod, TK[iKMOD], av, TK[iAV], bc(k_a, S4, 2), "c", ALU.mult)
                tt('g', kmod, TK[iKMOD], kmod, TK[iKMOD], bc(k_a, S4, 2), "c", ALU.subtract)
                op('v', [TK[iKMOD], "L"], [TK[iKMOD]], lambda e: e.scalar_tensor_tensor(out=kmod, in0=kmod, scalar=1.0, in1=kF, op0=ALU.add, op1=ALU.mult))
                tt('v', rtl, TK[iRTL], rF, "L", Pt, TK[iPT], ALU.mult)
                op('v', [TK[iKK], TK[iPTM]], [TK[iATL]], lambda e: e.scalar_tensor_tensor(out=atl, in0=kk, scalar=-1.0, in1=Ptm1, op0=ALU.mult, op1=ALU.mult))
                tt('g', btl, TK[iBTL], kk, TK[iKK], av, TK[iAV], ALU.mult)
                tt('v', btl, TK[iBTL], btl, TK[iBTL], Pinv, TK[iPINV], ALU.mult)
                tt('v', ktl, TK[iKTL], kmod, TK[iKMOD], Pinv, TK[iPINV], ALU.mult)
                tt('g', rkr, TK[iRKR], rF, "L", kmod, TK[iKMOD], ALU.mult)
                tt('g', rkr, TK[iRKR], rkr, TK[iRKR], bc(r_k, S4, 2), "c", ALU.mult)
                op('v', [TK[iPT], "c"], ["PTbd"], lambda e: e.tensor_tensor(out=PTbd[:], in0=bc(maskBD, S4, 1), in1=Pt[:, :, 127:128].to_broadcast(S4), op=ALU.mult))

                def mm_b(e):
                    ins = None
                    for j in range(4):
                        ins = e.matmul(PS[7][:, 2 * j:2 * j + 2], lhsT=rkr[:, j, :], rhs=sel2[:], start=True, stop=True)
                    return ins
                op('p', [TK[iRKR], "c"], [psk(7)], mm_b)
                op('v', [psk(7)], ["bon"], lambda e: e.tensor_copy(out=bon[:], in_=PS[7][:, 0:8]))
                atl_m = [TT[0], TT[1]]
                rtl_m = [TT[2], TT[3]]
                for hh in range(2):
                    op('v' if hh == 0 else 'g', [TK[iATL], "c"], [TK[hh]], lambda e, hh=hh: e.tensor_scalar(out=atl_m[hh], in0=atl, scalar1=sel2[:, hh:hh + 1], scalar2=None, op0=ALU.mult))
                    op('v' if hh == 0 else 'g', [TK[iRTL], "c"], [TK[2 + hh]], lambda e, hh=hh: e.tensor_scalar(out=rtl_m[hh], in0=rtl, scalar1=sel2[:, hh:hh + 1], scalar2=None, op0=ALU.mult))
                mark("elem")
                for src, srckey, dst, dkey, pb in [(vF, "L", VT, "VT", 0), (btl, TK[iBTL], BT, "BT", 1), (ktl, TK[iKTL], KT, "KT", 6)]:
                    def trf(e, src=src, pb=pb):
                        ins = None
                        for j in range(4):
                            ins = e.transpose(out=PS[pb][:, j * 128:(j + 1) * 128], in_=src[:, j, :], identity=ident[:])
                        return ins
                    op('p', [srckey, "c"], [psk(pb)], trf)
                    if pb == 1:
                        op('a', [psk(pb)], [dkey], lambda e, dst=dst, pb=pb: e.activation(out=dst[:], in_=PS[pb][:], func=AF.Copy))
                    else:
                        op('v', [psk(pb)], [dkey], lambda e, dst=dst, pb=pb: e.tensor_copy(out=dst[:], in_=PS[pb][:]))
                mark("trans")
                for hg in range(2):
                    def hsl(t, hh, hg=hg):
                        h = hg * 4 + hh
                        return t[(h % 2) * 64:(h % 2) * 64 + 64, h // 2, :]
                    specs = [("X", "A", btl, maskSL, 2), ("XT", btl, "A", maskSU, 0),
                             ("akT", ktl, "A", maskSU, 1), ("rbT", btl, "R", maskUI, 6), ("rkT", ktl, "R", maskUI, 7)]

                    def pick(t, hl, hg=hg):
                        h = hg * 4 + hl
                        j, hh = h // 2, h % 2
                        if isinstance(t, str):
                            return (atl_m if t == "A" else rtl_m)[hh][:, j, :]
                        return t[:, j, :]
                    for nm, lt, rt, mk, pb in specs:
                        def mmT(e, lt=lt, rt=rt, pb=pb, pick=pick):
                            ins = None
                            for hl in range(4):
                                ins = e.matmul(PS[pb][:, hl * 128:(hl + 1) * 128], lhsT=pick(lt, hl), rhs=pick(rt, hl), start=True, stop=True)
                            return ins
                        op('p', [TK[0], TK[1], TK[2], TK[3], TK[iBTL], TK[iKTL]], [psk(pb)], mmT)
                        op('v', [psk(pb), "c"], ["m_" + nm], lambda e, nm=nm, mk=mk, pb=pb: e.tensor_tensor(
                            out=mats[nm][:], in0=P3(pb), in1=bc(mk, S4, 1), op=ALU.mult))
                        mark("mats_" + nm)
                    op('g', ["m_XT", "c"], ["m_QT"], lambda e: e.tensor_tensor(out=mats["QT"][:], in0=mats["XT"][:], in1=bc(ident[:], S4, 1), op=ALU.add))
                    mark("mats_qt")
                    cur, curT, nx, nxT = "X", "XT", "X2", "X2T"
                    for step in range(6):
                        def sq(e, a=curT, b_=cur):
                            ins = None
                            for hh in range(4):
                                ins = e.matmul(PS[2][:, hh * 128:(hh + 1) * 128], lhsT=mats[a][:, hh, :], rhs=mats[b_][:, hh, :], start=True, stop=True)
                            return ins
                        op('p', ["m_" + cur, "m_" + curT], [psk(2)], sq)
                        op('a', [psk(2)], ["m_" + nx], lambda e, nx=nx: e.activation(out=mats[nx][:], in_=P3(2), func=AF.Copy))
                        if step < 5:
                            def sqT(e, a=cur, b_=curT):
                                ins = None
                                for hh in range(4):
                                    ins = e.matmul(PS[0][:, hh * 128:(hh + 1) * 128], lhsT=mats[a][:, hh, :], rhs=mats[b_][:, hh, :], start=True, stop=True)
                                return ins
                            op('p', ["m_" + cur, "m_" + curT], [psk(0)], sqT)
                            op('v', [psk(0)], ["m_" + nxT], lambda e, nxT=nxT: e.tensor_copy(out=mats[nxT][:], in_=P3(0)))

                        def qu(e, a=nx):
                            ins = None
                            for hh in range(4):
                                ins = e.matmul(PS[1][:, hh * 128:(hh + 1) * 128], lhsT=mats[a][:, hh, :], rhs=mats["QT"][:, hh, :], start=True, stop=True)
                            return ins
                        op('p', ["m_" + nx, "m_QT"], [psk(1)], qu)
                        op('v', [psk(1), "m_QT"], ["m_QT"], lambda e: e.tensor_tensor(out=mats["QT"][:], in0=mats["QT"][:], in1=P3(1), op=ALU.add))
                        cur, curT, nx, nxT = nx, nxT, cur, curT
                        mark("mats_s%d" % step)
                    mark("mats")
                    c0 = hg * 256

                    def mm_rhs(e, hg=hg):
                        ins = None
                        for hl in range(4):
                            h = hg * 4 + hl
                            j = h // 2
                            hh = h % 2
                            reg = PS[6][:, hl * 64:(hl + 1) * 64]
                            e.matmul(reg, lhsT=atl[:, j, :], rhs=Stp[:, j, hh * 64:(hh + 1) * 64], start=True, stop=False)
                            ins = e.matmul(reg, lhsT=mats["akT"][:, hl, :], rhs=VT[:, h * 64:(h + 1) * 64], start=False, stop=True)
                        return ins
                    op('p', [TK[iATL], "Stp", "m_akT", "VT"], [psk(6)], mm_rhs)
                    op('v', [psk(6)], ["RHS"], lambda e, c0=c0: e.tensor_copy(out=RHS[:, c0:c0 + 256], in_=PS[6][:, 0:256]))

                    def mm_sa(e, hg=hg):
                        ins = None
                        for hl in range(4):
                            h = hg * 4 + hl
                            ins = e.matmul(PS[7][:, hl * 64:(hl + 1) * 64], lhsT=mats["QT"][:, hl, :], rhs=RHS[:, h * 64:(h + 1) * 64], start=True, stop=True)
                        return ins
                    op('p', ["m_QT", "RHS"], [psk(7)], mm_sa)
                    op('v', [psk(7)], ["SA"], lambda e, c0=c0: e.tensor_copy(out=SA[:, c0:c0 + 256], in_=PS[7][:, 0:256]))

                    def mm_y(e, hg=hg):
                        ins = None
                        for hl in range(4):
                            h = hg * 4 + hl
                            j = h // 2
                            hh = h % 2
                            reg = PS[2][:, hl * 64:(hl + 1) * 64]
                            e.matmul(reg, lhsT=rtl[:, j, :], rhs=Stp[:, j, hh * 64:(hh + 1) * 64], start=True, stop=False)
                            e.matmul(reg, lhsT=mats["rbT"][:, hl, :], rhs=SA[:, h * 64:(h + 1) * 64], start=False, stop=False)
                            ins = e.matmul(reg, lhsT=mats["rkT"][:, hl, :], rhs=VT[:, h * 64:(h + 1) * 64], start=False, stop=True)
                        return ins
                    op('p', [TK[iRTL], "Stp", "m_rbT", "m_rkT", "SA", "VT"], [psk(2)], mm_y)
                    op('a', [psk(2)], ["Y"], lambda e, c0=c0: e.activation(out=Y[:, c0:c0 + 256], in_=PS[2][:, 0:256], func=AF.Copy))

                    def mm_st(e, hg=hg):
                        ins = None
                        for jj in range(2):
                            j = hg * 2 + jj
                            reg = PS[0][:, jj * 128:(jj + 1) * 128]
                            e.matmul(reg, lhsT=ident[:], rhs=Stp[:, j, :], start=True, stop=False)
                            e.matmul(reg, lhsT=BT[:, j * 128:(j + 1) * 128], rhs=SA[:, j * 128:(j + 1) * 128], start=False, stop=False)
                            ins = e.matmul(reg, lhsT=KT[:, j * 128:(j + 1) * 128], rhs=VT[:, j * 128:(j + 1) * 128], start=False, stop=True)
                        return ins
                    op('p', ["Stp", "BT", "KT", "SA", "VT", "c"], [psk(0)], mm_st)
                    op('v', [psk(0), "PTbd"], ["Stp"], lambda e, hg=hg: e.tensor_tensor(
                        out=Stp[:, 2 * hg:2 * hg + 2, :], in0=PS[0][:, 0:256].rearrange("p (a b) -> p a b", a=2),
                        in1=PTbd[:, 2 * hg:2 * hg + 2, :], op=ALU.mult))
                mark("chain")
                Y3 = Y[:].rearrange("p (h v) -> p h v", h=8)
                Ysq = SCR[:, iY2 * 512:(iY2 + 1) * 512]
                G8 = [128, 8, 64]
                op('v', ["Y"], ["gst"], lambda e: e.tensor_reduce(out=gst[:, 0, :], in_=Y3, axis=AX.X, op=ALU.add))
                op('a', ["Y"], [TK[iY2]], lambda e: e.activation(out=Ysq, in_=Y[:], func=AF.Square))
                op('v', [TK[iY2]], ["gst"], lambda e: e.tensor_reduce(out=gst[:, 1, :], in_=Ysq.rearrange("p (h v) -> p h v", h=8), axis=AX.X, op=ALU.add))
                op('v', ["gst"], ["gst"], lambda e: e.tensor_scalar(out=gst[:, 2, :], in0=gst[:, 0, :], scalar1=1.0 / 64, scalar2=None, op0=ALU.mult))
                op('v', ["gst"], ["gst"], lambda e: e.tensor_tensor(out=gst[:, 3, :], in0=gst[:, 2, :], in1=gst[:, 2, :], op=ALU.mult))
                op('v', ["gst"], ["gst"], lambda e: e.scalar_tensor_tensor(out=gst[:, 4, :], in0=gst[:, 1, :], scalar=1.0 / 64, in1=gst[:, 3, :], op0=ALU.mult, op1=ALU.subtract))
                op('a', ["gst"], ["gst"], lambda e: e.activation(out=gst[:, 5, :], in_=gst[:, 4, :], func=AF.Sqrt, bias=GN_EPS, scale=1.0))
                op('v', ["gst"], ["gst"], lambda e: e.reciprocal(out=gst[:, 5, :], in_=gst[:, 5, :]))
                op('v', ["Y", "gst"], ["Y"], lambda e: e.tensor_tensor(out=Y3, in0=Y3, in1=bc(gst[:, 2, :], G8, 2), op=ALU.subtract))
                op('v', ["Y", "gst"], ["Y"], lambda e: e.tensor_tensor(out=Y3, in0=Y3, in1=bc(gst[:, 5, :], G8, 2), op=ALU.mult))
                op('g', ["Y", "c"], ["Y"], lambda e: e.tensor_tensor(out=Y[:], in0=Y[:], in1=lnw[:], op=ALU.mult))
                op('g', ["Y", "c"], ["Y"], lambda e: e.tensor_tensor(out=Y[:], in0=Y[:], in1=lnb[:], op=ALU.add))
                op('v', ["VT", "bon"], [TK[iY2]], lambda e: e.tensor_tensor(out=Ysq.rearrange("p (h v) -> p h v", h=8), in0=VT[:].rearrange("p (h v) -> p h v", h=8), in1=bc(bon[:], G8, 2), op=ALU.mult))
                op('v', ["Y", TK[iY2]], ["Y"], lambda e: e.tensor_tensor(out=Y[:], in0=Y[:], in1=Ysq, op=ALU.add))
                op('v', ["Y", "gT"], ["Y"], lambda e: e.tensor_tensor(out=Y[:], in0=Y[:], in1=gT[:], op=ALU.mult))

                def tr_ro(e):
                    ins = None
                    for j in range(4):
                        ins = e.transpose(out=PS[0][:, j * 128:(j + 1) * 128], in_=Y[:, j * 128:(j + 1) * 128], identity=ident[:])
                    return ins
                op('p', ["Y", "c"], [psk(0)], tr_ro)
                op('a', [psk(0)], ["roF"], lambda e: e.activation(out=roF[:], in_=P3(0), func=AF.Copy))
                for half in range(2):
                    pb = 1 + half

                    def mm_o(e, half=half, pb=pb):
                        ins = None
                        for q in range(4):
                            dt_ = half * 4 + q
                            for kt in range(4):
                                ins = e.matmul(PS[pb][:, q * 128:(q + 1) * 128], lhsT=w_o[:, kt, dt_ * 128:(dt_ + 1) * 128], rhs=roF[:, kt, :], start=(kt == 0), stop=(kt == 3))
                        return ins
                    op('p', ["w_o", "roF"], [psk(pb)], mm_o)
                    op('v', [psk(pb), "gates"], ["mixed"], lambda e, half=half, pb=pb: e.tensor_tensor(out=mixed[:, half * 4:(half + 1) * 4, :], in0=P3(pb), in1=gates[:, half * 4:(half + 1) * 4, :], op=ALU.mult))
                mark("epi")
                opq[0] = qS
                def s5_group(kt, ts):
                    s5a, s5b, s5c, s5d_, btre, btim, wre, wim = [ST[:, i, :].rearrange("p (a b) -> p a b", a=4) for i in range(8)]
                    ka, kb, kc, kd, kbr, kbi, kwr, kwi = ["ST%d" % i for i in range(8)]
                    pa, pb_ = 3, 4
                    xr, xi = xre2[ts], xim2[ts]
                    xrk, xik = "xre%d" % ts, "xim%d" % ts
                    cn = cwn2[ts]
                    cnk = "cwn%d" % ts
                    cwk = "cw%d" % kt
                    Er = Ere[:, 4 * kt:4 * kt + 4, :]
                    Ei = Eim[:, 4 * kt:4 * kt + 4, :]

                    def mm_bu(e):
                        ins = None
                        for q in range(4):
                            j = 4 * kt + q
                            e.matmul(PS[pa][:, q * 128:(q + 1) * 128], lhsT=BBre[:, j, :], rhs=uB[:, kt, :], start=True, stop=True)
                            ins = e.matmul(PS[pb_][:, q * 128:(q + 1) * 128], lhsT=BBim[:, j, :], rhs=uB[:, kt, :], start=True, stop=True)
                        return ins
                    op('p', ["BB", "uB"], [psk(pa), psk(pb_)], mm_bu)
                    yield
                    tt('v', s5a, ka, Er, "E", P3(pa), psk(pa), ALU.mult)
                    yield
                    tt('v', s5b, kb, Ei, "E", P3(pb_), psk(pb_), ALU.mult)
                    yield
                    tt('g', btre, kbr, s5a, ka, s5b, kb, ALU.add)
                    yield
                    tt('v', s5c, kc, Er, "E", P3(pb_), psk(pb_), ALU.mult)
                    yield
                    tt('v', s5d_, kd, Ei, "E", P3(pa), psk(pa), ALU.mult)
                    yield
                    tt('g', btim, kbi, s5c, kc, s5d_, kd, ALU.subtract)
                    yield
                    for q in range(4):
                        j = 4 * kt + q
                        op('v', [kbr, cwk, "sp"], [kwr], lambda e, q=q, j=j: e.tensor_tensor_scan(out=wre[:, q, :], data0=sp[:, LAB, j:j + 1].to_broadcast([128, 128]), data1=btre[:, q, :], initial=cw[:, 0, j:j + 1], op0=ALU.mult, op1=ALU.add))
                        yield
                        op('v', [kbi, cwk, "sp"], [kwi], lambda e, q=q, j=j: e.tensor_tensor_scan(out=wim[:, q, :], data0=sp[:, LAB, j:j + 1].to_broadcast([128, 128]), data1=btim[:, q, :], initial=cw[:, 1, j:j + 1], op0=ALU.mult, op1=ALU.add))
                        yield
                    c128 = sp[:, CS2, 4 * kt:4 * kt + 4]
                    s128 = sp[:, SN2, 4 * kt:4 * kt + 4]
                    wr127 = wre[:, :, 127]
                    wi127 = wim[:, :, 127]
                    tt('g', cn[:, 0, :], cnk, c128, "sp", wr127, kwr, ALU.mult)
                    yield
                    tt('g', cn[:, 1, :], cnk, s128, "sp", wi127, kwi, ALU.mult)
                    yield
                    tt('g', cn[:, 2, :], cnk, s128, "sp", wr127, kwr, ALU.mult)
                    yield
                    tt('g', cn[:, 3, :], cnk, c128, "sp", wi127, kwi, ALU.mult)
                    yield
                    tt('g', cw[:, 0, 4 * kt:4 * kt + 4], cwk, cn[:, 0, :], cnk, cn[:, 1, :], cnk, ALU.subtract)
                    yield
                    tt('g', cw[:, 1, 4 * kt:4 * kt + 4], cwk, cn[:, 2, :], cnk, cn[:, 3, :], cnk, ALU.add)
                    yield
                    tt('v', s5a, ka, Er, "E", wre, kwr, ALU.mult)
                    yield
                    tt('v', s5b, kb, Ei, "E", wim, kwi, ALU.mult)
                    yield
                    tt('g', xr[:], xrk, s5a, ka, s5b, kb, ALU.subtract)
                    yield
                    tt('v', s5c, kc, Ei, "E", wre, kwr, ALU.mult)
                    yield
                    tt('v', s5d_, kd, Er, "E", wim, kwi, ALU.mult)
                    yield
                    tt('g', xi[:], xik, s5c, kc, s5d_, kd, ALU.add)
                    yield

                    def mm_c(e):
                        ins = None
                        for q in range(4):
                            j = 4 * kt + q
                            e.matmul(PS[5][:, kt * 128:(kt + 1) * 128], lhsT=CCre[:, j, :], rhs=xr[:, q, :], start=(q == 0), stop=False)
                            ins = e.matmul(PS[5][:, kt * 128:(kt + 1) * 128], lhsT=CCimN[:, j, :], rhs=xi[:, q, :], start=False, stop=(q == 3))
                        return ins
                    op('p', ["CC", xrk, xik], [psk(5)], mm_c)
                    yield

                for kt_ in range(4):
                    for _ in s5_group(kt_, 0):
                        pass
                s5a, s5b = ST[:, 0, :].rearrange("p (a b) -> p a b", a=4), ST[:, 1, :].rearrange("p (a b) -> p a b", a=4)
                ka, kb = "ST0", "ST1"
                opq[0] = None
                nR, nS = len(qR), len(qS)
                iR = iS = 0
                while iR < nR or iS < nS:
                    if iS >= nS or (iR < nR and (SEQ_STREAMS or iR * nS <= iS * nR)):
                        sch.op(*qR[iR])
                        iR += 1
                    else:
                        sch.op(*qS[iS])
                        iS += 1
                tt('g', s5a, ka, uF[:], "uF", bc(s5d, S4, 2), "c", ALU.mult)
                tt('v', s5a, ka, s5a, ka, P3(5), psk(5), ALU.add)
                gelu_tanh(op, s5a, ka, s5b, kb, ygB[:], "ygB")
                for half in range(2):
                    for vg in range(2):
                        pb = 6 + vg

                        def mm_g(e, half=half, vg=vg, pb=pb):
                            ins = None
                            for q in range(4):
                                col = vg * 8 + half * 4 + q
                                for kt in range(4):
                                    ins = e.matmul(PS[pb][:, q * 128:(q + 1) * 128], lhsT=w_glu[:, kt, col * 128:(col + 1) * 128], rhs=ygB[:, kt, :], start=(kt == 0), stop=(kt == 3))
                            return ins
                        op('p', ["w_glu", "ygB"], [psk(pb)], mm_g)
                    zh = zg[:, half * 4:(half + 1) * 4, :]
                    op('a', [psk(7)], ["zg"], lambda e, zh=zh: e.activation(out=zh, in_=P3(7), func=AF.Sigmoid))
                    tt('v', zh, "zg", zh, "zg", P3(6), psk(6), ALU.mult)
                    tt('g', zh, "zg", zh, "zg", gates[:, 8 + half * 4:8 + (half + 1) * 4, :], "gates", ALU.mult)
                    tt('v', mixedB[:, half * 4:(half + 1) * 4, :], "mixedB", zh, "zg", mixed[:, half * 4:(half + 1) * 4, :], "mixed", ALU.add)
                mark("s5")
                wviews = [w_[:].rearrange("p a b -> p (a b)") for w_ in wch]
                for kt in range(8):
                    wb = (NCT + kt) % 3
                    wk = "wch%d" % wb
                    dma('s', 'dw%d' % wb, wviews[wb], wo_d[kt], [], [wk])

                    def mm_h(e, kt=kt, wb=wb):
                        e.matmul(PS[1][:], lhsT=mixedB[:, kt, :], rhs=wviews[wb][:, 0:512], start=(kt == 0), stop=(kt == 7))
                        return e.matmul(PS[2][:], lhsT=mixedB[:, kt, :], rhs=wviews[wb][:, 512:1024], start=(kt == 0), stop=(kt == 7))
                    op('p', [wk, "mixedB"], [psk(1), psk(2)], mm_h)
                for half in range(2):
                    pb = 1 + half
                    op('v', [psk(pb), xk, "stat"], [xk], lambda e, half=half, pb=pb: e.scalar_tensor_tensor(
                        out=xcur[:, half * 512:(half + 1) * 512], in0=xcur[:, half * 512:(half + 1) * 512], scalar=stat[:, 1:2], in1=PS[pb][:], op0=ALU.mult, op1=ALU.add))
                dma('s', 'dh1', h1_d[it * 128:(it + 1) * 128, :], xcur[:], [xk], ["h1d%d" % it])
            sch.barrier()
         except _Stop:
            sch.barrier()

        if 2 in phases:
          with ExitStack() as es:
            def sb(name, shape, dt=F32):
                return es.enter_context(nc.sbuf_tensor("s_" + name, shape, dt))
            NCV = 6
            RB = 4
            cin = [sb("cin%d" % i, [128, RB, D]) for i in range(NCV)]
            cout = [sb("cout%d" % i, [128, RB, D], BF16) for i in range(NCV)]
            n = 0
            for src_d, dst_d in [(pu_d, tuv_d[:, 0, :]), (pvv_d, tuv_d[:, 1, :])]:
                sv_ = src_d.rearrange("(c r p) d -> c p r d", p=128, r=RB)
                dv_ = dst_d.rearrange("(c r p) d -> c p r d", p=128, r=RB)
                for c in range(16384 // (128 * RB)):
                    b = n % NCV
                    n += 1
                    dma('s', 'dci%d' % b, cin[b][:], sv_[c], [], ["cin%d" % b])
                    if n % 3 == 0:
                        op('v', ["cin%d" % b], ["cout%d" % b], lambda e, b=b: e.tensor_copy(out=cout[b][:], in_=cin[b][:]))
                    elif n % 3 == 1:
                        op('a', ["cin%d" % b], ["cout%d" % b], lambda e, b=b: e.activation(out=cout[b][:], in_=cin[b][:], func=AF.Copy))
                    else:
                        op('g', ["cin%d" % b], ["cout%d" % b], lambda e, b=b: e.tensor_copy(out=cout[b][:], in_=cin[b][:]))
                    dma('a', 'dco%d' % b, dv_[c], cout[b][:], ["cout%d" % b], ["tb"])
            sch.barrier()

        if 2 in phases:
          with ExitStack() as es:
            def sb(name, shape, dt=F32):
                return es.enter_context(nc.sbuf_tensor("s_" + name, shape, dt))
            ident = sb("ident2", [128, 128])
            gffn = sb("gffn", [128, D])
            gfin = sb("gfin", [128, D])
            wq = sb("wq", [128, 8, D])
            subk = sb("subk", [128, 8, 128])
            h1t = [sb("h1t%d" % i, [128, D]) for i in range(2)]
            xn2 = [sb("xn2_0", [128, D])] * 2
            xn2b = [sb("xn2b_%d" % i, [128, D], BF16) for i in range(2)]
            junkb = sb("junkb", [128, D], BF16)
            xn2F = sb("xn2F", [128, 8, 128])
            qF = sb("qF", [128, 8, 128])
            sc = sb("sc", [128, 16, 128])
            scr = sb("scr", [128, 16, 128])
            iota16 = sb("iota16", [128, 16])
            eq = sb("eq", [128, 8, 16, 16])
            junk = eq[:].rearrange("p a b c -> p (a b c)")
            sv = sb("sv", [128, 16, 16])
            siu = sb("siu", [128, 16, 16], U32)
            sif = sb("sif", [128, 16, 16])
            dsi = sb("dsi", [128, 8, 16])
            cand = sb("cand", [128, 8, 256])
            cv = sb("cv", [128, 8, 16])
            ciu = sb("ciu", [128, 8, 16], U32)
            iiu = sb("iiu", [128, 8, 16], U32)
            jju = sb("jju", [128, 8, 16], U32)
            iif = sb("iif", [128, 8, 16])
            jjf = sb("jjf", [128, 8, 16])
            i1 = sb("i1", [128, 8, 16])
            i2 = sb("i2", [128, 8, 16])
            lt = sb("lt", [128, 8, 16])
            gtmp = sb("gtmp", [128, 128])
            ei = [sb("ei%d" % i, [128, 128], I32) for i in range(2)]
            gate = [sb("gate%d" % i, [128, 8, 16]) for i in range(2)]
            sm = sb("sm", [128, 4, 8])
            hid = sb("hid", [128, 128])
            wgt = [sb("wgt%d" % i, [128, 128]) for i in range(2)]
            statA = sb("statA", [128, 8])
            statV = sb("statV", [128, 8])
            NDG = 4
            dg = [sb("dg%d" % i, [128, 128], BF16) for i in range(NDG)]
            NR = 20
            UV = [sb("UV%d" % i, [128, 2, D], BF16) for i in range(NR)]
            hid2 = [sb("hid%d" % i, [128, 128]) for i in range(2)]
            gt2 = [sb("gtg%d" % i, [128, 8]) for i in range(3)]
            h2 = sb("h2", [128, D])
            outt = sb("outt", [128, D])

            dma('s', 'dc2', ident[:], ident_d, [], ["c"])
            dma('s', 'dc2', gffn[:], gffn_d, [], ["c"])
            dma('s', 'dc2', iota16[:], iota_d, [], ["c"])
            dma('s', 'dc2', gfin[:], gfin_d, [], ["c"])
            dma('s', 'dc2', subk[:].rearrange("p a b -> p (a b)"), subk_d, [], ["c"])
            dma('s', 'dc2', wq[:], wq_d.rearrange("(kt p) c -> p kt c", p=128), [], ["c"])

            def rms(src, srck, dstat, dk, op=op):
                op('a', [srck], ["eq", dk], lambda e: e.activation(out=junk[:, 0:512], in_=src[:, 0:512], func=AF.Square, accum_out=dstat[:, 0:1]))
                op('a', [srck], ["eq", dk], lambda e: e.activation(out=junk[:, 512:1024], in_=src[:, 512:1024], func=AF.Square, accum_out=dstat[:, 3:4]))
                op('v', [dk], [dk], lambda e: e.tensor_tensor(out=dstat[:, 0:1], in0=dstat[:, 0:1], in1=dstat[:, 3:4], op=ALU.add))
                op('a', [dk], [dk], lambda e: e.activation(out=dstat[:, 1:2], in_=dstat[:, 0:1], func=AF.Sqrt, bias=1e-6, scale=1.0 / D))
                op('v', [dk], [dk], lambda e: e.reciprocal(out=dstat[:, 2:3], in_=dstat[:, 1:2]))

            def top16_multi(segs, segk, width, vouts, iouts, okeys):
                n = len(segs)
                scrv = [scr[:].rearrange("p a b -> p (a b)")[:, i * width:(i + 1) * width] for i in range(n)]
                sk = ["scr%d" % i for i in range(n)]
                for i in range(n):
                    qop('v', [segk], [okeys[i]], lambda e, i=i: e.max(out=vouts[i][:, 0:8], in_=segs[i]))
                for i in range(n):
                    qop('v', [segk, okeys[i]], [okeys[i]], lambda e, i=i: e.max_index(out=iouts[i][:, 0:8], in_max=vouts[i][:, 0:8], in_values=segs[i]))
                for i in range(n):
                    qop('v', [segk, okeys[i]], [sk[i]], lambda e, i=i: e.match_replace(out=scrv[i], in_to_replace=vouts[i][:, 0:8], in_values=segs[i], imm_value=NEG))
                for i in range(n):
                    qop('v', [sk[i]], [okeys[i]], lambda e, i=i: e.max(out=vouts[i][:, 8:16], in_=scrv[i]))
                for i in range(n):
                    qop('v', [sk[i], okeys[i]], [okeys[i]], lambda e, i=i: e.max_index(out=iouts[i][:, 8:16], in_max=vouts[i][:, 8:16], in_values=scrv[i]))

            gcount = [0, 0, 0]

            Aq = []

            def qop(*a):
                Aq.append(('op', a))

            def qdma(*a, **k):
                Aq.append(('dma', a, k))

            def drain(n=None):
                k = 0
                while Aq and (n is None or k < n):
                    item = Aq.pop(0)
                    if item[0] == 'op':
                        op(*item[1])
                    else:
                        dma(*item[1], **item[2])
                    k += 1

            def stage_A(it):
                p = it % 2
                hk = "h1t%d" % p
                hcur = h1t[p]
                xk = "xn2_0"
                xc = xn2[0]
                qdma('s', 'dh%d' % p, hcur[:], h1_d[it * 128:(it + 1) * 128, :], ["h1d%d" % it], [hk])
                rms(hcur, hk, statA, "statA", op=qop)
                qop('v', [hk, "statA", "c"], [xk], lambda e: e.scalar_tensor_tensor(out=xc[:], in0=hcur[:], scalar=statA[:, 2:3], in1=gffn[:], op0=ALU.mult, op1=ALU.mult))
                qop('a', [xk], ["xn2b_%d" % p], lambda e: e.activation(out=xn2b[p][:], in_=xc[:], func=AF.Copy))
                for half in range(2):
                    def tr2(e, half=half):
                        ins = None
                        for q in range(4):
                            kt = half * 4 + q
                            ins = e.transpose(out=PS[half][:, q * 128:(q + 1) * 128], in_=xc[:, kt * 128:(kt + 1) * 128], identity=ident[:])
                        return ins
                    qop('p', [xk, "c"], [psk(half)], tr2)
                    if half == 0:
                        qop('v', [psk(0)], ["xn2F"], lambda e: e.tensor_copy(out=xn2F[:, 0:4, :], in_=P3(0)))
                    else:
                        qop('a', [psk(1)], ["xn2F"], lambda e: e.activation(out=xn2F[:, 4:8, :], in_=P3(1), func=AF.Copy))
                for half in range(2):
                    pb = 2 + half

                    def mm_q(e, half=half, pb=pb):
                        ins = None
                        for q in range(4):
                            ct = half * 4 + q
                            for kt in range(8):
                                ins = e.matmul(PS[pb][:, q * 128:(q + 1) * 128], lhsT=wq[:, kt, ct * 128:(ct + 1) * 128], rhs=xn2F[:, kt, :], start=(kt == 0), stop=(kt == 7))
                        return ins
                    qop('p', ["c", "xn2F"], [psk(pb)], mm_q)
                    if half == 0:
                        qop('v', [psk(pb)], ["qF"], lambda e, pb=pb: e.tensor_copy(out=qF[:, 0:4, :], in_=P3(pb)))
                    else:
                        qop('a', [psk(pb)], ["qF"], lambda e, pb=pb: e.activation(out=qF[:, 4:8, :], in_=P3(pb), func=AF.Copy))
                sc4 = sc[:].rearrange("p (h c) n -> p h c n", c=2)
                for hg in range(2):
                    for c in range(2):
                        pb = 4 + c

                        def mm_s(e, hg=hg, c=c, pb=pb):
                            ins = None
                            for q in range(4):
                                h = hg * 4 + q
                                ins = e.matmul(PS[pb][:, q * 128:(q + 1) * 128], lhsT=qF[c * 64:(c + 1) * 64, h, :], rhs=subk[c * 64:(c + 1) * 64, h, :], start=True, stop=True)
                            return ins
                        qop('p', ["qF", "c"], [psk(pb)], mm_s)
                        if c == 0:
                            qop('v', [psk(pb)], ["sc"], lambda e, hg=hg, c=c, pb=pb: e.tensor_copy(out=sc4[:, hg * 4:(hg + 1) * 4, c, :], in_=P3(pb)))
                        else:
                            qop('a', [psk(pb)], ["sc"], lambda e, hg=hg, c=c, pb=pb: e.activation(out=sc4[:, hg * 4:(hg + 1) * 4, c, :], in_=P3(pb), func=AF.Copy))

            svk = ["svi%d" % i for i in range(16)]
            cvk = ["cvi%d" % i for i in range(8)]

            def stage_A2(it):
                p = it % 2
                top16_multi([sc[:, i, :] for i in range(16)], "sc", 128, [sv[:, i, :] for i in range(16)], [siu[:, i, :] for i in range(16)], svk)
                for h in range(8):
                    qop('v', [svk[2 * h], svk[2 * h + 1]], ["cand"], lambda e, h=h: e.tensor_tensor(
                        out=cand[:, h, :].rearrange("p (i j) -> p i j", i=16),
                        in0=bc(sv[:, 2 * h, :], [128, 16, 16], 2), in1=bc(sv[:, 2 * h + 1, :], [128, 16, 16], 1), op=ALU.add))
                top16_multi([cand[:, h, :] for h in range(8)], "cand", 256, [cv[:, h, :] for h in range(8)], [ciu[:, h, :] for h in range(8)], cvk)
                qop('v', cvk, ["iiu"], lambda e: e.tensor_single_scalar(out=iiu[:], in_=ciu[:], scalar=4, op=ALU.logical_shift_right))
                qop('v', cvk, ["jju"], lambda e: e.tensor_single_scalar(out=jju[:], in_=ciu[:], scalar=15, op=ALU.bitwise_and))
                qop('v', ["iiu"], ["iif"], lambda e: e.tensor_copy(out=iif[:], in_=iiu[:]))
                qop('v', ["jju"], ["jjf"], lambda e: e.tensor_copy(out=jjf[:], in_=jju[:]))
                qop('v', svk, ["sif"], lambda e: e.tensor_copy(out=sif[:], in_=siu[:]))
                sif4 = sif[:].rearrange("p (h c) k -> p h c k", c=2)
                E4 = [128, 8, 16, 16]
                iota4 = iota16[:].unsqueeze(1).unsqueeze(1).to_broadcast(E4)
                for (idxf, idk, c_, dst, dk) in [(iif, "iif", 0, i1, "i1"), (jjf, "jjf", 1, i2, "i2")]:
                    qop('v', [idk, "c", "eq"], ["eq"], lambda e, idxf=idxf: e.tensor_tensor(out=eq[:], in0=idxf[:].unsqueeze(3).to_broadcast(E4), in1=iota4, op=ALU.is_equal))
                    qop('v', ["eq", "sif"], ["eq"], lambda e, c_=c_: e.tensor_tensor(out=eq[:], in0=eq[:], in1=sif4[:, :, c_, :].unsqueeze(2).to_broadcast(E4), op=ALU.mult))
                    qop('v', ["eq"], [dk], lambda e, dst=dst: e.tensor_reduce(out=dst[:], in_=eq[:], axis=AX.X, op=ALU.add))
                qop('v', ["i1", "i2"], ["i1"], lambda e: e.scalar_tensor_tensor(out=i1[:], in0=i1[:], scalar=128.0, in1=i2[:], op0=ALU.mult, op1=ALU.add))
                qop('v', ["i1"], ["ei%d" % p], lambda e: e.tensor_copy(out=ei[p][:], in_=i1[:].rearrange("p h k -> p (h k)")))

            def stage_A3(it):
                p = it % 2
                gk = "gate%d" % p
                g_ = gate[p]
                qop('v', cvk, ["sm"], lambda e: e.tensor_reduce(out=sm[:, 0, :], in_=cv[:], axis=AX.X, op=ALU.max))
                qop('v', cvk + ["sm"], [gk], lambda e: e.tensor_tensor(out=g_[:], in0=cv[:], in1=bc(sm[:, 0, :], [128, 8, 16], 2), op=ALU.subtract))
                qop('a', [gk], [gk], lambda e: e.activation(out=g_[:], in_=g_[:], func=AF.Exp))
                qop('v', [gk], ["sm"], lambda e: e.tensor_reduce(out=sm[:, 1, :], in_=g_[:], axis=AX.X, op=ALU.add))
                qop('v', ["sm"], ["sm"], lambda e: e.reciprocal(out=sm[:, 2, :], in_=sm[:, 1, :]))
                qop('v', [gk, "sm"], [gk], lambda e: e.tensor_tensor(out=g_[:], in0=g_[:], in1=bc(sm[:, 2, :], [128, 8, 16], 2), op=ALU.mult))

            GS = 4
            NGR = 128 // GS
            slot_buf = {}

            def dots(it, g):
                p = it % 2
                hd = hid2[p]
                hk_ = "hid%d" % p
                if g == 0:
                    op('v', [], [hk_], lambda e: e.memset(hd[:], 0.0))
                for s_ in range(g * GS, (g + 1) * GS):
                    b = gcount[0] % NR
                    gcount[0] += 1
                    slot_buf[(it, s_)] = b
                    dma('g', 'duv%d' % b, UV[b][:].rearrange("p a d -> p (a d)"), tuv_d.rearrange("e a d -> e (a d)"), ["ei%d" % p], ["UV%d" % b],
                        in_offset=bass.IndirectOffsetOnAxis(ap=ei[p][:, s_:s_ + 1], axis=0))
                    op('v', ["UV%d" % b, "xn2b_%d" % p], ["junkb", hk_], lambda e, b=b, s_=s_: e.scalar_tensor_tensor(
                        out=junkb[:], in0=UV[b][:, 0, :], scalar=1.0, in1=xn2b[p][:], op0=ALU.mult, op1=ALU.mult, accum_out=hd[:, s_:s_ + 1]))

            def weights1(it, g):
                p = it % 2
                hd = hid2[p]
                hk_ = "hid%d" % p
                q = g % 3
                sl = slice(g * GS, (g + 1) * GS)
                src = hd[:, sl]
                tmp = gt2[q][:, 0:GS]
                tk_ = "gtg%d" % q
                op('v', [hk_], [tk_], lambda e: e.tensor_tensor(out=tmp, in0=src, in1=src, op=ALU.mult))
                op('v', [tk_], [tk_], lambda e: e.tensor_scalar(out=tmp, in0=tmp, scalar1=0.044715, scalar2=1.0, op0=ALU.mult, op1=ALU.add))
                op('v', [tk_, hk_], [tk_], lambda e: e.tensor_tensor(out=tmp, in0=tmp, in1=src, op=ALU.mult))
                op('a', [tk_], [tk_], lambda e: e.activation(out=tmp, in_=tmp, func=AF.Sigmoid, scale=1.5957691216057308))

            def weights2(it, g):
                p = it % 2
                hd = hid2[p]
                hk_ = "hid%d" % p
                wk = "wgt%d" % p
                q = g % 3
                sl = slice(g * GS, (g + 1) * GS)
                tmp = gt2[q][:, 0:GS]
                tk_ = "gtg%d" % q
                op('v', [tk_, hk_], [wk], lambda e: e.tensor_tensor(out=wgt[p][:, sl], in0=tmp, in1=hd[:, sl], op=ALU.mult))
                op('v', [wk, "gate%d" % p], [wk], lambda e: e.tensor_tensor(out=wgt[p][:, sl], in0=wgt[p][:, sl], in1=gate[p][:].rearrange("p h k -> p (h k)")[:, sl], op=ALU.mult))

            def accum(it, g):
                p = it % 2
                wk = "wgt%d" % p
                for s_ in range(g * GS, (g + 1) * GS):
                    b = slot_buf.pop((it, s_))
                    r = gcount[2] % NDG
                    gcount[2] += 1
                    op('a', [wk, "c"], ["dg%d" % r], lambda e, r=r, s_=s_: e.activation(out=dg[r][:], in_=ident[:], func=AF.Copy, scale=wgt[p][:, s_:s_ + 1]))

                    def mm_v(e, b=b, r=r, s_=s_):
                        e.matmul(PS[6][:], lhsT=dg[r][:], rhs=UV[b][:, 1, 0:512], start=(s_ == 0), stop=(s_ == 127))
                        return e.matmul(PS[7][:], lhsT=dg[r][:], rhs=UV[b][:, 1, 512:1024], start=(s_ == 0), stop=(s_ == 127))
                    op('p', ["dg%d" % r, "UV%d" % b], [psk(6), psk(7)], mm_v)

            def stage_Vf(it):
                p = it % 2
                hk = "h1t%d" % p
                hcur = h1t[p]
                op('v', [psk(6), hk], ["h2"], lambda e: e.tensor_tensor(out=h2[:, 0:512], in0=PS[6][:], in1=hcur[:, 0:512], op=ALU.add))
                op('v', [psk(7), hk], ["h2"], lambda e: e.tensor_tensor(out=h2[:, 512:1024], in0=PS[7][:], in1=hcur[:, 512:1024], op=ALU.add))
                rms(h2, "h2", statV, "statV")
                op('v', ["h2", "statV", "c"], ["outt"], lambda e: e.scalar_tensor_tensor(out=outt[:], in0=h2[:], scalar=statV[:, 2:3], in1=gfin[:], op0=ALU.mult, op1=ALU.mult))
                dma('s', 'dout', out_d[it * 128:(it + 1) * 128, :], outt[:], ["outt"], ["od%d" % it])

            stage_A(0)
            stage_A2(0)
            stage_A3(0)
            drain()
            hist = []
            def retire(k):
                it_, g_ = hist[k]
                weights2(it_, g_)
                accum(it_, g_)
                if g_ == NGR - 1:
                    stage_Vf(it_)
            for it in range(NT):
                per = 0
                if it + 1 < NT:
                    stage_A(it + 1)
                    stage_A2(it + 1)
                    stage_A3(it + 1)
                    per = (len(Aq) + NGR - 9) // (NGR - 8)
                for g in range(NGR):
                    dots(it, g)
                    hist.append((it, g))
                    if len(hist) >= 3:
                        retire(-3)
                    if len(hist) >= 2:
                        weights1(*hist[-2])
                    if per and g >= 2:
                        drain(per)
                drain()
            retire(-2)
            weights1(*hist[-1])
            retire(-1)
            sch.barrier()
    return nc


def make_inputs(inp, b):
    f = lambda a: np.ascontiguousarray(np.asarray(a), dtype=np.float32)
    colT = lambda v, n: f(np.asarray(v).reshape(n, 128).T)
    m = {}
    m["x"] = f(inp["x"][b])
    m["w_in"] = f(inp["w_in"][0])
    m["gainP"] = colT(inp["norm_mix"][0], 8)
    m["bgate"] = colT(inp["b_gate"][0], 16)
    m["mu"] = colT(inp["mu_rwkv"][0], 14)
    pv = np.stack([colT(inp["w0"][0], 4), colT(inp["a0"][0], 4), colT(inp["k_k"][0], 4), colT(inp["k_a"][0], 4),
                   colT(np.asarray(inp["r_k"][0]).reshape(512), 4), colT(np.asarray(inp["s5_d"][0]).reshape(512), 4)], axis=1)
    m["pvec"] = f(pv)
    m["lora"] = f(np.concatenate([np.asarray(inp["w_lora_up"][0]), np.asarray(inp["a_lora_up"][0])], axis=0))
    m["glora"] = f(inp["g_lora_up"][0])
    m["lnw"] = f(np.broadcast_to(np.asarray(inp["ln_x_w"][0])[None, :], (128, 512)))
    m["lnb"] = f(np.broadcast_to(np.asarray(inp["ln_x_b"][0])[None, :], (128, 512)))
    m["w_o"] = f(inp["w_o_rwkv"][0])
    m["w_glu"] = f(inp["w_glu_s5"][0])
    m["w_out"] = f(inp["w_out"][0])
    a_re = np.asarray(inp["s5_a_re"][0]).reshape(16, 128).T
    a_im = np.asarray(inp["s5_a_im"][0]).reshape(16, 128).T
    ldt = np.repeat(np.asarray(inp["s5_log_dt"][0]), 64).reshape(16, 128).T
    m["s5p"] = f(np.stack([a_re, a_im, ldt], axis=1))
    bbre = np.zeros((128, 16, 128), np.float32)
    bbim = np.zeros((128, 16, 128), np.float32)
    ccre = np.zeros((128, 16, 128), np.float32)
    ccim = np.zeros((128, 16, 128), np.float32)
    b_re = np.asarray(inp["s5_b_re"][0]); b_im = np.asarray(inp["s5_b_im"][0])
    c_re = np.asarray(inp["s5_c_re"][0]); c_im = np.asarray(inp["s5_c_im"][0])
    for g in range(32):
        j, gl, g8 = g // 2, g % 2, g % 8
        bbre[g8 * 16:(g8 + 1) * 16, j, gl * 64:(gl + 1) * 64] = b_re[g].T
        bbim[g8 * 16:(g8 + 1) * 16, j, gl * 64:(gl + 1) * 64] = b_im[g].T
        ccre[gl * 64:(gl + 1) * 64, j, g8 * 16:(g8 + 1) * 16] = c_re[g].T
        ccim[gl * 64:(gl + 1) * 64, j, g8 * 16:(g8 + 1) * 16] = c_im[g].T
    m["bbre"] = bbre.reshape(128, 2048)
    m["bbim"] = bbim.reshape(128, 2048)
    m["ccre"] = ccre.reshape(128, 2048)
    m["ccim"] = ccim.reshape(128, 2048)
    m["gffn"] = f(np.broadcast_to(np.asarray(inp["norm_ffn"][0])[None, :], (128, D)))
    m["gfin"] = f(np.broadcast_to(np.asarray(inp["norm_final"])[None, :], (128, D)))
    m["wq"] = f(inp["peer_wq"][0])
    m["subk"] = f(np.asarray(inp["peer_subkeys"][0]).transpose(1, 3, 0, 2).reshape(128, 1024))
    m["peer_u"] = f(inp["peer_u"][0])
    m["peer_v"] = f(inp["peer_v"][0])
    m["ident"] = np.eye(128, dtype=np.float32)
    p = np.arange(128)[:, None]
    jx = np.arange(128)[None, :]
    masks = np.zeros((128, 5, 128), np.float32)
    masks[:, 0] = (jx < p)
    masks[:, 1] = (jx > p)
    masks[:, 2] = (jx >= p)
    masks[:, 3] = ((jx // 64) == (p // 64))
    masks[:, 4] = 1.0
    masks[:, 4, 0] = 0.0
    m["masks"] = masks
    sel2 = np.zeros((128, 2), np.float32)
    sel2[:64, 0] = 1.0
    sel2[64:, 1] = 1.0
    m["sel2"] = sel2
    m["iota16"] = np.ascontiguousarray(np.broadcast_to(np.arange(16, dtype=np.float32)[None, :], (128, 16)))
    return m


_NC_CACHE = {}


def kernel(**inputs):
    n = 8
    if "nc" not in _NC_CACHE:
        _NC_CACHE["nc"] = build_nc()
    nc = _NC_CACHE["nc"]
    in_maps = [make_inputs(inputs, b) for b in range(n)]
    res = run_bass_kernel_spmd(nc, in_maps, core_ids=list(range(n)))
    out = np.stack([np.asarray(r["out"], dtype=np.float32) for r in res.results], axis=0)
    return out
```

```python
import numpy as np
from contextlib import ExitStack
import concourse.bass as bass
import concourse.mybir as mybir
from concourse.bass_utils import run_bass_kernel_spmd

F32 = mybir.dt.float32
BF16 = mybir.dt.bfloat16
I32 = mybir.dt.int32
U32 = mybir.dt.uint32
ALU = mybir.AluOpType
AF = mybir.ActivationFunctionType
AX = mybir.AxisListType

D = 1024
NRW = 1792
NCOL = 4352
SEQ = 4096
NCT = 34
C0 = float(np.exp(-0.5))
PI = float(np.pi)
GN_EPS = 64e-5
NB = 16
NEG = -1.0e30
SEQ_STREAMS = False


class Sched:
    def __init__(self, nc, es):
        self.nc = nc
        self.es = es
        self.engs = {'v': nc.vector, 'a': nc.scalar, 'p': nc.tensor, 'g': nc.gpsimd, 's': nc.sync}
        self.sems = {}
        self.val = {}
        self.waited = {e: {} for e in self.engs}
        self.lastw = {}
        self.readers = {}
        self.nins = 0
        for e in 'vapg':
            self._mk(e)

    def _mk(self, key):
        self.sems[key] = self.es.enter_context(self.nc.semaphore('sem_' + key))
        self.val[key] = 0

    def _wait(self, e, k, v):
        if self.waited[e].get(k, 0) >= v:
            return
        self.engs[e].wait_ge(self.sems[k], v)
        self.waited[e][k] = v

    def _deps(self, e, reads, writes):
        for b in reads:
            if b in self.lastw:
                self._wait(e, *self.lastw[b])
        for b in writes:
            if b in self.lastw:
                self._wait(e, *self.lastw[b])
            for k, v in self.readers.get(b, {}).items():
                self._wait(e, k, v)

    def _commit(self, tok, reads, writes):
        k, v = tok
        for b in reads:
            self.readers.setdefault(b, {})[k] = v
        for b in writes:
            self.lastw[b] = tok
            self.readers[b] = {}

    def op(self, e, reads, writes, fn):
        self._deps(e, reads, writes)
        ins = fn(self.engs[e])
        self.val[e] += 1
        ins.then_inc(self.sems[e], 1)
        self._commit((e, self.val[e]), reads, writes)
        self.nins += 1

    def dma(self, q, semkey, out, in_, reads, writes, in_offset=None):
        if semkey not in self.sems:
            self._mk(semkey)
        self._deps(q, reads, writes)
        if self.val[semkey] > 0:
            self._wait(q, semkey, self.val[semkey])
        eng = self.engs[q]
        if in_offset is not None:
            ins = eng.indirect_dma_start(out=out, out_offset=None, in_=in_, in_offset=in_offset)
        else:
            ins = eng.dma_start(out=out, in_=in_)
        self.val[semkey] += 16
        ins.then_inc(self.sems[semkey], 16)
        self._commit((semkey, self.val[semkey]), reads, writes)
        self.nins += 1

    def barrier(self):
        for e in self.engs:
            for k, v in self.val.items():
                if v > 0:
                    self._wait(e, k, v)
        self.lastw = {}
        self.readers = {}


def bc(ap, shape, axis):
    return ap.unsqueeze(axis).to_broadcast(shape)


def gelu_tanh(op, src, srck, tmp, tmpk, dst, dstk, sq_eng='g'):
    op(sq_eng, [srck], [tmpk], lambda e: e.tensor_tensor(out=tmp, in0=src, in1=src, op=ALU.mult))
    op('v', [tmpk], [tmpk], lambda e: e.tensor_scalar(out=tmp, in0=tmp, scalar1=0.044715, scalar2=1.0, op0=ALU.mult, op1=ALU.add))
    op('v', [tmpk, srck], [tmpk], lambda e: e.tensor_tensor(out=tmp, in0=tmp, in1=src, op=ALU.mult))
    op('a', [tmpk], [tmpk], lambda e: e.activation(out=tmp, in_=tmp, func=AF.Sigmoid, scale=1.5957691216057308))
    op('v', [tmpk, srck], [dstk], lambda e: e.tensor_tensor(out=dst, in0=tmp, in1=src, op=ALU.mult))


class _Stop(Exception):
    pass


def build_nc(NT=32, debug=False, phases=(1, 2), stop=None):
    nc = bass.Bass("TRN2", target_bir_lowering=False)

    def mark(name):
        if stop == name:
            raise _Stop()

    def din(name, shape, dt=F32):
        return nc.dram_tensor(name, shape, dt, kind="ExternalInput").ap()

    x_d = din("x", [SEQ, D])
    w_in_d = din("w_in", [D, NCOL])
    gainP_d = din("gainP", [128, 8])
    bgate_d = din("bgate", [128, 16])
    mu_d = din("mu", [128, 14])
    pv_d = din("pvec", [128, 6, 4])
    lora_d = din("lora", [128, 512])
    glora_d = din("glora", [128, 512])
    lnw_d = din("lnw", [128, 512])
    lnb_d = din("lnb", [128, 512])
    w_o_d = din("w_o", [512, D])
    w_glu_d = din("w_glu", [512, 2 * D])
    w_out_d = din("w_out", [D, D])
    s5p_d = din("s5p", [128, 3, 16])
    bbre_d = din("bbre", [128, 2048])
    bbim_d = din("bbim", [128, 2048])
    ccre_d = din("ccre", [128, 2048])
    ccim_d = din("ccim", [128, 2048])
    gffn_d = din("gffn", [128, D])
    gfin_d = din("gfin", [128, D])
    wq_d = din("wq", [D, D])
    subk_d = din("subk", [128, 1024])
    pu_d = din("peer_u", [16384, D])
    pvv_d = din("peer_v", [16384, D])
    ident_d = din("ident", [128, 128])
    masks_d = din("masks", [128, 5, 128])
    sel2_d = din("sel2", [128, 2])
    iota_d = din("iota16", [128, 16])
    out_d = nc.dram_tensor("out", [SEQ, D], F32, kind="ExternalOutput").ap()
    h1_d = nc.dram_tensor("h1s", [SEQ, D], F32, kind="ExternalOutput" if debug else "Internal").ap()
    wsc_d = nc.dram_tensor("wsc", [NCT, 128, 1024], BF16, kind="Internal").ap()
    wo_d = nc.dram_tensor("wosc", [8, 128, 1024], BF16, kind="Internal").ap()
    tuv_d = nc.dram_tensor("tuv", [16384, 2, D], BF16, kind="Internal").ap()

    with ExitStack() as es0:
        sch = Sched(nc, es0)
        op = sch.op
        dma = sch.dma
        PS = [es0.enter_context(nc.psum_tensor("ps%d" % i, [128, 512], F32)) for i in range(8)]

        def psk(i):
            return "ps%d" % i

        def P3(i):
            return PS[i][:].rearrange("p (a b) -> p a b", a=4)

        if 1 in phases:
         try:
          with ExitStack() as es:
            def sb(name, shape, dt=F32):
                return es.enter_context(nc.sbuf_tensor("s_" + name, shape, dt))

            opq = [None]

            def op(*a):
                if opq[0] is None:
                    sch.op(*a)
                else:
                    opq[0].append(a)

            ident = sb("ident", [128, 128])
            masks = sb("masks", [128, 5, 128])
            sel2 = sb("sel2", [128, 2])
            gainP = sb("gainP", [128, 8])
            bgate = sb("bgate", [128, 16])
            mu = sb("mu", [128, 14])
            pvec = sb("pvec", [128, 6, 4])
            lnw = sb("lnw", [128, 512])
            lnb = sb("lnb", [128, 512])
            s5p = sb("s5p", [128, 3, 16])
            sp = sb("sp", [128, 24, 16])
            lora = sb("lora", [128, 512], BF16)
            glora = sb("glora", [128, 512], BF16)
            w_o = sb("w_o", [128, 4, 1024], BF16)
            w_glu = sb("w_glu", [128, 4, 2048], BF16)
            BBre = sb("BBre", [128, 16, 128], BF16)
            BBim = sb("BBim", [128, 16, 128], BF16)
            CCre = sb("CCre", [128, 16, 128], BF16)
            CCimN = sb("CCimN", [128, 16, 128], BF16)
            Ere = sb("Ere", [128, 16, 128])
            Eim = sb("Eim", [128, 16, 128])
            SCR = sb("SCR", [128, 8192])
            esp = ExitStack()
            stgb = [esp.enter_context(nc.sbuf_tensor("s_stgb%d" % i, [128, 1024], BF16)) for i in range(2)]

            for t_sb, t_d in [(ident, ident_d), (masks, masks_d), (sel2, sel2_d), (gainP, gainP_d),
                              (bgate, bgate_d), (mu, mu_d), (pvec, pv_d), (lnw, lnw_d), (lnb, lnb_d),
                              (s5p, s5p_d)]:
                dma('s', 'dc', t_sb[:], t_d, [], ["c"])
            maskSL = masks[:, 0, :]
            maskSU = masks[:, 1, :]
            maskUI = masks[:, 2, :]
            maskBD = masks[:, 3, :]
            scanmask = masks[:, 4, :]

            stg = [SCR[:, 0:2048], SCR[:, 2048:4096]]
            tmpA = SCR[:, 4096:6144]
            tmpB = SCR[:, 6144:8192]

            w_in_v = w_in_d.rearrange("(kt p) c -> p kt c", p=128)
            for c in range(NCT):
                b = c % 2
                sg = "stg%d" % b
                sgb = "stgb%d" % b
                dma('s', 'dst%d' % b, stg[b][:, 0:1024].rearrange("p (k c) -> p k c", k=8),
                    w_in_v[:, :, c * 128:(c + 1) * 128], [], [sg])
                op('v', [sg, "c"], [sgb], lambda e, b=b: e.tensor_tensor(
                    out=stgb[b][:].rearrange("p (k c) -> p k c", k=8),
                    in0=stg[b][:, 0:1024].rearrange("p (k c) -> p k c", k=8),
                    in1=bc(gainP[:], [128, 8, 128], 2), op=ALU.mult))
                dma('s', 'dsb%d' % b, wsc_d[c], stgb[b][:], [sgb], ["wsc%d" % c])

            ldn = [0]

            def load_cast(dst_ap, src_ap, width, dstkey):
                b = ldn[0] % 2
                ldn[0] += 1
                sg = "stg%d" % b
                dma('s', 'dst%d' % b, stg[b][:, 0:width], src_ap, [], [sg])
                if b == 0:
                    op('v', [sg], [dstkey], lambda e: e.tensor_copy(out=dst_ap, in_=stg[b][:, 0:width]))
                else:
                    op('a', [sg], [dstkey], lambda e: e.activation(out=dst_ap, in_=stg[b][:, 0:width], func=AF.Copy))

            load_cast(lora[:], lora_d, 512, "lora")
            load_cast(glora[:], glora_d, 512, "glora")
            for k in range(4):
                load_cast(w_o[:, k, :], w_o_d[k * 128:(k + 1) * 128, :], 1024, "w_o")
            for k in range(4):
                load_cast(w_glu[:, k, :], w_glu_d[k * 128:(k + 1) * 128, :], 2048, "w_glu")
            for k in range(8):
                b = k % 2
                dma('s', 'dst%d' % b, stg[b][:, 0:1024], w_out_d[k * 128:(k + 1) * 128, :], [], ["stg%d" % b])
                op('v', ["stg%d" % b], ["stgb%d" % b], lambda e, b=b: e.tensor_copy(out=stgb[b][:], in_=stg[b][:, 0:1024]))
                dma('s', 'dsb%d' % b, wo_d[k], stgb[b][:], ["stgb%d" % b], ["wo%d" % k])
            load_cast(BBre[:].rearrange("p a b -> p (a b)"), bbre_d, 2048, "BB")
            load_cast(BBim[:].rearrange("p a b -> p (a b)"), bbim_d, 2048, "BB")

            def V2(i):
                return sp[:, i, :]
            a_re = s5p[:, 0, :]
            a_im = s5p[:, 1, :]
            ldt = s5p[:, 2, :]
            DT, TH, LAB, CS, SN, R, M_, LRE, LIM, NRE, DEN, FRE, FIM, T1, T2, CS2, SN2 = range(17)

            def vv(out_i, a, b_, o):
                op('v', ["c", "sp"], ["sp"], lambda e: e.tensor_tensor(out=V2(out_i), in0=a, in1=b_, op=o))
            op('a', ["c"], ["sp"], lambda e: e.activation(out=V2(DT), in_=ldt, func=AF.Exp))
            vv(TH, a_im, V2(DT), ALU.mult)
            vv(T1, a_re, V2(DT), ALU.mult)
            op('a', ["sp"], ["sp"], lambda e: e.activation(out=V2(LAB), in_=V2(T1), func=AF.Exp))

            def sin_of(dst, shift):
                op('v', ["sp"], ["sp"], lambda e: e.tensor_scalar(out=V2(R), in0=V2(TH), scalar1=float(shift), scalar2=None, op0=ALU.add))
                for _ in range(4):
                    op('v', ["sp"], ["sp"], lambda e: e.tensor_scalar(out=V2(M_), in0=V2(R), scalar1=PI, scalar2=-2.0 * PI, op0=ALU.is_ge, op1=ALU.mult))
                    vv(R, V2(R), V2(M_), ALU.add)
                op('a', ["sp"], ["sp"], lambda e: e.activation(out=V2(dst), in_=V2(R), func=AF.Sin))
            sin_of(SN, 0.0)
            sin_of(CS, PI / 2)
            vv(LRE, V2(LAB), V2(CS), ALU.mult)
            vv(LIM, V2(LAB), V2(SN), ALU.mult)
            op('v', ["sp"], ["sp"], lambda e: e.tensor_scalar(out=V2(NRE), in0=V2(LRE), scalar1=-1.0, scalar2=None, op0=ALU.add))
            vv(T1, a_re, a_re, ALU.mult)
            vv(T2, a_im, a_im, ALU.mult)
            vv(DEN, V2(T1), V2(T2), ALU.add)
            op('v', ["sp"], ["sp"], lambda e: e.reciprocal(out=V2(DEN), in_=V2(DEN)))
            vv(T1, V2(NRE), a_re, ALU.mult)
            vv(T2, V2(LIM), a_im, ALU.mult)
            vv(T1, V2(T1), V2(T2), ALU.add)
            vv(FRE, V2(T1), V2(DEN), ALU.mult)
            vv(T1, V2(LIM), a_re, ALU.mult)
            vv(T2, V2(NRE), a_im, ALU.mult)
            vv(T1, V2(T1), V2(T2), ALU.subtract)
            vv(FIM, V2(T1), V2(DEN), ALU.mult)

            dma('s', 'dst0', stg[0], ccre_d, [], ["stg0"])
            dma('s', 'dst1', stg[1], ccim_d, [], ["stg1"])

            def c3(a):
                return a.rearrange("p (j m) -> p j m", j=16)
            fre_b = bc(V2(FRE), [128, 16, 128], 2)
            fim_b = bc(V2(FIM), [128, 16, 128], 2)
            op('v', ["stg0", "sp"], ["tmpA"], lambda e: e.tensor_tensor(out=c3(tmpA), in0=c3(stg[0]), in1=fre_b, op=ALU.mult))
            op('v', ["stg1", "sp"], ["tmpB"], lambda e: e.tensor_tensor(out=c3(tmpB), in0=c3(stg[1]), in1=fim_b, op=ALU.mult))
            op('v', ["tmpA", "tmpB"], ["CC"], lambda e: e.tensor_tensor(out=CCre[:].rearrange("p a b -> p (a b)"), in0=tmpA, in1=tmpB, op=ALU.subtract))
            op('v', ["stg0", "sp", "CC"], ["tmpA"], lambda e: e.tensor_tensor(out=c3(tmpA), in0=c3(stg[0]), in1=fim_b, op=ALU.mult))
            op('v', ["stg1", "sp", "CC"], ["tmpB"], lambda e: e.tensor_tensor(out=c3(tmpB), in0=c3(stg[1]), in1=fre_b, op=ALU.mult))
            op('v', ["tmpA", "tmpB"], ["tmpA"], lambda e: e.tensor_tensor(out=tmpA, in0=tmpA, in1=tmpB, op=ALU.add))
            op('v', ["tmpA"], ["CC"], lambda e: e.tensor_scalar(out=CCimN[:].rearrange("p a b -> p (a b)"), in0=tmpA, scalar1=-1.0, scalar2=None, op0=ALU.mult))

            op('v', [], ["E"], lambda e: e.memset(Ere[:, :, 0:1], 1.0))
            op('v', [], ["E"], lambda e: e.memset(Eim[:, :, 0:1], 0.0))
            op('v', ["sp"], ["sp"], lambda e: e.tensor_copy(out=V2(CS2), in_=V2(CS)))
            op('v', ["sp"], ["sp"], lambda e: e.tensor_copy(out=V2(SN2), in_=V2(SN)))
            et0 = tmpA[:, 0:1024].rearrange("p (a b) -> p a b", a=16)
            et1 = tmpB[:, 0:1024].rearrange("p (a b) -> p a b", a=16)
            for lv in range(7):
                m = 1 << lv
                shp = [128, 16, m]
                cb = bc(V2(CS2), shp, 2)
                sbb = bc(V2(SN2), shp, 2)
                op('v', ["E", "sp", "tmpA"], ["tmpA"], lambda e, m=m, cb=cb: e.tensor_tensor(out=et0[:, :, 0:m], in0=Ere[:, :, 0:m], in1=cb, op=ALU.mult))
                op('v', ["E", "sp", "tmpB"], ["tmpB"], lambda e, m=m, sbb=sbb: e.tensor_tensor(out=et1[:, :, 0:m], in0=Eim[:, :, 0:m], in1=sbb, op=ALU.mult))
                op('v', ["tmpA", "tmpB", "E"], ["E"], lambda e, m=m: e.tensor_tensor(out=Ere[:, :, m:2 * m], in0=et0[:, :, 0:m], in1=et1[:, :, 0:m], op=ALU.subtract))
                op('v', ["E", "sp", "tmpA"], ["tmpA"], lambda e, m=m, sbb=sbb: e.tensor_tensor(out=et0[:, :, 0:m], in0=Ere[:, :, 0:m], in1=sbb, op=ALU.mult))
                op('v', ["E", "sp", "tmpB"], ["tmpB"], lambda e, m=m, cb=cb: e.tensor_tensor(out=et1[:, :, 0:m], in0=Eim[:, :, 0:m], in1=cb, op=ALU.mult))
                op('v', ["tmpA", "tmpB", "E"], ["E"], lambda e, m=m: e.tensor_tensor(out=Eim[:, :, m:2 * m], in0=et0[:, :, 0:m], in1=et1[:, :, 0:m], op=ALU.add))
                vv(T1, V2(CS2), V2(CS2), ALU.mult)
                vv(T2, V2(SN2), V2(SN2), ALU.mult)
                op('v', ["sp"], ["sp"], lambda e: e.scalar_tensor_tensor(out=V2(SN2), in0=V2(CS2), scalar=2.0, in1=V2(SN2), op0=ALU.mult, op1=ALU.mult))
                vv(CS2, V2(T1), V2(T2), ALU.subtract)

            sch.barrier()
            esp.close()
            mark("prep")

            PF = sb("PF", [128, 14, 129])
            Stp = sb("Stp", [128, 4, 128])
            cw = sb("cw", [128, 2, 16])
            op('v', [], ["PF"], lambda e: e.memset(PF[:], 0.0))
            op('v', [], ["Stp"], lambda e: e.memset(Stp[:], 0.0))
            op('v', [], ["cw0", "cw1", "cw2", "cw3"], lambda e: e.memset(cw[:], 0.0))

            xt = [sb("xt%d" % i, [128, D]) for i in range(2)]
            stat = sb("stat", [128, 8])
            xnF = sb("xnF", [128, 8, 128], BF16)
            wch = [sb("wch%d" % i, [128, 8, 128], BF16) for i in range(3)]
            L = sb("L", [128, 14, 128])
            uF = sb("uF", [128, 4, 128])
            uB = sb("uB", [128, 4, 128], BF16)
            gates = sb("gates", [128, 16, 128], BF16)
            lorain = sb("lorain", [128, 128], BF16)
            sgx = sb("sgx", [128, 128], BF16)
            TT = [SCR[:, i * 512:(i + 1) * 512].rearrange("p (a b) -> p a b", a=4) for i in range(16)]
            TK = ["T%d" % i for i in range(16)]
            (iSIG, iCS, iPT, iPTM, iPINV, iAV, iKK, iTMP, iKMOD, iRTL, iATL, iBTL, iKTL, iRKR, iY2, iX) = range(16)
            VT = sb("VT", [128, 512])
            BT = sb("BT", [128, 512])
            KT = sb("KT", [128, 512])
            gT = sb("gT", [128, 512])
            mats = {nm: sb("m_" + nm, [128, 4, 128]) for nm in ["X", "XT", "X2", "X2T", "QT", "akT", "rbT", "rkT"]}
            RHS = sb("RHS", [128, 512])
            SA = sb("SA", [128, 512])
            PTbd = sb("PTbd", [128, 4, 128])
            Y = sb("Y", [128, 512])
            gst = sb("gst", [128, 6, 8])
            bon = sb("bon", [128, 8])
            roF = sb("roF", [128, 4, 128], BF16)
            mixed = sb("mixed", [128, 8, 128])
            mixedB = sb("mixedB", [128, 8, 128], BF16)
            xre2 = [sb("xre%d" % i, [128, 4, 128], BF16) for i in range(2)]
            xim2 = [sb("xim%d" % i, [128, 4, 128], BF16) for i in range(2)]
            ygB = sb("ygB", [128, 4, 128], BF16)
            zg = sb("zg", [128, 8, 128])
            ST = sb("ST", [128, 8, 512])
            cwn2 = [sb("cwn%d" % i, [128, 4, 4]) for i in range(2)]

            w0 = pvec[:, 0, :]
            a0 = pvec[:, 1, :]
            k_k = pvec[:, 2, :]
            k_a = pvec[:, 3, :]
            r_k = pvec[:, 4, :]
            s5d = pvec[:, 5, :]
            S4 = [128, 4, 128]

            def tt(eng, o, ok, a, ak, b_, bk, alu):
                op(eng, [ak, bk], [ok], lambda e: e.tensor_tensor(out=o, in0=a, in1=b_, op=alu))

            dma('s', 'dx0', xt[0][:], x_d[0:128, :], [], ["xt0"])

            for it in range(NT):
                xb = it % 2
                xk = "xt%d" % xb
                xcur = xt[xb]
                if it + 1 < NT:
                    dma('s', 'dx%d' % (1 - xb), xt[1 - xb][:], x_d[(it + 1) * 128:(it + 2) * 128, :], [], ["xt%d" % (1 - xb)])
                op('a', [xk], [TK[iX], "stat"], lambda e: e.activation(out=SCR[:, iX * 512:iX * 512 + 512], in_=xcur[:, 0:512], func=AF.Square, accum_out=stat[:, 0:1]))
                op('a', [xk], [TK[iX], "stat"], lambda e: e.activation(out=SCR[:, iX * 512:iX * 512 + 512], in_=xcur[:, 512:1024], func=AF.Square, accum_out=stat[:, 3:4]))
                op('v', ["stat"], ["stat"], lambda e: e.tensor_tensor(out=stat[:, 0:1], in0=stat[:, 0:1], in1=stat[:, 3:4], op=ALU.add))
                op('a', ["stat"], ["stat"], lambda e: e.activation(out=stat[:, 1:2], in_=stat[:, 0:1], func=AF.Sqrt, bias=1e-6, scale=1.0 / D))
                op('v', ["stat"], ["stat"], lambda e: e.reciprocal(out=stat[:, 2:3], in_=stat[:, 1:2]))
                op('v', [xk, "stat"], [xk], lambda e: e.tensor_scalar(out=xcur[:], in0=xcur[:], scalar1=stat[:, 2:3], scalar2=None, op0=ALU.mult))
                mark("norm")
                for half in range(2):
                    pk = psk(half)

                    def tr(e, half=half):
                        ins = None
                        for q in range(4):
                            kt = half * 4 + q
                            ins = e.transpose(out=PS[half][:, q * 128:(q + 1) * 128], in_=xcur[:, kt * 128:(kt + 1) * 128], identity=ident[:])
                        return ins
                    op('p', [xk, "c"], [pk], tr)
                    if half == 0:
                        op('v', [pk], ["xnF"], lambda e: e.tensor_copy(out=xnF[:, 0:4, :], in_=P3(0)))
                    else:
                        op('a', [pk], ["xnF"], lambda e: e.activation(out=xnF[:, 4:8, :], in_=P3(1), func=AF.Copy))
                mark("xnF")
                for c in range(NCT):
                    mark("proj%d" % c)
                    wb = c % 3
                    wk = "wch%d" % wb
                    dma('s', 'dw%d' % wb, wch[wb][:].rearrange("p a b -> p (a b)"), wsc_d[c], ["wsc%d" % c], [wk])
                    pb = 2 + (c % 4)
                    pk = psk(pb)

                    def mm(e, wb=wb, pb=pb):
                        ins = None
                        for kt in range(8):
                            ins = e.matmul(PS[pb][:, 0:128], lhsT=wch[wb][:, kt, :], rhs=xnF[:, kt, :], start=(kt == 0), stop=(kt == 7))
                        return ins
                    op('p', [wk, "xnF"], [pk], mm)
                    if c < 14:
                        if c % 2 == 0:
                            op('v', [pk], ["PF"], lambda e, c=c, pb=pb: e.tensor_copy(out=PF[:, c, 1:129], in_=PS[pb][:, 0:128]))
                        else:
                            op('a', [pk], ["PF"], lambda e, c=c, pb=pb: e.activation(out=PF[:, c, 1:129], in_=PS[pb][:, 0:128], func=AF.Copy))
                    elif c < 18:
                        op('v', [pk], ["uF"], lambda e, c=c, pb=pb: e.tensor_copy(out=uF[:, c - 14, :], in_=PS[pb][:, 0:128]))
                        op('a', ["uF"], ["uB"], lambda e, c=c, pb=pb: e.activation(out=uB[:, c - 14, :], in_=uF[:, c - 14, :], func=AF.Copy))
                    else:
                        op('a', [pk, "c"], ["gates"], lambda e, c=c, pb=pb: e.activation(out=gates[:, c - 18, :], in_=PS[pb][:, 0:128], func=AF.Sigmoid, bias=bgate[:, c - 18:c - 17]))
                mark("proj")
                op('v', ["PF"], ["L"], lambda e: e.tensor_tensor(out=L[:], in0=PF[:, :, 0:128], in1=PF[:, :, 1:129], op=ALU.subtract))
                op('g', ["L", "c"], ["L"], lambda e: e.tensor_tensor(out=L[:], in0=L[:], in1=bc(mu[:], [128, 14, 128], 2), op=ALU.mult))
                op('v', ["L", "PF"], ["L"], lambda e: e.tensor_tensor(out=L[:], in0=L[:], in1=PF[:, :, 1:129], op=ALU.add))
                op('v', ["PF"], ["PF"], lambda e: e.tensor_copy(out=PF[:, :, 0:1], in_=PF[:, :, 128:129]))
                qR, qS = [], []
                opq[0] = qR
                rF = L[:, 0:4, :]
                kF = L[:, 4:8, :]
                vF = L[:, 8:12, :]
                sig, cs, Pt, Ptm1, Pinv, av, kk, tmp, kmod, rtl, atl, btl, ktl, rkr = [TT[i] for i in range(14)]
                op('a', ["L"], ["lorain"], lambda e: e.activation(out=lorain[0:64, :], in_=L[0:64, 12, :], func=AF.Tanh))
                op('v', ["L"], ["lorain"], lambda e: e.tensor_copy(out=lorain[64:128, :], in_=L[64:128, 12, :]))
                op('a', ["L"], ["sgx"], lambda e: e.activation(out=sgx[:], in_=L[:, 13, :], func=AF.Sigmoid))

                def mm_lw(e):
                    ins = None
                    for j in range(4):
                        ins = e.matmul(PS[0][:, j * 128:(j + 1) * 128], lhsT=lora[0:64, j * 128:(j + 1) * 128], rhs=lorain[0:64, :], start=True, stop=True)
                    return ins
                op('p', ["lora", "lorain"], [psk(0)], mm_lw)

                def mm_la(e):
                    ins = None
                    for j in range(4):
                        ins = e.matmul(PS[1][:, j * 128:(j + 1) * 128], lhsT=lora[64:128, j * 128:(j + 1) * 128], rhs=lorain[64:128, :], start=True, stop=True)
                    return ins
                op('p', ["lora", "lorain"], [psk(1)], mm_la)
                op('p', ["glora", "sgx"], [psk(6)], lambda e: e.matmul(PS[6][:], lhsT=sgx[:], rhs=glora[:], start=True, stop=True))
                for j in range(4):
                    op('a', [psk(0), "c"], [TK[iSIG]], lambda e, j=j: e.activation(out=sig[:, j, :], in_=PS[0][:, j * 128:(j + 1) * 128], func=AF.Sigmoid, bias=w0[:, j:j + 1]))
                    op('a', [psk(1), "c"], [TK[iAV]], lambda e, j=j: e.activation(out=av[:, j, :], in_=PS[1][:, j * 128:(j + 1) * 128], func=AF.Sigmoid, bias=a0[:, j:j + 1]))
                op('v', [psk(6)], ["gT"], lambda e: e.tensor_copy(out=gT[:], in_=PS[6][:]))
                for j in range(4):
                    op('v', [TK[iSIG], "c"], [TK[iCS]], lambda e, j=j: e.tensor_tensor_scan(out=cs[:, j, :], data0=scanmask, data1=sig[:, j, :], initial=0.0, op0=ALU.mult, op1=ALU.add))
                op('a', [TK[iCS]], [TK[iPT]], lambda e: e.activation(out=Pt, in_=cs, func=AF.Exp, scale=-C0))
                op('a', [TK[iCS]], [TK[iPINV]], lambda e: e.activation(out=Pinv, in_=cs, func=AF.Exp, scale=C0))
                tt('v', Ptm1, TK[iPTM], cs, TK[iCS], sig, TK[iSIG], ALU.subtract)
                op('a', [TK[iPTM]], [TK[iPTM]], lambda e: e.activation(out=Ptm1, in_=Ptm1, func=AF.Exp, scale=-C0))
                tt('g', kk, TK[iKK], kF, "L", bc(k_k, S4, 2), "c", ALU.mult)
                tt('g', tmp, TK[iTMP], kk, TK[iKK], kk, TK[iKK], ALU.mult)

                def mm_n(e):
                    ins = None
                    for j in range(4):
                        ins = e.matmul(PS[7][:, j * 128:(j + 1) * 128], lhsT=maskBD, rhs=tmp[:, j, :], start=True, stop=True)
                    return ins
                op('p', [TK[iTMP], "c"], [psk(7)], mm_n)
                op('a', [psk(7)], [TK[iTMP]], lambda e: e.activation(out=tmp, in_=P3(7), func=AF.Sqrt))
                op('v', [TK[iTMP]], [TK[iTMP]], lambda e: e.tensor_scalar(out=tmp, in0=tmp, scalar1=1e-12, scalar2=None, op0=ALU.max))
                op('v', [TK[iTMP]], [TK[iTMP]], lambda e: e.reciprocal(out=tmp, in_=tmp))
                tt('v', kk, TK[iKK], kk, TK[iKK], tmp, TK[iTMP], ALU.mult)
                tt('g', kmod, TK[iKMOD], av, TK[iAV], bc(k_a, S4, 2), "c", ALU.mult)
                tt('g', kmod, TK[iKMOD], kmod, TK[iKMOD], bc(k_a, S4, 2), "c", ALU.subtract)
                op('v', [TK[iKMOD], "L"], [TK[iKMOD]], lambda e: e.scalar_tensor_tensor(out=kmod, in0=kmod, scalar=1.0, in1=kF, op0=ALU.add, op1=ALU.mult))
                tt('v', rtl, TK[iRTL], rF, "L", Pt, TK[iPT], ALU.mult)
                op('v', [TK[iKK], TK[iPTM]], [TK[iATL]], lambda e: e.scalar_tensor_tensor(out=atl, in0=kk, scalar=-1.0, in1=Ptm1, op0=ALU.mult, op1=ALU.mult))
                tt('g', btl, TK[iBTL], kk, TK[iKK], av, TK[iAV], ALU.mult)
                tt('v', btl, TK[iBTL], btl, TK[iBTL], Pinv, TK[iPINV], ALU.mult)
                tt('v', ktl, TK[iKTL], kmod, TK[iKMOD], Pinv, TK[iPINV], ALU.mult)
                tt('g', rkr, TK[iRKR], rF, "L", kmod, TK[iKMOD], ALU.mult)
                tt('g', rkr, TK[iRKR], rkr, TK[iRKR], bc(r_k, S4, 2), "c", ALU.mult)
                op('v', [TK[iPT], "c"], ["PTbd"], lambda e: e.tensor_tensor(out=PTbd[:], in0=bc(maskBD, S4, 1), in1=Pt[:, :, 127:128].to_broadcast(S4), op=ALU.mult))

                def mm_b(e):
                    ins = None
                    for j in range(4):
                        ins = e.matmul(PS[7][:, 2 * j:2 * j + 2], lhsT=rkr[:, j, :], rhs=sel2[:], start=True, stop=True)
                    return ins
                op('p', [TK[iRKR], "c"], [psk(7)], mm_b)
                op('v', [psk(7)], ["bon"], lambda e: e.tensor_copy(out=bon[:], in_=PS[7][:, 0:8]))
                atl_m = [TT[0], TT[1]]
                rtl_m = [TT[2], TT[3]]
                for hh in range(2):
                    op('v' if hh == 0 else 'g', [TK[iATL], "c"], [TK[hh]], lambda e, hh=hh: e.tensor_scalar(out=atl_m[hh], in0=atl, scalar1=sel2[:, hh:hh + 1], scalar2=None, op0=ALU.mult))
                    op('v' if hh == 0 else 'g', [TK[iRTL], "c"], [TK[2 + hh]], lambda e, hh=hh: e.tensor_scalar(out=rtl_m[hh], in0=rtl, scalar1=sel2[:, hh:hh + 1], scalar2=None, op0=ALU.mult))
                mark("elem")
                for src, srckey, dst, dkey, pb in [(vF, "L", VT, "VT", 0), (btl, TK[iBTL], BT, "BT", 1), (ktl, TK[iKTL], KT, "KT", 6)]:
                    def trf(e, src=src, pb=pb):
                        ins = None
                        for j in range(4):
                            ins = e.transpose(out=PS[pb][:, j * 128:(j + 1) * 128], in_=src[:, j, :], identity=ident[:])
                        return ins
                    op('p', [srckey, "c"], [psk(pb)], trf)
                    if pb == 1:
                        op('a', [psk(pb)], [dkey], lambda e, dst=dst, pb=pb: e.activation(out=dst[:], in_=PS[pb][:], func=AF.Copy))
                    else:
                        op('v', [psk(pb)], [dkey], lambda e, dst=dst, pb=pb: e.tensor_copy(out=dst[:], in_=PS[pb][:]))
                mark("trans")
                for hg in range(2):
                    def hsl(t, hh, hg=hg):
                        h = hg * 4 + hh
                        return t[(h % 2) * 64:(h % 2) * 64 + 64, h // 2, :]
                    specs = [("X", "A", btl, maskSL, 2), ("XT", btl, "A", maskSU, 0),
                             ("akT", ktl, "A", maskSU, 1), ("rbT", btl, "R", maskUI, 6), ("rkT", ktl, "R", maskUI, 7)]

                    def pick(t, hl, hg=hg):
                        h = hg * 4 + hl
                        j, hh = h // 2, h % 2
                        if isinstance(t, str):
                            return (atl_m if t == "A" else rtl_m)[hh][:, j, :]
                        return t[:, j, :]
                    for nm, lt, rt, mk, pb in specs:
                        def mmT(e, lt=lt, rt=rt, pb=pb, pick=pick):
                            ins = None
                            for hl in range(4):
                                ins = e.matmul(PS[pb][:, hl * 128:(hl + 1) * 128], lhsT=pick(lt, hl), rhs=pick(rt, hl), start=True, stop=True)
                            return ins
                        op('p', [TK[0], TK[1], TK[2], TK[3], TK[iBTL], TK[iKTL]], [psk(pb)], mmT)
                        op('v', [psk(pb), "c"], ["m_" + nm], lambda e, nm=nm, mk=mk, pb=pb: e.tensor_tensor(
                            out=mats[nm][:], in0=P3(pb), in1=bc(mk, S4, 1), op=ALU.mult))
                        mark("mats_" + nm)
                    op('g', ["m_XT", "c"], ["m_QT"], lambda e: e.tensor_tensor(out=mats["QT"][:], in0=mats["XT"][:], in1=bc(ident[:], S4, 1), op=ALU.add))
                    mark("mats_qt")
                    cur, curT, nx, nxT = "X", "XT", "X2", "X2T"
                    for step in range(6):
                        def sq(e, a=curT, b_=cur):
                            ins = None
                            for hh in range(4):
                                ins = e.matmul(PS[2][:, hh * 128:(hh + 1) * 128], lhsT=mats[a][:, hh, :], rhs=mats[b_][:, hh, :], start=True, stop=True)
                            return ins
                        op('p', ["m_" + cur, "m_" + curT], [psk(2)], sq)
                        op('a', [psk(2)], ["m_" + nx], lambda e, nx=nx: e.activation(out=mats[nx][:], in_=P3(2), func=AF.Copy))
                        if step < 5:
                            def sqT(e, a=cur, b_=curT):
                                ins = None
                                for hh in range(4):
                                    ins = e.matmul(PS[0][:, hh * 128:(hh + 1) * 128], lhsT=mats[a][:, hh, :], rhs=mats[b_][:, hh, :], start=True, stop=True)
                                return ins
                            op('p', ["m_" + cur, "m_" + curT], [psk(0)], sqT)
                            op('v', [psk(0)], ["m_" + nxT], lambda e, nxT=nxT: e.tensor_copy(out=mats[nxT][:], in_=P3(0)))

                        def qu(e, a=nx):
                            ins = None
                            for hh in range(4):
                                ins = e.matmul(PS[1][:, hh * 128:(hh + 1) * 128], lhsT=mats[a][:, hh, :], rhs=mats["QT"][:, hh, :], start=True, stop=True)
                            return ins
                        op('p', ["m_" + nx, "m_QT"], [psk(1)], qu)
                        op('v', [psk(1), "m_QT"], ["m_QT"], lambda e: e.tensor_tensor(out=mats["QT"][:], in0=mats["QT"][:], in1=P3(1), op=ALU.add))
                        cur, curT, nx, nxT = nx, nxT, cur, curT
                        mark("mats_s%d" % step)
                    mark("mats")
                    c0 = hg * 256

                    def mm_rhs(e, hg=hg):
                        ins = None
                        for hl in range(4):
                            h = hg * 4 + hl
                            j = h // 2
                            hh = h % 2
                            reg = PS[6][:, hl * 64:(hl + 1) * 64]
                            e.matmul(reg, lhsT=atl[:, j, :], rhs=Stp[:, j, hh * 64:(hh + 1) * 64], start=True, stop=False)
                            ins = e.matmul(reg, lhsT=mats["akT"][:, hl, :], rhs=VT[:, h * 64:(h + 1) * 64], start=False, stop=True)
                        return ins
                    op('p', [TK[iATL], "Stp", "m_akT", "VT"], [psk(6)], mm_rhs)
                    op('v', [psk(6)], ["RHS"], lambda e, c0=c0: e.tensor_copy(out=RHS[:, c0:c0 + 256], in_=PS[6][:, 0:256]))

                    def mm_sa(e, hg=hg):
                        ins = None
                        for hl in range(4):
                            h = hg * 4 + hl
                            ins = e.matmul(PS[7][:, hl * 64:(hl + 1) * 64], lhsT=mats["QT"][:, hl, :], rhs=RHS[:, h * 64:(h + 1) * 64], start=True, stop=True)
                        return ins
                    op('p', ["m_QT", "RHS"], [psk(7)], mm_sa)
                    op('v', [psk(7)], ["SA"], lambda e, c0=c0: e.tensor_copy(out=SA[:, c0:c0 + 256], in_=PS[7][:, 0:256]))

                    def mm_y(e, hg=hg):
                        ins = None
                        for hl in range(4):
                            h = hg * 4 + hl
                            j = h // 2
                            hh = h % 2
                            reg = PS[2][:, hl * 64:(hl + 1) * 64]
                            e.matmul(reg, lhsT=rtl[:, j, :], rhs=Stp[:, j, hh * 64:(hh + 1) * 64], start=True, stop=False)
                            e.matmul(reg, lhsT=mats["rbT"][:, hl, :], rhs=SA[:, h * 64:(h + 1) * 64], start=False, stop=False)
                            ins = e.matmul(reg, lhsT=mats["rkT"][:, hl, :], rhs=VT[:, h * 64:(h + 1) * 64], start=False, stop=True)
                        return ins
                    op('p', [TK[iRTL], "Stp", "m_rbT", "m_rkT", "SA", "VT"], [psk(2)], mm_y)
                    op('a', [psk(2)], ["Y"], lambda e, c0=c0: e.activation(out=Y[:, c0:c0 + 256], in_=PS[2][:, 0:256], func=AF.Copy))

                    def mm_st(e, hg=hg):
                        ins = None
                        for jj in range(2):
                            j = hg * 2 + jj
                            reg = PS[0][:, jj * 128:(jj + 1) * 128]
                            e.matmul(reg, lhsT=ident[:], rhs=Stp[:, j, :], start=True, stop=False)
                            e.matmul(reg, lhsT=BT[:, j * 128:(j + 1) * 128], rhs=SA[:, j * 128:(j + 1) * 128], start=False, stop=False)
                            ins = e.matmul(reg, lhsT=KT[:, j * 128:(j + 1) * 128], rhs=VT[:, j * 128:(j + 1) * 128], start=False, stop=True)
                        return ins
                    op('p', ["Stp", "BT", "KT", "SA", "VT", "c"], [psk(0)], mm_st)
                    op('v', [psk(0), "PTbd"], ["Stp"], lambda e, hg=hg: e.tensor_tensor(
                        out=Stp[:, 2 * hg:2 * hg + 2, :], in0=PS[0][:, 0:256].rearrange("p (a b) -> p a b", a=2),
                        in1=PTbd[:, 2 * hg:2 * hg + 2, :], op=ALU.mult))
                mark("chain")
                Y3 = Y[:].rearrange("p (h v) -> p h v", h=8)
                Ysq = SCR[:, iY2 * 512:(iY2 + 1) * 512]
                G8 = [128, 8, 64]
                op('v', ["Y"], ["gst"], lambda e: e.tensor_reduce(out=gst[:, 0, :], in_=Y3, axis=AX.X, op=ALU.add))
                op('a', ["Y"], [TK[iY2]], lambda e: e.activation(out=Ysq, in_=Y[:], func=AF.Square))
                op('v', [TK[iY2]], ["gst"], lambda e: e.tensor_reduce(out=gst[:, 1, :], in_=Ysq.rearrange("p (h v) -> p h v", h=8), axis=AX.X, op=ALU.add))
                op('v', ["gst"], ["gst"], lambda e: e.tensor_scalar(out=gst[:, 2, :], in0=gst[:, 0, :], scalar1=1.0 / 64, scalar2=None, op0=ALU.mult))
                op('v', ["gst"], ["gst"], lambda e: e.tensor_tensor(out=gst[:, 3, :], in0=gst[:, 2, :], in1=gst[:, 2, :], op=ALU.mult))
                op('v', ["gst"], ["gst"], lambda e: e.scalar_tensor_tensor(out=gst[:, 4, :], in0=gst[:, 1, :], scalar=1.0 / 64, in1=gst[:, 3, :], op0=ALU.mult, op1=ALU.subtract))
                op('a', ["gst"], ["gst"], lambda e: e.activation(out=gst[:, 5, :], in_=gst[:, 4, :], func=AF.Sqrt, bias=GN_EPS, scale=1.0))
                op('v', ["gst"], ["gst"], lambda e: e.reciprocal(out=gst[:, 5, :], in_=gst[:, 5, :]))
                op('v', ["Y", "gst"], ["Y"], lambda e: e.tensor_tensor(out=Y3, in0=Y3, in1=bc(gst[:, 2, :], G8, 2), op=ALU.subtract))
                op('v', ["Y", "gst"], ["Y"], lambda e: e.tensor_tensor(out=Y3, in0=Y3, in1=bc(gst[:, 5, :], G8, 2), op=ALU.mult))
                op('g', ["Y", "c"], ["Y"], lambda e: e.tensor_tensor(out=Y[:], in0=Y[:], in1=lnw[:], op=ALU.mult))
                op('g', ["Y", "c"], ["Y"], lambda e: e.tensor_tensor(out=Y[:], in0=Y[:], in1=lnb[:], op=ALU.add))
                op('v', ["VT", "bon"], [TK[iY2]], lambda e: e.tensor_tensor(out=Ysq.rearrange("p (h v) -> p h v", h=8), in0=VT[:].rearrange("p (h v) -> p h v", h=8), in1=bc(bon[:], G8, 2), op=ALU.mult))
                op('v', ["Y", TK[iY2]], ["Y"], lambda e: e.tensor_tensor(out=Y[:], in0=Y[:], in1=Ysq, op=ALU.add))
                op('v', ["Y", "gT"], ["Y"], lambda e: e.tensor_tensor(out=Y[:], in0=Y[:], in1=gT[:], op=ALU.mult))

                def tr_ro(e):
                    ins = None
                    for j in range(4):
                        ins = e.transpose(out=PS[0][:, j * 128:(j + 1) * 128], in_=Y[:, j * 128:(j + 1) * 128], identity=ident[:])
                    return ins
                op('p', ["Y", "c"], [psk(0)], tr_ro)
                op('a', [psk(0)], ["roF"], lambda e: e.activation(out=roF[:], in_=P3(0), func=AF.Copy))
                for half in range(2):
                    pb = 1 + half

                    def mm_o(e, half=half, pb=pb):
                        ins = None
                        for q in range(4):
                            dt_ = half * 4 + q
                            for kt in range(4):
                                ins = e.matmul(PS[pb][:, q * 128:(q + 1) * 128], lhsT=w_o[:, kt, dt_ * 128:(dt_ + 1) * 128], rhs=roF[:, kt, :], start=(kt == 0), stop=(kt == 3))
                        return ins
                    op('p', ["w_o", "roF"], [psk(pb)], mm_o)
                    op('v', [psk(pb), "gates"], ["mixed"], lambda e, half=half, pb=pb: e.tensor_tensor(out=mixed[:, half * 4:(half + 1) * 4, :], in0=P3(pb), in1=gates[:, half * 4:(half + 1) * 4, :], op=ALU.mult))
                mark("epi")
                opq[0] = qS
                def s5_group(kt, ts):
                    s5a, s5b, s5c, s5d_, btre, btim, wre, wim = [ST[:, i, :].rearrange("p (a b) -> p a b", a=4) for i in range(8)]
                    ka, kb, kc, kd, kbr, kbi, kwr, kwi = ["ST%d" % i for i in range(8)]
                    pa, pb_ = 3, 4
                    xr, xi = xre2[ts], xim2[ts]
                    xrk, xik = "xre%d" % ts, "xim%d" % ts
                    cn = cwn2[ts]
                    cnk = "cwn%d" % ts
                    cwk = "cw%d" % kt
                    Er = Ere[:, 4 * kt:4 * kt + 4, :]
                    Ei = Eim[:, 4 * kt:4 * kt + 4, :]

                    def mm_bu(e):
                        ins = None
                        for q in range(4):
                            j = 4 * kt + q
                            e.matmul(PS[pa][:, q * 128:(q + 1) * 128], lhsT=BBre[:, j, :], rhs=uB[:, kt, :], start=True, stop=True)
                            ins = e.matmul(PS[pb_][:, q * 128:(q + 1) * 128], lhsT=BBim[:, j, :], rhs=uB[:, kt, :], start=True, stop=True)
                        return ins
                    op('p', ["BB", "uB"], [psk(pa), psk(pb_)], mm_bu)
                    yield
                    tt('v', s5a, ka, Er, "E", P3(pa), psk(pa), ALU.mult)
                    yield
                    tt('v', s5b, kb, Ei, "E", P3(pb_), psk(pb_), ALU.mult)
                    yield
                    tt('g', btre, kbr, s5a, ka, s5b, kb, ALU.add)
                    yield
                    tt('v', s5c, kc, Er, "E", P3(pb_), psk(pb_), ALU.mult)
                    yield
                    tt('v', s5d_, kd, Ei, "E", P3(pa), psk(pa), ALU.mult)
                    yield
                    tt('g', btim, kbi, s5c, kc, s5d_, kd, ALU.subtract)
                    yield
                    for q in range(4):
                        j = 4 * kt + q
                        op('v', [kbr, cwk, "sp"], [kwr], lambda e, q=q, j=j: e.tensor_tensor_scan(out=wre[:, q, :], data0=sp[:, LAB, j:j + 1].to_broadcast([128, 128]), data1=btre[:, q, :], initial=cw[:, 0, j:j + 1], op0=ALU.mult, op1=ALU.add))
                        yield
                        op('v', [kbi, cwk, "sp"], [kwi], lambda e, q=q, j=j: e.tensor_tensor_scan(out=wim[:, q, :], data0=sp[:, LAB, j:j + 1].to_broadcast([128, 128]), data1=btim[:, q, :], initial=cw[:, 1, j:j + 1], op0=ALU.mult, op1=ALU.add))
                        yield
                    c128 = sp[:, CS2, 4 * kt:4 * kt + 4]
                    s128 = sp[:, SN2, 4 * kt:4 * kt + 4]
                    wr127 = wre[:, :, 127]
                    wi127 = wim[:, :, 127]
                    tt('g', cn[:, 0, :], cnk, c128, "sp", wr127, kwr, ALU.mult)
                    yield
                    tt('g', cn[:, 1, :], cnk, s128, "sp", wi127, kwi, ALU.mult)
                    yield
                    tt('g', cn[:, 2, :], cnk, s128, "sp", wr127, kwr, ALU.mult)
                    yield
                    tt('g', cn[:, 3, :], cnk, c128, "sp", wi127, kwi, ALU.mult)
                    yield
                    tt('g', cw[:, 0, 4 * kt:4 * kt + 4], cwk, cn[:, 0, :], cnk, cn[:, 1, :], cnk, ALU.subtract)
                    yield
                    tt('g', cw[:, 1, 4 * kt:4 * kt + 4], cwk, cn[:, 2, :], cnk, cn[:, 3, :], cnk, ALU.add)
                    yield
                    tt('v', s5a, ka, Er, "E", wre, kwr, ALU.mult)
                    yield
                    tt('v', s5b, kb, Ei, "E", wim, kwi, ALU.mult)
                    yield
                    tt('g', xr[:], xrk, s5a, ka, s5b, kb, ALU.subtract)
                    yield
                    tt('v', s5c, kc, Ei, "E", wre, kwr, ALU.mult)
                    yield
                    tt('v', s5d_, kd, Er, "E", wim, kwi, ALU.mult)
                    yield
                    tt('g', xi[:], xik, s5c, kc, s5d_, kd, ALU.add)
                    yield

                    def mm_c(e):
                        ins = None
                        for q in range(4):
                            j = 4 * kt + q
                            e.matmul(PS[5][:, kt * 128:(kt + 1) * 128], lhsT=CCre[:, j, :], rhs=xr[:, q, :], start=(q == 0), stop=False)
                            ins = e.matmul(PS[5][:, kt * 128:(kt + 1) * 128], lhsT=CCimN[:, j, :], rhs=xi[:, q, :], start=False, stop=(q == 3))
                        return ins
                    op('p', ["CC", xrk, xik], [psk(5)], mm_c)
                    yield

                for kt_ in range(4):
                    for _ in s5_group(kt_, 0):
                        pass
                s5a, s5b = ST[:, 0, :].rearrange("p (a b) -> p a b", a=4), ST[:, 1, :].rearrange("p (a b) -> p a b", a=4)
                ka, kb = "ST0", "ST1"
                opq[0] = None
                nR, nS = len(qR), len(qS)
                iR = iS = 0
                while iR < nR or iS < nS:
                    if iS >= nS or (iR < nR and (SEQ_STREAMS or iR * nS <= iS * nR)):
                        sch.op(*qR[iR])
                        iR += 1
                    else:
                        sch.op(*qS[iS])
                        iS += 1
                tt('g', s5a, ka, uF[:], "uF", bc(s5d, S4, 2), "c", ALU.mult)
                tt('v', s5a, ka, s5a, ka, P3(5), psk(5), ALU.add)
                gelu_tanh(op, s5a, ka, s5b, kb, ygB[:], "ygB")
                for half in range(2):
                    for vg in range(2):
                        pb = 6 + vg

                        def mm_g(e, half=half, vg=vg, pb=pb):
                            ins = None
                            for q in range(4):
                                col = vg * 8 + half * 4 + q
                                for kt in range(4):
                                    ins = e.matmul(PS[pb][:, q * 128:(q + 1) * 128], lhsT=w_glu[:, kt, col * 128:(col + 1) * 128], rhs=ygB[:, kt, :], start=(kt == 0), stop=(kt == 3))
                            return ins
                        op('p', ["w_glu", "ygB"], [psk(pb)], mm_g)
                    zh = zg[:, half * 4:(half + 1) * 4, :]
                    op('a', [psk(7)], ["zg"], lambda e, zh=zh: e.activation(out=zh, in_=P3(7), func=AF.Sigmoid))
                    tt('v', zh, "zg", zh, "zg", P3(6), psk(6), ALU.mult)
                    tt('g', zh, "zg", zh, "zg", gates[:, 8 + half * 4:8 + (half + 1) * 4, :], "gates", ALU.mult)
                    tt('v', mixedB[:, half * 4:(half + 1) * 4, :], "mixedB", zh, "zg", mixed[:, half * 4:(half + 1) * 4, :], "mixed", ALU.add)
                mark("s5")
                wviews = [w_[:].rearrange("p a b -> p (a b)") for w_ in wch]
                for kt in range(8):
                    wb = (NCT + kt) % 3
                    wk = "wch%d" % wb
                    dma('s', 'dw%d' % wb, wviews[wb], wo_d[kt], [], [wk])

                    def mm_h(e, kt=kt, wb=wb):
                        e.matmul(PS[1][:], lhsT=mixedB[:, kt, :], rhs=wviews[wb][:, 0:512], start=(kt == 0), stop=(kt == 7))
                        return e.matmul(PS[2][:], lhsT=mixedB[:, kt, :], rhs=wviews[wb][:, 512:1024], start=(kt == 0), stop=(kt == 7))
                    op('p', [wk, "mixedB"], [psk(1), psk(2)], mm_h)
                for half in range(2):
                    pb = 1 + half
                    op('v', [psk(pb), xk, "stat"], [xk], lambda e, half=half, pb=pb: e.scalar_tensor_tensor(
                        out=xcur[:, half * 512:(half + 1) * 512], in0=xcur[:, half * 512:(half + 1) * 512], scalar=stat[:, 1:2], in1=PS[pb][:], op0=ALU.mult, op1=ALU.add))
                dma('s', 'dh1', h1_d[it * 128:(it + 1) * 128, :], xcur[:], [xk], ["h1d%d" % it])
            sch.barrier()
         except _Stop:
            sch.barrier()

        if 2 in phases:
          with ExitStack() as es:
            def sb(name, shape, dt=F32):
                return es.enter_context(nc.sbuf_tensor("s_" + name, shape, dt))
            NCV = 6
            RB = 4
            cin = [sb("cin%d" % i, [128, RB, D]) for i in range(NCV)]
            cout = [sb("cout%d" % i, [128, RB, D], BF16) for i in range(NCV)]
            n = 0
            for src_d, dst_d in [(pu_d, tuv_d[:, 0, :]), (pvv_d, tuv_d[:, 1, :])]:
                sv_ = src_d.rearrange("(c r p) d -> c p r d", p=128, r=RB)
                dv_ = dst_d.rearrange("(c r p) d -> c p r d", p=128, r=RB)
                for c in range(16384 // (128 * RB)):
                    b = n % NCV
                    n += 1
                    dma('s', 'dci%d' % b, cin[b][:], sv_[c], [], ["cin%d" % b])
                    if n % 3 == 0:
                        op('v', ["cin%d" % b], ["cout%d" % b], lambda e, b=b: e.tensor_copy(out=cout[b][:], in_=cin[b][:]))
                    elif n % 3 == 1:
                        op('a', ["cin%d" % b], ["cout%d" % b], lambda e, b=b: e.activation(out=cout[b][:], in_=cin[b][:], func=AF.Copy))
                    else:
                        op('g', ["cin%d" % b], ["cout%d" % b], lambda e, b=b: e.tensor_copy(out=cout[b][:], in_=cin[b][:]))
                    dma('a', 'dco%d' % b, dv_[c], cout[b][:], ["cout%d" % b], ["tb"])
            sch.barrier()

        if 2 in phases:
          with ExitStack() as es:
            def sb(name, shape, dt=F32):
                return es.enter_context(nc.sbuf_tensor("s_" + name, shape, dt))
            ident = sb("ident2", [128, 128])
            gffn = sb("gffn", [128, D])
            gfin = sb("gfin", [128, D])
            wq = sb("wq", [128, 8, D])
            subk = sb("subk", [128, 8, 128])
            h1t = [sb("h1t%d" % i, [128, D]) for i in range(2)]
            xn2 = [sb("xn2_0", [128, D])] * 2
            xn2b = [sb("xn2b_%d" % i, [128, D], BF16) for i in range(2)]
            junkb = sb("junkb", [128, D], BF16)
            xn2F = sb("xn2F", [128, 8, 128])
            qF = sb("qF", [128, 8, 128])
            sc = sb("sc", [128, 16, 128])
            scr = sb("scr", [128, 16, 128])
            iota16 = sb("iota16", [128, 16])
            eq = sb("eq", [128, 8, 16, 16])
            junk = eq[:].rearrange("p a b c -> p (a b c)")
            sv = sb("sv", [128, 16, 16])
            siu = sb("siu", [128, 16, 16], U32)
            sif = sb("sif", [128, 16, 16])
            dsi = sb("dsi", [128, 8, 16])
            cand = sb("cand", [128, 8, 256])
            cv = sb("cv", [128, 8, 16])
            ciu = sb("ciu", [128, 8, 16], U32)
            iiu = sb("iiu", [128, 8, 16], U32)
            jju = sb("jju", [128, 8, 16], U32)
            iif = sb("iif", [128, 8, 16])
            jjf = sb("jjf", [128, 8, 16])
            i1 = sb("i1", [128, 8, 16])
            i2 = sb("i2", [128, 8, 16])
            lt = sb("lt", [128, 8, 16])
            gtmp = sb("gtmp", [128, 128])
            ei = [sb("ei%d" % i, [128, 128], I32) for i in range(2)]
            gate = [sb("gate%d" % i, [128, 8, 16]) for i in range(2)]
            sm = sb("sm", [128, 4, 8])
            hid = sb("hid", [128, 128])
            wgt = [sb("wgt%d" % i, [128, 128]) for i in range(2)]
            statA = sb("statA", [128, 8])
            statV = sb("statV", [128, 8])
            NDG = 4
            dg = [sb("dg%d" % i, [128, 128], BF16) for i in range(NDG)]
            NR = 20
            UV = [sb("UV%d" % i, [128, 2, D], BF16) for i in range(NR)]
            hid2 = [sb("hid%d" % i, [128, 128]) for i in range(2)]
            gt2 = [sb("gtg%d" % i, [128, 8]) for i in range(3)]
            h2 = sb("h2", [128, D])
            outt = sb("outt", [128, D])

            dma('s', 'dc2', ident[:], ident_d, [], ["c"])
            dma('s', 'dc2', gffn[:], gffn_d, [], ["c"])
            dma('s', 'dc2', iota16[:], iota_d, [], ["c"])
            dma('s', 'dc2', gfin[:], gfin_d, [], ["c"])
            dma('s', 'dc2', subk[:].rearrange("p a b -> p (a b)"), subk_d, [], ["c"])
            dma('s', 'dc2', wq[:], wq_d.rearrange("(kt p) c -> p kt c", p=128), [], ["c"])

            def rms(src, srck, dstat, dk, op=op):
                op('a', [srck], ["eq", dk], lambda e: e.activation(out=junk[:, 0:512], in_=src[:, 0:512], func=AF.Square, accum_out=dstat[:, 0:1]))
                op('a', [srck], ["eq", dk], lambda e: e.activation(out=junk[:, 512:1024], in_=src[:, 512:1024], func=AF.Square, accum_out=dstat[:, 3:4]))
                op('v', [dk], [dk], lambda e: e.tensor_tensor(out=dstat[:, 0:1], in0=dstat[:, 0:1], in1=dstat[:, 3:4], op=ALU.add))
                op('a', [dk], [dk], lambda e: e.activation(out=dstat[:, 1:2], in_=dstat[:, 0:1], func=AF.Sqrt, bias=1e-6, scale=1.0 / D))
                op('v', [dk], [dk], lambda e: e.reciprocal(out=dstat[:, 2:3], in_=dstat[:, 1:2]))

            def top16_multi(segs, segk, width, vouts, iouts, okeys):
                n = len(segs)
                scrv = [scr[:].rearrange("p a b -> p (a b)")[:, i * width:(i + 1) * width] for i in range(n)]
                sk = ["scr%d" % i for i in range(n)]
                for i in range(n):
                    qop('v', [segk], [okeys[i]], lambda e, i=i: e.max(out=vouts[i][:, 0:8], in_=segs[i]))
                for i in range(n):
                    qop('v', [segk, okeys[i]], [okeys[i]], lambda e, i=i: e.max_index(out=iouts[i][:, 0:8], in_max=vouts[i][:, 0:8], in_values=segs[i]))
                for i in range(n):
                    qop('v', [segk, okeys[i]], [sk[i]], lambda e, i=i: e.match_replace(out=scrv[i], in_to_replace=vouts[i][:, 0:8], in_values=segs[i], imm_value=NEG))
                for i in range(n):
                    qop('v', [sk[i]], [okeys[i]], lambda e, i=i: e.max(out=vouts[i][:, 8:16], in_=scrv[i]))
                for i in range(n):
                    qop('v', [sk[i], okeys[i]], [okeys[i]], lambda e, i=i: e.max_index(out=iouts[i][:, 8:16], in_max=vouts[i][:, 8:16], in_values=scrv[i]))

            gcount = [0, 0, 0]

            Aq = []

            def qop(*a):
                Aq.append(('op', a))

            def qdma(*a, **k):
                Aq.append(('dma', a, k))

            def drain(n=None):
                k = 0
                while Aq and (n is None or k < n):
                    item = Aq.pop(0)
                    if item[0] == 'op':
                        op(*item[1])
                    else:
                        dma(*item[1], **item[2])
                    k += 1

            def stage_A(it):
                p = it % 2
                hk = "h1t%d" % p
                hcur = h1t[p]
                xk = "xn2_0"
                xc = xn2[0]
                qdma('s', 'dh%d' % p, hcur[:], h1_d[it * 128:(it + 1) * 128, :], ["h1d%d" % it], [hk])
                rms(hcur, hk, statA, "statA", op=qop)
                qop('v', [hk, "statA", "c"], [xk], lambda e: e.scalar_tensor_tensor(out=xc[:], in0=hcur[:], scalar=statA[:, 2:3], in1=gffn[:], op0=ALU.mult, op1=ALU.mult))
                qop('a', [xk], ["xn2b_%d" % p], lambda e: e.activation(out=xn2b[p][:], in_=xc[:], func=AF.Copy))
                for half in range(2):
                    def tr2(e, half=half):
                        ins = None
                        for q in range(4):
                            kt = half * 4 + q
                            ins = e.transpose(out=PS[half][:, q * 128:(q + 1) * 128], in_=xc[:, kt * 128:(kt + 1) * 128], identity=ident[:])
                        return ins
                    qop('p', [xk, "c"], [psk(half)], tr2)
                    if half == 0:
                        qop('a', [psk(0)], ["xn2F"], lambda e: e.activation(out=xn2F[:, 0:4, :], in_=P3(0), func=AF.Copy))
                    else:
                        qop('a', [psk(1)], ["xn2F"], lambda e: e.activation(out=xn2F[:, 4:8, :], in_=P3(1), func=AF.Copy))
                for half in range(2):
                    pb = 2 + half

                    def mm_q(e, half=half, pb=pb):
                        ins = None
                        for q in range(4):
                            ct = half * 4 + q
                            for kt in range(8):
                                ins = e.matmul(PS[pb][:, q * 128:(q + 1) * 128], lhsT=wq[:, kt, ct * 128:(ct + 1) * 128], rhs=xn2F[:, kt, :], start=(kt == 0), stop=(kt == 7))
                        return ins
                    qop('p', ["c", "xn2F"], [psk(pb)], mm_q)
                    if half == 0:
                        qop('a', [psk(pb)], ["qF"], lambda e, pb=pb: e.activation(out=qF[:, 0:4, :], in_=P3(pb), func=AF.Copy))
                    else:
                        qop('a', [psk(pb)], ["qF"], lambda e, pb=pb: e.activation(out=qF[:, 4:8, :], in_=P3(pb), func=AF.Copy))
                sc4 = sc[:].rearrange("p (h c) n -> p h c n", c=2)
                for hg in range(2):
                    for c in range(2):
                        pb = 4 + c

                        def mm_s(e, hg=hg, c=c, pb=pb):
                            ins = None
                            for q in range(4):
                                h = hg * 4 + q
                                ins = e.matmul(PS[pb][:, q * 128:(q + 1) * 128], lhsT=qF[c * 64:(c + 1) * 64, h, :], rhs=subk[c * 64:(c + 1) * 64, h, :], start=True, stop=True)
                            return ins
                        qop('p', ["qF", "c"], [psk(pb)], mm_s)
                        if c == 0:
                            qop('a', [psk(pb)], ["sc"], lambda e, hg=hg, c=c, pb=pb: e.activation(out=sc4[:, hg * 4:(hg + 1) * 4, c, :], in_=P3(pb), func=AF.Copy))
                        else:
                            qop('a', [psk(pb)], ["sc"], lambda e, hg=hg, c=c, pb=pb: e.activation(out=sc4[:, hg * 4:(hg + 1) * 4, c, :], in_=P3(pb), func=AF.Copy))

            svk = ["svi%d" % i for i in range(16)]
            cvk = ["cvi%d" % i for i in range(8)]

            def stage_A2(it):
                p = it % 2
                top16_multi([sc[:, i, :] for i in range(16)], "sc", 128, [sv[:, i, :] for i in range(16)], [siu[:, i, :] for i in range(16)], svk)
                for h in range(8):
                    qop('v', [svk[2 * h], svk[2 * h + 1]], ["cand"], lambda e, h=h: e.tensor_tensor(
                        out=cand[:, h, :].rearrange("p (i j) -> p i j", i=16),
                        in0=bc(sv[:, 2 * h, :], [128, 16, 16], 2), in1=bc(sv[:, 2 * h + 1, :], [128, 16, 16], 1), op=ALU.add))
                top16_multi([cand[:, h, :] for h in range(8)], "cand", 256, [cv[:, h, :] for h in range(8)], [ciu[:, h, :] for h in range(8)], cvk)
                qop('v', cvk, ["iiu"], lambda e: e.tensor_single_scalar(out=iiu[:], in_=ciu[:], scalar=4, op=ALU.logical_shift_right))
                qop('v', cvk, ["jju"], lambda e: e.tensor_single_scalar(out=jju[:], in_=ciu[:], scalar=15, op=ALU.bitwise_and))
                qop('v', ["iiu"], ["iif"], lambda e: e.tensor_copy(out=iif[:], in_=iiu[:]))
                qop('v', ["jju"], ["jjf"], lambda e: e.tensor_copy(out=jjf[:], in_=jju[:]))
                qop('v', svk, ["sif"], lambda e: e.tensor_copy(out=sif[:], in_=siu[:]))
                sif4 = sif[:].rearrange("p (h c) k -> p h c k", c=2)
                E4 = [128, 8, 16, 16]
                iota4 = iota16[:].unsqueeze(1).unsqueeze(1).to_broadcast(E4)
                for (idxf, idk, c_, dst, dk) in [(iif, "iif", 0, i1, "i1"), (jjf, "jjf", 1, i2, "i2")]:
                    qop('v', [idk, "c", "eq"], ["eq"], lambda e, idxf=idxf: e.tensor_tensor(out=eq[:], in0=idxf[:].unsqueeze(3).to_broadcast(E4), in1=iota4, op=ALU.is_equal))
                    qop('v', ["eq", "sif"], ["eq"], lambda e, c_=c_: e.tensor_tensor(out=eq[:], in0=eq[:], in1=sif4[:, :, c_, :].unsqueeze(2).to_broadcast(E4), op=ALU.mult))
                    qop('v', ["eq"], [dk], lambda e, dst=dst: e.tensor_reduce(out=dst[:], in_=eq[:], axis=AX.X, op=ALU.add))
                qop('v', ["i1", "i2"], ["i1"], lambda e: e.scalar_tensor_tensor(out=i1[:], in0=i1[:], scalar=128.0, in1=i2[:], op0=ALU.mult, op1=ALU.add))
                qop('v', ["i1"], ["ei%d" % p], lambda e: e.tensor_copy(out=ei[p][:], in_=i1[:].rearrange("p h k -> p (h k)")))

            def stage_A3(it):
                p = it % 2
                gk = "gate%d" % p
                g_ = gate[p]
                qop('v', cvk, ["sm"], lambda e: e.tensor_reduce(out=sm[:, 0, :], in_=cv[:], axis=AX.X, op=ALU.max))
                qop('v', cvk + ["sm"], [gk], lambda e: e.tensor_tensor(out=g_[:], in0=cv[:], in1=bc(sm[:, 0, :], [128, 8, 16], 2), op=ALU.subtract))
                qop('a', [gk], [gk], lambda e: e.activation(out=g_[:], in_=g_[:], func=AF.Exp))
                qop('v', [gk], ["sm"], lambda e: e.tensor_reduce(out=sm[:, 1, :], in_=g_[:], axis=AX.X, op=ALU.add))
                qop('v', ["sm"], ["sm"], lambda e: e.reciprocal(out=sm[:, 2, :], in_=sm[:, 1, :]))
                qop('v', [gk, "sm"], [gk], lambda e: e.tensor_tensor(out=g_[:], in0=g_[:], in1=bc(sm[:, 2, :], [128, 8, 16], 2), op=ALU.mult))

            GS = 4
            NGR = 128 // GS
            slot_buf = {}

            def dots(it, g):
                p = it % 2
                hd = hid2[p]
                hk_ = "hid%d" % p
                if g == 0:
                    op('v', [], [hk_], lambda e: e.memset(hd[:], 0.0))
                for s_ in range(g * GS, (g + 1) * GS):
                    b = gcount[0] % NR
                    gcount[0] += 1
                    slot_buf[(it, s_)] = b
                    dma('g', 'duv%d' % b, UV[b][:].rearrange("p a d -> p (a d)"), tuv_d.rearrange("e a d -> e (a d)"), ["ei%d" % p], ["UV%d" % b],
                        in_offset=bass.IndirectOffsetOnAxis(ap=ei[p][:, s_:s_ + 1], axis=0))
                    op('v', ["UV%d" % b, "xn2b_%d" % p], ["junkb", hk_], lambda e, b=b, s_=s_: e.scalar_tensor_tensor(
                        out=junkb[:], in0=UV[b][:, 0, :], scalar=1.0, in1=xn2b[p][:], op0=ALU.mult, op1=ALU.mult, accum_out=hd[:, s_:s_ + 1]))

            def weights1(it, g):
                p = it % 2
                hd = hid2[p]
                hk_ = "hid%d" % p
                q = g % 3
                sl = slice(g * GS, (g + 1) * GS)
                src = hd[:, sl]
                tmp = gt2[q][:, 0:GS]
                tk_ = "gtg%d" % q
                op('v', [hk_], [tk_], lambda e: e.tensor_tensor(out=tmp, in0=src, in1=src, op=ALU.mult))
                op('v', [tk_], [tk_], lambda e: e.tensor_scalar(out=tmp, in0=tmp, scalar1=0.044715, scalar2=1.0, op0=ALU.mult, op1=ALU.add))
                op('v', [tk_, hk_], [tk_], lambda e: e.tensor_tensor(out=tmp, in0=tmp, in1=src, op=ALU.mult))
                op('a', [tk_], [tk_], lambda e: e.activation(out=tmp, in_=tmp, func=AF.Sigmoid, scale=1.5957691216057308))

            def weights2(it, g):
                p = it % 2
                hd = hid2[p]
                hk_ = "hid%d" % p
                wk = "wgt%d" % p
                q = g % 3
                sl = slice(g * GS, (g + 1) * GS)
                tmp = gt2[q][:, 0:GS]
                tk_ = "gtg%d" % q
                op('v', [tk_, hk_], [wk], lambda e: e.tensor_tensor(out=wgt[p][:, sl], in0=tmp, in1=hd[:, sl], op=ALU.mult))
                op('v', [wk, "gate%d" % p], [wk], lambda e: e.tensor_tensor(out=wgt[p][:, sl], in0=wgt[p][:, sl], in1=gate[p][:].rearrange("p h k -> p (h k)")[:, sl], op=ALU.mult))

            def accum(it, g):
                p = it % 2
                wk = "wgt%d" % p
                for s_ in range(g * GS, (g + 1) * GS):
                    b = slot_buf.pop((it, s_))
                    r = gcount[2] % NDG
                    gcount[2] += 1
                    op('a', [wk, "c"], ["dg%d" % r], lambda e, r=r, s_=s_: e.activation(out=dg[r][:], in_=ident[:], func=AF.Copy, scale=wgt[p][:, s_:s_ + 1]))

                    def mm_v(e, b=b, r=r, s_=s_):
                        e.matmul(PS[6][:], lhsT=dg[r][:], rhs=UV[b][:, 1, 0:512], start=(s_ == 0), stop=(s_ == 127))
                        return e.matmul(PS[7][:], lhsT=dg[r][:], rhs=UV[b][:, 1, 512:1024], start=(s_ == 0), stop=(s_ == 127))
                    op('p', ["dg%d" % r, "UV%d" % b], [psk(6), psk(7)], mm_v)

            def stage_Vf(it):
                p = it % 2
                hk = "h1t%d" % p
                hcur = h1t[p]
                op('v', [psk(6), hk], ["h2"], lambda e: e.tensor_tensor(out=h2[:, 0:512], in0=PS[6][:], in1=hcur[:, 0:512], op=ALU.add))
                op('v', [psk(7), hk], ["h2"], lambda e: e.tensor_tensor(out=h2[:, 512:1024], in0=PS[7][:], in1=hcur[:, 512:1024], op=ALU.add))
                rms(h2, "h2", statV, "statV")
                op('v', ["h2", "statV", "c"], ["outt"], lambda e: e.scalar_tensor_tensor(out=outt[:], in0=h2[:], scalar=statV[:, 2:3], in1=gfin[:], op0=ALU.mult, op1=ALU.mult))
                dma('s', 'dout', out_d[it * 128:(it + 1) * 128, :], outt[:], ["outt"], ["od%d" % it])

            stage_A(0)
            stage_A2(0)
            stage_A3(0)
            drain()
            hist = []
            def retire(k):
                it_, g_ = hist[k]
                weights2(it_, g_)
                accum(it_, g_)
                if g_ == NGR - 1:
                    stage_Vf(it_)
            for it in range(NT):
                per = 0
                if it + 1 < NT:
                    stage_A(it + 1)
                    stage_A2(it + 1)
                    stage_A3(it + 1)
                    per = (len(Aq) + NGR - 9) // (NGR - 8)
                for g in range(NGR):
                    dots(it, g)
                    hist.append((it, g))
                    if len(hist) >= 3:
                        retire(-3)
                    if len(hist) >= 2:
                        weights1(*hist[-2])
                    if per and g >= 2:
                        drain(per)
                drain()
            retire(-2)
            weights1(*hist[-1])
            retire(-1)
            sch.barrier()
    return nc


def make_inputs(inp, b):
    f = lambda a: np.ascontiguousarray(np.asarray(a), dtype=np.float32)
    colT = lambda v, n: f(np.asarray(v).reshape(n, 128).T)
    m = {}
    m["x"] = f(inp["x"][b])
    m["w_in"] = f(inp["w_in"][0])
    m["gainP"] = colT(inp["norm_mix"][0], 8)
    m["bgate"] = colT(inp["b_gate"][0], 16)
    m["mu"] = colT(inp["mu_rwkv"][0], 14)
    pv = np.stack([colT(inp["w0"][0], 4), colT(inp["a0"][0], 4), colT(inp["k_k"][0], 4), colT(inp["k_a"][0], 4),
                   colT(np.asarray(inp["r_k"][0]).reshape(512), 4), colT(np.asarray(inp["s5_d"][0]).reshape(512), 4)], axis=1)
    m["pvec"] = f(pv)
    m["lora"] = f(np.concatenate([np.asarray(inp["w_lora_up"][0]), np.asarray(inp["a_lora_up"][0])], axis=0))
    m["glora"] = f(inp["g_lora_up"][0])
    m["lnw"] = f(np.broadcast_to(np.asarray(inp["ln_x_w"][0])[None, :], (128, 512)))
    m["lnb"] = f(np.broadcast_to(np.asarray(inp["ln_x_b"][0])[None, :], (128, 512)))
    m["w_o"] = f(inp["w_o_rwkv"][0])
    m["w_glu"] = f(inp["w_glu_s5"][0])
    m["w_out"] = f(inp["w_out"][0])
    a_re = np.asarray(inp["s5_a_re"][0]).reshape(16, 128).T
    a_im = np.asarray(inp["s5_a_im"][0]).reshape(16, 128).T
    ldt = np.repeat(np.asarray(inp["s5_log_dt"][0]), 64).reshape(16, 128).T
    m["s5p"] = f(np.stack([a_re, a_im, ldt], axis=1))
    bbre = np.zeros((128, 16, 128), np.float32)
    bbim = np.zeros((128, 16, 128), np.float32)
    ccre = np.zeros((128, 16, 128), np.float32)
    ccim = np.zeros((128, 16, 128), np.float32)
    b_re = np.asarray(inp["s5_b_re"][0]); b_im = np.asarray(inp["s5_b_im"][0])
    c_re = np.asarray(inp["s5_c_re"][0]); c_im = np.asarray(inp["s5_c_im"][0])
    for g in range(32):
        j, gl, g8 = g // 2, g % 2, g % 8
        bbre[g8 * 16:(g8 + 1) * 16, j, gl * 64:(gl + 1) * 64] = b_re[g].T
        bbim[g8 * 16:(g8 + 1) * 16, j, gl * 64:(gl + 1) * 64] = b_im[g].T
        ccre[gl * 64:(gl + 1) * 64, j, g8 * 16:(g8 + 1) * 16] = c_re[g].T
        ccim[gl * 64:(gl + 1) * 64, j, g8 * 16:(g8 + 1) * 16] = c_im[g].T
    m["bbre"] = bbre.reshape(128, 2048)
    m["bbim"] = bbim.reshape(128, 2048)
    m["ccre"] = ccre.reshape(128, 2048)
    m["ccim"] = ccim.reshape(128, 2048)
    m["gffn"] = f(np.broadcast_to(np.asarray(inp["norm_ffn"][0])[None, :], (128, D)))
    m["gfin"] = f(np.broadcast_to(np.asarray(inp["norm_final"])[None, :], (128, D)))
    m["wq"] = f(inp["peer_wq"][0])
    m["subk"] = f(np.asarray(inp["peer_subkeys"][0]).transpose(1, 3, 0, 2).reshape(128, 1024))
    m["peer_u"] = f(inp["peer_u"][0])
    m["peer_v"] = f(inp["peer_v"][0])
    m["ident"] = np.eye(128, dtype=np.float32)
    p = np.arange(128)[:, None]
    jx = np.arange(128)[None, :]
    masks = np.zeros((128, 5, 128), np.float32)
    masks[:, 0] = (jx < p)
    masks[:, 1] = (jx > p)
    masks[:, 2] = (jx >= p)
    masks[:, 3] = ((jx // 64) == (p // 64))
    masks[:, 4] = 1.0
    masks[:, 4, 0] = 0.0
    m["masks"] = masks
    sel2 = np.zeros((128, 2), np.float32)
    sel2[:64, 0] = 1.0
    sel2[64:, 1] = 1.0
    m["sel2"] = sel2
    m["iota16"] = np.ascontiguousarray(np.broadcast_to(np.arange(16, dtype=np.float32)[None, :], (128, 16)))
    return m


_NC_CACHE = {}


def kernel(**inputs):
    n = 8
    if "nc" not in _NC_CACHE:
        _NC_CACHE["nc"] = build_nc()
    nc = _NC_CACHE["nc"]
    in_maps = [make_inputs(inputs, b) for b in range(n)]
    res = run_bass_kernel_spmd(nc, in_maps, core_ids=list(range(n)))
    out = np.stack([np.asarray(r["out"], dtype=np.float32) for r in res.results], axis=0)
    return out
```
